# Optimizing a Trainium2 kernel written in Bass

```python
import jax, jax.numpy as jnp
from jax import lax
import numpy as np

D_MODEL = 1024
BATCH = 4
SEQ = 8192
DEPTH = 2

CHUNK = 64
N_META = 16
Q_BLOCK = 128
EPS = 1e-6

D_MIX = D_MODEL
N_GROUPS = 4
GROUP_WIDTH = D_MIX // N_GROUPS

LRU_WIDTH = GROUP_WIDTH
LRU_BLOCKS = 4
LRU_BLOCK = LRU_WIDTH // LRU_BLOCKS
CONV_WIDTH = 4
LRU_C = 8.0
HG_HEADS = 4
HG_DV = GROUP_WIDTH // HG_HEADS
HG_DK = 64
HG_CHUNK = 16
MLA_HEADS = 4
MLA_NOPE = 64
MLA_ROPE = 32
MLA_V = GROUP_WIDTH // MLA_HEADS
MLA_Q_RANK = 192
MLA_KV_RANK = 128
ROPE_THETA = 10000.0
FOX_HEADS = 4
FOX_HD = GROUP_WIDTH // FOX_HEADS
D_FF = 2816
N_EXPERTS = 8
TOP_K = 2
D_FF_EXPERT = 3584
N_DENSE = (DEPTH + 1) // 2
N_MOE = DEPTH // 2

IN_SIZES = (LRU_WIDTH, LRU_WIDTH,
            HG_HEADS * HG_DK, HG_HEADS * HG_DK, HG_HEADS * HG_DV, HG_HEADS * HG_DV,
            MLA_Q_RANK, MLA_KV_RANK, MLA_ROPE,
            FOX_HEADS * FOX_HD, FOX_HEADS * FOX_HD, FOX_HEADS * FOX_HD, FOX_HEADS)
IN_COLS = sum(IN_SIZES)

kernel_name = "hymba_hybrid_rglru_hgrn2_mla_fox_moe"

F32 = jnp.float32


def rmsnorm(x, g):
    xf = x.astype(F32)
    y = xf * lax.rsqrt(jnp.mean(xf * xf, axis=-1, keepdims=True) + EPS)
    return (y * g.astype(F32)).astype(x.dtype)


def group_rmsnorm(y, g):
    B, Lp, _ = y.shape
    yg = y.reshape(B, Lp, N_GROUPS, GROUP_WIDTH)
    return rmsnorm(yg, g.reshape(N_GROUPS, GROUP_WIDTH)).reshape(B, Lp, D_MIX)


def chunk_ids(pos):
    return jnp.where(pos < N_META, 0, 1 + (pos - N_META) // CHUNK)


def rope(t, pos):
    half = t.shape[-1] // 2
    inv = ROPE_THETA ** (-jnp.arange(half, dtype=F32) / half)
    ang = pos.astype(F32)[:, None] * inv[None, :]
    cos = jnp.cos(ang)[:, None, :]
    sin = jnp.sin(ang)[:, None, :]
    t1 = t[..., :half].astype(F32)
    t2 = t[..., half:].astype(F32)
    return jnp.concatenate([t1 * cos - t2 * sin, t1 * sin + t2 * cos], axis=-1).astype(t.dtype)


def blocked_attention(q, k, v, score_mod):
    B, H, Lp, dk = q.shape
    nb = Lp // Q_BLOCK
    scale = dk ** -0.5
    qb = q.reshape(B, H, nb, Q_BLOCK, dk).transpose(2, 0, 1, 3, 4)

    def one_block(args):
        q_blk, blk = args
        s = jnp.einsum('bhqd,bhkd->bhqk', q_blk, k).astype(F32) * scale
        p = jax.nn.softmax(score_mod(s, blk * Q_BLOCK), axis=-1)
        return jnp.einsum('bhqk,bhkd->bhqd', p.astype(v.dtype), v)

    o = lax.map(one_block, (qb, jnp.arange(nb)))
    return o.transpose(1, 2, 0, 3, 4).reshape(B, H, Lp, v.shape[-1])


def _linear_combine(e1, e2):
    a1, b1 = e1
    a2, b2 = e2
    return a1 * a2, a2 * b1 + b2


def rglru_mixer(xb, gb, conv_w, conv_b, wa, ba, wx, bx, lam):
    B, Lp, W = xb.shape
    xp = jnp.pad(xb, ((0, 0), (CONV_WIDTH - 1, 0), (0, 0)))
    u = conv_b + xp[:, 0:Lp] * conv_w[0]
    for j in range(1, CONV_WIDTH):
        u = u + xp[:, j:j + Lp] * conv_w[j]
    ub = u.reshape(B, Lp, LRU_BLOCKS, LRU_BLOCK)
    r = jax.nn.sigmoid(jnp.einsum('blnc,ncd->blnd', ub, wa) + ba).reshape(B, Lp, W)
    i = jax.nn.sigmoid(jnp.einsum('blnc,ncd->blnd', ub, wx) + bx).reshape(B, Lp, W)
    log_a = -LRU_C * r.astype(F32) * jax.nn.softplus(-lam.astype(F32))
    a = jnp.exp(log_a)
    b = jnp.sqrt(-jnp.expm1(2.0 * log_a)) * (i * u).astype(F32)
    _, h = lax.associative_scan(_linear_combine, (a, b), axis=1)
    return h.astype(xb.dtype) * jax.nn.gelu(gb)


def hgrn2_mixer(q, fz, v, g, lb):
    B, Lp, _ = q.shape
    nc = Lp // HG_CHUNK
    logf = jnp.logaddexp(jnp.log(lb), jnp.log1p(-lb) + jax.nn.log_sigmoid(fz.astype(F32)))
    k = -jnp.expm1(logf)

    def heads(t, d):
        return t.reshape(B, nc, HG_CHUNK, HG_HEADS, d).transpose(0, 3, 1, 2, 4)

    qh = heads(q.astype(F32), HG_DK)
    kh = heads(k, HG_DK)
    lfh = heads(logf, HG_DK)
    vh = heads(v.astype(F32), HG_DV)
    b = jnp.cumsum(lfh, axis=3)
    b_end = b[:, :, :, -1:, :]
    q_dec = qh * jnp.exp(b)
    k_dec = kh * jnp.exp(-b)
    tri = jnp.tril(jnp.ones((HG_CHUNK, HG_CHUNK), dtype=bool))
    att = jnp.where(tri, jnp.einsum('bhntd,bhnsd->bhnts', q_dec, k_dec), 0.0)
    o_intra = jnp.einsum('bhnts,bhnsv->bhntv', att, vh)
    u_chunk = jnp.einsum('bhnsd,bhnsv->bhndv', kh * jnp.exp(b_end - b), vh)
    dec_chunk = jnp.exp(b_end[:, :, :, 0, :])

    def step(S, inp):
        dec, uc = inp
        return dec[..., None] * S + uc, S

    S0 = jnp.zeros((B, HG_HEADS, HG_DK, HG_DV), F32)
    _, S_prev = lax.scan(step, S0, (jnp.moveaxis(dec_chunk, 2, 0), jnp.moveaxis(u_chunk, 2, 0)))
    o_inter = jnp.einsum('bhntd,nbhdv->bhntv', q_dec, S_prev)
    o = (o_intra + o_inter).transpose(0, 2, 3, 1, 4).reshape(B, Lp, HG_HEADS * HG_DV)
    return o.astype(g.dtype) * jax.nn.silu(g)


def mla_mixer(cq, ckv, kr, pos, cid, gq, w_uq, gkv, w_ukv, gqn, gkn):
    B, Lp, _ = cq.shape
    q = (rmsnorm(cq, gq) @ w_uq).reshape(B, Lp, MLA_HEADS, MLA_NOPE + MLA_ROPE)
    kv = (rmsnorm(ckv, gkv) @ w_ukv).reshape(B, Lp, MLA_HEADS, MLA_NOPE + MLA_V)
    k_nope, v = kv[..., :MLA_NOPE], kv[..., MLA_NOPE:]
    k = jnp.concatenate([k_nope, jnp.broadcast_to(kr[:, :, None, :], (B, Lp, MLA_HEADS, MLA_ROPE))], axis=-1)
    q = rmsnorm(q, gqn)
    k = rmsnorm(k, gkn)
    q = jnp.concatenate([q[..., :MLA_NOPE], rope(q[..., MLA_NOPE:], pos)], axis=-1)
    k = jnp.concatenate([k[..., :MLA_NOPE], rope(k[..., MLA_NOPE:], pos)], axis=-1)

    def chunk_mask(s, q0):
        qc = lax.dynamic_slice_in_dim(cid, q0, Q_BLOCK)
        return jnp.where(cid[None, :] <= qc[:, None], s, -jnp.inf)

    o = blocked_attention(q.transpose(0, 2, 1, 3), k.transpose(0, 2, 1, 3),
                          v.transpose(0, 2, 1, 3), chunk_mask)
    return o.transpose(0, 2, 1, 3).reshape(B, Lp, MLA_HEADS * MLA_V)


def fox_mixer(q, k, v, fz, pos, bf, gqn, gkn):
    B, Lp, _ = q.shape

    def heads(t):
        return t.reshape(B, Lp, FOX_HEADS, FOX_HD)

    qh = rmsnorm(heads(q), gqn).transpose(0, 2, 1, 3)
    kh = rmsnorm(heads(k), gkn).transpose(0, 2, 1, 3)
    vh = heads(v).transpose(0, 2, 1, 3)
    logf = jax.nn.log_sigmoid((fz + bf).astype(F32))
    c = jnp.cumsum(logf, axis=1).transpose(0, 2, 1)

    def decay_mask(s, q0):
        cq = lax.dynamic_slice_in_dim(c, q0, Q_BLOCK, axis=2)
        qp = q0 + jnp.arange(Q_BLOCK)
        bias = cq[..., :, None] - c[..., None, :]
        return jnp.where(pos[None, :] <= qp[:, None], s + bias, -jnp.inf)

    o = blocked_attention(qh, kh, vh, decay_mask)
    return o.transpose(0, 2, 1, 3).reshape(B, Lp, FOX_HEADS * FOX_HD)


def swiglu(u, wg, wu, wd):
    return (jax.nn.silu(u @ wg) * (u @ wu)) @ wd


def moe_swiglu(u, w_router, w_gate, w_up, w_down):
    logits = jnp.einsum('bld,de->ble', u, w_router).astype(F32)
    top_v, top_i = lax.top_k(logits, TOP_K)
    gates = jax.nn.softmax(top_v, axis=-1)
    dense_gate = jnp.sum(jax.nn.one_hot(top_i, N_EXPERTS, dtype=F32) * gates[..., None], axis=-2)
    out = jnp.zeros_like(u)
    for e in range(N_EXPERTS):
        ye = swiglu(u, w_gate[e], w_up[e], w_down[e])
        out = out + dense_gate[..., e:e + 1].astype(u.dtype) * ye
    return out


def setup_inputs(seed: int = 0) -> dict:
    key = jax.random.key(seed)
    it = iter(jax.random.split(key, 40))

    def nrm(shape, scale):
        return jax.random.normal(next(it), shape, F32) * scale

    def gain(shape):
        return 1.0 + nrm(shape, 0.05)

    u = jax.random.uniform(next(it), (DEPTH, LRU_WIDTH), F32, minval=0.9, maxval=0.999)
    a0 = u ** (1.0 / LRU_C)
    lru_lambda = jnp.log(a0) - jnp.log1p(-a0)
    return {
        "x": nrm((BATCH, SEQ, D_MODEL), 1.0),
        "meta": nrm((N_META, D_MODEL), 1.0),
        "norm1_g": gain((DEPTH, D_MODEL)),
        "norm2_g": gain((DEPTH, D_MODEL)),
        "w_in": nrm((DEPTH, D_MODEL, IN_COLS), D_MODEL ** -0.5),
        "w_out": nrm((DEPTH, D_MIX, D_MODEL), D_MIX ** -0.5),
        "out_norm_g": gain((DEPTH, D_MIX)),
        "lru_conv_w": nrm((DEPTH, CONV_WIDTH, LRU_WIDTH), CONV_WIDTH ** -0.5),
        "lru_conv_b": nrm((DEPTH, LRU_WIDTH), 0.02),
        "lru_wa": nrm((DEPTH, LRU_BLOCKS, LRU_BLOCK, LRU_BLOCK), LRU_BLOCK ** -0.5),
        "lru_ba": nrm((DEPTH, LRU_BLOCKS, LRU_BLOCK), 0.02),
        "lru_wx": nrm((DEPTH, LRU_BLOCKS, LRU_BLOCK, LRU_BLOCK), LRU_BLOCK ** -0.5),
        "lru_bx": nrm((DEPTH, LRU_BLOCKS, LRU_BLOCK), 0.02),
        "lru_lambda": lru_lambda,
        "hg_lb_logits": nrm((DEPTH, HG_HEADS * HG_DK), 1.0),
        "mla_gq": gain((DEPTH, MLA_Q_RANK)),
        "mla_w_uq": nrm((DEPTH, MLA_Q_RANK, MLA_HEADS * (MLA_NOPE + MLA_ROPE)), MLA_Q_RANK ** -0.5),
        "mla_gkv": gain((DEPTH, MLA_KV_RANK)),
        "mla_w_ukv": nrm((DEPTH, MLA_KV_RANK, MLA_HEADS * (MLA_NOPE + MLA_V)), MLA_KV_RANK ** -0.5),
        "mla_gqn": gain((DEPTH, MLA_NOPE + MLA_ROPE)),
        "mla_gkn": gain((DEPTH, MLA_NOPE + MLA_ROPE)),
        "fox_gqn": gain((DEPTH, FOX_HD)),
        "fox_gkn": gain((DEPTH, FOX_HD)),
        "fox_bf": 2.0 + nrm((DEPTH, FOX_HEADS), 0.5),
        "ffn_w_gate": nrm((N_DENSE, D_MODEL, D_FF), D_MODEL ** -0.5),
        "ffn_w_up": nrm((N_DENSE, D_MODEL, D_FF), D_MODEL ** -0.5),
        "ffn_w_down": nrm((N_DENSE, D_FF, D_MODEL), D_FF ** -0.5),
        "moe_w_router": nrm((N_MOE, D_MODEL, N_EXPERTS), D_MODEL ** -0.5),
        "moe_w_gate": nrm((N_MOE, N_EXPERTS, D_MODEL, D_FF_EXPERT), D_MODEL ** -0.5),
        "moe_w_up": nrm((N_MOE, N_EXPERTS, D_MODEL, D_FF_EXPERT), D_MODEL ** -0.5),
        "moe_w_down": nrm((N_MOE, N_EXPERTS, D_FF_EXPERT, D_MODEL), D_FF_EXPERT ** -0.5),
    }


def reference(x, meta, norm1_g, norm2_g, w_in, w_out, out_norm_g,
              lru_conv_w, lru_conv_b, lru_wa, lru_ba, lru_wx, lru_bx, lru_lambda,
              hg_lb_logits,
              mla_gq, mla_w_uq, mla_gkv, mla_w_ukv, mla_gqn, mla_gkn,
              fox_gqn, fox_gkn, fox_bf,
              ffn_w_gate, ffn_w_up, ffn_w_down,
              moe_w_router, moe_w_gate, moe_w_up, moe_w_down):
    B = x.shape[0]
    L = N_META + x.shape[1]
    Lp = -(-L // Q_BLOCK) * Q_BLOCK
    h = jnp.concatenate([jnp.broadcast_to(meta[None].astype(x.dtype), (B, N_META, D_MODEL)), x], axis=1)
    h = jnp.pad(h, ((0, 0), (0, Lp - L), (0, 0)))
    pos = jnp.arange(Lp, dtype=jnp.int32)
    cid = chunk_ids(pos)
    lb_cum = jnp.cumsum(jax.nn.softmax(hg_lb_logits.astype(F32), axis=0), axis=0)
    split_at = np.cumsum(IN_SIZES)[:-1].tolist()
    for l in range(DEPTH):
        u = rmsnorm(h, norm1_g[l])
        z = u @ w_in[l]
        (xa, ga, hq, hf, hi, hg, cq, ckv, kr, fq, fk, fv, ff) = jnp.split(z, split_at, axis=-1)
        ya = rglru_mixer(xa, ga, lru_conv_w[l], lru_conv_b[l], lru_wa[l], lru_ba[l],
                         lru_wx[l], lru_bx[l], lru_lambda[l])
        yb = hgrn2_mixer(hq, hf, hi, hg, lb_cum[l] - lb_cum[0])
        yc = mla_mixer(cq, ckv, kr, pos, cid, mla_gq[l], mla_w_uq[l], mla_gkv[l], mla_w_ukv[l],
                       mla_gqn[l], mla_gkn[l])
        yd = fox_mixer(fq, fk, fv, ff, pos, fox_bf[l], fox_gqn[l], fox_gkn[l])
        y = group_rmsnorm(jnp.concatenate([ya, yb, yc, yd], axis=-1), out_norm_g[l])
        h = h + y @ w_out[l]
        u = rmsnorm(h, norm2_g[l])
        if l % 2 == 0:
            h = h + swiglu(u, ffn_w_gate[l // 2], ffn_w_up[l // 2], ffn_w_down[l // 2])
        else:
            h = h + moe_swiglu(u, moe_w_router[l // 2], moe_w_gate[l // 2],
                               moe_w_up[l // 2], moe_w_down[l // 2])
    return h[:, N_META:L]
```

```python
import contextlib
import os
import numpy as np
import ml_dtypes
import concourse.bass as bass
import concourse.mybir as mybir
from concourse.bass_utils import run_bass_kernel_spmd

F32 = mybir.dt.float32
BF16 = mybir.dt.bfloat16
AF = mybir.ActivationFunctionType
ALU = mybir.AluOpType
AX = mybir.AxisListType

ENGS = ("pe", "act", "dve", "pool", "sp")
D = 1024
NMETA = 16
EPS = 1e-6
IN_COLS = 2660
D_FF = 2816
D_FFE = 3584
NEXP = 8


class Buf:
    __slots__ = ("name", "w", "r")

    def __init__(self, name=""):
        self.name = name
        self.w = None
        self.r = {}


class DSem:
    __slots__ = ("h", "cnt", "key")

    def __init__(self, h, key):
        self.h = h
        self.cnt = 0
        self.key = key


class TT:
    __slots__ = ("t", "b")

    def __init__(self, t, b=None, name=""):
        self.t = t
        self.b = b if b is not None else Buf(name)

    def __getitem__(self, k):
        return self.t[k]


def _bufs(xs):
    out = []
    for x in xs:
        if x is None:
            continue
        out.append(x.b if isinstance(x, TT) else x)
    return out


class Prog:
    def __init__(self, nc, stack):
        self.nc = nc
        self.stack = stack
        self.q = {e: [] for e in ENGS}
        self.cnt = {e: 0 for e in ENGS}
        self.seen = {e: {} for e in ENGS}
        self.semh = {}
        for e in ENGS:
            h = stack.enter_context(nc.semaphore("es_" + e))
            self.semh[("E", e)] = h
        self.nd = 0
        self.all_dsems = []

    def dsem(self, name=None):
        self.nd += 1
        key = ("D", self.nd)
        h = self.stack.enter_context(self.nc.semaphore(name or f"ds{self.nd}"))
        self.semh[key] = h
        d = DSem(h, key)
        self.all_dsems.append(d)
        return d

    def _waits(self, eng, reads, writes, extra=()):
        need = {}

        def add(ev, raw):
            if ev is None:
                return
            k, v = ev
            if k == ("E", eng) and not raw:
                return
            if need.get(k, 0) < v:
                need[k] = v
        for b in reads:
            add(b.w, True)
        for b in writes:
            add(b.w, False)
            for k, v in b.r.items():
                add((k, v), False)
        for ev in extra:
            add(ev, True)
        out = []
        seen = self.seen[eng]
        for k, v in need.items():
            if seen.get(k, 0) < v:
                seen[k] = v
                out.append((k, v))
        return out

    def _mark(self, ev, reads, writes):
        k, v = ev
        for b in reads:
            if b.r.get(k, 0) < v:
                b.r[k] = v
        for b in writes:
            b.w = ev
            b.r = {}

    def op(self, eng, fn, reads=(), writes=()):
        reads = _bufs(reads)
        writes = _bufs(writes)
        waits = self._waits(eng, reads, writes)
        self.cnt[eng] += 1
        ev = (("E", eng), self.cnt[eng])
        self._mark(ev, reads, writes)
        self.q[eng].append((waits, fn, (("E", eng), 1)))
        return ev

    def dma(self, eng, out, in_, ds, reads=(), writes=(), newgroup=True, **kw):
        reads = _bufs(reads)
        writes = _bufs(writes)
        extra = ()
        if newgroup and ds.cnt > 0:
            extra = ((ds.key, ds.cnt),)
        waits = self._waits(eng, reads, writes, extra)
        ds.cnt += 16
        ev = (ds.key, ds.cnt)
        self._mark(ev, reads, writes)
        self.q[eng].append((waits, lambda e: e.dma_start(out=out, in_=in_, **kw), (ds.key, 16)))
        return ev

    def commit(self, ds, bufs):
        for b in _bufs(bufs):
            if b.w is not None and b.w[0] == ds.key:
                b.w = (ds.key, ds.cnt)
            if ds.key in b.r:
                b.r[ds.key] = ds.cnt

    def wait_all(self, eng, evs):
        waits = self._waits(eng, (), (), evs)
        self.q[eng].append((waits, None, None))

    def emit(self):
        nc = self.nc
        semh = self.semh
        with nc.Block() as block:
            def mk(ename):
                def body(e):
                    for waits, fn, inc in self.q[ename]:
                        for k, v in waits:
                            e.wait_ge(semh[k], v)
                        if fn is None:
                            continue
                        ins = fn(e)
                        if inc is not None:
                            ins.then_inc(semh[inc[0]], inc[1])
                return body
            block.tensor(mk("pe"))
            block.scalar(mk("act"))
            block.vector(mk("dve"))
            block.gpsimd(mk("pool"))
            block.sync(mk("sp"))


def make_consts(T):
    s = np.arange(128)[:, None]
    t = np.arange(128)[None, :]
    c = {}
    c["c_ident"] = np.eye(128, dtype=np.float32)
    c["c_triblk"] = ((s // 32 == t // 32) & (s <= t)).astype(np.float32)
    c["c_blkones"] = (s // 32 == t // 32).astype(np.float32)
    c["c_chunkind"] = (s // 32 == np.arange(4)[None, :]).astype(np.float32)
    c["c_trifull"] = (s <= t).astype(np.float32)
    c["c_sel127"] = np.broadcast_to((s == 127), (128, 128)).astype(np.float32).copy()
    c["c_sel15"] = np.broadcast_to((s == 15), (128, 128)).astype(np.float32).copy()
    c["c_sel64"] = np.broadcast_to((s == 64), (128, 128)).astype(np.float32).copy()
    c["c_sel8"] = np.broadcast_to((s == 8), (128, 128)).astype(np.float32).copy()
    c["c_mask_mla"] = (s // 64 <= t // 64).astype(np.float32)
    pos = np.arange(T, dtype=np.float32)
    inv = (10000.0 ** (-np.arange(16, dtype=np.float32) / 16)).astype(np.float32)
    ang = pos[:, None] * inv[None, :]
    c["c_cos"] = np.cos(ang).astype(np.float32)
    c["c_sin"] = np.sin(ang).astype(np.float32)
    return c


def tile_list(NT):
    return [(0, NMETA)] + [(NMETA + 128 * i, 128) for i in range(NT)]


class Builder:
    def __init__(self, NT, depth=2, debug=False, stop_after=None, moe_tokens=None):
        self.NT = NT
        self.T = NMETA + 128 * NT
        self.depth = depth
        self.debug = debug
        self.stop_after = stop_after
        self.tiles = tile_list(NT)
        self.nc = bass.Bass("TRN2", target_bir_lowering=False)
        self.inputs = {}
        self.outputs = {}
        self.moe_tokens = moe_tokens if moe_tokens is not None else (128 * NT) // 2

    def din(self, name, shape, dt=F32):
        t = self.nc.dram_tensor(name, list(shape), dt, kind="ExternalInput")
        self.inputs[name] = (tuple(shape), dt)
        return t.ap()

    def dscratch(self, name, shape, dt=F32, dbg=False):
        if dbg and self.debug:
            t = self.nc.dram_tensor(name, list(shape), dt, kind="ExternalOutput")
            self.outputs[name] = (tuple(shape), dt)
        else:
            t = self.nc.dram_tensor(name, list(shape), dt, kind="Internal")
        return t.ap()

    def dout(self, name, shape, dt=F32):
        t = self.nc.dram_tensor(name, list(shape), dt, kind="ExternalOutput")
        self.outputs[name] = (tuple(shape), dt)
        return t.ap()

    def sb(self, st, name, shape, dt=F32):
        self._uid = getattr(self, "_uid", 0) + 1
        return TT(st.enter_context(self.nc.sbuf_tensor(f"s{self._uid}_{name}", list(shape), dt)), name=name)

    def ring(self, st, name, shape, dt, n):
        return [self.sb(st, f"{name}{i}", shape, dt) for i in range(n)]

    def build(self):
        nc = self.nc
        T = self.T
        NT = self.NT
        depth = self.depth
        I = {}
        I["xin"] = self.din("xin", [T, D])
        I["sel"] = self.din("sel", [128, 2])
        for nm, shp in [("c_ident", [128, 128]), ("c_triblk", [128, 128]), ("c_blkones", [128, 128]),
                        ("c_chunkind", [128, 4]), ("c_trifull", [128, 128]), ("c_sel127", [128, 128]),
                        ("c_sel15", [128, 128]), ("c_sel64", [128, 128]), ("c_sel8", [128, 128]),
                        ("c_mask_mla", [128, 128]), ("c_cos", [T, 16]), ("c_sin", [T, 16])]:
            I[nm] = self.din(nm, shp)
        L = depth
        I["w_in"] = self.din("w_in", [L, D, IN_COLS])
        I["w_out"] = self.din("w_out", [L, D, D])
        I["g1T"] = self.din("g1T", [L, 128, 8])
        I["g2T"] = self.din("g2T", [L, 128, 8])
        I["og"] = self.din("og", [L, D])
        I["conv_wT"] = self.din("conv_wT", [L, 128, 2, 4])
        I["conv_b"] = self.din("conv_b", [L, 128, 2])
        I["lru_ba"] = self.din("lru_ba", [L, 128, 2])
        I["lru_bx"] = self.din("lru_bx", [L, 128, 2])
        I["lru_lam"] = self.din("lru_lam", [L, 128, 2])
        I["lru_wa"] = self.din("lru_wa", [L, 4, 64, 64])
        I["lru_wx"] = self.din("lru_wx", [L, 4, 64, 64])
        I["hg_lb"] = self.din("hg_lb", [2, 256])
        I["mla_gq"] = self.din("mla_gq", [L, 192])
        I["mla_w_uq"] = self.din("mla_w_uq", [L, 192, 384])
        I["mla_gkv"] = self.din("mla_gkv", [L, 128])
        I["mla_w_ukv"] = self.din("mla_w_ukv", [L, 128, 512])
        I["mla_gqn"] = self.din("mla_gqn", [L, 96])
        I["mla_gkn"] = self.din("mla_gkn", [L, 96])
        I["fox_gqn"] = self.din("fox_gqn", [L, 64])
        I["fox_gkn"] = self.din("fox_gkn", [L, 64])
        I["fox_bf"] = self.din("fox_bf", [L, 4])
        I["ffn_wg"] = self.din("ffn_wg", [D, D_FF])
        I["ffn_wu"] = self.din("ffn_wu", [D, D_FF])
        I["ffn_wd"] = self.din("ffn_wd", [D_FF, D])
        if depth > 1:
            I["moe_wr"] = self.din("moe_wr", [D, NEXP])
            I["moe_wg"] = self.din("moe_wg", [NEXP, D, D_FFE])
            I["moe_wu"] = self.din("moe_wu", [NEXP, D, D_FFE])
            I["moe_wd"] = self.din("moe_wd", [NEXP, D_FFE, D])
        self.I = I
        S = {}
        dbg = True
        S["hA"] = self.dscratch("hA", [T, D], F32, dbg)
        S["hmid"] = self.dscratch("hmid", [T, D], F32, dbg)
        S["Y"] = self.dscratch("Y", [T, D], F32, dbg)
        S["u2T"] = self.dscratch("u2T", [D, T], BF16, dbg)
        S["QTc"] = self.dscratch("QTc", [4, 96, T], BF16, dbg)
        S["KTc"] = self.dscratch("KTc", [4, 96, T], BF16, dbg)
        S["Vc"] = self.dscratch("Vc", [4, 128, NT + 1, 65], BF16, dbg)
        S["QTd"] = self.dscratch("QTd", [2, 128, T], BF16, dbg)
        S["KTd"] = self.dscratch("KTd", [2, 128, T], BF16, dbg)
        S["Vd"] = self.dscratch("Vd", [4, 128, NT + 1, 65], BF16, dbg)
        S["gateT"] = self.dscratch("gateT", [NEXP, T], F32, dbg)
        self.S = S
        n_out = self.moe_tokens if depth > 1 else 128 * NT
        self.out_ap = self.dout("out", [n_out, D])

        with contextlib.ExitStack() as st:
            P = Prog(nc, st)
            self.P = P
            self.bank = [TT(st.enter_context(nc.psum_tensor(f"bank{i}", [128, 512], F32)), name=f"bank{i}")
                         for i in range(8)]
            with contextlib.ExitStack() as cst:
                self.load_consts(cst)
                for l in range(depth):
                    hsrc = I["xin"] if l == 0 else S["hA"]
                    self.phase_1a(l, hsrc)
                    if self.stop_after == ("1a", l):
                        break
                    self.phase_1b(l)
                    if self.stop_after == ("1b", l):
                        break
                    self.phase_1c(l, hsrc)
                    if self.stop_after == ("1c", l):
                        break
                    if l == 0:
                        self.phase_ffn(S["hA"])
                    else:
                        self.phase_moe()
            P.wait_all("sp", self.final_evs)
            P.emit()
        return nc

    def load_consts(self, st):
        P = self.P
        I = self.I
        self.final_evs = []
        self.ds_const = P.dsem("ds_const")
        C = {}
        for nm in ["c_ident", "c_triblk", "c_blkones", "c_trifull", "c_sel127", "c_sel15", "c_sel64", "c_sel8",
                   "c_mask_mla"]:
            C[nm] = self.sb(st, nm, [128, 128], F32)
            P.dma("sp", C[nm][:], I[nm][:, :], self.ds_const, writes=[C[nm]], newgroup=False)
        C["c_chunkind"] = self.sb(st, "c_chunkind", [128, 4], F32)
        P.dma("sp", C["c_chunkind"][:], I["c_chunkind"][:, :], self.ds_const, writes=[C["c_chunkind"]], newgroup=False)
        C["sel"] = self.sb(st, "sel", [128, 2], F32)
        P.dma("sp", C["sel"][:], I["sel"][:, :], self.ds_const, writes=[C["sel"]], newgroup=False)
        P.commit(self.ds_const, list(C.values()))
        C["identb"] = self.sb(st, "identb", [128, 128], BF16)
        P.op("pool", lambda e: e.tensor_copy(out=C["identb"][:], in_=C["c_ident"][:]), reads=[C["c_ident"]], writes=[C["identb"]])
        C["hgmask"] = self.sb(st, "hgmask", [128, 128], BF16)
        P.op("pool", lambda e: e.tensor_copy(out=C["hgmask"][:], in_=C["c_triblk"][:]), reads=[C["c_triblk"]], writes=[C["hgmask"]])
        C["foxmask"] = self.sb(st, "foxmask", [128, 128], BF16)
        P.op("pool", lambda e: e.tensor_copy(out=C["foxmask"][:], in_=C["c_trifull"][:]), reads=[C["c_trifull"]], writes=[C["foxmask"]])
        C["mlamask"] = self.sb(st, "mlamask", [128, 128], BF16)
        P.op("pool", lambda e: e.tensor_copy(out=C["mlamask"][:], in_=C["c_mask_mla"][:]), reads=[C["c_mask_mla"]], writes=[C["mlamask"]])
        self.C = C
        self.call = self.sb(st, "c_all", [128, self.NT + 1, 4], F32)
        P.op("pool", lambda e: e.memset(self.call[:], 0.0), writes=[self.call])

    def mm(self, out, lhsT, rhs, start, stop, reads, writes, **kw):
        self.P.op("pe", lambda e: e.matmul(out, lhsT=lhsT, rhs=rhs, start=start, stop=stop, **kw), reads=reads, writes=writes)

    def tr(self, out, in_, ident, reads, writes):
        self.P.op("pe", lambda e: e.transpose(out=out, in_=in_, identity=ident), reads=reads, writes=writes)

    def act(self, out, in_, func, reads, writes, **kw):
        self.P.op("act", lambda e: e.activation(out=out, in_=in_, func=func, **kw), reads=reads, writes=writes)

    def tt(self, eng, out, in0, in1, op, reads, writes):
        self.P.op(eng, lambda e: e.tensor_tensor(out=out, in0=in0, in1=in1, op=op), reads=reads, writes=writes)

    def ts(self, eng, out, in0, s1, s2, op0, op1, reads, writes):
        if op1 is None:
            self.P.op(eng, lambda e: e.tensor_scalar(out=out, in0=in0, scalar1=s1, scalar2=None, op0=op0), reads=reads, writes=writes)
        else:
            self.P.op(eng, lambda e: e.tensor_scalar(out=out, in0=in0, scalar1=s1, scalar2=s2, op0=op0, op1=op1), reads=reads, writes=writes)

    def stt(self, eng, out, in0, scalar, in1, op0, op1, reads, writes):
        self.P.op(eng, lambda e: e.scalar_tensor_tensor(out=out, in0=in0, scalar=scalar, in1=in1, op0=op0, op1=op1), reads=reads, writes=writes)

    def cp(self, eng, out, in_, reads, writes):
        if eng == "act":
            self.P.op("act", lambda e: e.copy(out=out, in_=in_), reads=reads, writes=writes)
        else:
            self.P.op(eng, lambda e: e.tensor_copy(out=out, in_=in_), reads=reads, writes=writes)

    def rstd(self, ss, out, n, scale, reads_w):
        self.act(out, ss, AF.Sqrt, reads=reads_w, writes=reads_w, scale=scale, bias=EPS)
        self.P.op("dve", lambda e: e.reciprocal(out=out, in_=out), reads=reads_w, writes=reads_w)

    def phase_1a(self, l, hsrc):
        P, I, S, C, nc = self.P, self.I, self.S, self.C, self.nc
        bank = self.bank
        NT, T = self.NT, self.T
        call = self.call
        with contextlib.ExitStack() as st:
            sb = lambda name, shape, dt=F32: self.sb(st, "a_" + name, shape, dt)
            dsw = P.dsem()
            dsl = P.dsem()
            w_in = sb("w_in", [128, 8, IN_COLS], BF16)
            wv = I["w_in"][l].rearrange("(k p) c -> p k c", p=128)
            for k in range(8):
                P.dma("pool", w_in[:, k, :], wv[:, k, :], dsw, writes=[w_in], newgroup=False)
            wuq = sb("wuq", [128, 2, 384], BF16)
            P.dma("pool", wuq[:, 0, :], I["mla_w_uq"][l, 0:128, :], dsw, writes=[wuq], newgroup=False)
            P.dma("pool", wuq[0:64, 1, :], I["mla_w_uq"][l, 128:192, :], dsw, writes=[wuq], newgroup=False)
            wukv = sb("wukv", [128, 512], BF16)
            P.dma("pool", wukv[:], I["mla_w_ukv"][l, :, :], dsw, writes=[wukv], newgroup=False)
            g1T = sb("g1T", [128, 8])
            P.dma("sp", g1T[:], I["g1T"][l], dsl, writes=[g1T], newgroup=False)
            cw = sb("cw", [128, 2, 4]); cb = sb("cb", [128, 2]); ba = sb("ba", [128, 2]); bx = sb("bx", [128, 2])
            lam = sb("lam", [128, 2])
            for tdst, nm in [(cw, "conv_wT"), (cb, "conv_b"), (ba, "lru_ba"), (bx, "lru_bx"), (lam, "lru_lam")]:
                P.dma("sp", tdst[:], I[nm][l], dsl, writes=[tdst], newgroup=False)
            waf = sb("waf", [128, 2, 128]); wxf = sb("wxf", [128, 2, 128])
            wab = sb("wab", [128, 2, 128], BF16); wxb = sb("wxb", [128, 2, 128], BF16)
            P.op("pool", lambda e: e.memset(waf[:], 0.0), writes=[waf])
            P.op("pool", lambda e: e.memset(wxf[:], 0.0), writes=[wxf])
            for nb in range(4):
                r0 = (nb % 2) * 64
                P.dma("sp", waf[r0:r0 + 64, nb // 2, r0:r0 + 64], I["lru_wa"][l, nb], dsl, writes=[waf], newgroup=False)
                P.dma("sp", wxf[r0:r0 + 64, nb // 2, r0:r0 + 64], I["lru_wx"][l, nb], dsl, writes=[wxf], newgroup=False)
            defer = []
            defer.append(lambda: self.cp("pool", wab[:], waf[:], [waf], [wab]))
            defer.append(lambda: self.cp("pool", wxb[:], wxf[:], [wxf], [wxb]))
            cA = sb("cA", [128, 2]); cA2 = sb("cA2", [128, 2])
            defer.append(lambda: self.act(cA[:], lam[:], AF.Exp, [lam], [cA], scale=-1.0))
            defer.append(lambda: self.act(cA[:], cA[:], AF.Ln, [cA], [cA], bias=1.0))
            defer.append(lambda: self.ts("dve", cA2[:], cA[:], -16.0, None, ALU.mult, None, [cA], [cA2]))
            defer.append(lambda: self.ts("dve", cA[:], cA[:], -8.0, None, ALU.mult, None, [cA], [cA]))
            bcs = []

            def bc(name, src, w):
                t = sb(name, [128, w])
                P.dma("sp", t[:], src.partition_broadcast(128), dsl, writes=[t], newgroup=False)
                bcs.append(t)
                return t
            gq_bc = bc("gq_bc", I["mla_gq"][l], 192)
            gkv_bc = bc("gkv_bc", I["mla_gkv"][l], 128)
            gqn_bc = bc("gqn_bc", I["mla_gqn"][l], 96)
            gkn_bc = bc("gkn_bc", I["mla_gkn"][l], 96)
            fgq_bc = bc("fgq_bc", I["fox_gqn"][l], 64)
            fgk_bc = bc("fgk_bc", I["fox_gkn"][l], 64)
            fbf_bc = bc("fbf_bc", I["fox_bf"][l], 4)
            lb_bc = sb("lb_bc", [128, 256]); oml_bc = sb("oml_bc", [128, 256])
            if l == 0:
                P.op("pool", lambda e: e.memset(lb_bc[:], 0.0), writes=[lb_bc])
                P.op("pool", lambda e: e.memset(oml_bc[:], 1.0), writes=[oml_bc])
            else:
                lg0 = bc("lg0", I["hg_lb"][0], 256)
                lg1 = bc("lg1", I["hg_lb"][1], 256)
                defer.append(lambda: self.tt("dve", lb_bc[:], lg1[:], lg0[:], ALU.subtract, [lg0, lg1], [lb_bc]))
                defer.append(lambda: self.act(lb_bc[:], lb_bc[:], AF.Sigmoid, [lb_bc], [lb_bc]))
                defer.append(lambda: self.ts("dve", oml_bc[:], lb_bc[:], -1.0, 1.0, ALU.mult, ALU.add, [lb_bc], [oml_bc]))
            P.commit(dsw, [w_in, wuq, wukv])
            P.commit(dsl, [g1T, cw, cb, ba, bx, lam, waf, wxf] + bcs)
            for fn in defer:
                fn()
            xe = sb("xe", [128, 2, 3 + 128])
            P.op("pool", lambda e: e.memset(xe[:], 0.0), writes=[xe])
            hprev = sb("hprev", [128, 2])
            P.op("pool", lambda e: e.memset(hprev[:], 0.0), writes=[hprev])
            Sst = [sb(f"Sst{h}", [64, 64]) for h in range(4)]
            for h in range(4):
                P.op("pool", lambda e, h=h: e.memset(Sst[h][:], 0.0), writes=[Sst[h]])
            Sbf = [[sb(f"Sbf{h}_{r}", [64, 64], BF16) for r in range(2)] for h in range(4)]
            hqm = [sb(f"hqm{c}", [64, 4, 128], BF16) for c in range(4)]
            hkem = [sb(f"hkem{c}", [128, 256], BF16) for c in range(4)]
            for c in range(4):
                P.op("pool", lambda e, c=c: e.memset(hqm[c][:], 0.0), writes=[hqm[c]])
            R2 = 2
            ht = self.ring(st, "a_ht", [128, D], F32, R2)
            junk = sb("junk", [128, D], BF16)
            ss = self.ring(st, "a_ss", [128, 8], F32, R2)
            ub = self.ring(st, "a_ub", [128, D], BF16, R2)
            uT = self.ring(st, "a_uT", [128, 8, 128], BF16, R2)
            zt = self.ring(st, "a_zt", [128, 2148], F32, R2)
            ysb = self.ring(st, "a_ysb", [128, 512], F32, R2)
            cs = self.ring(st, "a_cs", [128, 32], F32, R2)
            dsh = [P.dsem() for _ in range(R2)]
            dsy = [P.dsem() for _ in range(R2)]
            dsq = [P.dsem() for _ in range(R2)]
            lu = sb("lu", [128, 128]); lub = sb("lub", [128, 128], BF16)
            lr = sb("lr", [128, 128]); li = sb("li", [128, 128]); la = sb("la", [128, 128]); lm = sb("lm", [128, 128])
            lbb = sb("lbb", [128, 128]); lhs = sb("lhs", [128, 128]); lg = sb("lg", [128, 128]); lya = sb("lya", [128, 2, 128])
            hsig = sb("hsig", [128, 256]); hlogf = sb("hlogf", [128, 256]); hkk = sb("hkk", [128, 256])
            heb = sb("heb", [128, 256]); henb = sb("henb", [128, 256]); hebe = sb("hebe", [128, 256])
            hqd = sb("hqd", [128, 256], BF16); hkd = sb("hkd", [128, 256], BF16); hke = sb("hke", [128, 256], BF16)
            hkdf = sb("hkdf", [128, 256]); hvb = sb("hvb", [128, 256], BF16); hsg = sb("hsg", [128, 256])
            hqdT = sb("hqdT", [64, 4, 128], BF16); hkdT = sb("hkdT", [64, 4, 128], BF16)
            hdec = sb("hdec", [64, 4, 4]); hattm = sb("hattm", [128, 4, 128], BF16)
            mjunk = sb("mjunk", [128, 512]); mss = sb("mss", [128, 16])
            cqn = sb("cqn", [128, 192], BF16); ckvn = sb("ckvn", [128, 128], BF16)
            cqT = sb("cqT", [128, 2, 128], BF16); ckvT = sb("ckvT", [128, 128], BF16)
            qf = sb("qf", [128, 4, 96]); kf = sb("kf", [128, 4, 96])
            qb = sb("qb", [128, 4, 96], BF16); kb = sb("kb", [128, 4, 96], BF16)
            rt = sb("rt", [128, 4, 4, 16])
            vaug = self.ring(st, "a_vaug", [128, 4, 65], BF16, R2)
            qkT = self.ring(st, "a_qkT", [96, 8, 128], BF16, R2)
            fqf = sb("fqf", [128, 4, 64]); fqb = sb("fqb", [128, 256], BF16); fkb = sb("fkb", [128, 256], BF16)
            fvaug = self.ring(st, "a_fvaug", [128, 4, 65], BF16, R2)
            fqkT = self.ring(st, "a_fqkT", [128, 4, 128], BF16, R2)
            flog = sb("flog", [128, 4])
            for r in range(R2):
                P.op("pool", lambda e, r=r: e.memset(vaug[r][:], 1.0), writes=[vaug[r]])
                P.op("pool", lambda e, r=r: e.memset(fvaug[r][:], 1.0), writes=[fvaug[r]])
            identb = C["identb"]; identf = C["c_ident"]

            for j, (p0, n) in enumerate(self.tiles):
                r = j % R2
                H, SS, UB, UT, ZT, YS, CS = ht[r], ss[r], ub[r], uT[r], zt[r], ysb[r], cs[r]
                def load(jn):
                    pp, nn = self.tiles[jn]
                    rr = jn % R2
                    P.dma("sp", ht[rr][0:nn, :], hsrc[pp:pp + nn, :], dsh[rr], writes=[ht[rr]])
                    P.dma("sp", cs[rr][0:nn, 0:16], I["c_cos"][pp:pp + nn, :], dsh[rr], writes=[cs[rr]], newgroup=False)
                    P.dma("sp", cs[rr][0:nn, 16:32], I["c_sin"][pp:pp + nn, :], dsh[rr], writes=[cs[rr]], newgroup=False)
                    P.commit(dsh[rr], [ht[rr], cs[rr]])
                if j == 0:
                    load(0)
                if j + 1 < len(self.tiles):
                    load(j + 1)
                P.op("dve", lambda e, SS=SS: e.memset(SS[:], 0.0), writes=[SS])
                self.act(junk[0:n, :], H[0:n, :], AF.Square, [H, SS], [junk, SS], accum_out=SS[0:n, 0:1])
                self.act(SS[0:n, 1:2], SS[0:n, 0:1], AF.Sqrt, [SS], [SS], scale=1.0 / D, bias=EPS)
                P.op("dve", lambda e, SS=SS, n=n: e.reciprocal(out=SS[0:n, 1:2], in_=SS[0:n, 1:2]), reads=[SS], writes=[SS])
                self.act(UB[0:n, :], H[0:n, :], AF.Copy, [H, SS], [UB], scale=SS[0:n, 1:2])
                b0 = bank[0]
                b0v = b0.t[:].bitcast(BF16)
                for k in range(8):
                    self.tr(b0v[:, k * 128:k * 128 + n], UB[0:n, k * 128:(k + 1) * 128], identb[0:n, 0:n], [UB, identb], [b0])
                self.tt("dve", UT[:, :, 0:n], b0v[:, :].rearrange("p (k t) -> p k t", k=8)[:, :, 0:n],
                        g1T[:, :].unsqueeze(2).broadcast_to([128, 8, n]), ALU.mult, [g1T], [UT, b0])
                b1 = bank[1]
                for m in range(4):
                    for k in range(8):
                        self.mm(b1[:, m * 128:m * 128 + n], w_in[:, k, m * 128:(m + 1) * 128], UT[:, k, 0:n], k == 0, k == 7, [w_in, UT], [b1])
                chunks = [(512, 1024), (1024, 1536), (1536, 1888), (1888, 2400), (2400, 2660)]
                for ci, (c0, c1) in enumerate(chunks):
                    bz = bank[2 + (ci % 2)]
                    wd = c1 - c0
                    for k in range(8):
                        self.mm(bz[0:n, 0:wd], UT[:, k, 0:n], w_in[:, k, c0:c1], k == 0, k == 7, [UT, w_in], [bz])
                    self.cp("act" if ci % 2 == 0 else "dve", ZT[0:n, c0 - 512:c1 - 512], bz[0:n, 0:wd], [], [ZT, bz])
                steps = os.environ.get("K_STEPS", "lru,hgrn,mla,fox").split(",")
                for cc in (range(2) if "lru" in steps else []):
                    self.cp("act", xe[:, cc, 3:3 + n], b1[:, cc * 128:cc * 128 + n], [], [xe, b1])
                    self.ts("dve", lu[:, 0:n], xe[:, cc, 0:n], cw[:, cc, 0:1], cb[:, cc:cc + 1], ALU.mult, ALU.add, [xe, cw, cb], [lu])
                    for jj in range(1, 4):
                        self.stt("dve", lu[:, 0:n], xe[:, cc, jj:jj + n], cw[:, cc, jj:jj + 1], lu[:, 0:n], ALU.mult, ALU.add, [xe, cw, lu], [lu])
                    self.cp("pool", xe[:, cc, 0:3], xe[:, cc, n:n + 3], [xe], [xe])
                    self.cp("pool", lub[:, 0:n], lu[:, 0:n], [lu], [lub])
                    b4 = bank[4]
                    self.mm(b4[:, 0:n], wab[:, cc, :], lub[:, 0:n], True, True, [wab, lub], [b4])
                    self.mm(b4[:, 128:128 + n], wxb[:, cc, :], lub[:, 0:n], True, True, [wxb, lub], [b4])
                    self.act(lr[:, 0:n], b4[:, 0:n], AF.Sigmoid, [ba], [lr, b4], bias=ba[:, cc:cc + 1])
                    self.act(li[:, 0:n], b4[:, 128:128 + n], AF.Sigmoid, [bx], [li, b4], bias=bx[:, cc:cc + 1])
                    self.act(la[:, 0:n], lr[:, 0:n], AF.Exp, [lr, cA], [la], scale=cA[:, cc:cc + 1])
                    self.act(lm[:, 0:n], lr[:, 0:n], AF.Exp, [lr, cA2], [lm], scale=cA2[:, cc:cc + 1])
                    self.act(lm[:, 0:n], lm[:, 0:n], AF.Sqrt, [lm], [lm], scale=-1.0, bias=1.0)
                    self.tt("dve", lbb[:, 0:n], lm[:, 0:n], li[:, 0:n], ALU.mult, [lm, li], [lbb])
                    self.tt("dve", lbb[:, 0:n], lbb[:, 0:n], lu[:, 0:n], ALU.mult, [lbb, lu], [lbb])
                    P.op("dve", lambda e, n=n, cc=cc: e.tensor_tensor_scan(out=lhs[:, 0:n], data0=la[:, 0:n], data1=lbb[:, 0:n],
                                                                        initial=hprev[:, cc:cc + 1], op0=ALU.mult, op1=ALU.add),
                         reads=[la, lbb, hprev], writes=[lhs])
                    self.cp("dve", hprev[:, cc:cc + 1], lhs[:, n - 1:n], [lhs], [hprev])
                    self.act(lg[:, 0:n], b1[:, (2 + cc) * 128:(2 + cc) * 128 + n], AF.Gelu_apprx_tanh, [], [lg, b1])
                    self.tt("dve", lya[:, cc, 0:n], lhs[:, 0:n], lg[:, 0:n], ALU.mult, [lhs, lg], [lya])
                b5 = bank[5]
                for cc in (range(2) if "lru" in steps else []):
                    self.tr(b5[0:n, cc * 128:(cc + 1) * 128], lya[:, cc, 0:n], identf[:, :], [lya, identf], [b5])
                if "lru" in steps:
                    self.cp("act", YS[0:n, 0:256], b5[0:n, 0:256], [], [YS, b5])
                if "hgrn" in steps:
                    nch = (n + 31) // 32
                    hq_ = ZT[0:n, 0:256]; hf_ = ZT[0:n, 256:512]; hv_ = ZT[0:n, 512:768]; hg_ = ZT[0:n, 768:1024]
                    self.act(hsig[0:n, :], hf_, AF.Sigmoid, [ZT], [hsig])
                    self.tt("dve", hsig[0:n, :], hsig[0:n, :], oml_bc[0:n, :], ALU.mult, [hsig, oml_bc], [hsig])
                    self.tt("dve", hsig[0:n, :], hsig[0:n, :], lb_bc[0:n, :], ALU.add, [hsig, lb_bc], [hsig])
                    self.act(hlogf[0:n, :], hsig[0:n, :], AF.Ln, [hsig], [hlogf])
                    self.ts("pool", hkk[0:n, :], hsig[0:n, :], -1.0, 1.0, ALU.mult, ALU.add, [hsig], [hkk])
                    b4 = bank[4]
                    self.mm(b4[0:n, 0:256], C["c_triblk"][0:n, 0:n], hlogf[0:n, :], True, True, [C["c_triblk"], hlogf], [b4])
                    b6 = bank[6]
                    self.mm(b6[0:n, 0:256], C["c_blkones"][0:n, 0:n], hlogf[0:n, :], True, True, [C["c_blkones"], hlogf], [b6])
                    self.act(heb[0:n, :], b4[0:n, 0:256], AF.Exp, [], [heb, b4])
                    self.act(henb[0:n, :], b4[0:n, 0:256], AF.Exp, [], [henb, b4], scale=-1.0)
                    self.act(hebe[0:n, :], b6[0:n, 0:256], AF.Exp, [], [hebe, b6])
                    self.tt("dve", hqd[0:n, :], hq_, heb[0:n, :], ALU.mult, [ZT, heb], [hqd])
                    self.tt("dve", hkdf[0:n, :], hkk[0:n, :], henb[0:n, :], ALU.mult, [hkk, henb], [hkdf])
                    self.cp("pool", hkd[0:n, :], hkdf[0:n, :], [hkdf], [hkd])
                    self.tt("dve", hke[0:n, :], hkdf[0:n, :], hebe[0:n, :], ALU.mult, [hkdf, hebe], [hke])
                    self.cp("pool", hvb[0:n, :], hv_, [ZT], [hvb])
                    self.act(hsg[0:n, :], hg_, AF.Silu, [ZT], [hsg])
                    b7 = bank[7]
                    for h in range(4):
                        self.mm(b7[0:64, h * 4:h * 4 + nch], hlogf[0:n, h * 64:(h + 1) * 64], C["c_chunkind"][0:n, 0:nch], True, True,
                                [hlogf, C["c_chunkind"]], [b7])
                    self.act(hdec[:, :, 0:nch], b7[0:64, 0:16].rearrange("p (a c) -> p a c", a=4)[:, :, 0:nch], AF.Exp, [], [hdec, b7])
                    b5v = b5.t[:].bitcast(BF16)
                    for h in range(4):
                        self.tr(b5v[0:64, h * 128:h * 128 + n], hqd[0:n, h * 64:(h + 1) * 64], identb[0:n, 0:n], [hqd, identb], [b5])
                        self.tr(b5v[0:64, 512 + h * 128:512 + h * 128 + n], hkd[0:n, h * 64:(h + 1) * 64], identb[0:n, 0:n], [hkd, identb], [b5])
                    b5q = b5v[0:64, 0:512].rearrange("p (a t) -> p a t", a=4)
                    self.cp("act", hqdT[:, :, 0:n], b5q[:, :, 0:n], [], [hqdT, b5])
                    self.cp("act", hkdT[:, :, 0:n], b5v[0:64, 512:1024].rearrange("p (a t) -> p a t", a=4)[:, :, 0:n], [], [hkdT, b5])
                    for c in range(nch):
                        cn = min(32, n - 32 * c)
                        self.cp("pool", hqm[c][:, :, 32 * c:32 * c + cn], hqdT[:, :, 32 * c:32 * c + cn], [hqdT], [hqm[c]])
                        self.ts("dve" if c % 2 == 0 else "pool", hkem[c][0:n, :], hke[0:n, :], C["c_chunkind"][0:n, c:c + 1], None, ALU.mult, None,
                                [hke, C["c_chunkind"]], [hkem[c]])
                    for h in range(4):
                        self.mm(b6[0:n, h * 128:h * 128 + n], hkdT[:, h, 0:n], hqdT[:, h, 0:n], True, True, [hkdT, hqdT], [b6])
                    self.tt("dve", hattm[0:n, :, 0:n], b6[0:n, :].rearrange("p (h t) -> p h t", h=4)[:, :, 0:n],
                            C["hgmask"][0:n, 0:n].unsqueeze(1).broadcast_to([n, 4, n]), ALU.mult, [C["hgmask"]], [hattm, b6])
                    for h in range(4):
                        bu = bank[2 + h // 2]
                        for c in range(nch):
                            col = ((h % 2) * 4 + c) * 64
                            self.mm(bu[0:64, col:col + 64], hkem[c][0:n, h * 64:(h + 1) * 64], hvb[0:n, h * 64:(h + 1) * 64], True, True,
                                    [hkem[c], hvb], [bu])
                    for h in range(4):
                        self.mm(b4[0:n, h * 64:(h + 1) * 64], hattm[0:n, h, 0:n], hvb[0:n, h * 64:(h + 1) * 64], h == 0, False, [hattm, hvb], [b4],
                                skip_group_check=True)
                    for h in range(4):
                        bu = bank[2 + h // 2]
                        for c in range(nch):
                            gidx = (j - 1) * 4 + c + 1 if j > 0 else 0
                            slot_prev = Sbf[h][(gidx - 1) % 2]
                            if gidx > 0:
                                self.mm(b4[0:n, h * 64:(h + 1) * 64], hqm[c][:, h, 0:n], slot_prev[:, :], False, True, [hqm[c], slot_prev], [b4],
                                        skip_group_check=True)
                            col = ((h % 2) * 4 + c) * 64
                            self.stt("dve", Sst[h][:], Sst[h][:], hdec[:, h, c:c + 1], bu[0:64, col:col + 64], ALU.mult, ALU.add,
                                     [Sst[h], hdec], [Sst[h], bu])
                            slot = Sbf[h][gidx % 2]
                            self.cp("pool", slot[:], Sst[h][:], [Sst[h]], [slot])
                    self.tt("dve", YS[0:n, 256:512], b4[0:n, 0:256], hsg[0:n, :], ALU.mult, [hsg], [YS, b4])
                    P.dma("sp", S["Y"][p0:p0 + n, 0:512], YS[0:n, :], dsy[r], reads=[YS], writes=[Buf()])
                if "mla" in steps:
                    cq_ = ZT[0:n, 1024:1216]; ckv_ = ZT[0:n, 1216:1344]; kr_ = ZT[0:n, 1344:1376]
                    P.op("dve", lambda e: e.memset(mss[:], 0.0), writes=[mss])
                    self.act(mjunk[0:n, 0:192], cq_, AF.Square, [ZT, mss], [mjunk, mss], accum_out=mss[0:n, 0:1])
                    self.act(mjunk[0:n, 0:128], ckv_, AF.Square, [ZT, mss], [mjunk, mss], accum_out=mss[0:n, 1:2])
                    self.act(mss[0:n, 2:3], mss[0:n, 0:1], AF.Sqrt, [mss], [mss], scale=1.0 / 192, bias=EPS)
                    self.act(mss[0:n, 3:4], mss[0:n, 1:2], AF.Sqrt, [mss], [mss], scale=1.0 / 128, bias=EPS)
                    P.op("dve", lambda e, n=n: e.reciprocal(out=mss[0:n, 2:4], in_=mss[0:n, 2:4]), reads=[mss], writes=[mss])
                    self.stt("dve", cqn[0:n, :], cq_, mss[0:n, 2:3], gq_bc[0:n, :], ALU.mult, ALU.mult, [ZT, mss, gq_bc], [cqn])
                    self.stt("dve", ckvn[0:n, :], ckv_, mss[0:n, 3:4], gkv_bc[0:n, :], ALU.mult, ALU.mult, [ZT, mss, gkv_bc], [ckvn])
                    self.tr(b5v[:, 0:n], cqn[0:n, 0:128], identb[0:n, 0:n], [cqn, identb], [b5])
                    self.tr(b5v[0:64, 128:128 + n], cqn[0:n, 128:192], identb[0:n, 0:n], [cqn, identb], [b5])
                    self.tr(b5v[:, 256:256 + n], ckvn[0:n, 0:128], identb[0:n, 0:n], [ckvn, identb], [b5])
                    self.cp("act", cqT[:, 0, 0:n], b5v[:, 0:n], [], [cqT, b5])
                    self.cp("act", cqT[0:64, 1, 0:n], b5v[0:64, 128:128 + n], [], [cqT, b5])
                    self.cp("act", ckvT[:, 0:n], b5v[:, 256:256 + n], [], [ckvT, b5])
                    self.mm(b6[0:n, 0:384], cqT[:, 0, 0:n], wuq[:, 0, :], True, False, [cqT, wuq], [b6])
                    self.mm(b6[0:n, 0:384], cqT[0:64, 1, 0:n], wuq[0:64, 1, :], False, True, [cqT, wuq], [b6])
                    self.mm(b7[0:n, 0:512], ckvT[:, 0:n], wukv[:, :], True, True, [ckvT, wukv], [b7])
                    self.cp("act", qf[0:n, :, :], b6[0:n, 0:384].rearrange("p (h c) -> p h c", h=4), [], [qf, b6])
                    b7h = b7[0:n, 0:512].rearrange("p (h c) -> p h c", h=4)
                    self.cp("dve", kf[0:n, :, 0:64], b7h[:, :, 0:64], [], [kf, b7])
                    VA = vaug[r]
                    self.cp("act", VA[0:n, :, 0:64], b7h[:, :, 64:128], [], [VA, b7])
                    self.cp("pool", kf[0:n, :, 64:96], kr_.unsqueeze(1).broadcast_to([n, 4, 32]), [ZT], [kf])
                    for (src, gbc, dst, col) in [(qf, gqn_bc, qb, 4), (kf, gkn_bc, kb, 8)]:
                        self.act(mjunk[0:n, 0:384], src[0:n, :, :].rearrange("p h c -> p (h c)"), AF.Square, [src], [mjunk])
                        P.op("dve", lambda e, n=n, col=col: e.tensor_reduce(out=mss[0:n, col:col + 4], in_=mjunk[0:n, 0:384].rearrange("p (h c) -> p h c", h=4),
                                                                        axis=AX.X, op=ALU.add), reads=[mjunk], writes=[mss])
                        self.act(mss[0:n, col:col + 4], mss[0:n, col:col + 4], AF.Sqrt, [mss], [mss], scale=1.0 / 96, bias=EPS)
                        P.op("dve", lambda e, n=n, col=col: e.reciprocal(out=mss[0:n, col:col + 4], in_=mss[0:n, col:col + 4]), reads=[mss], writes=[mss])
                        self.tt("dve", src[0:n, :, :], src[0:n, :, :], mss[0:n, col:col + 4].unsqueeze(2).broadcast_to([n, 4, 96]), ALU.mult, [src, mss], [src])
                        self.tt("pool", src[0:n, :, :], src[0:n, :, :], gbc[0:n, :].unsqueeze(1).broadcast_to([n, 4, 96]), ALU.mult, [src, gbc], [src])
                        cosb = CS[0:n, 0:16].unsqueeze(1).broadcast_to([n, 4, 16])
                        sinb = CS[0:n, 16:32].unsqueeze(1).broadcast_to([n, 4, 16])
                        t1 = src[0:n, :, 64:80]; t2 = src[0:n, :, 80:96]
                        self.tt("dve", rt[0:n, :, 0, :], t1, cosb, ALU.mult, [src, CS], [rt])
                        self.tt("pool", rt[0:n, :, 1, :], t2, sinb, ALU.mult, [src, CS], [rt])
                        self.tt("dve", rt[0:n, :, 2, :], t1, sinb, ALU.mult, [src, CS], [rt])
                        self.tt("pool", rt[0:n, :, 3, :], t2, cosb, ALU.mult, [src, CS], [rt])
                        self.cp("act", dst[0:n, :, 0:64], src[0:n, :, 0:64], [src], [dst])
                        self.tt("dve", dst[0:n, :, 64:80], rt[0:n, :, 0, :], rt[0:n, :, 1, :], ALU.subtract, [rt], [dst])
                        self.tt("dve", dst[0:n, :, 80:96], rt[0:n, :, 2, :], rt[0:n, :, 3, :], ALU.add, [rt], [dst])
                    QK = qkT[r]
                    for h in range(4):
                        self.tr(b5v[0:96, h * 128:h * 128 + n], qb[0:n, h, :], identb[0:n, 0:n], [qb, identb], [b5])
                        self.tr(b5v[0:96, (4 + h) * 128:(4 + h) * 128 + n], kb[0:n, h, :], identb[0:n, 0:n], [kb, identb], [b5])
                    self.cp("act", QK[:, :, 0:n], b5v[0:96, :].rearrange("p (a t) -> p a t", a=8)[:, :, 0:n], [], [QK, b5])
                    P.dma("sp", S["QTc"][:, :, p0:p0 + n].rearrange("h d t -> d h t"), QK[:, 0:4, 0:n], dsq[r], reads=[QK], writes=[Buf()])
                    P.dma("sp", S["KTc"][:, :, p0:p0 + n].rearrange("h d t -> d h t"), QK[:, 4:8, 0:n], dsq[r], reads=[QK], writes=[Buf()], newgroup=False)
                    P.dma("sp", S["Vc"][:, 0:n, j, :].rearrange("h p c -> p h c"), VA[0:n, :, :], dsq[r], reads=[VA], writes=[Buf()], newgroup=False)
                if "fox" in steps:
                    fq_ = ZT[0:n, 1376:1632]; fk_ = ZT[0:n, 1632:1888]; fv_ = ZT[0:n, 1888:2144]; ff_ = ZT[0:n, 2144:2148]
                    for (src, gbc, dst, col) in [(fq_, fgq_bc, fqb, 12), (fk_, fgk_bc, fkb, 12)]:
                        self.act(mjunk[0:n, 0:256], src, AF.Square, [ZT], [mjunk])
                        P.op("dve", lambda e, n=n, col=col: e.tensor_reduce(out=mss[0:n, col:col + 4], in_=mjunk[0:n, 0:256].rearrange("p (h c) -> p h c", h=4),
                                                                        axis=AX.X, op=ALU.add), reads=[mjunk], writes=[mss])
                        self.act(mss[0:n, col:col + 4], mss[0:n, col:col + 4], AF.Sqrt, [mss], [mss], scale=1.0 / 64, bias=EPS)
                        P.op("dve", lambda e, n=n, col=col: e.reciprocal(out=mss[0:n, col:col + 4], in_=mss[0:n, col:col + 4]), reads=[mss], writes=[mss])
                        self.tt("dve", fqf[0:n, :, :], src.rearrange("p (h c) -> p h c", h=4), mss[0:n, col:col + 4].unsqueeze(2).broadcast_to([n, 4, 64]), ALU.mult, [ZT, mss], [fqf])
                        self.tt("pool", dst[0:n, :].rearrange("p (h c) -> p h c", h=4), fqf[0:n, :, :], gbc[0:n, :].unsqueeze(1).broadcast_to([n, 4, 64]), ALU.mult, [fqf, gbc], [dst])
                    FV = fvaug[r]
                    self.cp("act", FV[0:n, :, 0:64], fv_.rearrange("p (h c) -> p h c", h=4), [ZT], [FV])
                    FQK = fqkT[r]
                    for hp in range(2):
                        self.tr(b5v[:, hp * 128:hp * 128 + n], fqb[0:n, hp * 128:(hp + 1) * 128], identb[0:n, 0:n], [fqb, identb], [b5])
                        self.tr(b5v[:, (2 + hp) * 128:(2 + hp) * 128 + n], fkb[0:n, hp * 128:(hp + 1) * 128], identb[0:n, 0:n], [fkb, identb], [b5])
                    self.cp("act", FQK[:, :, 0:n], b5v[:, 0:512].rearrange("p (a t) -> p a t", a=4)[:, :, 0:n], [], [FQK, b5])
                    P.dma("sp", S["QTd"][:, :, p0:p0 + n].rearrange("a d t -> d a t"), FQK[:, 0:2, 0:n], dsq[r], reads=[FQK], writes=[Buf()], newgroup=False)
                    P.dma("sp", S["KTd"][:, :, p0:p0 + n].rearrange("a d t -> d a t"), FQK[:, 2:4, 0:n], dsq[r], reads=[FQK], writes=[Buf()], newgroup=False)
                    P.dma("sp", S["Vd"][:, 0:n, j, :].rearrange("h p c -> p h c"), FV[0:n, :, :], dsq[r], reads=[FV], writes=[Buf()], newgroup=False)
                    P.commit(dsq[r], [qkT[r], vaug[r], FQK, FV])
                    self.tt("dve", flog[0:n, :], ff_, fbf_bc[0:n, :], ALU.add, [ZT, fbf_bc], [flog])
                    self.act(flog[0:n, :], flog[0:n, :], AF.Sigmoid, [flog], [flog])
                    self.act(flog[0:n, :], flog[0:n, :], AF.Ln, [flog], [flog])
                    if j == 0:
                        self.mm(b6[0:n, 0:4], C["c_trifull"][0:n, 0:n], flog[0:n, :], True, True, [C["c_trifull"], flog], [b6])
                    else:
                        pn = self.tiles[j - 1][1]
                        selp = C["c_sel15"] if pn == 16 else C["c_sel127"]
                        self.mm(b6[0:n, 0:4], C["c_trifull"][0:n, 0:n], flog[0:n, :], True, False, [C["c_trifull"], flog], [b6])
                        self.mm(b6[0:n, 0:4], selp[0:pn, 0:n], call[0:pn, j - 1, :], False, True, [selp, call], [b6])
                    self.cp("dve", call[0:n, j, :], b6[0:n, 0:4], [], [call, b6])
            evs = [(d.key, d.cnt) for d in dsy + dsq if d.cnt > 0]
            self.barrier(evs)

    def barrier(self, evs=()):
        P = self.P
        evs = list(evs)
        for e in ("pe", "act", "dve", "pool"):
            if P.cnt[e] > 0:
                evs.append((("E", e), P.cnt[e]))
        for d in P.all_dsems:
            if d.cnt > 0:
                evs.append((d.key, d.cnt))
        for e in ("sp", "pool", "act", "pe", "dve"):
            P.wait_all(e, evs)

    def phase_1b(self, l):
        P, I, S, C, nc = self.P, self.I, self.S, self.C, self.nc
        bank = self.bank
        NT, T = self.NT, self.T
        call = self.call
        tiles = self.tiles
        with contextlib.ExitStack() as st:
            sb = lambda name, shape, dt=F32: self.sb(st, "b_" + name, shape, dt)
            b7 = bank[7]
            for i, (p0, n) in enumerate(tiles):
                sel = C["c_sel8"] if n == 16 else C["c_sel64"]
                self.mm(b7[:, i * 4:(i + 1) * 4], sel[0:n, :], call[0:n, i, :], True, True, [sel, call], [b7])
            cref = sb("cref", [128, NT + 1, 4])
            self.cp("dve", cref[:].rearrange("p a b -> p (a b)"), b7[:, 0:(NT + 1) * 4], [], [cref, b7])
            negc = sb("negc", [128, 4, NT + 1])
            self.ts("dve", negc[:], call[:].rearrange("p j h -> p h j"), -1.0, None, ALU.mult, None, [call], [negc])
            bias = self.ring(st, "b_bias", [128, NT + 1], F32, 2)
            KT = self.ring(st, "b_KT", [128, T], BF16, 2)
            QT = self.ring(st, "b_QT", [128, T], BF16, 2)
            V = self.ring(st, "b_V", [128, NT + 1, 65], BF16, 2)
            dsk = [P.dsem() for _ in range(2)]
            dsv = [P.dsem() for _ in range(2)]
            pt = self.ring(st, "b_pt", [128, 128], BF16, 3)
            osb = self.ring(st, "b_osb", [128, 64], F32, 4)
            rden = self.ring(st, "b_rden", [128, 1], F32, 4)
            dso = [P.dsem() for _ in range(4)]
            units = []
            for h in range(4):
                units.append(("mla", h))
            for h in range(4):
                units.append(("fox", h))
            loaded = {}
            nload = [0]

            def load(u):
                kind, h = u
                if kind == "mla":
                    key = ("mla", h)
                else:
                    key = ("fox", h // 2)
                if key in loaded:
                    return loaded[key]
                r = nload[0] % 2
                nload[0] += 1
                if kind == "mla":
                    P.dma("sp", KT[r][0:96, :], S["KTc"][h], dsk[r], writes=[KT[r]])
                    P.dma("sp", QT[r][0:96, :], S["QTc"][h], dsk[r], writes=[QT[r]], newgroup=False)
                else:
                    P.dma("sp", KT[r][:, :], S["KTd"][h // 2], dsk[r], writes=[KT[r]])
                    P.dma("sp", QT[r][:, :], S["QTd"][h // 2], dsk[r], writes=[QT[r]], newgroup=False)
                P.commit(dsk[r], [KT[r], QT[r]])
                loaded[key] = r
                return r
            nv = [0]

            def loadv(u):
                kind, h = u
                r = nv[0] % 2
                nv[0] += 1
                src = S["Vc"] if kind == "mla" else S["Vd"]
                P.dma("sp", V[r][:], src[h], dsv[r], writes=[V[r]])
                return r
            cnt_s = 0
            cnt_o = 0
            cnt_st = 0
            nxt = None
            for ui, u in enumerate(units):
                kind, h = u
                if nxt is None:
                    r = load(u); rv = loadv(u)
                else:
                    r, rv = nxt
                fox = kind == "fox"
                rows = slice(0, 96) if not fox else slice((h % 2) * 64, (h % 2) * 64 + 64)
                scale = (96.0 if not fox else 64.0) ** -0.5
                mask = C["foxmask"] if fox else C["mlamask"]
                ycol = (512 if not fox else 768) + h * 64
                for i, (p0, n) in enumerate(tiles):
                    if i == 1 and ui + 1 < len(units):
                        nxt = (load(units[ui + 1]), loadv(units[ui + 1]))
                    if fox:
                        B_ = bias[i % 2]
                        self.ts("dve", B_[:, 0:i + 1], negc[:, h, 0:i + 1], cref[:, i, h:h + 1], None, ALU.add, None, [negc, cref], [B_])
                    ob = bank[4 + (cnt_o % 2)]
                    for jj in range(i + 1):
                        k0, kn = tiles[jj]
                        sbk = bank[cnt_s % 3]
                        PT = pt[cnt_s % 3]
                        cnt_s += 1
                        self.mm(sbk[0:kn, 0:n], KT[r][rows, k0:k0 + kn], QT[r][rows, p0:p0 + n], True, True, [KT[r], QT[r]], [sbk])
                        if fox:
                            self.act(PT[0:kn, 0:n], sbk[0:kn, 0:n], AF.Exp, [B_], [PT, sbk], scale=scale, bias=B_[0:kn, jj:jj + 1])
                        else:
                            self.act(PT[0:kn, 0:n], sbk[0:kn, 0:n], AF.Exp, [], [PT, sbk], scale=scale)
                        if jj == i:
                            self.tt("pool", PT[0:kn, 0:n], PT[0:kn, 0:n], mask[0:kn, 0:n], ALU.mult, [PT, mask], [PT])
                        self.mm(ob[0:n, 0:65], PT[0:kn, 0:n], V[rv][0:kn, jj, :], jj == 0, jj == i, [PT, V[rv]], [ob])
                    cnt_o += 1
                    ro = cnt_st % 4
                    cnt_st += 1
                    P.op("dve", lambda e, ob=ob, n=n, ro=ro: e.reciprocal(out=rden[ro][0:n, :], in_=ob[0:n, 64:65]), reads=[], writes=[rden[ro], ob])
                    self.ts("dve", osb[ro][0:n, :], ob[0:n, 0:64], rden[ro][0:n, 0:1], None, ALU.mult, None, [rden[ro]], [osb[ro], ob])
                    P.dma("sp", S["Y"][p0:p0 + n, ycol:ycol + 64], osb[ro][0:n, :], dso[ro], reads=[osb[ro]], writes=[Buf()])
            self.barrier([(d.key, d.cnt) for d in dso if d.cnt > 0])

    def phase_1c(self, l, hsrc):
        P, I, S, C, nc = self.P, self.I, self.S, self.C, self.nc
        bank = self.bank
        NT, T = self.NT, self.T
        moe = (l % 2 == 1)
        with contextlib.ExitStack() as st:
            sb = lambda name, shape, dt=F32: self.sb(st, "c_" + name, shape, dt)
            dsw = P.dsem()
            wout = sb("wout", [128, 8, D], BF16)
            wv = I["w_out"][l].rearrange("(k p) c -> p k c", p=128)
            for k in range(8):
                P.dma("pool", wout[:, k, :], wv[:, k, :], dsw, writes=[wout], newgroup=False)
            og_bc = sb("og_bc", [128, D])
            P.dma("sp", og_bc[:], I["og"][l].partition_broadcast(128), dsw, writes=[og_bc], newgroup=False)
            g2T = sb("g2T", [128, 8])
            P.dma("sp", g2T[:], I["g2T"][l], dsw, writes=[g2T], newgroup=False)
            if moe:
                wr = sb("wr", [128, 8, NEXP])
                P.dma("sp", wr[:], I["moe_wr"].rearrange("(k p) e -> p k e", p=128), dsw, writes=[wr], newgroup=False)
                P.commit(dsw, [wr])
            P.commit(dsw, [wout, og_bc, g2T])
            R2 = 2
            Yt = self.ring(st, "c_Yt", [128, D], F32, R2)
            Ht = self.ring(st, "c_Ht", [128, D], F32, R2)
            dsl = [P.dsem() for _ in range(R2)]
            junk = sb("junk", [128, D])
            ss = self.ring(st, "c_ss", [128, 16], F32, R2)
            yn = sb("yn", [128, D], BF16)
            ynT = sb("ynT", [128, 8, 128], BF16)
            hm = self.ring(st, "c_hm", [128, D], F32, R2)
            dsh = [P.dsem() for _ in range(R2)]
            u2b = sb("u2b", [128, D], BF16)
            u2f = sb("u2f", [128, D], F32)
            u2Tf = sb("u2Tf", [128, 8, 128], F32)
            u2T = self.ring(st, "c_u2T", [128, 8, 128], BF16, R2)
            dsu = [P.dsem() for _ in range(R2)]
            gl = sb("gl", [128, 8]); gl2 = sb("gl2", [128, 8]); gm1 = sb("gm1", [128, 8]); gm2 = sb("gm2", [128, 8])
            gsc = sb("gsc", [128, 8]); gate = sb("gate", [128, 8])
            gT = self.ring(st, "c_gT", [8, 128], F32, R2)
            identb = C["identb"]; identf = C["c_ident"]

            def load(jn):
                pp, nn = self.tiles[jn]
                rr = jn % R2
                P.dma("sp", Yt[rr][0:nn, :], S["Y"][pp:pp + nn, :], dsl[rr], writes=[Yt[rr]])
                P.dma("sp", Ht[rr][0:nn, :], hsrc[pp:pp + nn, :], dsl[rr], writes=[Ht[rr]], newgroup=False)
                P.commit(dsl[rr], [Yt[rr], Ht[rr]])
            for j, (p0, n) in enumerate(self.tiles):
                r = j % R2
                if j == 0:
                    load(0)
                if j + 1 < len(self.tiles):
                    load(j + 1)
                Y_, H_, SS, HM, U2T = Yt[r], Ht[r], ss[r], hm[r], u2T[r]
                self.act(junk[0:n, :], Y_[0:n, :], AF.Square, [Y_], [junk])
                P.op("dve", lambda e, n=n, SS=SS: e.tensor_reduce(out=SS[0:n, 0:4], in_=junk[0:n, :].rearrange("p (g c) -> p g c", g=4), axis=AX.X, op=ALU.add),
                     reads=[junk], writes=[SS])
                self.act(SS[0:n, 0:4], SS[0:n, 0:4], AF.Sqrt, [SS], [SS], scale=1.0 / 256, bias=EPS)
                P.op("dve", lambda e, n=n, SS=SS: e.reciprocal(out=SS[0:n, 0:4], in_=SS[0:n, 0:4]), reads=[SS], writes=[SS])
                for g in range(4):
                    self.stt("dve", yn[0:n, g * 256:(g + 1) * 256], Y_[0:n, g * 256:(g + 1) * 256], SS[0:n, g:g + 1],
                             og_bc[0:n, g * 256:(g + 1) * 256], ALU.mult, ALU.mult, [Y_, SS, og_bc], [yn])
                b0 = bank[0]
                b0v = b0.t[:].bitcast(BF16)
                for k in range(8):
                    self.tr(b0v[:, k * 128:k * 128 + n], yn[0:n, k * 128:(k + 1) * 128], identb[0:n, 0:n], [yn, identb], [b0])
                self.cp("act", ynT[:, :, 0:n], b0v[:, :].rearrange("p (k t) -> p k t", k=8)[:, :, 0:n], [], [ynT, b0])
                for c in range(2):
                    bo = bank[2 + c]
                    for k in range(8):
                        self.mm(bo[0:n, 0:512], ynT[:, k, 0:n], wout[:, k, c * 512:(c + 1) * 512], k == 0, k == 7, [ynT, wout], [bo])
                    self.tt("dve", HM[0:n, c * 512:(c + 1) * 512], bo[0:n, 0:512], H_[0:n, c * 512:(c + 1) * 512], ALU.add, [H_], [HM, bo])
                P.dma("sp", S["hmid"][p0:p0 + n, :], HM[0:n, :], dsh[r], reads=[HM], writes=[Buf()])
                P.op("dve", lambda e, SS=SS: e.memset(SS[:, 8:9], 0.0), writes=[SS])
                self.act(junk[0:n, :], HM[0:n, :], AF.Square, [HM, SS], [junk, SS], accum_out=SS[0:n, 8:9])
                self.act(SS[0:n, 9:10], SS[0:n, 8:9], AF.Sqrt, [SS], [SS], scale=1.0 / D, bias=EPS)
                P.op("dve", lambda e, n=n, SS=SS: e.reciprocal(out=SS[0:n, 9:10], in_=SS[0:n, 9:10]), reads=[SS], writes=[SS])
                b1 = bank[1]
                if not moe:
                    self.act(u2b[0:n, :], HM[0:n, :], AF.Copy, [HM, SS], [u2b], scale=SS[0:n, 9:10])
                    b1v = b1.t[:].bitcast(BF16)
                    for k in range(8):
                        self.tr(b1v[:, k * 128:k * 128 + n], u2b[0:n, k * 128:(k + 1) * 128], identb[0:n, 0:n], [u2b, identb], [b1])
                    self.tt("dve", U2T[:, :, 0:n], b1v[:, :].rearrange("p (k t) -> p k t", k=8)[:, :, 0:n],
                            g2T[:, :].unsqueeze(2).broadcast_to([128, 8, n]), ALU.mult, [g2T], [U2T, b1])
                else:
                    self.act(u2f[0:n, :], HM[0:n, :], AF.Copy, [HM, SS], [u2f], scale=SS[0:n, 9:10])
                    for half in range(2):
                        bb = bank[5 + half]
                        for kk in range(4):
                            k = half * 4 + kk
                            self.tr(bb[:, kk * 128:kk * 128 + n], u2f[0:n, k * 128:(k + 1) * 128], identf[0:n, 0:n], [u2f, identf], [bb])
                        self.tt("dve", u2Tf[:, half * 4:half * 4 + 4, 0:n], bb[:, :].rearrange("p (k t) -> p k t", k=4)[:, :, 0:n],
                                g2T[:, half * 4:half * 4 + 4].unsqueeze(2).broadcast_to([128, 4, n]), ALU.mult, [g2T], [u2Tf, bb])
                    self.cp("pool", U2T[:, :, 0:n], u2Tf[:, :, 0:n], [u2Tf], [U2T])
                    b7 = bank[7]
                    for k in range(8):
                        self.mm(b7[0:n, 0:NEXP], u2Tf[:, k, 0:n], wr[:, k, :], k == 0, k == 7, [u2Tf, wr], [b7])
                    self.cp("act", gl[0:n, :], b7[0:n, 0:NEXP], [], [gl, b7])
                    P.op("dve", lambda e, n=n: e.tensor_reduce(out=gsc[0:n, 0:1], in_=gl[0:n, :], axis=AX.X, op=ALU.max), reads=[gl], writes=[gsc])
                    self.ts("dve", gm1[0:n, :], gl[0:n, :], gsc[0:n, 0:1], None, ALU.is_equal, None, [gl, gsc], [gm1])
                    self.stt("dve", gl2[0:n, :], gm1[0:n, :], -1e30, gl[0:n, :], ALU.mult, ALU.add, [gm1, gl], [gl2])
                    P.op("dve", lambda e, n=n: e.tensor_reduce(out=gsc[0:n, 1:2], in_=gl2[0:n, :], axis=AX.X, op=ALU.max), reads=[gl2], writes=[gsc])
                    self.ts("dve", gm2[0:n, :], gl2[0:n, :], gsc[0:n, 1:2], None, ALU.is_equal, None, [gl2, gsc], [gm2])
                    self.tt("dve", gsc[0:n, 2:3], gsc[0:n, 1:2], gsc[0:n, 0:1], ALU.subtract, [gsc], [gsc])
                    self.act(gsc[0:n, 3:4], gsc[0:n, 2:3], AF.Exp, [gsc], [gsc])
                    self.ts("dve", gsc[0:n, 4:5], gsc[0:n, 3:4], 1.0, None, ALU.add, None, [gsc], [gsc])
                    P.op("dve", lambda e, n=n: e.reciprocal(out=gsc[0:n, 4:5], in_=gsc[0:n, 4:5]), reads=[gsc], writes=[gsc])
                    self.tt("dve", gsc[0:n, 5:6], gsc[0:n, 3:4], gsc[0:n, 4:5], ALU.mult, [gsc], [gsc])
                    self.ts("dve", gate[0:n, :], gm1[0:n, :], gsc[0:n, 4:5], None, ALU.mult, None, [gm1, gsc], [gate])
                    self.stt("dve", gate[0:n, :], gm2[0:n, :], gsc[0:n, 5:6], gate[0:n, :], ALU.mult, ALU.add, [gm2, gsc, gate], [gate])
                    self.tr(b7[0:NEXP, 128:128 + n], gate[0:n, :], identf[0:n, 0:n], [gate, identf], [b7])
                    self.cp("act", gT[r][:, 0:n], b7[0:NEXP, 128:128 + n], [], [gT[r], b7])
                    P.dma("sp", S["gateT"][:, p0:p0 + n], gT[r][:, 0:n], dsu[r], reads=[gT[r]], writes=[Buf()])
                P.dma("sp", S["u2T"].rearrange("(k p) t -> p k t", p=128)[:, :, p0:p0 + n], U2T[:, :, 0:n], dsu[r], reads=[U2T], writes=[Buf()],
                      newgroup=not moe)
                P.commit(dsu[r], [U2T, gT[r]])
            self.barrier([(d.key, d.cnt) for d in dsh + dsu if d.cnt > 0])

    def ffn_core(self, st, tok_sets, wsets, out_fn, blend, gated):
        P, I, S, C, nc = self.P, self.I, self.S, self.C, self.nc
        bank = self.bank
        sb = lambda name, shape, dt=F32: self.sb(st, "f_" + name, shape, dt)
        TSMAX = max(sum(n for (_, n) in ts) for ts in tok_sets)
        NTT = max(len(ts) for ts in tok_sets)
        u2 = sb("u2", [128, 8, TSMAX], BF16)
        acc = sb("acc", [128, NTT, D], F32)
        dsx = P.dsem(); dsa = P.dsem(); dsg = [P.dsem() for _ in range(2)]
        if blend:
            u2B = sb("u2B", [128, 8, TSMAX], BF16)
            accB = self.ring(st, "f_accB", [128, D], F32, 2)
            dsb = [P.dsem() for _ in range(2)]
            selA = C["sel"][:, 0:1]; selB = C["sel"][:, 1:2]
        if gated:
            gbc = self.ring(st, "f_gbc", [128, TSMAX], F32, 2)
            gbcB = self.ring(st, "f_gbcB", [128, TSMAX], F32, 2)
        GW = 512
        wg = self.ring(st, "f_wg", [128, 8, GW], BF16, 2)
        wu = self.ring(st, "f_wu", [128, 8, GW], BF16, 2)
        wd = self.ring(st, "f_wd", [128, 4, D], BF16, 2)
        dsw = [P.dsem() for _ in range(2)]
        sg = self.ring(st, "f_sg", [128, 512], F32, 2)
        hid = self.ring(st, "f_hid", [128, 512], BF16, 8)
        dso = [P.dsem() for _ in range(4)]
        u2Tv = S["u2T"].rearrange("(k p) t -> p k t", p=128)
        groups = []
        for (wg_ap, wu_ap, wd_ap, F, e) in wsets:
            f0 = 0
            while f0 < F:
                fw = min(GW, F - f0)
                groups.append((wg_ap, wu_ap, wd_ap, f0, fw, e))
                f0 += fw
        gcount = 0
        hcount = 0

        def loadw(gi):
            wg_ap, wu_ap, wd_ap, f0, fw, e = groups[gi % len(groups)]
            r = gi % 2
            wgv = wg_ap.rearrange("(k p) f -> p k f", p=128)
            wuv = wu_ap.rearrange("(k p) f -> p k f", p=128)
            for k in range(0, 8, 2):
                P.dma("pool", wg[r][:, k:k + 2, 0:fw], wgv[:, k:k + 2, f0:f0 + fw], dsw[r], writes=[wg[r]], newgroup=(k == 0))
            for k in range(0, 8, 2):
                P.dma("pool", wu[r][:, k:k + 2, 0:fw], wuv[:, k:k + 2, f0:f0 + fw], dsw[r], writes=[wu[r]], newgroup=False)
            nb = fw // 128
            P.dma("pool", wd[r][:, 0:nb, :], wd_ap[f0:f0 + fw, :].rearrange("(b p) d -> p b d", p=128), dsw[r], writes=[wd[r]], newgroup=False)
            P.commit(dsw[r], [wg[r], wu[r], wd[r]])
        total_groups = len(groups) * len(tok_sets)
        loadw(0)
        for si, ts in enumerate(tok_sets):
            col = 0
            cols = []
            for ti, (rows, n) in enumerate(ts):
                cols.append(col)
                if not blend:
                    r0 = rows
                    P.dma("sp", u2[:, :, col:col + n], u2Tv[:, :, r0:r0 + n], dsx, writes=[u2], newgroup=(ti == 0))
                    P.dma("sp", acc[0:n, ti, :], S["hmid"][r0:r0 + n, :], dsa, writes=[acc], newgroup=(ti == 0))
                else:
                    rA, rB = rows
                    P.dma("sp", u2[:, :, col:col + n], u2Tv[:, :, rA:rA + n], dsx, writes=[u2], newgroup=(ti == 0))
                    P.dma("sp", u2B[:, :, col:col + n], u2Tv[:, :, rB:rB + n], dsx, writes=[u2B], newgroup=False)
                    P.dma("sp", acc[0:n, ti, :], S["hmid"][rA:rA + n, :], dsa, writes=[acc], newgroup=(ti == 0))
                    ab = accB[ti % 2]
                    P.dma("sp", ab[0:n, :], S["hmid"][rB:rB + n, :], dsb[ti % 2], writes=[ab])
                    self.ts("pool", acc[0:n, ti, :], acc[0:n, ti, :], selA[0:n, :], None, ALU.mult, None, [acc, C["sel"]], [acc])
                    self.stt("dve", acc[0:n, ti, :], ab[0:n, :], selB[0:n, :], acc[0:n, ti, :], ALU.mult, ALU.add, [ab, C["sel"], acc], [acc])
                col += n
            TS = col
            P.commit(dsx, [u2] + ([u2B] if blend else []))
            if blend:
                for k in range(8):
                    self.ts("pool", u2[:, k, 0:TS], u2[:, k, 0:TS], selA, None, ALU.mult, None, [u2, C["sel"]], [u2])
                    self.stt("dve", u2[:, k, 0:TS], u2B[:, k, 0:TS], selB, u2[:, k, 0:TS], ALU.mult, ALU.add, [u2B, C["sel"], u2], [u2])
            chunks = []
            cur = []
            cw_ = 0
            for ti, (rows, n) in enumerate(ts):
                if cw_ + n > 512:
                    chunks.append(cur); cur = []; cw_ = 0
                cur.append(ti); cw_ += n
            if cur:
                chunks.append(cur)
            cur_e = None
            for gi_local in range(len(groups)):
                gi = gcount
                gcount += 1
                wg_ap, wu_ap, wd_ap, f0, fw, e = groups[gi_local]
                r = gi % 2
                if gi + 1 < total_groups:
                    loadw(gi + 1)
                nb = fw // 128
                if gated and e != cur_e:
                    cur_e = e
                    G = gbc[e % 2]
                    if not blend:
                        raise NotImplementedError
                    col = 0
                    GB = gbcB[e % 2]
                    for ti, (rows, n) in enumerate(ts):
                        rA, rB = rows
                        P.dma("sp", G[:, col:col + n], S["gateT"][e, rA:rA + n].partition_broadcast(128), dsg[e % 2], writes=[G], newgroup=(ti == 0))
                        P.dma("sp", GB[:, col:col + n], S["gateT"][e, rB:rB + n].partition_broadcast(128), dsg[e % 2], writes=[GB], newgroup=False)
                        col += n
                    P.commit(dsg[e % 2], [G, GB])
                    self.ts("pool", G[:, 0:TS], G[:, 0:TS], selA, None, ALU.mult, None, [G, C["sel"]], [G])
                    self.stt("dve", G[:, 0:TS], GB[:, 0:TS], selB, G[:, 0:TS], ALU.mult, ALU.add, [GB, C["sel"], G], [G])
                for ch in chunks:
                    c0 = cols[ch[0]]
                    cw = sum(ts[ti][1] for ti in ch)
                    hslots = []
                    for fb in range(nb):
                        bg = bank[(hcount % 2) * 2]
                        bu = bank[(hcount % 2) * 2 + 1]
                        H_ = hid[hcount % 8]
                        SG = sg[hcount % 2]
                        hcount += 1
                        for k in range(8):
                            self.mm(bg[:, 0:cw], wg[r][:, k, fb * 128:(fb + 1) * 128], u2[:, k, c0:c0 + cw], k == 0, k == 7, [wg[r], u2], [bg])
                        for k in range(8):
                            self.mm(bu[:, 0:cw], wu[r][:, k, fb * 128:(fb + 1) * 128], u2[:, k, c0:c0 + cw], k == 0, k == 7, [wu[r], u2], [bu])
                        self.act(SG[:, 0:cw], bg[:, 0:cw], AF.Silu, [], [SG, bg])
                        if gated:
                            self.tt("pool", SG[:, 0:cw], SG[:, 0:cw], G[:, c0:c0 + cw], ALU.mult, [SG, G], [SG])
                        self.tt("dve", H_[:, 0:cw], bu[:, 0:cw], SG[:, 0:cw], ALU.mult, [SG], [H_, bu])
                        hslots.append(H_)
                    for ti in ch:
                        n = ts[ti][1]
                        lc = cols[ti] - c0
                        for half in range(2):
                            bd = bank[4 + 2 * (ti % 2) + half]
                            for fb in range(nb):
                                self.mm(bd[0:n, 0:512], hslots[fb][:, lc:lc + n], wd[r][:, fb, half * 512:(half + 1) * 512], fb == 0, fb == nb - 1,
                                        [hslots[fb], wd[r]], [bd])
                            self.tt("dve", acc[0:n, ti, half * 512:(half + 1) * 512], bd[0:n, 0:512], acc[0:n, ti, half * 512:(half + 1) * 512], ALU.add,
                                    [acc], [acc, bd])
            for ti, (rows, n) in enumerate(ts):
                dst = out_fn(si, ti)
                if dst is None:
                    continue
                ev = P.dma("sp", dst, acc[0:n, ti, :], dso[ti % 4], reads=[acc], writes=[Buf()])
                self.final_evs.append(ev)
        self.barrier([(d.key, d.cnt) for d in dso if d.cnt > 0])

    def phase_ffn(self, hdst):
        I = self.I
        T = self.T
        rows = [(r0, min(128, T - r0)) for r0 in range(0, T, 128)]
        TSN = 8
        tok_sets = [rows[i:i + TSN] for i in range(0, len(rows), TSN)]
        if len(tok_sets) > 1 and len(tok_sets[-1]) == 1:
            tok_sets[-2] = tok_sets[-2] + tok_sets[-1]
            tok_sets.pop()
        final = hdst is None

        def out_fn(si, ti):
            r0, n = tok_sets[si][ti]
            if not final:
                return hdst[r0:r0 + n, :]
            lo = max(r0, NMETA)
            return None if True else None
        with contextlib.ExitStack() as st:
            self.ffn_core(st, tok_sets, [(I["ffn_wg"], I["ffn_wu"], I["ffn_wd"], D_FF, 0)], out_fn, blend=False, gated=False)

    def phase_moe(self):
        I = self.I
        NTOK = self.moe_tokens
        ntile = NTOK // 128
        TSN = 8
        tiles_ab = [((NMETA + 128 * i, NMETA + NTOK + 128 * i), 128) for i in range(ntile)]
        tok_sets = [tiles_ab[i:i + TSN] for i in range(0, ntile, TSN)]
        out = self.out_ap

        def out_fn(si, ti):
            i = si * TSN + ti
            return out[128 * i:128 * (i + 1), :]
        wsets = [(I["moe_wg"][e], I["moe_wu"][e], I["moe_wd"][e], D_FFE, e) for e in range(NEXP)]
        with contextlib.ExitStack() as st:
            self.ffn_core(st, tok_sets, wsets, out_fn, blend=True, gated=True)


def prep_inputs(inp, b, half, NT, depth=2):
    f = lambda a: np.ascontiguousarray(np.asarray(a, dtype=np.float32))
    T = NMETA + 128 * NT
    m = {}
    m["xin"] = f(np.concatenate([inp["meta"], inp["x"][b]], axis=0))
    sel = np.zeros((128, 2), np.float32)
    sel[:, half] = 1.0
    m["sel"] = sel
    m.update(make_consts(T))
    L = depth
    colmaj = lambda v: f(np.asarray(v).reshape(L, -1, 128).transpose(0, 2, 1))
    m["w_in"] = f(inp["w_in"][:L]); m["w_out"] = f(inp["w_out"][:L])
    m["g1T"] = colmaj(inp["norm1_g"][:L]); m["g2T"] = colmaj(inp["norm2_g"][:L])
    m["og"] = f(inp["out_norm_g"][:L])
    cw = np.asarray(inp["lru_conv_w"][:L])
    m["conv_wT"] = f(cw.transpose(0, 2, 1).reshape(L, 2, 128, 4).transpose(0, 2, 1, 3))
    m["conv_b"] = colmaj(inp["lru_conv_b"][:L])
    m["lru_ba"] = colmaj(np.asarray(inp["lru_ba"][:L]).reshape(L, 256))
    m["lru_bx"] = colmaj(np.asarray(inp["lru_bx"][:L]).reshape(L, 256))
    m["lru_lam"] = colmaj(inp["lru_lambda"][:L])
    m["lru_wa"] = f(inp["lru_wa"][:L]); m["lru_wx"] = f(inp["lru_wx"][:L])
    m["hg_lb"] = f(inp["hg_lb_logits"][:2])
    for k in ["mla_gq", "mla_w_uq", "mla_gkv", "mla_w_ukv", "mla_gqn", "mla_gkn", "fox_gqn", "fox_gkn", "fox_bf"]:
        m[k] = f(inp[k][:L])
    m["ffn_wg"] = f(inp["ffn_w_gate"][0]); m["ffn_wu"] = f(inp["ffn_w_up"][0]); m["ffn_wd"] = f(inp["ffn_w_down"][0])
    if depth > 1:
        m["moe_wr"] = f(inp["moe_w_router"][0]); m["moe_wg"] = f(inp["moe_w_gate"][0])
        m["moe_wu"] = f(inp["moe_w_up"][0]); m["moe_wd"] = f(inp["moe_w_down"][0])
    return m


_CACHE = {}


def _get_program(NT):
    if NT not in _CACHE:
        b = Builder(NT, depth=2, debug=False)
        nc = b.build()
        _CACHE[NT] = (b, nc)
    return _CACHE[NT]


def kernel(**inputs):
    x = np.asarray(inputs["x"])
    Bsz, SEQ, _ = x.shape
    NT = SEQ // 128
    b, nc = _get_program(NT)
    half_tokens = b.moe_tokens
    n_cores = 8
    in_maps = []
    shared = None
    for c in range(n_cores):
        bi, half = c % Bsz, c // Bsz
        if shared is None:
            m = prep_inputs(inputs, bi, half, NT, depth=2)
            shared = m
        else:
            m = dict(shared)
            m["xin"] = np.ascontiguousarray(np.concatenate([np.asarray(inputs["meta"], np.float32), np.asarray(x[bi], np.float32)], axis=0))
            sel = np.zeros((128, 2), np.float32)
            sel[:, half] = 1.0
            m["sel"] = sel
        in_maps.append({k: v for k, v in m.items() if k in b.inputs})
    res = run_bass_kernel_spmd(nc, in_maps, core_ids=list(range(n_cores)))
    out = np.empty((Bsz, SEQ, D), np.float32)
    for c in range(n_cores):
        bi, half = c % Bsz, c // Bsz
        out[bi, half * half_tokens:(half + 1) * half_tokens] = np.asarray(res.results[c]["out"])
    return out
```

```python
import contextlib
import os
import numpy as np
import ml_dtypes
import concourse.bass as bass
import concourse.mybir as mybir
from concourse.bass_utils import run_bass_kernel_spmd

F32 = mybir.dt.float32
BF16 = mybir.dt.bfloat16
AF = mybir.ActivationFunctionType
ALU = mybir.AluOpType
AX = mybir.AxisListType

ENGS = ("pe", "act", "dve", "pool", "sp")
D = 1024
NMETA = 16
EPS = 1e-6
IN_COLS = 2660
D_FF = 2816
D_FFE = 3584
NEXP = 8


class Buf:
    __slots__ = ("name", "w", "r")

    def __init__(self, name=""):
        self.name = name
        self.w = None
        self.r = {}


class DSem:
    __slots__ = ("h", "cnt", "key")

    def __init__(self, h, key):
        self.h = h
        self.cnt = 0
        self.key = key


class TT:
    __slots__ = ("t", "b")

    def __init__(self, t, b=None, name=""):
        self.t = t
        self.b = b if b is not None else Buf(name)

    def __getitem__(self, k):
        return self.t[k]


def _bufs(xs):
    out = []
    for x in xs:
        if x is None:
            continue
        out.append(x.b if isinstance(x, TT) else x)
    return out


class Prog:
    def __init__(self, nc, stack):
        self.nc = nc
        self.stack = stack
        self.q = {e: [] for e in ENGS}
        self.cnt = {e: 0 for e in ENGS}
        self.seen = {e: {} for e in ENGS}
        self.semh = {}
        for e in ENGS:
            h = stack.enter_context(nc.semaphore("es_" + e))
            self.semh[("E", e)] = h
        self.nd = 0
        self.all_dsems = []

    def dsem(self, name=None):
        self.nd += 1
        key = ("D", self.nd)
        h = self.stack.enter_context(self.nc.semaphore(name or f"ds{self.nd}"))
        self.semh[key] = h
        d = DSem(h, key)
        self.all_dsems.append(d)
        return d

    def _waits(self, eng, reads, writes, extra=()):
        need = {}

        def add(ev, raw):
            if ev is None:
                return
            k, v = ev
            if k == ("E", eng) and not raw:
                return
            if need.get(k, 0) < v:
                need[k] = v
        for b in reads:
            add(b.w, True)
        for b in writes:
            add(b.w, False)
            for k, v in b.r.items():
                add((k, v), False)
        for ev in extra:
            add(ev, True)
        out = []
        seen = self.seen[eng]
        for k, v in need.items():
            if seen.get(k, 0) < v:
                seen[k] = v
                out.append((k, v))
        return out

    def _mark(self, ev, reads, writes):
        k, v = ev
        for b in reads:
            if b.r.get(k, 0) < v:
                b.r[k] = v
        for b in writes:
            b.w = ev
            b.r = {}

    def op(self, eng, fn, reads=(), writes=()):
        reads = _bufs(reads)
        writes = _bufs(writes)
        waits = self._waits(eng, reads, writes)
        self.cnt[eng] += 1
        ev = (("E", eng), self.cnt[eng])
        self._mark(ev, reads, writes)
        self.q[eng].append((waits, fn, (("E", eng), 1)))
        return ev

    def dma(self, eng, out, in_, ds, reads=(), writes=(), newgroup=True, **kw):
        reads = _bufs(reads)
        writes = _bufs(writes)
        extra = ()
        if newgroup and ds.cnt > 0:
            extra = ((ds.key, ds.cnt),)
        waits = self._waits(eng, reads, writes, extra)
        ds.cnt += 16
        ev = (ds.key, ds.cnt)
        self._mark(ev, reads, writes)
        self.q[eng].append((waits, lambda e: e.dma_start(out=out, in_=in_, **kw), (ds.key, 16)))
        return ev

    def commit(self, ds, bufs):
        for b in _bufs(bufs):
            if b.w is not None and b.w[0] == ds.key:
                b.w = (ds.key, ds.cnt)
            if ds.key in b.r:
                b.r[ds.key] = ds.cnt

    def wait_all(self, eng, evs):
        waits = self._waits(eng, (), (), evs)
        self.q[eng].append((waits, None, None))

    def emit(self):
        nc = self.nc
        semh = self.semh
        with nc.Block() as block:
            def mk(ename):
                def body(e):
                    for waits, fn, inc in self.q[ename]:
                        for k, v in waits:
                            e.wait_ge(semh[k], v)
                        if fn is None:
                            continue
                        ins = fn(e)
                        if inc is not None:
                            ins.then_inc(semh[inc[0]], inc[1])
                return body
            block.tensor(mk("pe"))
            block.scalar(mk("act"))
            block.vector(mk("dve"))
            block.gpsimd(mk("pool"))
            block.sync(mk("sp"))


def make_consts(T):
    s = np.arange(128)[:, None]
    t = np.arange(128)[None, :]
    c = {}
    c["c_ident"] = np.eye(128, dtype=np.float32)
    c["c_triblk"] = ((s // 32 == t // 32) & (s <= t)).astype(np.float32)
    c["c_blkones"] = (s // 32 == t // 32).astype(np.float32)
    c["c_chunkind"] = (s // 32 == np.arange(4)[None, :]).astype(np.float32)
    c["c_trifull"] = (s <= t).astype(np.float32)
    c["c_sel127"] = np.broadcast_to((s == 127), (128, 128)).astype(np.float32).copy()
    c["c_sel15"] = np.broadcast_to((s == 15), (128, 128)).astype(np.float32).copy()
    c["c_sel64"] = np.broadcast_to((s == 64), (128, 128)).astype(np.float32).copy()
    c["c_sel8"] = np.broadcast_to((s == 8), (128, 128)).astype(np.float32).copy()
    c["c_mask_mla"] = (s // 64 <= t // 64).astype(np.float32)
    pos = np.arange(T, dtype=np.float32)
    inv = (10000.0 ** (-np.arange(16, dtype=np.float32) / 16)).astype(np.float32)
    ang = pos[:, None] * inv[None, :]
    c["c_cos"] = np.cos(ang).astype(np.float32)
    c["c_sin"] = np.sin(ang).astype(np.float32)
    return c


def tile_list(NT):
    return [(0, NMETA)] + [(NMETA + 128 * i, 128) for i in range(NT)]


class Builder:
    def __init__(self, NT, depth=2, debug=False, stop_after=None, moe_tokens=None):
        self.NT = NT
        self.T = NMETA + 128 * NT
        self.depth = depth
        self.debug = debug
        self.stop_after = stop_after
        self.tiles = tile_list(NT)
        self.nc = bass.Bass("TRN2", target_bir_lowering=False)
        self.inputs = {}
        self.outputs = {}
        self.moe_tokens = moe_tokens if moe_tokens is not None else (128 * NT) // 2

    def din(self, name, shape, dt=F32):
        t = self.nc.dram_tensor(name, list(shape), dt, kind="ExternalInput")
        self.inputs[name] = (tuple(shape), dt)
        return t.ap()

    def dscratch(self, name, shape, dt=F32, dbg=False):
        if dbg and self.debug:
            t = self.nc.dram_tensor(name, list(shape), dt, kind="ExternalOutput")
            self.outputs[name] = (tuple(shape), dt)
        else:
            t = self.nc.dram_tensor(name, list(shape), dt, kind="Internal")
        return t.ap()

    def dout(self, name, shape, dt=F32):
        t = self.nc.dram_tensor(name, list(shape), dt, kind="ExternalOutput")
        self.outputs[name] = (tuple(shape), dt)
        return t.ap()

    def sb(self, st, name, shape, dt=F32):
        self._uid = getattr(self, "_uid", 0) + 1
        return TT(st.enter_context(self.nc.sbuf_tensor(f"s{self._uid}_{name}", list(shape), dt)), name=name)

    def ring(self, st, name, shape, dt, n):
        return [self.sb(st, f"{name}{i}", shape, dt) for i in range(n)]

    def build(self):
        nc = self.nc
        T = self.T
        NT = self.NT
        depth = self.depth
        I = {}
        I["xin"] = self.din("xin", [T, D])
        I["sel"] = self.din("sel", [128, 2])
        for nm, shp in [("c_ident", [128, 128]), ("c_triblk", [128, 128]), ("c_blkones", [128, 128]),
                        ("c_chunkind", [128, 4]), ("c_trifull", [128, 128]), ("c_sel127", [128, 128]),
                        ("c_sel15", [128, 128]), ("c_sel64", [128, 128]), ("c_sel8", [128, 128]),
                        ("c_mask_mla", [128, 128]), ("c_cos", [T, 16]), ("c_sin", [T, 16])]:
            I[nm] = self.din(nm, shp)
        L = depth
        I["w_in"] = self.din("w_in", [L, D, IN_COLS])
        I["w_out"] = self.din("w_out", [L, D, D])
        I["g1T"] = self.din("g1T", [L, 128, 8])
        I["g2T"] = self.din("g2T", [L, 128, 8])
        I["og"] = self.din("og", [L, D])
        I["conv_wT"] = self.din("conv_wT", [L, 128, 2, 4])
        I["conv_b"] = self.din("conv_b", [L, 128, 2])
        I["lru_ba"] = self.din("lru_ba", [L, 128, 2])
        I["lru_bx"] = self.din("lru_bx", [L, 128, 2])
        I["lru_lam"] = self.din("lru_lam", [L, 128, 2])
        I["lru_wa"] = self.din("lru_wa", [L, 4, 64, 64])
        I["lru_wx"] = self.din("lru_wx", [L, 4, 64, 64])
        I["hg_lb"] = self.din("hg_lb", [2, 256])
        I["mla_gq"] = self.din("mla_gq", [L, 192])
        I["mla_w_uq"] = self.din("mla_w_uq", [L, 192, 384])
        I["mla_gkv"] = self.din("mla_gkv", [L, 128])
        I["mla_w_ukv"] = self.din("mla_w_ukv", [L, 128, 512])
        I["mla_gqn"] = self.din("mla_gqn", [L, 96])
        I["mla_gkn"] = self.din("mla_gkn", [L, 96])
        I["fox_gqn"] = self.din("fox_gqn", [L, 64])
        I["fox_gkn"] = self.din("fox_gkn", [L, 64])
        I["fox_bf"] = self.din("fox_bf", [L, 4])
        I["ffn_wg"] = self.din("ffn_wg", [D, D_FF])
        I["ffn_wu"] = self.din("ffn_wu", [D, D_FF])
        I["ffn_wd"] = self.din("ffn_wd", [D_FF, D])
        if depth > 1:
            I["moe_wr"] = self.din("moe_wr", [D, NEXP])
            I["moe_wg"] = self.din("moe_wg", [NEXP, D, D_FFE])
            I["moe_wu"] = self.din("moe_wu", [NEXP, D, D_FFE])
            I["moe_wd"] = self.din("moe_wd", [NEXP, D_FFE, D])
        self.I = I
        S = {}
        dbg = True
        S["hA"] = self.dscratch("hA", [T, D], F32, dbg)
        S["hmid"] = self.dscratch("hmid", [T, D], F32, dbg)
        S["Y"] = self.dscratch("Y", [T, D], F32, dbg)
        S["u2T"] = self.dscratch("u2T", [D, T], BF16, dbg)
        S["QTc"] = self.dscratch("QTc", [4, 96, T], BF16, dbg)
        S["KTc"] = self.dscratch("KTc", [4, 96, T], BF16, dbg)
        S["Vc"] = self.dscratch("Vc", [4, 128, NT + 1, 65], BF16, dbg)
        S["QTd"] = self.dscratch("QTd", [2, 128, T], BF16, dbg)
        S["KTd"] = self.dscratch("KTd", [2, 128, T], BF16, dbg)
        S["Vd"] = self.dscratch("Vd", [4, 128, NT + 1, 65], BF16, dbg)
        S["gate"] = self.dscratch("gate", [T, NEXP], F32, dbg)
        self.S = S
        n_out = self.moe_tokens if depth > 1 else 128 * NT
        self.out_ap = self.dout("out", [n_out, D])

        with contextlib.ExitStack() as st:
            P = Prog(nc, st)
            self.P = P
            self.bank = [TT(st.enter_context(nc.psum_tensor(f"bank{i}", [128, 512], F32)), name=f"bank{i}")
                         for i in range(8)]
            with contextlib.ExitStack() as cst:
                self.load_consts(cst)
                for l in range(depth):
                    hsrc = I["xin"] if l == 0 else S["hA"]
                    self.phase_1a(l, hsrc)
                    if self.stop_after == ("1a", l):
                        break
                    self.phase_1b(l)
                    if self.stop_after == ("1b", l):
                        break
                    self.phase_1c(l, hsrc)
                    if self.stop_after == ("1c", l):
                        break
                    if l == 0:
                        self.phase_ffn(S["hA"])
                    else:
                        self.phase_moe()
            P.wait_all("sp", self.final_evs)
            P.emit()
        return nc

    def load_consts(self, st):
        P = self.P
        I = self.I
        self.final_evs = []
        self.ds_const = P.dsem("ds_const")
        C = {}
        for nm in ["c_ident", "c_triblk", "c_blkones", "c_trifull", "c_sel127", "c_sel15", "c_sel64", "c_sel8",
                   "c_mask_mla"]:
            C[nm] = self.sb(st, nm, [128, 128], F32)
            P.dma("sp", C[nm][:], I[nm][:, :], self.ds_const, writes=[C[nm]], newgroup=False)
        C["c_chunkind"] = self.sb(st, "c_chunkind", [128, 4], F32)
        P.dma("sp", C["c_chunkind"][:], I["c_chunkind"][:, :], self.ds_const, writes=[C["c_chunkind"]], newgroup=False)
        C["sel"] = self.sb(st, "sel", [128, 2], F32)
        P.dma("sp", C["sel"][:], I["sel"][:, :], self.ds_const, writes=[C["sel"]], newgroup=False)
        P.commit(self.ds_const, list(C.values()))
        C["identb"] = self.sb(st, "identb", [128, 128], BF16)
        P.op("pool", lambda e: e.tensor_copy(out=C["identb"][:], in_=C["c_ident"][:]), reads=[C["c_ident"]], writes=[C["identb"]])
        C["hgmask"] = self.sb(st, "hgmask", [128, 128], BF16)
        P.op("pool", lambda e: e.tensor_copy(out=C["hgmask"][:], in_=C["c_triblk"][:]), reads=[C["c_triblk"]], writes=[C["hgmask"]])
        C["foxmask"] = self.sb(st, "foxmask", [128, 128], BF16)
        P.op("pool", lambda e: e.tensor_copy(out=C["foxmask"][:], in_=C["c_trifull"][:]), reads=[C["c_trifull"]], writes=[C["foxmask"]])
        C["mlamask"] = self.sb(st, "mlamask", [128, 128], BF16)
        P.op("pool", lambda e: e.tensor_copy(out=C["mlamask"][:], in_=C["c_mask_mla"][:]), reads=[C["c_mask_mla"]], writes=[C["mlamask"]])
        self.C = C
        self.call = self.sb(st, "c_all", [128, self.NT + 1, 4], F32)
        P.op("pool", lambda e: e.memset(self.call[:], 0.0), writes=[self.call])

    def mm(self, out, lhsT, rhs, start, stop, reads, writes, **kw):
        self.P.op("pe", lambda e: e.matmul(out, lhsT=lhsT, rhs=rhs, start=start, stop=stop, **kw), reads=reads, writes=writes)

    def tr(self, out, in_, ident, reads, writes):
        self.P.op("pe", lambda e: e.transpose(out=out, in_=in_, identity=ident), reads=reads, writes=writes)

    def act(self, out, in_, func, reads, writes, **kw):
        self.P.op("act", lambda e: e.activation(out=out, in_=in_, func=func, **kw), reads=reads, writes=writes)

    def tt(self, eng, out, in0, in1, op, reads, writes):
        self.P.op(eng, lambda e: e.tensor_tensor(out=out, in0=in0, in1=in1, op=op), reads=reads, writes=writes)

    def ts(self, eng, out, in0, s1, s2, op0, op1, reads, writes):
        if op1 is None:
            self.P.op(eng, lambda e: e.tensor_scalar(out=out, in0=in0, scalar1=s1, scalar2=None, op0=op0), reads=reads, writes=writes)
        else:
            self.P.op(eng, lambda e: e.tensor_scalar(out=out, in0=in0, scalar1=s1, scalar2=s2, op0=op0, op1=op1), reads=reads, writes=writes)

    def stt(self, eng, out, in0, scalar, in1, op0, op1, reads, writes):
        self.P.op(eng, lambda e: e.scalar_tensor_tensor(out=out, in0=in0, scalar=scalar, in1=in1, op0=op0, op1=op1), reads=reads, writes=writes)

    def cp(self, eng, out, in_, reads, writes):
        if eng == "act":
            self.P.op("act", lambda e: e.copy(out=out, in_=in_), reads=reads, writes=writes)
        else:
            self.P.op(eng, lambda e: e.tensor_copy(out=out, in_=in_), reads=reads, writes=writes)

    def rsq(self, out, in_, scale, rw):
        self.act(out, in_, AF.Ln, rw, rw, scale=scale, bias=EPS)
        self.act(out, out, AF.Exp, rw, rw, scale=-0.5)

    def sigm(self, out, in_, reads, writes, bias=None):
        if bias is None:
            self.act(out, in_, AF.Exp, reads, writes, scale=-1.0)
        else:
            self.act(out, in_, AF.Exp, reads, writes, scale=-1.0, bias=bias)
        w2 = [writes[0]]
        self.act(out, out, AF.Ln, w2, w2, bias=1.0)
        self.act(out, out, AF.Exp, w2, w2, scale=-1.0)

    def rstd(self, ss, out, n, scale, reads_w):
        self.act(out, ss, AF.Sqrt, reads=reads_w, writes=reads_w, scale=scale, bias=EPS)
        self.P.op("dve", lambda e: e.reciprocal(out=out, in_=out), reads=reads_w, writes=reads_w)

    def phase_1a(self, l, hsrc):
        P, I, S, C, nc = self.P, self.I, self.S, self.C, self.nc
        bank = self.bank
        NT, T = self.NT, self.T
        call = self.call
        with contextlib.ExitStack() as st:
            sb = lambda name, shape, dt=F32: self.sb(st, "a_" + name, shape, dt)
            dsw = P.dsem()
            dsl = P.dsem()
            w_in = sb("w_in", [128, 8, IN_COLS], BF16)
            wv = I["w_in"][l].rearrange("(k p) c -> p k c", p=128)
            for k in range(8):
                P.dma("pool", w_in[:, k, :], wv[:, k, :], dsw, writes=[w_in], newgroup=False)
            wuq = sb("wuq", [128, 2, 384], BF16)
            P.dma("pool", wuq[:, 0, :], I["mla_w_uq"][l, 0:128, :], dsw, writes=[wuq], newgroup=False)
            P.dma("pool", wuq[0:64, 1, :], I["mla_w_uq"][l, 128:192, :], dsw, writes=[wuq], newgroup=False)
            wukv = sb("wukv", [128, 512], BF16)
            P.dma("pool", wukv[:], I["mla_w_ukv"][l, :, :], dsw, writes=[wukv], newgroup=False)
            g1T = sb("g1T", [128, 8])
            P.dma("sp", g1T[:], I["g1T"][l], dsl, writes=[g1T], newgroup=False)
            cw = sb("cw", [128, 2, 4]); cb = sb("cb", [128, 2]); ba = sb("ba", [128, 2]); bx = sb("bx", [128, 2])
            lam = sb("lam", [128, 2])
            for tdst, nm in [(cw, "conv_wT"), (cb, "conv_b"), (ba, "lru_ba"), (bx, "lru_bx"), (lam, "lru_lam")]:
                P.dma("sp", tdst[:], I[nm][l], dsl, writes=[tdst], newgroup=False)
            waf = sb("waf", [128, 2, 128]); wxf = sb("wxf", [128, 2, 128])
            wab = sb("wab", [128, 2, 128], BF16); wxb = sb("wxb", [128, 2, 128], BF16)
            P.op("pool", lambda e: e.memset(waf[:], 0.0), writes=[waf])
            P.op("pool", lambda e: e.memset(wxf[:], 0.0), writes=[wxf])
            for nb in range(4):
                r0 = (nb % 2) * 64
                P.dma("sp", waf[r0:r0 + 64, nb // 2, r0:r0 + 64], I["lru_wa"][l, nb], dsl, writes=[waf], newgroup=False)
                P.dma("sp", wxf[r0:r0 + 64, nb // 2, r0:r0 + 64], I["lru_wx"][l, nb], dsl, writes=[wxf], newgroup=False)
            defer = []
            nba = sb("nba", [128, 2]); nbx = sb("nbx", [128, 2])
            defer.append(lambda: self.ts("dve", nba[:], ba[:], -1.0, None, ALU.mult, None, [ba], [nba]))
            defer.append(lambda: self.ts("dve", nbx[:], bx[:], -1.0, None, ALU.mult, None, [bx], [nbx]))
            defer.append(lambda: self.cp("pool", wab[:], waf[:], [waf], [wab]))
            defer.append(lambda: self.cp("pool", wxb[:], wxf[:], [wxf], [wxb]))
            cA = sb("cA", [128, 2]); cA2 = sb("cA2", [128, 2])
            defer.append(lambda: self.act(cA[:], lam[:], AF.Exp, [lam], [cA], scale=-1.0))
            defer.append(lambda: self.act(cA[:], cA[:], AF.Ln, [cA], [cA], bias=1.0))
            defer.append(lambda: self.ts("dve", cA2[:], cA[:], -16.0, None, ALU.mult, None, [cA], [cA2]))
            defer.append(lambda: self.ts("dve", cA[:], cA[:], -8.0, None, ALU.mult, None, [cA], [cA]))
            bcs = []

            def bc(name, src, w):
                t = sb(name, [128, w])
                P.dma("sp", t[:], src.partition_broadcast(128), dsl, writes=[t], newgroup=False)
                bcs.append(t)
                return t
            gq_bc = bc("gq_bc", I["mla_gq"][l], 192)
            gkv_bc = bc("gkv_bc", I["mla_gkv"][l], 128)
            gqn_bc = bc("gqn_bc", I["mla_gqn"][l], 96)
            gkn_bc = bc("gkn_bc", I["mla_gkn"][l], 96)
            fgq_bc = bc("fgq_bc", I["fox_gqn"][l], 64)
            fgk_bc = bc("fgk_bc", I["fox_gkn"][l], 64)
            fbf_bc = bc("fbf_bc", I["fox_bf"][l], 4)
            lb_bc = sb("lb_bc", [128, 256]); oml_bc = sb("oml_bc", [128, 256])
            if l == 0:
                P.op("pool", lambda e: e.memset(lb_bc[:], 0.0), writes=[lb_bc])
                P.op("pool", lambda e: e.memset(oml_bc[:], 1.0), writes=[oml_bc])
            else:
                lg0 = bc("lg0", I["hg_lb"][0], 256)
                lg1 = bc("lg1", I["hg_lb"][1], 256)
                defer.append(lambda: self.tt("dve", lb_bc[:], lg1[:], lg0[:], ALU.subtract, [lg0, lg1], [lb_bc]))
                defer.append(lambda: self.act(lb_bc[:], lb_bc[:], AF.Sigmoid, [lb_bc], [lb_bc]))
                defer.append(lambda: self.ts("dve", oml_bc[:], lb_bc[:], -1.0, 1.0, ALU.mult, ALU.add, [lb_bc], [oml_bc]))
            P.commit(dsw, [w_in, wuq, wukv])
            P.commit(dsl, [g1T, cw, cb, ba, bx, lam, waf, wxf] + bcs)
            for fn in defer:
                fn()
            xe = sb("xe", [128, 2, 3 + 128])
            P.op("pool", lambda e: e.memset(xe[:], 0.0), writes=[xe])
            hprev = sb("hprev", [128, 2])
            P.op("pool", lambda e: e.memset(hprev[:], 0.0), writes=[hprev])
            Sst = [sb(f"Sst{h}", [64, 64]) for h in range(4)]
            for h in range(4):
                P.op("pool", lambda e, h=h: e.memset(Sst[h][:], 0.0), writes=[Sst[h]])
            Sbf = [[sb(f"Sbf{h}_{r}", [64, 64], BF16) for r in range(2)] for h in range(4)]
            hqm = [sb(f"hqm{c}", [64, 4, 128], BF16) for c in range(4)]
            hkem = [sb(f"hkem{c}", [128, 256], BF16) for c in range(4)]
            for c in range(4):
                P.op("pool", lambda e, c=c: e.memset(hqm[c][:], 0.0), writes=[hqm[c]])
            R2 = 2
            ht = self.ring(st, "a_ht", [128, D], F32, R2)
            junk = sb("junk", [128, D], BF16)
            ss = self.ring(st, "a_ss", [128, 8], F32, R2)
            ub = self.ring(st, "a_ub", [128, D], BF16, R2)
            uT = self.ring(st, "a_uT", [128, 8, 128], BF16, R2)
            zt = self.ring(st, "a_zt", [128, 2148], F32, R2)
            ysb = self.ring(st, "a_ysb", [128, 512], F32, R2)
            cs = self.ring(st, "a_cs", [128, 32], F32, R2)
            dsh = [P.dsem() for _ in range(R2)]
            dsy = [P.dsem() for _ in range(R2)]
            dsq = [P.dsem() for _ in range(R2)]
            lu = sb("lu", [128, 128]); lub = sb("lub", [128, 128], BF16)
            lr = sb("lr", [128, 128]); li = sb("li", [128, 128]); la = sb("la", [128, 128]); lm = sb("lm", [128, 128])
            lbb = sb("lbb", [128, 128]); lhs = sb("lhs", [128, 128]); lg = sb("lg", [128, 2, 128]); lya = sb("lya", [128, 2, 128])
            hsig = sb("hsig", [128, 256]); hlogf = sb("hlogf", [128, 256]); hkk = sb("hkk", [128, 256])
            heb = sb("heb", [128, 256]); henb = sb("henb", [128, 256]); hebe = sb("hebe", [128, 256])
            hqd = sb("hqd", [128, 256], BF16); hkd = sb("hkd", [128, 256], BF16); hke = sb("hke", [128, 256], BF16)
            hkdf = sb("hkdf", [128, 256]); hvb = sb("hvb", [128, 256], BF16); hsg = sb("hsg", [128, 256])
            hqdT = sb("hqdT", [64, 4, 128], BF16); hkdT = sb("hkdT", [64, 4, 128], BF16)
            hdec = sb("hdec", [64, 4, 4]); hattm = sb("hattm", [128, 4, 128], BF16)
            mjunk = sb("mjunk", [128, 512]); mss = sb("mss", [128, 16])
            cqn = sb("cqn", [128, 192], BF16); ckvn = sb("ckvn", [128, 128], BF16)
            cqT = sb("cqT", [128, 2, 128], BF16); ckvT = sb("ckvT", [128, 128], BF16)
            qf = sb("qf", [128, 4, 96]); kf = sb("kf", [128, 4, 96])
            qb = sb("qb", [128, 4, 96], BF16); kb = sb("kb", [128, 4, 96], BF16)
            rt = sb("rt", [128, 4, 4, 16])
            vaug = self.ring(st, "a_vaug", [128, 4, 65], BF16, R2)
            qkT = self.ring(st, "a_qkT", [96, 8, 128], BF16, R2)
            fqf = sb("fqf", [128, 4, 64]); fqb = sb("fqb", [128, 256], BF16); fkb = sb("fkb", [128, 256], BF16)
            fvaug = self.ring(st, "a_fvaug", [128, 4, 65], BF16, R2)
            fqkT = self.ring(st, "a_fqkT", [128, 4, 128], BF16, R2)
            flog = sb("flog", [128, 4])
            for r in range(R2):
                P.op("pool", lambda e, r=r: e.memset(vaug[r][:], 1.0), writes=[vaug[r]])
                P.op("pool", lambda e, r=r: e.memset(fvaug[r][:], 1.0), writes=[fvaug[r]])
            identb = C["identb"]; identf = C["c_ident"]

            for j, (p0, n) in enumerate(self.tiles):
                r = j % R2
                H, SS, UB, UT, ZT, YS, CS = ht[r], ss[r], ub[r], uT[r], zt[r], ysb[r], cs[r]
                def load(jn):
                    pp, nn = self.tiles[jn]
                    rr = jn % R2
                    P.dma("sp", ht[rr][0:nn, :], hsrc[pp:pp + nn, :], dsh[rr], writes=[ht[rr]])
                    P.dma("sp", cs[rr][0:nn, 0:16], I["c_cos"][pp:pp + nn, :], dsh[rr], writes=[cs[rr]], newgroup=False)
                    P.dma("sp", cs[rr][0:nn, 16:32], I["c_sin"][pp:pp + nn, :], dsh[rr], writes=[cs[rr]], newgroup=False)
                    P.commit(dsh[rr], [ht[rr], cs[rr]])
                if j == 0:
                    load(0)
                if j + 1 < len(self.tiles):
                    load(j + 1)
                P.op("dve", lambda e, SS=SS: e.memset(SS[:], 0.0), writes=[SS])
                self.act(junk[0:n, :], H[0:n, :], AF.Square, [H, SS], [junk, SS], accum_out=SS[0:n, 0:1])
                self.rsq(SS[0:n, 1:2], SS[0:n, 0:1], 1.0 / D, [SS])
                self.act(UB[0:n, :], H[0:n, :], AF.Copy, [H, SS], [UB], scale=SS[0:n, 1:2])
                b0 = bank[0]
                b0v = b0.t[:].bitcast(BF16)
                for k in range(8):
                    self.tr(b0v[:, k * 128:k * 128 + n], UB[0:n, k * 128:(k + 1) * 128], identb[0:n, 0:n], [UB, identb], [b0])
                self.tt("dve", UT[:, :, 0:n], b0v[:, :].rearrange("p (k t) -> p k t", k=8)[:, :, 0:n],
                        g1T[:, :].unsqueeze(2).broadcast_to([128, 8, n]), ALU.mult, [g1T], [UT, b0])
                b1 = bank[1]
                for m in range(4):
                    for k in range(8):
                        self.mm(b1[:, m * 128:m * 128 + n], w_in[:, k, m * 128:(m + 1) * 128], UT[:, k, 0:n], k == 0, k == 7, [w_in, UT], [b1])
                chunks = [(512, 1024), (1024, 1536), (1536, 1888), (1888, 2400), (2400, 2660)]
                for ci, (c0, c1) in enumerate(chunks):
                    bz = bank[2 + (ci % 2)]
                    wd = c1 - c0
                    for k in range(8):
                        self.mm(bz[0:n, 0:wd], UT[:, k, 0:n], w_in[:, k, c0:c1], k == 0, k == 7, [UT, w_in], [bz])
                    self.cp("act" if ci % 2 == 0 else "dve", ZT[0:n, c0 - 512:c1 - 512], bz[0:n, 0:wd], [], [ZT, bz])
                steps = os.environ.get("K_STEPS", "lru,hgrn,mla,fox").split(",")
                for cc in (range(2) if "lru" in steps else []):
                    self.act(lg[:, cc, 0:n], b1[:, (2 + cc) * 128:(2 + cc) * 128 + n], AF.Gelu_apprx_tanh, [], [lg, b1])
                for cc in (range(2) if "lru" in steps else []):
                    self.cp("act", xe[:, cc, 3:3 + n], b1[:, cc * 128:cc * 128 + n], [], [xe, b1])
                    self.ts("dve", lu[:, 0:n], xe[:, cc, 0:n], cw[:, cc, 0:1], cb[:, cc:cc + 1], ALU.mult, ALU.add, [xe, cw, cb], [lu])
                    for jj in range(1, 4):
                        self.stt("dve", lu[:, 0:n], xe[:, cc, jj:jj + n], cw[:, cc, jj:jj + 1], lu[:, 0:n], ALU.mult, ALU.add, [xe, cw, lu], [lu])
                    self.cp("pool", xe[:, cc, 0:3], xe[:, cc, n:n + 3], [xe], [xe])
                    self.cp("pool", lub[:, 0:n], lu[:, 0:n], [lu], [lub])
                    b4 = bank[4]
                    self.mm(b4[:, 0:n], wab[:, cc, :], lub[:, 0:n], True, True, [wab, lub], [b4])
                    self.mm(b4[:, 128:128 + n], wxb[:, cc, :], lub[:, 0:n], True, True, [wxb, lub], [b4])
                    self.sigm(lr[:, 0:n], b4[:, 0:n], [nba], [lr, b4], bias=nba[:, cc:cc + 1])
                    self.sigm(li[:, 0:n], b4[:, 128:128 + n], [nbx], [li, b4], bias=nbx[:, cc:cc + 1])
                    self.act(la[:, 0:n], lr[:, 0:n], AF.Exp, [lr, cA], [la], scale=cA[:, cc:cc + 1])
                    self.act(lm[:, 0:n], lr[:, 0:n], AF.Exp, [lr, cA2], [lm], scale=cA2[:, cc:cc + 1])
                    self.act(lm[:, 0:n], lm[:, 0:n], AF.Ln, [lm], [lm], scale=-1.0, bias=1.0)
                    self.act(lm[:, 0:n], lm[:, 0:n], AF.Exp, [lm], [lm], scale=0.5)
                    self.tt("dve", lbb[:, 0:n], lm[:, 0:n], li[:, 0:n], ALU.mult, [lm, li], [lbb])
                    self.tt("dve", lbb[:, 0:n], lbb[:, 0:n], lu[:, 0:n], ALU.mult, [lbb, lu], [lbb])
                    P.op("dve", lambda e, n=n, cc=cc: e.tensor_tensor_scan(out=lhs[:, 0:n], data0=la[:, 0:n], data1=lbb[:, 0:n],
                                                                        initial=hprev[:, cc:cc + 1], op0=ALU.mult, op1=ALU.add),
                         reads=[la, lbb, hprev], writes=[lhs])
                    self.cp("dve", hprev[:, cc:cc + 1], lhs[:, n - 1:n], [lhs], [hprev])
                    self.tt("dve", lya[:, cc, 0:n], lhs[:, 0:n], lg[:, cc, 0:n], ALU.mult, [lhs, lg], [lya])
                b5 = bank[5]
                for cc in (range(2) if "lru" in steps else []):
                    self.tr(b5[0:n, cc * 128:(cc + 1) * 128], lya[:, cc, 0:n], identf[:, :], [lya, identf], [b5])
                if "lru" in steps:
                    self.cp("act", YS[0:n, 0:256], b5[0:n, 0:256], [], [YS, b5])
                if "hgrn" in steps:
                    nch = (n + 31) // 32
                    hq_ = ZT[0:n, 0:256]; hf_ = ZT[0:n, 256:512]; hv_ = ZT[0:n, 512:768]; hg_ = ZT[0:n, 768:1024]
                    self.sigm(hsig[0:n, :], hf_, [ZT], [hsig])
                    self.tt("dve", hsig[0:n, :], hsig[0:n, :], oml_bc[0:n, :], ALU.mult, [hsig, oml_bc], [hsig])
                    self.tt("dve", hsig[0:n, :], hsig[0:n, :], lb_bc[0:n, :], ALU.add, [hsig, lb_bc], [hsig])
                    self.act(hlogf[0:n, :], hsig[0:n, :], AF.Ln, [hsig], [hlogf])
                    self.ts("pool", hkk[0:n, :], hsig[0:n, :], -1.0, 1.0, ALU.mult, ALU.add, [hsig], [hkk])
                    b4 = bank[4]
                    self.mm(b4[0:n, 0:256], C["c_triblk"][0:n, 0:n], hlogf[0:n, :], True, True, [C["c_triblk"], hlogf], [b4])
                    b6 = bank[6]
                    self.mm(b6[0:n, 0:256], C["c_blkones"][0:n, 0:n], hlogf[0:n, :], True, True, [C["c_blkones"], hlogf], [b6])
                    self.act(heb[0:n, :], b4[0:n, 0:256], AF.Exp, [], [heb, b4])
                    self.act(henb[0:n, :], b4[0:n, 0:256], AF.Exp, [], [henb, b4], scale=-1.0)
                    self.act(hebe[0:n, :], b6[0:n, 0:256], AF.Exp, [], [hebe, b6])
                    self.tt("dve", hqd[0:n, :], hq_, heb[0:n, :], ALU.mult, [ZT, heb], [hqd])
                    self.tt("dve", hkdf[0:n, :], hkk[0:n, :], henb[0:n, :], ALU.mult, [hkk, henb], [hkdf])
                    self.cp("pool", hkd[0:n, :], hkdf[0:n, :], [hkdf], [hkd])
                    self.tt("dve", hke[0:n, :], hkdf[0:n, :], hebe[0:n, :], ALU.mult, [hkdf, hebe], [hke])
                    self.cp("pool", hvb[0:n, :], hv_, [ZT], [hvb])
                    self.sigm(hsg[0:n, :], hg_, [ZT], [hsg])
                    self.tt("pool", hsg[0:n, :], hsg[0:n, :], hg_, ALU.mult, [hsg, ZT], [hsg])
                    b7 = bank[7]
                    for h in range(4):
                        self.mm(b7[0:64, h * 4:h * 4 + nch], hlogf[0:n, h * 64:(h + 1) * 64], C["c_chunkind"][0:n, 0:nch], True, True,
                                [hlogf, C["c_chunkind"]], [b7])
                    self.act(hdec[:, :, 0:nch], b7[0:64, 0:16].rearrange("p (a c) -> p a c", a=4)[:, :, 0:nch], AF.Exp, [], [hdec, b7])
                    b5v = b5.t[:].bitcast(BF16)
                    for h in range(4):
                        self.tr(b5v[0:64, h * 128:h * 128 + n], hqd[0:n, h * 64:(h + 1) * 64], identb[0:n, 0:n], [hqd, identb], [b5])
                        self.tr(b5v[0:64, 512 + h * 128:512 + h * 128 + n], hkd[0:n, h * 64:(h + 1) * 64], identb[0:n, 0:n], [hkd, identb], [b5])
                    b5q = b5v[0:64, 0:512].rearrange("p (a t) -> p a t", a=4)
                    self.cp("act", hqdT[:, :, 0:n], b5q[:, :, 0:n], [], [hqdT, b5])
                    self.cp("act", hkdT[:, :, 0:n], b5v[0:64, 512:1024].rearrange("p (a t) -> p a t", a=4)[:, :, 0:n], [], [hkdT, b5])
                    for c in range(nch):
                        cn = min(32, n - 32 * c)
                        self.cp("pool", hqm[c][:, :, 32 * c:32 * c + cn], hqdT[:, :, 32 * c:32 * c + cn], [hqdT], [hqm[c]])
                        if c % 2 == 0:
                            self.ts("dve", hkem[c][0:n, :], hke[0:n, :], C["c_chunkind"][0:n, c:c + 1], None, ALU.mult, None,
                                    [hke, C["c_chunkind"]], [hkem[c]])
                        else:
                            self.act(hkem[c][0:n, :], hke[0:n, :], AF.Copy, [hke, C["c_chunkind"]], [hkem[c]], scale=C["c_chunkind"][0:n, c:c + 1])
                    for h in range(4):
                        self.mm(b6[0:n, h * 128:h * 128 + n], hkdT[:, h, 0:n], hqdT[:, h, 0:n], True, True, [hkdT, hqdT], [b6])
                    self.tt("dve", hattm[0:n, :, 0:n], b6[0:n, :].rearrange("p (h t) -> p h t", h=4)[:, :, 0:n],
                            C["hgmask"][0:n, 0:n].unsqueeze(1).broadcast_to([n, 4, n]), ALU.mult, [C["hgmask"]], [hattm, b6])
                    for h in range(4):
                        bu = bank[2 + h // 2]
                        for c in range(nch):
                            col = ((h % 2) * 4 + c) * 64
                            self.mm(bu[0:64, col:col + 64], hkem[c][0:n, h * 64:(h + 1) * 64], hvb[0:n, h * 64:(h + 1) * 64], True, True,
                                    [hkem[c], hvb], [bu])
                    for h in range(4):
                        self.mm(b4[0:n, h * 64:(h + 1) * 64], hattm[0:n, h, 0:n], hvb[0:n, h * 64:(h + 1) * 64], h == 0, False, [hattm, hvb], [b4],
                                skip_group_check=True)
                    for c in range(nch):
                        for h in range(4):
                            bu = bank[2 + h // 2]
                            gidx = (j - 1) * 4 + c + 1 if j > 0 else 0
                            slot_prev = Sbf[h][(gidx - 1) % 2]
                            if gidx > 0:
                                self.mm(b4[0:n, h * 64:(h + 1) * 64], hqm[c][:, h, 0:n], slot_prev[:, :], False, True, [hqm[c], slot_prev], [b4],
                                        skip_group_check=True)
                            col = ((h % 2) * 4 + c) * 64
                            self.stt("dve", Sst[h][:], Sst[h][:], hdec[:, h, c:c + 1], bu[0:64, col:col + 64], ALU.mult, ALU.add,
                                     [Sst[h], hdec], [Sst[h], bu])
                            slot = Sbf[h][gidx % 2]
                            self.cp("pool", slot[:], Sst[h][:], [Sst[h]], [slot])
                    self.tt("dve", YS[0:n, 256:512], b4[0:n, 0:256], hsg[0:n, :], ALU.mult, [hsg], [YS, b4])
                    P.dma("sp", S["Y"][p0:p0 + n, 0:512], YS[0:n, :], dsy[r], reads=[YS], writes=[Buf()])
                if "mla" in steps:
                    cq_ = ZT[0:n, 1024:1216]; ckv_ = ZT[0:n, 1216:1344]; kr_ = ZT[0:n, 1344:1376]
                    P.op("dve", lambda e: e.memset(mss[:], 0.0), writes=[mss])
                    self.act(mjunk[0:n, 0:192], cq_, AF.Square, [ZT, mss], [mjunk, mss], accum_out=mss[0:n, 0:1])
                    self.act(mjunk[0:n, 0:128], ckv_, AF.Square, [ZT, mss], [mjunk, mss], accum_out=mss[0:n, 1:2])
                    self.rsq(mss[0:n, 2:3], mss[0:n, 0:1], 1.0 / 192, [mss])
                    self.rsq(mss[0:n, 3:4], mss[0:n, 1:2], 1.0 / 128, [mss])
                    self.stt("dve", cqn[0:n, :], cq_, mss[0:n, 2:3], gq_bc[0:n, :], ALU.mult, ALU.mult, [ZT, mss, gq_bc], [cqn])
                    self.stt("dve", ckvn[0:n, :], ckv_, mss[0:n, 3:4], gkv_bc[0:n, :], ALU.mult, ALU.mult, [ZT, mss, gkv_bc], [ckvn])
                    self.tr(b5v[:, 0:n], cqn[0:n, 0:128], identb[0:n, 0:n], [cqn, identb], [b5])
                    self.tr(b5v[0:64, 128:128 + n], cqn[0:n, 128:192], identb[0:n, 0:n], [cqn, identb], [b5])
                    self.tr(b5v[:, 256:256 + n], ckvn[0:n, 0:128], identb[0:n, 0:n], [ckvn, identb], [b5])
                    self.cp("act", cqT[:, 0, 0:n], b5v[:, 0:n], [], [cqT, b5])
                    self.cp("act", cqT[0:64, 1, 0:n], b5v[0:64, 128:128 + n], [], [cqT, b5])
                    self.cp("act", ckvT[:, 0:n], b5v[:, 256:256 + n], [], [ckvT, b5])
                    self.mm(b6[0:n, 0:384], cqT[:, 0, 0:n], wuq[:, 0, :], True, False, [cqT, wuq], [b6])
                    self.mm(b6[0:n, 0:384], cqT[0:64, 1, 0:n], wuq[0:64, 1, :], False, True, [cqT, wuq], [b6])
                    self.mm(b7[0:n, 0:512], ckvT[:, 0:n], wukv[:, :], True, True, [ckvT, wukv], [b7])
                    self.cp("act", qf[0:n, :, :], b6[0:n, 0:384].rearrange("p (h c) -> p h c", h=4), [], [qf, b6])
                    b7h = b7[0:n, 0:512].rearrange("p (h c) -> p h c", h=4)
                    self.cp("dve", kf[0:n, :, 0:64], b7h[:, :, 0:64], [], [kf, b7])
                    VA = vaug[r]
                    self.cp("act", VA[0:n, :, 0:64], b7h[:, :, 64:128], [], [VA, b7])
                    self.cp("pool", kf[0:n, :, 64:96], kr_.unsqueeze(1).broadcast_to([n, 4, 32]), [ZT], [kf])
                    for (src, gbc, dst, col) in [(qf, gqn_bc, qb, 4), (kf, gkn_bc, kb, 8)]:
                        self.act(mjunk[0:n, 0:384], src[0:n, :, :].rearrange("p h c -> p (h c)"), AF.Square, [src], [mjunk])
                        P.op("dve", lambda e, n=n, col=col: e.tensor_reduce(out=mss[0:n, col:col + 4], in_=mjunk[0:n, 0:384].rearrange("p (h c) -> p h c", h=4),
                                                                        axis=AX.X, op=ALU.add), reads=[mjunk], writes=[mss])
                        self.rsq(mss[0:n, col:col + 4], mss[0:n, col:col + 4], 1.0 / 96, [mss])
                        self.tt("dve", src[0:n, :, :], src[0:n, :, :], mss[0:n, col:col + 4].unsqueeze(2).broadcast_to([n, 4, 96]), ALU.mult, [src, mss], [src])
                        self.tt("pool", src[0:n, :, :], src[0:n, :, :], gbc[0:n, :].unsqueeze(1).broadcast_to([n, 4, 96]), ALU.mult, [src, gbc], [src])
                        cosb = CS[0:n, 0:16].unsqueeze(1).broadcast_to([n, 4, 16])
                        sinb = CS[0:n, 16:32].unsqueeze(1).broadcast_to([n, 4, 16])
                        t1 = src[0:n, :, 64:80]; t2 = src[0:n, :, 80:96]
                        self.tt("dve", rt[0:n, :, 0, :], t1, cosb, ALU.mult, [src, CS], [rt])
                        self.tt("pool", rt[0:n, :, 1, :], t2, sinb, ALU.mult, [src, CS], [rt])
                        self.tt("dve", rt[0:n, :, 2, :], t1, sinb, ALU.mult, [src, CS], [rt])
                        self.tt("pool", rt[0:n, :, 3, :], t2, cosb, ALU.mult, [src, CS], [rt])
                        self.cp("act", dst[0:n, :, 0:64], src[0:n, :, 0:64], [src], [dst])
                        self.tt("dve", dst[0:n, :, 64:80], rt[0:n, :, 0, :], rt[0:n, :, 1, :], ALU.subtract, [rt], [dst])
                        self.tt("dve", dst[0:n, :, 80:96], rt[0:n, :, 2, :], rt[0:n, :, 3, :], ALU.add, [rt], [dst])
                    QK = qkT[r]
                    for h in range(4):
                        self.tr(b5v[0:96, h * 128:h * 128 + n], qb[0:n, h, :], identb[0:n, 0:n], [qb, identb], [b5])
                        self.tr(b5v[0:96, (4 + h) * 128:(4 + h) * 128 + n], kb[0:n, h, :], identb[0:n, 0:n], [kb, identb], [b5])
                    self.cp("act", QK[:, :, 0:n], b5v[0:96, :].rearrange("p (a t) -> p a t", a=8)[:, :, 0:n], [], [QK, b5])
                    P.dma("sp", S["QTc"][:, :, p0:p0 + n].rearrange("h d t -> d h t"), QK[:, 0:4, 0:n], dsq[r], reads=[QK], writes=[Buf()])
                    P.dma("sp", S["KTc"][:, :, p0:p0 + n].rearrange("h d t -> d h t"), QK[:, 4:8, 0:n], dsq[r], reads=[QK], writes=[Buf()], newgroup=False)
                    P.dma("sp", S["Vc"][:, 0:n, j, :].rearrange("h p c -> p h c"), VA[0:n, :, :], dsq[r], reads=[VA], writes=[Buf()], newgroup=False)
                if "fox" in steps:
                    fq_ = ZT[0:n, 1376:1632]; fk_ = ZT[0:n, 1632:1888]; fv_ = ZT[0:n, 1888:2144]; ff_ = ZT[0:n, 2144:2148]
                    for (src, gbc, dst, col) in [(fq_, fgq_bc, fqb, 12), (fk_, fgk_bc, fkb, 12)]:
                        self.act(mjunk[0:n, 0:256], src, AF.Square, [ZT], [mjunk])
                        P.op("dve", lambda e, n=n, col=col: e.tensor_reduce(out=mss[0:n, col:col + 4], in_=mjunk[0:n, 0:256].rearrange("p (h c) -> p h c", h=4),
                                                                        axis=AX.X, op=ALU.add), reads=[mjunk], writes=[mss])
                        self.rsq(mss[0:n, col:col + 4], mss[0:n, col:col + 4], 1.0 / 64, [mss])
                        self.tt("dve", fqf[0:n, :, :], src.rearrange("p (h c) -> p h c", h=4), mss[0:n, col:col + 4].unsqueeze(2).broadcast_to([n, 4, 64]), ALU.mult, [ZT, mss], [fqf])
                        self.tt("pool", dst[0:n, :].rearrange("p (h c) -> p h c", h=4), fqf[0:n, :, :], gbc[0:n, :].unsqueeze(1).broadcast_to([n, 4, 64]), ALU.mult, [fqf, gbc], [dst])
                    FV = fvaug[r]
                    self.cp("act", FV[0:n, :, 0:64], fv_.rearrange("p (h c) -> p h c", h=4), [ZT], [FV])
                    FQK = fqkT[r]
                    for hp in range(2):
                        self.tr(b5v[:, hp * 128:hp * 128 + n], fqb[0:n, hp * 128:(hp + 1) * 128], identb[0:n, 0:n], [fqb, identb], [b5])
                        self.tr(b5v[:, (2 + hp) * 128:(2 + hp) * 128 + n], fkb[0:n, hp * 128:(hp + 1) * 128], identb[0:n, 0:n], [fkb, identb], [b5])
                    self.cp("act", FQK[:, :, 0:n], b5v[:, 0:512].rearrange("p (a t) -> p a t", a=4)[:, :, 0:n], [], [FQK, b5])
                    P.dma("sp", S["QTd"][:, :, p0:p0 + n].rearrange("a d t -> d a t"), FQK[:, 0:2, 0:n], dsq[r], reads=[FQK], writes=[Buf()], newgroup=False)
                    P.dma("sp", S["KTd"][:, :, p0:p0 + n].rearrange("a d t -> d a t"), FQK[:, 2:4, 0:n], dsq[r], reads=[FQK], writes=[Buf()], newgroup=False)
                    P.dma("sp", S["Vd"][:, 0:n, j, :].rearrange("h p c -> p h c"), FV[0:n, :, :], dsq[r], reads=[FV], writes=[Buf()], newgroup=False)
                    P.commit(dsq[r], [qkT[r], vaug[r], FQK, FV])
                    self.tt("dve", flog[0:n, :], ff_, fbf_bc[0:n, :], ALU.add, [ZT, fbf_bc], [flog])
                    self.act(flog[0:n, :], flog[0:n, :], AF.Exp, [flog], [flog], scale=-1.0)
                    self.act(flog[0:n, :], flog[0:n, :], AF.Ln, [flog], [flog], bias=1.0)
                    self.ts("dve", flog[0:n, :], flog[0:n, :], -1.0, None, ALU.mult, None, [flog], [flog])
                    if j == 0:
                        self.mm(b6[0:n, 0:4], C["c_trifull"][0:n, 0:n], flog[0:n, :], True, True, [C["c_trifull"], flog], [b6])
                    else:
                        pn = self.tiles[j - 1][1]
                        selp = C["c_sel15"] if pn == 16 else C["c_sel127"]
                        self.mm(b6[0:n, 0:4], C["c_trifull"][0:n, 0:n], flog[0:n, :], True, False, [C["c_trifull"], flog], [b6])
                        self.mm(b6[0:n, 0:4], selp[0:pn, 0:n], call[0:pn, j - 1, :], False, True, [selp, call], [b6])
                    self.cp("dve", call[0:n, j, :], b6[0:n, 0:4], [], [call, b6])
            evs = [(d.key, d.cnt) for d in dsy + dsq if d.cnt > 0]
            self.barrier(evs)

    def barrier(self, evs=()):
        P = self.P
        evs = list(evs)
        for e in ("pe", "act", "dve", "pool"):
            if P.cnt[e] > 0:
                evs.append((("E", e), P.cnt[e]))
        for d in P.all_dsems:
            if d.cnt > 0:
                evs.append((d.key, d.cnt))
        for e in ("sp", "pool", "act", "pe", "dve"):
            P.wait_all(e, evs)

    def phase_1b(self, l):
        P, I, S, C, nc = self.P, self.I, self.S, self.C, self.nc
        bank = self.bank
        NT, T = self.NT, self.T
        call = self.call
        tiles = self.tiles
        with contextlib.ExitStack() as st:
            sb = lambda name, shape, dt=F32: self.sb(st, "b_" + name, shape, dt)
            b7 = bank[7]
            for i, (p0, n) in enumerate(tiles):
                sel = C["c_sel8"] if n == 16 else C["c_sel64"]
                self.mm(b7[:, i * 4:(i + 1) * 4], sel[0:n, :], call[0:n, i, :], True, True, [sel, call], [b7])
            cref = sb("cref", [128, NT + 1, 4])
            self.cp("dve", cref[:].rearrange("p a b -> p (a b)"), b7[:, 0:(NT + 1) * 4], [], [cref, b7])
            negc = sb("negc", [128, 4, NT + 1])
            self.ts("dve", negc[:], call[:].rearrange("p j h -> p h j"), -1.0, None, ALU.mult, None, [call], [negc])
            bias = self.ring(st, "b_bias", [128, NT + 1], F32, 4)
            KT = self.ring(st, "b_KT", [128, T], BF16, 2)
            QT = self.ring(st, "b_QT", [128, T], BF16, 2)
            V = self.ring(st, "b_V", [128, NT + 1, 65], BF16, 2)
            dsk = [P.dsem() for _ in range(2)]
            dsv = [P.dsem() for _ in range(2)]
            pt = self.ring(st, "b_pt", [128, 128], BF16, 4)
            osb = self.ring(st, "b_osb", [128, 64], F32, 4)
            rden = self.ring(st, "b_rden", [128, 1], F32, 4)
            dso = [P.dsem() for _ in range(4)]
            units = []
            for h in range(4):
                units.append(("mla", h))
            for h in range(4):
                units.append(("fox", h))
            loaded = {}
            nload = [0]

            def load(u):
                kind, h = u
                if kind == "mla":
                    key = ("mla", h)
                else:
                    key = ("fox", h // 2)
                if key in loaded:
                    return loaded[key]
                r = nload[0] % 2
                nload[0] += 1
                if kind == "mla":
                    P.dma("sp", KT[r][0:96, :], S["KTc"][h], dsk[r], writes=[KT[r]])
                    P.dma("sp", QT[r][0:96, :], S["QTc"][h], dsk[r], writes=[QT[r]], newgroup=False)
                else:
                    P.dma("sp", KT[r][:, :], S["KTd"][h // 2], dsk[r], writes=[KT[r]])
                    P.dma("sp", QT[r][:, :], S["QTd"][h // 2], dsk[r], writes=[QT[r]], newgroup=False)
                P.commit(dsk[r], [KT[r], QT[r]])
                loaded[key] = r
                return r
            nv = [0]

            def loadv(u):
                kind, h = u
                r = nv[0] % 2
                nv[0] += 1
                src = S["Vc"] if kind == "mla" else S["Vd"]
                P.dma("sp", V[r][:], src[h], dsv[r], writes=[V[r]])
                return r
            LA = 3
            NSB = 4
            ring_of = {}
            steps = []
            for ui, u in enumerate(units):
                for i in range(len(tiles)):
                    for jj in range(i + 1):
                        steps.append((ui, i, jj))
            ring_of[0] = (load(units[0]), loadv(units[0]))

            def emit_qk(s):
                ui, i, jj = steps[s]
                kind, h = units[ui]
                fox = kind == "fox"
                r, rv = ring_of[ui]
                p0, n = tiles[i]
                if i == min(3, len(tiles) - 1) and jj == 0 and ui + 1 < len(units):
                    ring_of[ui + 1] = (load(units[ui + 1]), loadv(units[ui + 1]))
                if fox and jj == 0:
                    B_ = bias[(ui * len(tiles) + i) % 4]
                    self.ts("dve", B_[:, 0:i + 1], negc[:, h, 0:i + 1], cref[:, i, h:h + 1], None, ALU.add, None, [negc, cref], [B_])
                rows = slice(0, 96) if not fox else slice((h % 2) * 64, (h % 2) * 64 + 64)
                k0, kn = tiles[jj]
                sbk = bank[s % NSB]
                self.mm(sbk[0:kn, 0:n], KT[r][rows, k0:k0 + kn], QT[r][rows, p0:p0 + n], True, True, [KT[r], QT[r]], [sbk])

            def emit_rest(s):
                ui, i, jj = steps[s]
                kind, h = units[ui]
                fox = kind == "fox"
                r, rv = ring_of[ui]
                p0, n = tiles[i]
                k0, kn = tiles[jj]
                scale = (96.0 if not fox else 64.0) ** -0.5
                mask = C["foxmask"] if fox else C["mlamask"]
                ycol = (512 if not fox else 768) + h * 64
                sbk = bank[s % NSB]
                PT = pt[s % NSB]
                oidx = ui * len(tiles) + i
                ob = bank[4 + (oidx % 2)]
                if fox:
                    B_ = bias[(ui * len(tiles) + i) % 4]
                    self.act(PT[0:kn, 0:n], sbk[0:kn, 0:n], AF.Exp, [B_], [PT, sbk], scale=scale, bias=B_[0:kn, jj:jj + 1])
                else:
                    self.act(PT[0:kn, 0:n], sbk[0:kn, 0:n], AF.Exp, [], [PT, sbk], scale=scale)
                if jj == i:
                    self.tt("pool", PT[0:kn, 0:n], PT[0:kn, 0:n], mask[0:kn, 0:n], ALU.mult, [PT, mask], [PT])
                self.mm(ob[0:n, 0:65], PT[0:kn, 0:n], V[rv][0:kn, jj, :], jj == 0, jj == i, [PT, V[rv]], [ob])
                if jj == i:
                    ro = oidx % 4
                    P.op("dve", lambda e, ob=ob, n=n, ro=ro: e.reciprocal(out=rden[ro][0:n, :], in_=ob[0:n, 64:65]), reads=[], writes=[rden[ro], ob])
                    self.ts("dve", osb[ro][0:n, :], ob[0:n, 0:64], rden[ro][0:n, 0:1], None, ALU.mult, None, [rden[ro]], [osb[ro], ob])
                    P.dma("sp", S["Y"][p0:p0 + n, ycol:ycol + 64], osb[ro][0:n, :], dso[ro], reads=[osb[ro]], writes=[Buf()])
            for s in range(len(steps) + LA):
                if s < len(steps):
                    emit_qk(s)
                if s >= LA:
                    emit_rest(s - LA)
            self.barrier([(d.key, d.cnt) for d in dso if d.cnt > 0])

    def phase_1c(self, l, hsrc):
        P, I, S, C, nc = self.P, self.I, self.S, self.C, self.nc
        bank = self.bank
        NT, T = self.NT, self.T
        moe = (l % 2 == 1)
        with contextlib.ExitStack() as st:
            sb = lambda name, shape, dt=F32: self.sb(st, "c_" + name, shape, dt)
            dsw = P.dsem()
            wout = sb("wout", [128, 8, D], BF16)
            wv = I["w_out"][l].rearrange("(k p) c -> p k c", p=128)
            for k in range(8):
                P.dma("pool", wout[:, k, :], wv[:, k, :], dsw, writes=[wout], newgroup=False)
            og_bc = sb("og_bc", [128, D])
            P.dma("sp", og_bc[:], I["og"][l].partition_broadcast(128), dsw, writes=[og_bc], newgroup=False)
            g2T = sb("g2T", [128, 8])
            P.dma("sp", g2T[:], I["g2T"][l], dsw, writes=[g2T], newgroup=False)
            if moe:
                wr = sb("wr", [128, 8, NEXP])
                P.dma("sp", wr[:], I["moe_wr"].rearrange("(k p) e -> p k e", p=128), dsw, writes=[wr], newgroup=False)
                P.commit(dsw, [wr])
            P.commit(dsw, [wout, og_bc, g2T])
            R2 = 2
            Yt = self.ring(st, "c_Yt", [128, D], F32, R2)
            Ht = self.ring(st, "c_Ht", [128, D], F32, R2)
            dsl = [P.dsem() for _ in range(R2)]
            junk = sb("junk", [128, D])
            ss = self.ring(st, "c_ss", [128, 16], F32, R2)
            yn = sb("yn", [128, D], BF16)
            ynT = sb("ynT", [128, 8, 128], BF16)
            hm = self.ring(st, "c_hm", [128, D], F32, R2)
            dsh = [P.dsem() for _ in range(R2)]
            u2b = sb("u2b", [128, D], BF16)
            u2f = sb("u2f", [128, D], F32)
            u2Tf = sb("u2Tf", [128, 8, 128], F32)
            u2T = self.ring(st, "c_u2T", [128, 8, 128], BF16, R2)
            dsu = [P.dsem() for _ in range(R2)]
            gl = sb("gl", [128, 8]); gl2 = sb("gl2", [128, 8]); gm1 = sb("gm1", [128, 8]); gm2 = sb("gm2", [128, 8])
            gsc = sb("gsc", [128, 8])
            gT = self.ring(st, "c_gT", [128, 8], F32, R2)
            identb = C["identb"]; identf = C["c_ident"]

            def load(jn):
                pp, nn = self.tiles[jn]
                rr = jn % R2
                P.dma("sp", Yt[rr][0:nn, :], S["Y"][pp:pp + nn, :], dsl[rr], writes=[Yt[rr]])
                P.dma("sp", Ht[rr][0:nn, :], hsrc[pp:pp + nn, :], dsl[rr], writes=[Ht[rr]], newgroup=False)
                P.commit(dsl[rr], [Yt[rr], Ht[rr]])
            for j, (p0, n) in enumerate(self.tiles):
                r = j % R2
                if j == 0:
                    load(0)
                if j + 1 < len(self.tiles):
                    load(j + 1)
                Y_, H_, SS, HM, U2T = Yt[r], Ht[r], ss[r], hm[r], u2T[r]
                self.act(junk[0:n, :], Y_[0:n, :], AF.Square, [Y_], [junk])
                P.op("dve", lambda e, n=n, SS=SS: e.tensor_reduce(out=SS[0:n, 0:4], in_=junk[0:n, :].rearrange("p (g c) -> p g c", g=4), axis=AX.X, op=ALU.add),
                     reads=[junk], writes=[SS])
                self.rsq(SS[0:n, 0:4], SS[0:n, 0:4], 1.0 / 256, [SS])
                for g in range(4):
                    self.stt("dve", yn[0:n, g * 256:(g + 1) * 256], Y_[0:n, g * 256:(g + 1) * 256], SS[0:n, g:g + 1],
                             og_bc[0:n, g * 256:(g + 1) * 256], ALU.mult, ALU.mult, [Y_, SS, og_bc], [yn])
                b0 = bank[0]
                b0v = b0.t[:].bitcast(BF16)
                for k in range(8):
                    self.tr(b0v[:, k * 128:k * 128 + n], yn[0:n, k * 128:(k + 1) * 128], identb[0:n, 0:n], [yn, identb], [b0])
                self.cp("act", ynT[:, :, 0:n], b0v[:, :].rearrange("p (k t) -> p k t", k=8)[:, :, 0:n], [], [ynT, b0])
                for c in range(2):
                    bo = bank[2 + c]
                    for k in range(8):
                        self.mm(bo[0:n, 0:512], ynT[:, k, 0:n], wout[:, k, c * 512:(c + 1) * 512], k == 0, k == 7, [ynT, wout], [bo])
                    self.tt("dve", HM[0:n, c * 512:(c + 1) * 512], bo[0:n, 0:512], H_[0:n, c * 512:(c + 1) * 512], ALU.add, [H_], [HM, bo])
                P.dma("sp", S["hmid"][p0:p0 + n, :], HM[0:n, :], dsh[r], reads=[HM], writes=[Buf()])
                P.op("dve", lambda e, SS=SS: e.memset(SS[:, 8:9], 0.0), writes=[SS])
                self.act(junk[0:n, :], HM[0:n, :], AF.Square, [HM, SS], [junk, SS], accum_out=SS[0:n, 8:9])
                self.rsq(SS[0:n, 9:10], SS[0:n, 8:9], 1.0 / D, [SS])
                b1 = bank[1]
                if not moe:
                    self.act(u2b[0:n, :], HM[0:n, :], AF.Copy, [HM, SS], [u2b], scale=SS[0:n, 9:10])
                    b1v = b1.t[:].bitcast(BF16)
                    for k in range(8):
                        self.tr(b1v[:, k * 128:k * 128 + n], u2b[0:n, k * 128:(k + 1) * 128], identb[0:n, 0:n], [u2b, identb], [b1])
                    self.tt("dve", U2T[:, :, 0:n], b1v[:, :].rearrange("p (k t) -> p k t", k=8)[:, :, 0:n],
                            g2T[:, :].unsqueeze(2).broadcast_to([128, 8, n]), ALU.mult, [g2T], [U2T, b1])
                else:
                    self.act(u2f[0:n, :], HM[0:n, :], AF.Copy, [HM, SS], [u2f], scale=SS[0:n, 9:10])
                    for half in range(2):
                        bb = bank[5 + half]
                        for kk in range(4):
                            k = half * 4 + kk
                            self.tr(bb[:, kk * 128:kk * 128 + n], u2f[0:n, k * 128:(k + 1) * 128], identf[0:n, 0:n], [u2f, identf], [bb])
                        self.tt("dve", u2Tf[:, half * 4:half * 4 + 4, 0:n], bb[:, :].rearrange("p (k t) -> p k t", k=4)[:, :, 0:n],
                                g2T[:, half * 4:half * 4 + 4].unsqueeze(2).broadcast_to([128, 4, n]), ALU.mult, [g2T], [u2Tf, bb])
                    self.cp("pool", U2T[:, :, 0:n], u2Tf[:, :, 0:n], [u2Tf], [U2T])
                    b7 = bank[7]
                    for k in range(8):
                        self.mm(b7[0:n, 0:NEXP], u2Tf[:, k, 0:n], wr[:, k, :], k == 0, k == 7, [u2Tf, wr], [b7])
                    self.cp("act", gl[0:n, :], b7[0:n, 0:NEXP], [], [gl, b7])
                    P.op("dve", lambda e, n=n: e.tensor_reduce(out=gsc[0:n, 0:1], in_=gl[0:n, :], axis=AX.X, op=ALU.max), reads=[gl], writes=[gsc])
                    self.ts("dve", gm1[0:n, :], gl[0:n, :], gsc[0:n, 0:1], None, ALU.is_equal, None, [gl, gsc], [gm1])
                    self.stt("dve", gl2[0:n, :], gm1[0:n, :], -1e30, gl[0:n, :], ALU.mult, ALU.add, [gm1, gl], [gl2])
                    P.op("dve", lambda e, n=n: e.tensor_reduce(out=gsc[0:n, 1:2], in_=gl2[0:n, :], axis=AX.X, op=ALU.max), reads=[gl2], writes=[gsc])
                    self.ts("dve", gm2[0:n, :], gl2[0:n, :], gsc[0:n, 1:2], None, ALU.is_equal, None, [gl2, gsc], [gm2])
                    self.tt("dve", gsc[0:n, 2:3], gsc[0:n, 1:2], gsc[0:n, 0:1], ALU.subtract, [gsc], [gsc])
                    self.act(gsc[0:n, 3:4], gsc[0:n, 2:3], AF.Exp, [gsc], [gsc])
                    self.ts("dve", gsc[0:n, 4:5], gsc[0:n, 3:4], 1.0, None, ALU.add, None, [gsc], [gsc])
                    P.op("dve", lambda e, n=n: e.reciprocal(out=gsc[0:n, 4:5], in_=gsc[0:n, 4:5]), reads=[gsc], writes=[gsc])
                    self.tt("dve", gsc[0:n, 5:6], gsc[0:n, 3:4], gsc[0:n, 4:5], ALU.mult, [gsc], [gsc])
                    gate = gT[r]
                    self.ts("dve", gate[0:n, :], gm1[0:n, :], gsc[0:n, 4:5], None, ALU.mult, None, [gm1, gsc], [gate])
                    self.stt("dve", gate[0:n, :], gm2[0:n, :], gsc[0:n, 5:6], gate[0:n, :], ALU.mult, ALU.add, [gm2, gsc, gate], [gate])
                    P.dma("sp", S["gate"][p0:p0 + n, :], gate[0:n, :], dsu[r], reads=[gate], writes=[Buf()])
                P.dma("sp", S["u2T"].rearrange("(k p) t -> p k t", p=128)[:, :, p0:p0 + n], U2T[:, :, 0:n], dsu[r], reads=[U2T], writes=[Buf()],
                      newgroup=not moe)
                P.commit(dsu[r], [U2T, gT[r]])
            self.barrier([(d.key, d.cnt) for d in dsh + dsu if d.cnt > 0])

    def ffn_core(self, st, tok_sets, wsets, out_fn, blend, gated):
        P, I, S, C, nc = self.P, self.I, self.S, self.C, self.nc
        bank = self.bank
        sb = lambda name, shape, dt=F32: self.sb(st, "f_" + name, shape, dt)
        TSMAX = max(sum(n for (_, n) in ts) for ts in tok_sets)
        NTT = max(len(ts) for ts in tok_sets)
        u2 = sb("u2", [128, 8, TSMAX], BF16)
        acc = sb("acc", [128, NTT, D], F32)
        dsx = P.dsem(); dsa = P.dsem(); dsg = P.dsem()
        SW = 256
        if blend:
            u2B = self.ring(st, "f_u2B", [128, 8, SW], BF16, 2)
            dsxb = [P.dsem() for _ in range(2)]
            accB = self.ring(st, "f_accB", [128, D], F32, 2)
            dsb = [P.dsem() for _ in range(2)]
            selA = C["sel"][:, 0:1]; selB = C["sel"][:, 1:2]
        if gated:
            gt = sb("gt", [128, NTT, NEXP], F32)
            gtB = sb("gtB", [128, NTT, NEXP], F32)
        GW = 512
        wg = self.ring(st, "f_wg", [128, 8, GW], BF16, 2)
        wu = self.ring(st, "f_wu", [128, 8, GW], BF16, 2)
        wd = self.ring(st, "f_wd", [128, 4, D], BF16, 2)
        dsw = [P.dsem() for _ in range(2)]
        sg = self.ring(st, "f_sg", [128, 512], F32, 2)
        hid = self.ring(st, "f_hid", [128, 512], BF16, 8)
        dso = [P.dsem() for _ in range(4)]
        u2Tv = S["u2T"].rearrange("(k p) t -> p k t", p=128)
        groups = []
        for (wg_ap, wu_ap, wd_ap, F, e) in wsets:
            f0 = 0
            while f0 < F:
                fw = min(GW, F - f0)
                groups.append((wg_ap, wu_ap, wd_ap, f0, fw, e))
                f0 += fw
        gcount = 0
        hcount = 0

        def loadw(gi):
            wg_ap, wu_ap, wd_ap, f0, fw, e = groups[gi % len(groups)]
            r = gi % 2
            wgv = wg_ap.rearrange("(k p) f -> p k f", p=128)
            wuv = wu_ap.rearrange("(k p) f -> p k f", p=128)
            for k in range(0, 8, 4):
                P.dma("pool", wg[r][:, k:k + 4, 0:fw], wgv[:, k:k + 4, f0:f0 + fw], dsw[r], writes=[wg[r]], newgroup=(k == 0))
            for k in range(0, 8, 4):
                P.dma("pool", wu[r][:, k:k + 4, 0:fw], wuv[:, k:k + 4, f0:f0 + fw], dsw[r], writes=[wu[r]], newgroup=False)
            nb = fw // 128
            P.dma("pool", wd[r][:, 0:nb, :], wd_ap[f0:f0 + fw, :].rearrange("(b p) d -> p b d", p=128), dsw[r], writes=[wd[r]], newgroup=False)
            P.commit(dsw[r], [wg[r], wu[r], wd[r]])
        total_groups = len(groups) * len(tok_sets)
        loadw(0)
        nstage = 0
        for si, ts in enumerate(tok_sets):
            col = 0
            cols = []
            for ti, (rows, n) in enumerate(ts):
                cols.append(col)
                col += n
            TS = col
            if not blend:
                r00 = ts[0][0]
                for ti, (rows, n) in enumerate(ts):
                    assert rows == r00 + cols[ti]
                    P.dma("sp", acc[0:n, ti, :], S["hmid"][rows:rows + n, :], dsa, writes=[acc], newgroup=(ti == 0))
                for c0 in range(0, TS, 512):
                    cw_ = min(512, TS - c0)
                    P.dma("sp", u2[:, :, c0:c0 + cw_], u2Tv[:, :, r00 + c0:r00 + c0 + cw_], dsx, writes=[u2], newgroup=(c0 == 0))
                P.commit(dsx, [u2])
                P.commit(dsa, [acc])
            else:
                rA0, rB0 = ts[0][0]
                for ti, (rows, n) in enumerate(ts):
                    rA, rB = rows
                    assert rA == rA0 + cols[ti] and rB == rB0 + cols[ti]
                    P.dma("sp", acc[0:n, ti, :], S["hmid"][rA:rA + n, :], dsa, writes=[acc], newgroup=(ti == 0))
                    if gated:
                        P.dma("sp", gt[0:n, ti, :], S["gate"][rA:rA + n, :], dsg, writes=[gt], newgroup=(ti == 0))
                        P.dma("sp", gtB[0:n, ti, :], S["gate"][rB:rB + n, :], dsg, writes=[gtB], newgroup=False)
                P.commit(dsa, [acc])
                if gated:
                    P.commit(dsg, [gt, gtB])
                for c0 in range(0, TS, 512):
                    cw_ = min(512, TS - c0)
                    P.dma("sp", u2[:, :, c0:c0 + cw_], u2Tv[:, :, rA0 + c0:rA0 + c0 + cw_], dsx, writes=[u2], newgroup=(c0 == 0))
                P.commit(dsx, [u2])
                for ti, (rows, n) in enumerate(ts):
                    rA, rB = rows
                    ab = accB[ti % 2]
                    P.dma("sp", ab[0:n, :], S["hmid"][rB:rB + n, :], dsb[ti % 2], writes=[ab])
                    self.act(acc[0:n, ti, :], acc[0:n, ti, :], AF.Copy, [acc, C["sel"]], [acc], scale=selA[0:n, :])
                    self.stt("dve", acc[0:n, ti, :], ab[0:n, :], selB[0:n, :], acc[0:n, ti, :], ALU.mult, ALU.add, [ab, C["sel"], acc], [acc])
                for c0 in range(0, TS, SW):
                    cw_ = min(SW, TS - c0)
                    ub_ = u2B[nstage % 2]
                    P.dma("sp", ub_[:, :, 0:cw_], u2Tv[:, :, rB0 + c0:rB0 + c0 + cw_], dsxb[nstage % 2], writes=[ub_])
                    nstage += 1
                    self.act(u2[:, :, c0:c0 + cw_], u2[:, :, c0:c0 + cw_], AF.Copy, [u2, C["sel"]], [u2], scale=selA)
                    self.stt("dve", u2[:, :, c0:c0 + cw_], ub_[:, :, 0:cw_], selB, u2[:, :, c0:c0 + cw_], ALU.mult, ALU.add, [ub_, C["sel"], u2], [u2])
                if gated:
                    self.act(gt[:], gt[:], AF.Copy, [gt, C["sel"]], [gt], scale=selA)
                    self.stt("dve", gt[:], gtB[:], selB, gt[:], ALU.mult, ALU.add, [gtB, C["sel"], gt], [gt])
            chunks = []
            cur = []
            cw_ = 0
            for ti, (rows, n) in enumerate(ts):
                if cw_ + n > 512:
                    chunks.append(cur); cur = []; cw_ = 0
                cur.append(ti); cw_ += n
            if cur:
                chunks.append(cur)
            for gi_local in range(len(groups)):
                gi = gcount
                gcount += 1
                wg_ap, wu_ap, wd_ap, f0, fw, e = groups[gi_local]
                r = gi % 2
                if gi + 1 < total_groups:
                    loadw(gi + 1)
                nb = fw // 128
                for ch in chunks:
                    c0 = cols[ch[0]]
                    cw = sum(ts[ti][1] for ti in ch)
                    hslots = []
                    for fb in range(nb):
                        bg = bank[(hcount % 2) * 2]
                        bu = bank[(hcount % 2) * 2 + 1]
                        H_ = hid[hcount % 8]
                        SG = sg[hcount % 2]
                        hcount += 1
                        for k in range(8):
                            self.mm(bg[:, 0:cw], wg[r][:, k, fb * 128:(fb + 1) * 128], u2[:, k, c0:c0 + cw], k == 0, k == 7, [wg[r], u2], [bg])
                        for k in range(8):
                            self.mm(bu[:, 0:cw], wu[r][:, k, fb * 128:(fb + 1) * 128], u2[:, k, c0:c0 + cw], k == 0, k == 7, [wu[r], u2], [bu])
                        self.act(SG[:, 0:cw], bg[:, 0:cw], AF.Silu, [], [SG, bg])
                        self.tt("dve", H_[:, 0:cw], bu[:, 0:cw], SG[:, 0:cw], ALU.mult, [SG], [H_, bu])
                        hslots.append(H_)
                    for ti in ch:
                        n = ts[ti][1]
                        lc = cols[ti] - c0
                        for half in range(2):
                            bd = bank[4 + 2 * (ti % 2) + half]
                            for fb in range(nb):
                                self.mm(bd[0:n, 0:512], hslots[fb][:, lc:lc + n], wd[r][:, fb, half * 512:(half + 1) * 512], fb == 0, fb == nb - 1,
                                        [hslots[fb], wd[r]], [bd])
                            a_ = acc[0:n, ti, half * 512:(half + 1) * 512]
                            if gated:
                                self.stt("dve", a_, bd[0:n, 0:512], gt[0:n, ti, e:e + 1], a_, ALU.mult, ALU.add, [acc, gt], [acc, bd])
                            else:
                                self.tt("dve", a_, bd[0:n, 0:512], a_, ALU.add, [acc], [acc, bd])
            for ti, (rows, n) in enumerate(ts):
                dst = out_fn(si, ti)
                if dst is None:
                    continue
                ev = P.dma("sp", dst, acc[0:n, ti, :], dso[ti % 4], reads=[acc], writes=[Buf()])
                self.final_evs.append(ev)
        self.barrier([(d.key, d.cnt) for d in dso if d.cnt > 0])

    def phase_ffn(self, hdst):
        I = self.I
        T = self.T
        rows = [(r0, min(128, T - r0)) for r0 in range(0, T, 128)]
        TSN = 16
        tok_sets = [rows[i:i + TSN] for i in range(0, len(rows), TSN)]
        if len(tok_sets) > 1 and len(tok_sets[-1]) == 1:
            tok_sets[-2] = tok_sets[-2] + tok_sets[-1]
            tok_sets.pop()
        final = hdst is None

        def out_fn(si, ti):
            r0, n = tok_sets[si][ti]
            if not final:
                return hdst[r0:r0 + n, :]
            lo = max(r0, NMETA)
            return None if True else None
        with contextlib.ExitStack() as st:
            self.ffn_core(st, tok_sets, [(I["ffn_wg"], I["ffn_wu"], I["ffn_wd"], D_FF, 0)], out_fn, blend=False, gated=False)

    def phase_moe(self):
        I = self.I
        NTOK = self.moe_tokens
        ntile = NTOK // 128
        TSN = 16
        tiles_ab = [((NMETA + 128 * i, NMETA + NTOK + 128 * i), 128) for i in range(ntile)]
        tok_sets = [tiles_ab[i:i + TSN] for i in range(0, ntile, TSN)]
        out = self.out_ap

        def out_fn(si, ti):
            i = si * TSN + ti
            return out[128 * i:128 * (i + 1), :]
        wsets = [(I["moe_wg"][e], I["moe_wu"][e], I["moe_wd"][e], D_FFE, e) for e in range(NEXP)]
        with contextlib.ExitStack() as st:
            self.ffn_core(st, tok_sets, wsets, out_fn, blend=True, gated=True)


def prep_inputs(inp, b, half, NT, depth=2):
    f = lambda a: np.ascontiguousarray(np.asarray(a, dtype=np.float32))
    T = NMETA + 128 * NT
    m = {}
    m["xin"] = f(np.concatenate([inp["meta"], inp["x"][b]], axis=0))
    sel = np.zeros((128, 2), np.float32)
    sel[:, half] = 1.0
    m["sel"] = sel
    m.update(make_consts(T))
    L = depth
    colmaj = lambda v: f(np.asarray(v).reshape(L, -1, 128).transpose(0, 2, 1))
    m["w_in"] = f(inp["w_in"][:L]); m["w_out"] = f(inp["w_out"][:L])
    m["g1T"] = colmaj(inp["norm1_g"][:L]); m["g2T"] = colmaj(inp["norm2_g"][:L])
    m["og"] = f(inp["out_norm_g"][:L])
    cw = np.asarray(inp["lru_conv_w"][:L])
    m["conv_wT"] = f(cw.transpose(0, 2, 1).reshape(L, 2, 128, 4).transpose(0, 2, 1, 3))
    m["conv_b"] = colmaj(inp["lru_conv_b"][:L])
    m["lru_ba"] = colmaj(np.asarray(inp["lru_ba"][:L]).reshape(L, 256))
    m["lru_bx"] = colmaj(np.asarray(inp["lru_bx"][:L]).reshape(L, 256))
    m["lru_lam"] = colmaj(inp["lru_lambda"][:L])
    m["lru_wa"] = f(inp["lru_wa"][:L]); m["lru_wx"] = f(inp["lru_wx"][:L])
    m["hg_lb"] = f(inp["hg_lb_logits"][:2])
    for k in ["mla_gq", "mla_w_uq", "mla_gkv", "mla_w_ukv", "mla_gqn", "mla_gkn", "fox_gqn", "fox_gkn", "fox_bf"]:
        m[k] = f(inp[k][:L])
    m["ffn_wg"] = f(inp["ffn_w_gate"][0]); m["ffn_wu"] = f(inp["ffn_w_up"][0]); m["ffn_wd"] = f(inp["ffn_w_down"][0])
    if depth > 1:
        m["moe_wr"] = f(inp["moe_w_router"][0]); m["moe_wg"] = f(inp["moe_w_gate"][0])
        m["moe_wu"] = f(inp["moe_w_up"][0]); m["moe_wd"] = f(inp["moe_w_down"][0])
    return m


_CACHE = {}


def _get_program(NT):
    if NT not in _CACHE:
        b = Builder(NT, depth=2, debug=False)
        nc = b.build()
        _CACHE[NT] = (b, nc)
    return _CACHE[NT]


def kernel(**inputs):
    x = np.asarray(inputs["x"])
    Bsz, SEQ, _ = x.shape
    NT = SEQ // 128
    b, nc = _get_program(NT)
    half_tokens = b.moe_tokens
    n_cores = 8
    in_maps = []
    shared = None
    for c in range(n_cores):
        bi, half = c % Bsz, c // Bsz
        if shared is None:
            m = prep_inputs(inputs, bi, half, NT, depth=2)
            shared = m
        else:
            m = dict(shared)
            m["xin"] = np.ascontiguousarray(np.concatenate([np.asarray(inputs["meta"], np.float32), np.asarray(x[bi], np.float32)], axis=0))
            sel = np.zeros((128, 2), np.float32)
            sel[:, half] = 1.0
            m["sel"] = sel
        in_maps.append({k: v for k, v in m.items() if k in b.inputs})
    res = run_bass_kernel_spmd(nc, in_maps, core_ids=list(range(n_cores)))
    out = np.empty((Bsz, SEQ, D), np.float32)
    for c in range(n_cores):
        bi, half = c % Bsz, c // Bsz
        out[bi, half * half_tokens:(half + 1) * half_tokens] = np.asarray(res.results[c]["out"])
    return out
```

```python
import contextlib
import os
import numpy as np
import ml_dtypes
import concourse.bass as bass
import concourse.mybir as mybir
from concourse.bass_utils import run_bass_kernel_spmd

F32 = mybir.dt.float32
BF16 = mybir.dt.bfloat16
AF = mybir.ActivationFunctionType
ALU = mybir.AluOpType
AX = mybir.AxisListType

ENGS = ("pe", "act", "dve", "pool", "sp")
D = 1024
NMETA = 16
EPS = 1e-6
IN_COLS = 2660
D_FF = 2816
D_FFE = 3584
NEXP = 8


class Buf:
    __slots__ = ("name", "w", "r")

    def __init__(self, name=""):
        self.name = name
        self.w = None
        self.r = {}


class DSem:
    __slots__ = ("h", "cnt", "key")

    def __init__(self, h, key):
        self.h = h
        self.cnt = 0
        self.key = key


class TT:
    __slots__ = ("t", "b")

    def __init__(self, t, b=None, name=""):
        self.t = t
        self.b = b if b is not None else Buf(name)

    def __getitem__(self, k):
        return self.t[k]


def _bufs(xs):
    out = []
    for x in xs:
        if x is None:
            continue
        out.append(x.b if isinstance(x, TT) else x)
    return out


class Prog:
    def __init__(self, nc, stack):
        self.nc = nc
        self.stack = stack
        self.q = {e: [] for e in ENGS}
        self.cnt = {e: 0 for e in ENGS}
        self.seen = {e: {} for e in ENGS}
        self.semh = {}
        for e in ENGS:
            h = stack.enter_context(nc.semaphore("es_" + e))
            self.semh[("E", e)] = h
        self.nd = 0
        self.all_dsems = []

    def dsem(self, name=None):
        self.nd += 1
        key = ("D", self.nd)
        h = self.stack.enter_context(self.nc.semaphore(name or f"ds{self.nd}"))
        self.semh[key] = h
        d = DSem(h, key)
        self.all_dsems.append(d)
        return d

    def _waits(self, eng, reads, writes, extra=()):
        need = {}

        def add(ev, raw):
            if ev is None:
                return
            k, v = ev
            if k == ("E", eng) and not raw:
                return
            if need.get(k, 0) < v:
                need[k] = v
        for b in reads:
            add(b.w, True)
        for b in writes:
            add(b.w, False)
            for k, v in b.r.items():
                add((k, v), False)
        for ev in extra:
            add(ev, True)
        out = []
        seen = self.seen[eng]
        for k, v in need.items():
            if seen.get(k, 0) < v:
                seen[k] = v
                out.append((k, v))
        return out

    def _mark(self, ev, reads, writes):
        k, v = ev
        for b in reads:
            if b.r.get(k, 0) < v:
                b.r[k] = v
        for b in writes:
            b.w = ev
            b.r = {}

    def op(self, eng, fn, reads=(), writes=()):
        reads = _bufs(reads)
        writes = _bufs(writes)
        waits = self._waits(eng, reads, writes)
        self.cnt[eng] += 1
        ev = (("E", eng), self.cnt[eng])
        self._mark(ev, reads, writes)
        self.q[eng].append((waits, fn, (("E", eng), 1)))
        return ev

    def dma(self, eng, out, in_, ds, reads=(), writes=(), newgroup=True, **kw):
        reads = _bufs(reads)
        writes = _bufs(writes)
        extra = ()
        if newgroup and ds.cnt > 0:
            extra = ((ds.key, ds.cnt),)
        waits = self._waits(eng, reads, writes, extra)
        ds.cnt += 16
        ev = (ds.key, ds.cnt)
        self._mark(ev, reads, writes)
        self.q[eng].append((waits, lambda e: e.dma_start(out=out, in_=in_, **kw), (ds.key, 16)))
        return ev

    def commit(self, ds, bufs):
        for b in _bufs(bufs):
            if b.w is not None and b.w[0] == ds.key:
                b.w = (ds.key, ds.cnt)
            if ds.key in b.r:
                b.r[ds.key] = ds.cnt

    def wait_all(self, eng, evs):
        waits = self._waits(eng, (), (), evs)
        self.q[eng].append((waits, None, None))

    def emit(self):
        nc = self.nc
        semh = self.semh
        with nc.Block() as block:
            def mk(ename):
                def body(e):
                    for waits, fn, inc in self.q[ename]:
                        for k, v in waits:
                            e.wait_ge(semh[k], v)
                        if fn is None:
                            continue
                        ins = fn(e)
                        if inc is not None:
                            ins.then_inc(semh[inc[0]], inc[1])
                return body
            block.tensor(mk("pe"))
            block.scalar(mk("act"))
            block.vector(mk("dve"))
            block.gpsimd(mk("pool"))
            block.sync(mk("sp"))


def make_consts(T):
    s = np.arange(128)[:, None]
    t = np.arange(128)[None, :]
    c = {}
    c["c_ident"] = np.eye(128, dtype=np.float32)
    c["c_triblk"] = ((s // 32 == t // 32) & (s <= t)).astype(np.float32)
    c["c_blkones"] = (s // 32 == t // 32).astype(np.float32)
    c["c_chunkind"] = (s // 32 == np.arange(4)[None, :]).astype(np.float32)
    c["c_trifull"] = (s <= t).astype(np.float32)
    c["c_sel127"] = np.broadcast_to((s == 127), (128, 128)).astype(np.float32).copy()
    c["c_sel15"] = np.broadcast_to((s == 15), (128, 128)).astype(np.float32).copy()
    c["c_sel64"] = np.broadcast_to((s == 64), (128, 128)).astype(np.float32).copy()
    c["c_sel8"] = np.broadcast_to((s == 8), (128, 128)).astype(np.float32).copy()
    c["c_mask_mla"] = (s // 64 <= t // 64).astype(np.float32)
    pos = np.arange(T, dtype=np.float32)
    inv = (10000.0 ** (-np.arange(16, dtype=np.float32) / 16)).astype(np.float32)
    ang = pos[:, None] * inv[None, :]
    c["c_cos"] = np.cos(ang).astype(np.float32)
    c["c_sin"] = np.sin(ang).astype(np.float32)
    return c


def tile_list(NT):
    return [(0, NMETA)] + [(NMETA + 128 * i, 128) for i in range(NT)]


class Builder:
    def __init__(self, NT, depth=2, debug=False, stop_after=None, moe_tokens=None):
        self.NT = NT
        self.T = NMETA + 128 * NT
        self.depth = depth
        self.debug = debug
        self.stop_after = stop_after
        self.tiles = tile_list(NT)
        self.nc = bass.Bass("TRN2", target_bir_lowering=False)
        self.inputs = {}
        self.outputs = {}
        self.moe_tokens = moe_tokens if moe_tokens is not None else (128 * NT) // 2

    def din(self, name, shape, dt=F32):
        t = self.nc.dram_tensor(name, list(shape), dt, kind="ExternalInput")
        self.inputs[name] = (tuple(shape), dt)
        return t.ap()

    def dscratch(self, name, shape, dt=F32, dbg=False):
        if dbg and self.debug:
            t = self.nc.dram_tensor(name, list(shape), dt, kind="ExternalOutput")
            self.outputs[name] = (tuple(shape), dt)
        else:
            t = self.nc.dram_tensor(name, list(shape), dt, kind="Internal")
        return t.ap()

    def dout(self, name, shape, dt=F32):
        t = self.nc.dram_tensor(name, list(shape), dt, kind="ExternalOutput")
        self.outputs[name] = (tuple(shape), dt)
        return t.ap()

    def sb(self, st, name, shape, dt=F32):
        self._uid = getattr(self, "_uid", 0) + 1
        return TT(st.enter_context(self.nc.sbuf_tensor(f"s{self._uid}_{name}", list(shape), dt)), name=name)

    def ring(self, st, name, shape, dt, n):
        return [self.sb(st, f"{name}{i}", shape, dt) for i in range(n)]

    def build(self):
        nc = self.nc
        T = self.T
        NT = self.NT
        depth = self.depth
        I = {}
        I["xin"] = self.din("xin", [T, D])
        I["sel"] = self.din("sel", [128, 2])
        for nm, shp in [("c_ident", [128, 128]), ("c_triblk", [128, 128]), ("c_blkones", [128, 128]),
                        ("c_chunkind", [128, 4]), ("c_trifull", [128, 128]), ("c_sel127", [128, 128]),
                        ("c_sel15", [128, 128]), ("c_sel64", [128, 128]), ("c_sel8", [128, 128]),
                        ("c_mask_mla", [128, 128]), ("c_cos", [T, 16]), ("c_sin", [T, 16])]:
            I[nm] = self.din(nm, shp)
        L = depth
        I["w_in"] = self.din("w_in", [L, D, IN_COLS])
        I["w_out"] = self.din("w_out", [L, D, D])
        I["g1T"] = self.din("g1T", [L, 128, 8])
        I["g2T"] = self.din("g2T", [L, 128, 8])
        I["og"] = self.din("og", [L, D])
        I["conv_wT"] = self.din("conv_wT", [L, 128, 2, 4])
        I["conv_b"] = self.din("conv_b", [L, 128, 2])
        I["lru_ba"] = self.din("lru_ba", [L, 128, 2])
        I["lru_bx"] = self.din("lru_bx", [L, 128, 2])
        I["lru_lam"] = self.din("lru_lam", [L, 128, 2])
        I["lru_wa"] = self.din("lru_wa", [L, 4, 64, 64])
        I["lru_wx"] = self.din("lru_wx", [L, 4, 64, 64])
        I["hg_lb"] = self.din("hg_lb", [2, 256])
        I["mla_gq"] = self.din("mla_gq", [L, 192])
        I["mla_w_uq"] = self.din("mla_w_uq", [L, 192, 384])
        I["mla_gkv"] = self.din("mla_gkv", [L, 128])
        I["mla_w_ukv"] = self.din("mla_w_ukv", [L, 128, 512])
        I["mla_gqn"] = self.din("mla_gqn", [L, 96])
        I["mla_gkn"] = self.din("mla_gkn", [L, 96])
        I["fox_gqn"] = self.din("fox_gqn", [L, 64])
        I["fox_gkn"] = self.din("fox_gkn", [L, 64])
        I["fox_bf"] = self.din("fox_bf", [L, 4])
        I["ffn_wg"] = self.din("ffn_wg", [D, D_FF])
        I["ffn_wu"] = self.din("ffn_wu", [D, D_FF])
        I["ffn_wd"] = self.din("ffn_wd", [D_FF, D])
        if depth > 1:
            I["moe_wr"] = self.din("moe_wr", [D, NEXP])
            I["moe_wg"] = self.din("moe_wg", [NEXP, D, D_FFE])
            I["moe_wu"] = self.din("moe_wu", [NEXP, D, D_FFE])
            I["moe_wd"] = self.din("moe_wd", [NEXP, D_FFE, D])
        self.I = I
        S = {}
        dbg = True
        S["hA"] = self.dscratch("hA", [T, D], F32, dbg)
        S["hmid"] = self.dscratch("hmid", [T, D], F32, dbg)
        S["Y"] = self.dscratch("Y", [T, D], F32, dbg)
        S["u2T"] = self.dscratch("u2T", [D, T], BF16, dbg)
        S["QTc"] = self.dscratch("QTc", [4, 96, T], BF16, dbg)
        S["KTc"] = self.dscratch("KTc", [4, 96, T], BF16, dbg)
        S["Vc"] = self.dscratch("Vc", [4, 128, NT + 1, 65], BF16, dbg)
        S["QTd"] = self.dscratch("QTd", [2, 128, T], BF16, dbg)
        S["KTd"] = self.dscratch("KTd", [2, 128, T], BF16, dbg)
        S["Vd"] = self.dscratch("Vd", [4, 128, NT + 1, 65], BF16, dbg)
        S["gate"] = self.dscratch("gate", [T, NEXP], F32, dbg)
        self.S = S
        n_out = self.moe_tokens if depth > 1 else 128 * NT
        self.out_ap = self.dout("out", [n_out, D])

        with contextlib.ExitStack() as st:
            P = Prog(nc, st)
            self.P = P
            self.bank = [TT(st.enter_context(nc.psum_tensor(f"bank{i}", [128, 512], F32)), name=f"bank{i}")
                         for i in range(8)]
            with contextlib.ExitStack() as cst:
                self.load_consts(cst)
                for l in range(depth):
                    hsrc = I["xin"] if l == 0 else S["hA"]
                    self.phase_1a(l, hsrc)
                    if self.stop_after == ("1a", l):
                        break
                    self.phase_1b(l)
                    if self.stop_after == ("1b", l):
                        break
                    self.phase_1c(l, hsrc)
                    if self.stop_after == ("1c", l):
                        break
                    if l == 0:
                        self.phase_ffn(S["hA"])
                    else:
                        self.phase_moe()
            P.wait_all("sp", self.final_evs)
            P.emit()
        return nc

    def load_consts(self, st):
        P = self.P
        I = self.I
        self.final_evs = []
        self.ds_const = P.dsem("ds_const")
        C = {}
        for nm in ["c_ident", "c_triblk", "c_blkones", "c_trifull", "c_sel127", "c_sel15", "c_sel64", "c_sel8",
                   "c_mask_mla"]:
            C[nm] = self.sb(st, nm, [128, 128], F32)
            P.dma("sp", C[nm][:], I[nm][:, :], self.ds_const, writes=[C[nm]], newgroup=False)
        C["c_chunkind"] = self.sb(st, "c_chunkind", [128, 4], F32)
        P.dma("sp", C["c_chunkind"][:], I["c_chunkind"][:, :], self.ds_const, writes=[C["c_chunkind"]], newgroup=False)
        C["sel"] = self.sb(st, "sel", [128, 2], F32)
        P.dma("sp", C["sel"][:], I["sel"][:, :], self.ds_const, writes=[C["sel"]], newgroup=False)
        P.commit(self.ds_const, list(C.values()))
        C["identb"] = self.sb(st, "identb", [128, 128], BF16)
        P.op("pool", lambda e: e.tensor_copy(out=C["identb"][:], in_=C["c_ident"][:]), reads=[C["c_ident"]], writes=[C["identb"]])
        C["hgmask"] = self.sb(st, "hgmask", [128, 128], BF16)
        P.op("pool", lambda e: e.tensor_copy(out=C["hgmask"][:], in_=C["c_triblk"][:]), reads=[C["c_triblk"]], writes=[C["hgmask"]])
        C["foxmask"] = self.sb(st, "foxmask", [128, 128], BF16)
        P.op("pool", lambda e: e.tensor_copy(out=C["foxmask"][:], in_=C["c_trifull"][:]), reads=[C["c_trifull"]], writes=[C["foxmask"]])
        C["mlamask"] = self.sb(st, "mlamask", [128, 128], BF16)
        P.op("pool", lambda e: e.tensor_copy(out=C["mlamask"][:], in_=C["c_mask_mla"][:]), reads=[C["c_mask_mla"]], writes=[C["mlamask"]])
        self.C = C
        self.call = self.sb(st, "c_all", [128, self.NT + 1, 4], F32)
        P.op("pool", lambda e: e.memset(self.call[:], 0.0), writes=[self.call])

    def mm(self, out, lhsT, rhs, start, stop, reads, writes, **kw):
        self.P.op("pe", lambda e: e.matmul(out, lhsT=lhsT, rhs=rhs, start=start, stop=stop, **kw), reads=reads, writes=writes)

    def tr(self, out, in_, ident, reads, writes):
        self.P.op("pe", lambda e: e.transpose(out=out, in_=in_, identity=ident), reads=reads, writes=writes)

    def act(self, out, in_, func, reads, writes, **kw):
        self.P.op("act", lambda e: e.activation(out=out, in_=in_, func=func, **kw), reads=reads, writes=writes)

    def tt(self, eng, out, in0, in1, op, reads, writes):
        self.P.op(eng, lambda e: e.tensor_tensor(out=out, in0=in0, in1=in1, op=op), reads=reads, writes=writes)

    def ts(self, eng, out, in0, s1, s2, op0, op1, reads, writes):
        if op1 is None:
            self.P.op(eng, lambda e: e.tensor_scalar(out=out, in0=in0, scalar1=s1, scalar2=None, op0=op0), reads=reads, writes=writes)
        else:
            self.P.op(eng, lambda e: e.tensor_scalar(out=out, in0=in0, scalar1=s1, scalar2=s2, op0=op0, op1=op1), reads=reads, writes=writes)

    def stt(self, eng, out, in0, scalar, in1, op0, op1, reads, writes):
        self.P.op(eng, lambda e: e.scalar_tensor_tensor(out=out, in0=in0, scalar=scalar, in1=in1, op0=op0, op1=op1), reads=reads, writes=writes)

    def cp(self, eng, out, in_, reads, writes):
        if eng == "act":
            self.P.op("act", lambda e: e.copy(out=out, in_=in_), reads=reads, writes=writes)
        else:
            self.P.op(eng, lambda e: e.tensor_copy(out=out, in_=in_), reads=reads, writes=writes)

    def rsq(self, out, in_, scale, rw):
        self.act(out, in_, AF.Ln, rw, rw, scale=scale, bias=EPS)
        self.act(out, out, AF.Exp, rw, rw, scale=-0.5)

    def sigm(self, out, in_, reads, writes, bias=None):
        if bias is None:
            self.act(out, in_, AF.Exp, reads, writes, scale=-1.0)
        else:
            self.act(out, in_, AF.Exp, reads, writes, scale=-1.0, bias=bias)
        w2 = [writes[0]]
        self.act(out, out, AF.Ln, w2, w2, bias=1.0)
        self.act(out, out, AF.Exp, w2, w2, scale=-1.0)

    def rstd(self, ss, out, n, scale, reads_w):
        self.act(out, ss, AF.Sqrt, reads=reads_w, writes=reads_w, scale=scale, bias=EPS)
        self.P.op("dve", lambda e: e.reciprocal(out=out, in_=out), reads=reads_w, writes=reads_w)

    def phase_1a(self, l, hsrc):
        P, I, S, C, nc = self.P, self.I, self.S, self.C, self.nc
        bank = self.bank
        NT, T = self.NT, self.T
        call = self.call
        with contextlib.ExitStack() as st:
            sb = lambda name, shape, dt=F32: self.sb(st, "a_" + name, shape, dt)
            dsw = P.dsem()
            dsl = P.dsem()
            w_in = sb("w_in", [128, 8, IN_COLS], BF16)
            wv = I["w_in"][l].rearrange("(k p) c -> p k c", p=128)
            for k in range(8):
                P.dma("pool", w_in[:, k, :], wv[:, k, :], dsw, writes=[w_in], newgroup=False)
            wuq = sb("wuq", [128, 2, 384], BF16)
            P.dma("pool", wuq[:, 0, :], I["mla_w_uq"][l, 0:128, :], dsw, writes=[wuq], newgroup=False)
            P.dma("pool", wuq[0:64, 1, :], I["mla_w_uq"][l, 128:192, :], dsw, writes=[wuq], newgroup=False)
            wukv = sb("wukv", [128, 512], BF16)
            P.dma("pool", wukv[:], I["mla_w_ukv"][l, :, :], dsw, writes=[wukv], newgroup=False)
            g1T = sb("g1T", [128, 8])
            P.dma("sp", g1T[:], I["g1T"][l], dsl, writes=[g1T], newgroup=False)
            cw = sb("cw", [128, 2, 4]); cb = sb("cb", [128, 2]); ba = sb("ba", [128, 2]); bx = sb("bx", [128, 2])
            lam = sb("lam", [128, 2])
            for tdst, nm in [(cw, "conv_wT"), (cb, "conv_b"), (ba, "lru_ba"), (bx, "lru_bx"), (lam, "lru_lam")]:
                P.dma("sp", tdst[:], I[nm][l], dsl, writes=[tdst], newgroup=False)
            waf = sb("waf", [128, 2, 128]); wxf = sb("wxf", [128, 2, 128])
            wab = sb("wab", [128, 2, 128], BF16); wxb = sb("wxb", [128, 2, 128], BF16)
            P.op("pool", lambda e: e.memset(waf[:], 0.0), writes=[waf])
            P.op("pool", lambda e: e.memset(wxf[:], 0.0), writes=[wxf])
            for nb in range(4):
                r0 = (nb % 2) * 64
                P.dma("sp", waf[r0:r0 + 64, nb // 2, r0:r0 + 64], I["lru_wa"][l, nb], dsl, writes=[waf], newgroup=False)
                P.dma("sp", wxf[r0:r0 + 64, nb // 2, r0:r0 + 64], I["lru_wx"][l, nb], dsl, writes=[wxf], newgroup=False)
            defer = []
            nba = sb("nba", [128, 2]); nbx = sb("nbx", [128, 2])
            defer.append(lambda: self.ts("dve", nba[:], ba[:], -1.0, None, ALU.mult, None, [ba], [nba]))
            defer.append(lambda: self.ts("dve", nbx[:], bx[:], -1.0, None, ALU.mult, None, [bx], [nbx]))
            defer.append(lambda: self.cp("pool", wab[:], waf[:], [waf], [wab]))
            defer.append(lambda: self.cp("pool", wxb[:], wxf[:], [wxf], [wxb]))
            cA = sb("cA", [128, 2]); cA2 = sb("cA2", [128, 2])
            defer.append(lambda: self.act(cA[:], lam[:], AF.Exp, [lam], [cA], scale=-1.0))
            defer.append(lambda: self.act(cA[:], cA[:], AF.Ln, [cA], [cA], bias=1.0))
            defer.append(lambda: self.ts("dve", cA2[:], cA[:], -16.0, None, ALU.mult, None, [cA], [cA2]))
            defer.append(lambda: self.ts("dve", cA[:], cA[:], -8.0, None, ALU.mult, None, [cA], [cA]))
            bcs = []

            def bc(name, src, w):
                t = sb(name, [128, w])
                P.dma("sp", t[:], src.partition_broadcast(128), dsl, writes=[t], newgroup=False)
                bcs.append(t)
                return t
            gq_bc = bc("gq_bc", I["mla_gq"][l], 192)
            gkv_bc = bc("gkv_bc", I["mla_gkv"][l], 128)
            gqn_bc = bc("gqn_bc", I["mla_gqn"][l], 96)
            gkn_bc = bc("gkn_bc", I["mla_gkn"][l], 96)
            fgq_bc = bc("fgq_bc", I["fox_gqn"][l], 64)
            fgk_bc = bc("fgk_bc", I["fox_gkn"][l], 64)
            fbf_bc = bc("fbf_bc", I["fox_bf"][l], 4)
            lb_bc = sb("lb_bc", [128, 256]); oml_bc = sb("oml_bc", [128, 256])
            if l == 0:
                P.op("pool", lambda e: e.memset(lb_bc[:], 0.0), writes=[lb_bc])
                P.op("pool", lambda e: e.memset(oml_bc[:], 1.0), writes=[oml_bc])
            else:
                lg0 = bc("lg0", I["hg_lb"][0], 256)
                lg1 = bc("lg1", I["hg_lb"][1], 256)
                defer.append(lambda: self.tt("dve", lb_bc[:], lg1[:], lg0[:], ALU.subtract, [lg0, lg1], [lb_bc]))
                defer.append(lambda: self.act(lb_bc[:], lb_bc[:], AF.Sigmoid, [lb_bc], [lb_bc]))
                defer.append(lambda: self.ts("dve", oml_bc[:], lb_bc[:], -1.0, 1.0, ALU.mult, ALU.add, [lb_bc], [oml_bc]))
            P.commit(dsw, [w_in, wuq, wukv])
            P.commit(dsl, [g1T, cw, cb, ba, bx, lam, waf, wxf] + bcs)
            for fn in defer:
                fn()
            xe = sb("xe", [128, 2, 3 + 128])
            P.op("pool", lambda e: e.memset(xe[:], 0.0), writes=[xe])
            hprev = sb("hprev", [128, 2])
            P.op("pool", lambda e: e.memset(hprev[:], 0.0), writes=[hprev])
            Sst = [sb(f"Sst{h}", [64, 64]) for h in range(4)]
            for h in range(4):
                P.op("pool", lambda e, h=h: e.memset(Sst[h][:], 0.0), writes=[Sst[h]])
            Sbf = [[sb(f"Sbf{h}_{r}", [64, 64], BF16) for r in range(2)] for h in range(4)]
            hqm = [sb(f"hqm{c}", [64, 4, 128], BF16) for c in range(4)]
            hkem = [sb(f"hkem{c}", [128, 256], BF16) for c in range(4)]
            for c in range(4):
                P.op("pool", lambda e, c=c: e.memset(hqm[c][:], 0.0), writes=[hqm[c]])
            R2 = 2
            ht = self.ring(st, "a_ht", [128, D], F32, R2)
            junk = sb("junk", [128, D], BF16)
            ss = self.ring(st, "a_ss", [128, 8], F32, R2)
            ub = self.ring(st, "a_ub", [128, D], BF16, R2)
            uT = self.ring(st, "a_uT", [128, 8, 128], BF16, R2)
            zt = self.ring(st, "a_zt", [128, 2148], F32, R2)
            ysb = self.ring(st, "a_ysb", [128, 512], F32, R2)
            cs = self.ring(st, "a_cs", [128, 32], F32, R2)
            dsh = [P.dsem() for _ in range(R2)]
            dsy = [P.dsem() for _ in range(R2)]
            dsq = [P.dsem() for _ in range(R2)]
            lu = sb("lu", [128, 128]); lub = sb("lub", [128, 128], BF16)
            lr = sb("lr", [128, 128]); li = sb("li", [128, 128]); la = sb("la", [128, 128]); lm = sb("lm", [128, 128])
            lbb = sb("lbb", [128, 128]); lhs = sb("lhs", [128, 128]); lg = sb("lg", [128, 2, 128]); lya = sb("lya", [128, 2, 128])
            hsig = sb("hsig", [128, 256]); hlogf = sb("hlogf", [128, 256]); hkk = sb("hkk", [128, 256])
            heb = sb("heb", [128, 256]); henb = sb("henb", [128, 256]); hebe = sb("hebe", [128, 256])
            hqd = sb("hqd", [128, 256], BF16); hkd = sb("hkd", [128, 256], BF16); hke = sb("hke", [128, 256], BF16)
            hkdf = sb("hkdf", [128, 256]); hvb = sb("hvb", [128, 256], BF16); hsg = sb("hsg", [128, 256])
            hqdT = sb("hqdT", [64, 4, 128], BF16); hkdT = sb("hkdT", [64, 4, 128], BF16)
            hdec = sb("hdec", [64, 4, 4]); hattm = sb("hattm", [128, 4, 128], BF16)
            mjunk = sb("mjunk", [128, 512]); mss = sb("mss", [128, 16])
            cqn = sb("cqn", [128, 192], BF16); ckvn = sb("ckvn", [128, 128], BF16)
            cqT = sb("cqT", [128, 2, 128], BF16); ckvT = sb("ckvT", [128, 128], BF16)
            qf = sb("qf", [128, 4, 96]); kf = sb("kf", [128, 4, 96])
            qb = sb("qb", [128, 4, 96], BF16); kb = sb("kb", [128, 4, 96], BF16)
            rt = sb("rt", [128, 4, 4, 16])
            vaug = self.ring(st, "a_vaug", [128, 4, 65], BF16, R2)
            qkT = self.ring(st, "a_qkT", [96, 8, 128], BF16, R2)
            fqf = sb("fqf", [128, 4, 64]); fqb = sb("fqb", [128, 256], BF16); fkb = sb("fkb", [128, 256], BF16)
            fvaug = self.ring(st, "a_fvaug", [128, 4, 65], BF16, R2)
            fqkT = self.ring(st, "a_fqkT", [128, 4, 128], BF16, R2)
            flog = sb("flog", [128, 4])
            for r in range(R2):
                P.op("pool", lambda e, r=r: e.memset(vaug[r][:], 1.0), writes=[vaug[r]])
                P.op("pool", lambda e, r=r: e.memset(fvaug[r][:], 1.0), writes=[fvaug[r]])
            identb = C["identb"]; identf = C["c_ident"]

            for j, (p0, n) in enumerate(self.tiles):
                r = j % R2
                H, SS, UB, UT, ZT, YS, CS = ht[r], ss[r], ub[r], uT[r], zt[r], ysb[r], cs[r]
                def load(jn):
                    pp, nn = self.tiles[jn]
                    rr = jn % R2
                    P.dma("sp", ht[rr][0:nn, :], hsrc[pp:pp + nn, :], dsh[rr], writes=[ht[rr]])
                    P.dma("sp", cs[rr][0:nn, 0:16], I["c_cos"][pp:pp + nn, :], dsh[rr], writes=[cs[rr]], newgroup=False)
                    P.dma("sp", cs[rr][0:nn, 16:32], I["c_sin"][pp:pp + nn, :], dsh[rr], writes=[cs[rr]], newgroup=False)
                    P.commit(dsh[rr], [ht[rr], cs[rr]])
                if j == 0:
                    load(0)
                if j + 1 < len(self.tiles):
                    load(j + 1)
                P.op("dve", lambda e, SS=SS: e.memset(SS[:], 0.0), writes=[SS])
                self.act(junk[0:n, :], H[0:n, :], AF.Square, [H, SS], [junk, SS], accum_out=SS[0:n, 0:1])
                self.rsq(SS[0:n, 1:2], SS[0:n, 0:1], 1.0 / D, [SS])
                self.act(UB[0:n, :], H[0:n, :], AF.Copy, [H, SS], [UB], scale=SS[0:n, 1:2])
                b0 = bank[0]
                b0v = b0.t[:].bitcast(BF16)
                for k in range(8):
                    self.tr(b0v[:, k * 128:k * 128 + n], UB[0:n, k * 128:(k + 1) * 128], identb[0:n, 0:n], [UB, identb], [b0])
                self.tt("dve", UT[:, :, 0:n], b0v[:, :].rearrange("p (k t) -> p k t", k=8)[:, :, 0:n],
                        g1T[:, :].unsqueeze(2).broadcast_to([128, 8, n]), ALU.mult, [g1T], [UT, b0])
                b1 = bank[1]
                for m in range(4):
                    for k in range(8):
                        self.mm(b1[:, m * 128:m * 128 + n], w_in[:, k, m * 128:(m + 1) * 128], UT[:, k, 0:n], k == 0, k == 7, [w_in, UT], [b1])
                chunks = [(512, 1024), (1024, 1536), (1536, 1888), (1888, 2400), (2400, 2660)]
                for ci, (c0, c1) in enumerate(chunks):
                    bz = bank[2 + (ci % 2)]
                    wd = c1 - c0
                    for k in range(8):
                        self.mm(bz[0:n, 0:wd], UT[:, k, 0:n], w_in[:, k, c0:c1], k == 0, k == 7, [UT, w_in], [bz])
                    self.cp("act" if ci % 2 == 0 else "dve", ZT[0:n, c0 - 512:c1 - 512], bz[0:n, 0:wd], [], [ZT, bz])
                steps = os.environ.get("K_STEPS", "lru,hgrn,mla,fox").split(",")
                for cc in (range(2) if "lru" in steps else []):
                    self.act(lg[:, cc, 0:n], b1[:, (2 + cc) * 128:(2 + cc) * 128 + n], AF.Gelu_apprx_tanh, [], [lg, b1])
                for cc in (range(2) if "lru" in steps else []):
                    self.cp("act", xe[:, cc, 3:3 + n], b1[:, cc * 128:cc * 128 + n], [], [xe, b1])
                    self.ts("dve", lu[:, 0:n], xe[:, cc, 0:n], cw[:, cc, 0:1], cb[:, cc:cc + 1], ALU.mult, ALU.add, [xe, cw, cb], [lu])
                    for jj in range(1, 4):
                        self.stt("dve", lu[:, 0:n], xe[:, cc, jj:jj + n], cw[:, cc, jj:jj + 1], lu[:, 0:n], ALU.mult, ALU.add, [xe, cw, lu], [lu])
                    self.cp("pool", xe[:, cc, 0:3], xe[:, cc, n:n + 3], [xe], [xe])
                    self.cp("pool", lub[:, 0:n], lu[:, 0:n], [lu], [lub])
                    b4 = bank[4]
                    self.mm(b4[:, 0:n], wab[:, cc, :], lub[:, 0:n], True, True, [wab, lub], [b4])
                    self.mm(b4[:, 128:128 + n], wxb[:, cc, :], lub[:, 0:n], True, True, [wxb, lub], [b4])
                    self.sigm(lr[:, 0:n], b4[:, 0:n], [nba], [lr, b4], bias=nba[:, cc:cc + 1])
                    self.sigm(li[:, 0:n], b4[:, 128:128 + n], [nbx], [li, b4], bias=nbx[:, cc:cc + 1])
                    self.act(la[:, 0:n], lr[:, 0:n], AF.Exp, [lr, cA], [la], scale=cA[:, cc:cc + 1])
                    self.act(lm[:, 0:n], lr[:, 0:n], AF.Exp, [lr, cA2], [lm], scale=cA2[:, cc:cc + 1])
                    self.act(lm[:, 0:n], lm[:, 0:n], AF.Ln, [lm], [lm], scale=-1.0, bias=1.0)
                    self.act(lm[:, 0:n], lm[:, 0:n], AF.Exp, [lm], [lm], scale=0.5)
                    self.tt("dve", lbb[:, 0:n], lm[:, 0:n], li[:, 0:n], ALU.mult, [lm, li], [lbb])
                    self.tt("dve", lbb[:, 0:n], lbb[:, 0:n], lu[:, 0:n], ALU.mult, [lbb, lu], [lbb])
                    P.op("dve", lambda e, n=n, cc=cc: e.tensor_tensor_scan(out=lhs[:, 0:n], data0=la[:, 0:n], data1=lbb[:, 0:n],
                                                                        initial=hprev[:, cc:cc + 1], op0=ALU.mult, op1=ALU.add),
                         reads=[la, lbb, hprev], writes=[lhs])
                    self.cp("dve", hprev[:, cc:cc + 1], lhs[:, n - 1:n], [lhs], [hprev])
                    self.tt("dve", lya[:, cc, 0:n], lhs[:, 0:n], lg[:, cc, 0:n], ALU.mult, [lhs, lg], [lya])
                b5 = bank[5]
                for cc in (range(2) if "lru" in steps else []):
                    self.tr(b5[0:n, cc * 128:(cc + 1) * 128], lya[:, cc, 0:n], identf[:, :], [lya, identf], [b5])
                if "lru" in steps:
                    self.cp("act", YS[0:n, 0:256], b5[0:n, 0:256], [], [YS, b5])
                if "hgrn" in steps:
                    nch = (n + 31) // 32
                    hq_ = ZT[0:n, 0:256]; hf_ = ZT[0:n, 256:512]; hv_ = ZT[0:n, 512:768]; hg_ = ZT[0:n, 768:1024]
                    self.sigm(hsig[0:n, :], hf_, [ZT], [hsig])
                    self.tt("dve", hsig[0:n, :], hsig[0:n, :], oml_bc[0:n, :], ALU.mult, [hsig, oml_bc], [hsig])
                    self.tt("dve", hsig[0:n, :], hsig[0:n, :], lb_bc[0:n, :], ALU.add, [hsig, lb_bc], [hsig])
                    self.act(hlogf[0:n, :], hsig[0:n, :], AF.Ln, [hsig], [hlogf])
                    self.ts("pool", hkk[0:n, :], hsig[0:n, :], -1.0, 1.0, ALU.mult, ALU.add, [hsig], [hkk])
                    b4 = bank[4]
                    self.mm(b4[0:n, 0:256], C["c_triblk"][0:n, 0:n], hlogf[0:n, :], True, True, [C["c_triblk"], hlogf], [b4])
                    b6 = bank[6]
                    self.mm(b6[0:n, 0:256], C["c_blkones"][0:n, 0:n], hlogf[0:n, :], True, True, [C["c_blkones"], hlogf], [b6])
                    self.act(heb[0:n, :], b4[0:n, 0:256], AF.Exp, [], [heb, b4])
                    self.act(henb[0:n, :], b4[0:n, 0:256], AF.Exp, [], [henb, b4], scale=-1.0)
                    self.act(hebe[0:n, :], b6[0:n, 0:256], AF.Exp, [], [hebe, b6])
                    self.tt("dve", hqd[0:n, :], hq_, heb[0:n, :], ALU.mult, [ZT, heb], [hqd])
                    self.tt("dve", hkdf[0:n, :], hkk[0:n, :], henb[0:n, :], ALU.mult, [hkk, henb], [hkdf])
                    self.cp("pool", hkd[0:n, :], hkdf[0:n, :], [hkdf], [hkd])
                    self.tt("dve", hke[0:n, :], hkdf[0:n, :], hebe[0:n, :], ALU.mult, [hkdf, hebe], [hke])
                    self.cp("pool", hvb[0:n, :], hv_, [ZT], [hvb])
                    self.sigm(hsg[0:n, :], hg_, [ZT], [hsg])
                    self.tt("pool", hsg[0:n, :], hsg[0:n, :], hg_, ALU.mult, [hsg, ZT], [hsg])
                    b7 = bank[7]
                    for h in range(4):
                        self.mm(b7[0:64, h * 4:h * 4 + nch], hlogf[0:n, h * 64:(h + 1) * 64], C["c_chunkind"][0:n, 0:nch], True, True,
                                [hlogf, C["c_chunkind"]], [b7])
                    self.act(hdec[:, :, 0:nch], b7[0:64, 0:16].rearrange("p (a c) -> p a c", a=4)[:, :, 0:nch], AF.Exp, [], [hdec, b7])
                    b5v = b5.t[:].bitcast(BF16)
                    for h in range(4):
                        self.tr(b5v[0:64, h * 128:h * 128 + n], hqd[0:n, h * 64:(h + 1) * 64], identb[0:n, 0:n], [hqd, identb], [b5])
                        self.tr(b5v[0:64, 512 + h * 128:512 + h * 128 + n], hkd[0:n, h * 64:(h + 1) * 64], identb[0:n, 0:n], [hkd, identb], [b5])
                    b5q = b5v[0:64, 0:512].rearrange("p (a t) -> p a t", a=4)
                    self.cp("act", hqdT[:, :, 0:n], b5q[:, :, 0:n], [], [hqdT, b5])
                    self.cp("act", hkdT[:, :, 0:n], b5v[0:64, 512:1024].rearrange("p (a t) -> p a t", a=4)[:, :, 0:n], [], [hkdT, b5])
                    for c in range(nch):
                        cn = min(32, n - 32 * c)
                        self.cp("pool", hqm[c][:, :, 32 * c:32 * c + cn], hqdT[:, :, 32 * c:32 * c + cn], [hqdT], [hqm[c]])
                        if c % 2 == 0:
                            self.ts("dve", hkem[c][0:n, :], hke[0:n, :], C["c_chunkind"][0:n, c:c + 1], None, ALU.mult, None,
                                    [hke, C["c_chunkind"]], [hkem[c]])
                        else:
                            self.act(hkem[c][0:n, :], hke[0:n, :], AF.Copy, [hke, C["c_chunkind"]], [hkem[c]], scale=C["c_chunkind"][0:n, c:c + 1])
                    for h in range(4):
                        self.mm(b6[0:n, h * 128:h * 128 + n], hkdT[:, h, 0:n], hqdT[:, h, 0:n], True, True, [hkdT, hqdT], [b6])
                    self.tt("dve", hattm[0:n, :, 0:n], b6[0:n, :].rearrange("p (h t) -> p h t", h=4)[:, :, 0:n],
                            C["hgmask"][0:n, 0:n].unsqueeze(1).broadcast_to([n, 4, n]), ALU.mult, [C["hgmask"]], [hattm, b6])
                    for h in range(4):
                        bu = bank[2 + h // 2]
                        for c in range(nch):
                            col = ((h % 2) * 4 + c) * 64
                            self.mm(bu[0:64, col:col + 64], hkem[c][0:n, h * 64:(h + 1) * 64], hvb[0:n, h * 64:(h + 1) * 64], True, True,
                                    [hkem[c], hvb], [bu])
                    for h in range(4):
                        self.mm(b4[0:n, h * 64:(h + 1) * 64], hattm[0:n, h, 0:n], hvb[0:n, h * 64:(h + 1) * 64], h == 0, False, [hattm, hvb], [b4],
                                skip_group_check=True)
                    for c in range(nch):
                        for h in range(4):
                            bu = bank[2 + h // 2]
                            gidx = (j - 1) * 4 + c + 1 if j > 0 else 0
                            slot_prev = Sbf[h][(gidx - 1) % 2]
                            if gidx > 0:
                                self.mm(b4[0:n, h * 64:(h + 1) * 64], hqm[c][:, h, 0:n], slot_prev[:, :], False, True, [hqm[c], slot_prev], [b4],
                                        skip_group_check=True)
                            col = ((h % 2) * 4 + c) * 64
                            self.stt("dve", Sst[h][:], Sst[h][:], hdec[:, h, c:c + 1], bu[0:64, col:col + 64], ALU.mult, ALU.add,
                                     [Sst[h], hdec], [Sst[h], bu])
                            slot = Sbf[h][gidx % 2]
                            self.cp("pool", slot[:], Sst[h][:], [Sst[h]], [slot])
                    self.tt("dve", YS[0:n, 256:512], b4[0:n, 0:256], hsg[0:n, :], ALU.mult, [hsg], [YS, b4])
                    P.dma("sp", S["Y"][p0:p0 + n, 0:512], YS[0:n, :], dsy[r], reads=[YS], writes=[Buf()])
                if "mla" in steps:
                    cq_ = ZT[0:n, 1024:1216]; ckv_ = ZT[0:n, 1216:1344]; kr_ = ZT[0:n, 1344:1376]
                    P.op("dve", lambda e: e.memset(mss[:], 0.0), writes=[mss])
                    self.act(mjunk[0:n, 0:192], cq_, AF.Square, [ZT, mss], [mjunk, mss], accum_out=mss[0:n, 0:1])
                    self.act(mjunk[0:n, 0:128], ckv_, AF.Square, [ZT, mss], [mjunk, mss], accum_out=mss[0:n, 1:2])
                    self.rsq(mss[0:n, 2:3], mss[0:n, 0:1], 1.0 / 192, [mss])
                    self.rsq(mss[0:n, 3:4], mss[0:n, 1:2], 1.0 / 128, [mss])
                    self.stt("dve", cqn[0:n, :], cq_, mss[0:n, 2:3], gq_bc[0:n, :], ALU.mult, ALU.mult, [ZT, mss, gq_bc], [cqn])
                    self.stt("dve", ckvn[0:n, :], ckv_, mss[0:n, 3:4], gkv_bc[0:n, :], ALU.mult, ALU.mult, [ZT, mss, gkv_bc], [ckvn])
                    self.tr(b5v[:, 0:n], cqn[0:n, 0:128], identb[0:n, 0:n], [cqn, identb], [b5])
                    self.tr(b5v[0:64, 128:128 + n], cqn[0:n, 128:192], identb[0:n, 0:n], [cqn, identb], [b5])
                    self.tr(b5v[:, 256:256 + n], ckvn[0:n, 0:128], identb[0:n, 0:n], [ckvn, identb], [b5])
                    self.cp("act", cqT[:, 0, 0:n], b5v[:, 0:n], [], [cqT, b5])
                    self.cp("act", cqT[0:64, 1, 0:n], b5v[0:64, 128:128 + n], [], [cqT, b5])
                    self.cp("act", ckvT[:, 0:n], b5v[:, 256:256 + n], [], [ckvT, b5])
                    self.mm(b6[0:n, 0:384], cqT[:, 0, 0:n], wuq[:, 0, :], True, False, [cqT, wuq], [b6])
                    self.mm(b6[0:n, 0:384], cqT[0:64, 1, 0:n], wuq[0:64, 1, :], False, True, [cqT, wuq], [b6])
                    self.mm(b7[0:n, 0:512], ckvT[:, 0:n], wukv[:, :], True, True, [ckvT, wukv], [b7])
                    self.cp("act", qf[0:n, :, :], b6[0:n, 0:384].rearrange("p (h c) -> p h c", h=4), [], [qf, b6])
                    b7h = b7[0:n, 0:512].rearrange("p (h c) -> p h c", h=4)
                    self.cp("dve", kf[0:n, :, 0:64], b7h[:, :, 0:64], [], [kf, b7])
                    VA = vaug[r]
                    self.cp("act", VA[0:n, :, 0:64], b7h[:, :, 64:128], [], [VA, b7])
                    self.cp("pool", kf[0:n, :, 64:96], kr_.unsqueeze(1).broadcast_to([n, 4, 32]), [ZT], [kf])
                    for (src, gbc, dst, col) in [(qf, gqn_bc, qb, 4), (kf, gkn_bc, kb, 8)]:
                        self.act(mjunk[0:n, 0:384], src[0:n, :, :].rearrange("p h c -> p (h c)"), AF.Square, [src], [mjunk])
                        P.op("dve", lambda e, n=n, col=col: e.tensor_reduce(out=mss[0:n, col:col + 4], in_=mjunk[0:n, 0:384].rearrange("p (h c) -> p h c", h=4),
                                                                        axis=AX.X, op=ALU.add), reads=[mjunk], writes=[mss])
                        self.rsq(mss[0:n, col:col + 4], mss[0:n, col:col + 4], 1.0 / 96, [mss])
                        self.tt("dve", src[0:n, :, :], src[0:n, :, :], mss[0:n, col:col + 4].unsqueeze(2).broadcast_to([n, 4, 96]), ALU.mult, [src, mss], [src])
                        self.tt("pool", src[0:n, :, :], src[0:n, :, :], gbc[0:n, :].unsqueeze(1).broadcast_to([n, 4, 96]), ALU.mult, [src, gbc], [src])
                        cosb = CS[0:n, 0:16].unsqueeze(1).broadcast_to([n, 4, 16])
                        sinb = CS[0:n, 16:32].unsqueeze(1).broadcast_to([n, 4, 16])
                        t1 = src[0:n, :, 64:80]; t2 = src[0:n, :, 80:96]
                        self.tt("dve", rt[0:n, :, 0, :], t1, cosb, ALU.mult, [src, CS], [rt])
                        self.tt("pool", rt[0:n, :, 1, :], t2, sinb, ALU.mult, [src, CS], [rt])
                        self.tt("dve", rt[0:n, :, 2, :], t1, sinb, ALU.mult, [src, CS], [rt])
                        self.tt("pool", rt[0:n, :, 3, :], t2, cosb, ALU.mult, [src, CS], [rt])
                        self.cp("act", dst[0:n, :, 0:64], src[0:n, :, 0:64], [src], [dst])
                        self.tt("dve", dst[0:n, :, 64:80], rt[0:n, :, 0, :], rt[0:n, :, 1, :], ALU.subtract, [rt], [dst])
                        self.tt("dve", dst[0:n, :, 80:96], rt[0:n, :, 2, :], rt[0:n, :, 3, :], ALU.add, [rt], [dst])
                    QK = qkT[r]
                    for h in range(4):
                        self.tr(b5v[0:96, h * 128:h * 128 + n], qb[0:n, h, :], identb[0:n, 0:n], [qb, identb], [b5])
                        self.tr(b5v[0:96, (4 + h) * 128:(4 + h) * 128 + n], kb[0:n, h, :], identb[0:n, 0:n], [kb, identb], [b5])
                    self.cp("act", QK[:, :, 0:n], b5v[0:96, :].rearrange("p (a t) -> p a t", a=8)[:, :, 0:n], [], [QK, b5])
                    P.dma("sp", S["QTc"][:, :, p0:p0 + n].rearrange("h d t -> d h t"), QK[:, 0:4, 0:n], dsq[r], reads=[QK], writes=[Buf()])
                    P.dma("sp", S["KTc"][:, :, p0:p0 + n].rearrange("h d t -> d h t"), QK[:, 4:8, 0:n], dsq[r], reads=[QK], writes=[Buf()], newgroup=False)
                    P.dma("sp", S["Vc"][:, 0:n, j, :].rearrange("h p c -> p h c"), VA[0:n, :, :], dsq[r], reads=[VA], writes=[Buf()], newgroup=False)
                if "fox" in steps:
                    fq_ = ZT[0:n, 1376:1632]; fk_ = ZT[0:n, 1632:1888]; fv_ = ZT[0:n, 1888:2144]; ff_ = ZT[0:n, 2144:2148]
                    for (src, gbc, dst, col) in [(fq_, fgq_bc, fqb, 12), (fk_, fgk_bc, fkb, 12)]:
                        self.act(mjunk[0:n, 0:256], src, AF.Square, [ZT], [mjunk])
                        P.op("dve", lambda e, n=n, col=col: e.tensor_reduce(out=mss[0:n, col:col + 4], in_=mjunk[0:n, 0:256].rearrange("p (h c) -> p h c", h=4),
                                                                        axis=AX.X, op=ALU.add), reads=[mjunk], writes=[mss])
                        self.rsq(mss[0:n, col:col + 4], mss[0:n, col:col + 4], 1.0 / 64, [mss])
                        self.tt("dve", fqf[0:n, :, :], src.rearrange("p (h c) -> p h c", h=4), mss[0:n, col:col + 4].unsqueeze(2).broadcast_to([n, 4, 64]), ALU.mult, [ZT, mss], [fqf])
                        self.tt("pool", dst[0:n, :].rearrange("p (h c) -> p h c", h=4), fqf[0:n, :, :], gbc[0:n, :].unsqueeze(1).broadcast_to([n, 4, 64]), ALU.mult, [fqf, gbc], [dst])
                    FV = fvaug[r]
                    self.cp("act", FV[0:n, :, 0:64], fv_.rearrange("p (h c) -> p h c", h=4), [ZT], [FV])
                    FQK = fqkT[r]
                    for hp in range(2):
                        self.tr(b5v[:, hp * 128:hp * 128 + n], fqb[0:n, hp * 128:(hp + 1) * 128], identb[0:n, 0:n], [fqb, identb], [b5])
                        self.tr(b5v[:, (2 + hp) * 128:(2 + hp) * 128 + n], fkb[0:n, hp * 128:(hp + 1) * 128], identb[0:n, 0:n], [fkb, identb], [b5])
                    self.cp("act", FQK[:, :, 0:n], b5v[:, 0:512].rearrange("p (a t) -> p a t", a=4)[:, :, 0:n], [], [FQK, b5])
                    P.dma("sp", S["QTd"][:, :, p0:p0 + n].rearrange("a d t -> d a t"), FQK[:, 0:2, 0:n], dsq[r], reads=[FQK], writes=[Buf()], newgroup=False)
                    P.dma("sp", S["KTd"][:, :, p0:p0 + n].rearrange("a d t -> d a t"), FQK[:, 2:4, 0:n], dsq[r], reads=[FQK], writes=[Buf()], newgroup=False)
                    P.dma("sp", S["Vd"][:, 0:n, j, :].rearrange("h p c -> p h c"), FV[0:n, :, :], dsq[r], reads=[FV], writes=[Buf()], newgroup=False)
                    P.commit(dsq[r], [qkT[r], vaug[r], FQK, FV])
                    self.tt("dve", flog[0:n, :], ff_, fbf_bc[0:n, :], ALU.add, [ZT, fbf_bc], [flog])
                    self.act(flog[0:n, :], flog[0:n, :], AF.Exp, [flog], [flog], scale=-1.0)
                    self.act(flog[0:n, :], flog[0:n, :], AF.Ln, [flog], [flog], bias=1.0)
                    self.ts("dve", flog[0:n, :], flog[0:n, :], -1.0, None, ALU.mult, None, [flog], [flog])
                    if j == 0:
                        self.mm(b6[0:n, 0:4], C["c_trifull"][0:n, 0:n], flog[0:n, :], True, True, [C["c_trifull"], flog], [b6])
                    else:
                        pn = self.tiles[j - 1][1]
                        selp = C["c_sel15"] if pn == 16 else C["c_sel127"]
                        self.mm(b6[0:n, 0:4], C["c_trifull"][0:n, 0:n], flog[0:n, :], True, False, [C["c_trifull"], flog], [b6])
                        self.mm(b6[0:n, 0:4], selp[0:pn, 0:n], call[0:pn, j - 1, :], False, True, [selp, call], [b6])
                    self.cp("dve", call[0:n, j, :], b6[0:n, 0:4], [], [call, b6])
            evs = [(d.key, d.cnt) for d in dsy + dsq if d.cnt > 0]
            self.barrier(evs)

    def barrier(self, evs=()):
        P = self.P
        evs = list(evs)
        for e in ("pe", "act", "dve", "pool"):
            if P.cnt[e] > 0:
                evs.append((("E", e), P.cnt[e]))
        for d in P.all_dsems:
            if d.cnt > 0:
                evs.append((d.key, d.cnt))
        for e in ("sp", "pool", "act", "pe", "dve"):
            P.wait_all(e, evs)

    def phase_1b(self, l):
        P, I, S, C, nc = self.P, self.I, self.S, self.C, self.nc
        bank = self.bank
        NT, T = self.NT, self.T
        call = self.call
        tiles = self.tiles
        WMAX = 4

        def make_blocks(W):
            blocks = [[0]]
            i = 1
            while i < len(tiles):
                blocks.append(list(range(i, min(i + W, len(tiles)))))
                i += W
            return blocks
        blocks_of = {"mla": make_blocks(4), "fox": make_blocks(2)}
        with contextlib.ExitStack() as st:
            sb = lambda name, shape, dt=F32: self.sb(st, "b_" + name, shape, dt)
            fblocks = blocks_of["fox"]
            b7 = bank[7]
            for bi, blk in enumerate(fblocks):
                i = blk[0]
                n = tiles[i][1]
                if len(blk) > 1:
                    sel = C["c_sel127"]
                else:
                    sel = C["c_sel8"] if n == 16 else C["c_sel64"]
                self.mm(b7[:, bi * 4:(bi + 1) * 4], sel[0:n, :], call[0:n, i, :], True, True, [sel, call], [b7])
            cref = sb("cref", [128, len(fblocks), 4])
            self.cp("dve", cref[:].rearrange("p a b -> p (a b)"), b7[:, 0:len(fblocks) * 4], [], [cref, b7])
            negc = sb("negc", [128, 4, NT + 1])
            self.ts("dve", negc[:], call[:].rearrange("p j h -> p h j"), -1.0, None, ALU.mult, None, [call], [negc])
            bias = self.ring(st, "b_bias", [128, NT + 1], F32, 4)
            KT = self.ring(st, "b_KT", [128, T], BF16, 2)
            QT = self.ring(st, "b_QT", [128, T], BF16, 2)
            V = self.ring(st, "b_V", [128, NT + 1, 65], BF16, 2)
            dsk = [P.dsem() for _ in range(2)]
            dsv = [P.dsem() for _ in range(2)]
            LA = 3
            NSB = 4
            pt = self.ring(st, "b_pt", [128, WMAX * 128], BF16, NSB)
            oT = self.ring(st, "b_oT", [65, WMAX * 128], F32, 2)
            osb = self.ring(st, "b_osb", [128, WMAX, 64], F32, 2)
            rden = self.ring(st, "b_rden", [128, WMAX], F32, 2)
            dso = [P.dsem() for _ in range(2)]
            identf = C["c_ident"]
            units = []
            for h in range(4):
                units.append(("mla", h))
            for h in range(4):
                units.append(("fox", h))
            loaded = {}
            nload = [0]

            def load(u):
                kind, h = u
                if kind == "mla":
                    key = ("mla", h)
                else:
                    key = ("fox", h // 2)
                if key in loaded:
                    return loaded[key]
                r = nload[0] % 2
                nload[0] += 1
                if kind == "mla":
                    P.dma("sp", KT[r][0:96, :], S["KTc"][h], dsk[r], writes=[KT[r]])
                    P.dma("sp", QT[r][0:96, :], S["QTc"][h], dsk[r], writes=[QT[r]], newgroup=False)
                else:
                    P.dma("sp", KT[r][:, :], S["KTd"][h // 2], dsk[r], writes=[KT[r]])
                    P.dma("sp", QT[r][:, :], S["QTd"][h // 2], dsk[r], writes=[QT[r]], newgroup=False)
                P.commit(dsk[r], [KT[r], QT[r]])
                loaded[key] = r
                return r
            nv = [0]

            def loadv(u):
                kind, h = u
                r = nv[0] % 2
                nv[0] += 1
                src = S["Vc"] if kind == "mla" else S["Vd"]
                P.dma("sp", V[r][:], src[h], dsv[r], writes=[V[r]])
                return r
            ring_of = {}
            steps = []
            ustart = {}
            uend = {}
            gblk = {}
            ng = 0
            for ui, (kind, h) in enumerate(units):
                ustart[ui] = len(steps)
                for bi, blk in enumerate(blocks_of[kind]):
                    gblk[(ui, bi)] = ng
                    ng += 1
                    for jj in range(blk[-1] + 1):
                        steps.append((ui, bi, jj))
                uend[ui] = len(steps)

            def geom(s):
                ui, bi, jj = steps[s]
                kind, h = units[ui]
                blk = blocks_of[kind][bi]
                i0, i1 = blk[0], blk[-1]
                a = max(i0, jj)
                qb0 = tiles[i0][0]
                q0 = tiles[a][0]
                q1 = tiles[i1][0] + tiles[i1][1]
                return ui, bi, jj, kind, h, blk, i0, i1, a, qb0, q0, q1

            def emit_qk(s):
                ui, bi, jj, kind, h, blk, i0, i1, a, qb0, q0, q1 = geom(s)
                fox = kind == "fox"
                if ui not in ring_of:
                    ring_of[ui] = (load(units[ui]), loadv(units[ui]))
                r, rv = ring_of[ui]
                if s - ustart[ui] == min(LA + 1, uend[ui] - ustart[ui] - 1) and ui + 1 < len(units) and (ui + 1) not in ring_of:
                    ring_of[ui + 1] = (load(units[ui + 1]), loadv(units[ui + 1]))
                if fox and jj == 0:
                    B_ = bias[gblk[(ui, bi)] % 4]
                    self.ts("dve", B_[:, 0:i1 + 1], negc[:, h, 0:i1 + 1], cref[:, bi, h:h + 1], None, ALU.add, None, [negc, cref], [B_])
                rows = slice(0, 96) if not fox else slice((h % 2) * 64, (h % 2) * 64 + 64)
                k0, kn = tiles[jj]
                sbk = bank[s % NSB]
                self.mm(sbk[0:kn, 0:q1 - q0], KT[r][rows, k0:k0 + kn], QT[r][rows, q0:q1], True, True, [KT[r], QT[r]], [sbk])

            def emit_rest(s):
                ui, bi, jj, kind, h, blk, i0, i1, a, qb0, q0, q1 = geom(s)
                fox = kind == "fox"
                r, rv = ring_of[ui]
                k0, kn = tiles[jj]
                wq = q1 - q0
                scale = (96.0 if not fox else 64.0) ** -0.5
                mask = C["foxmask"] if fox else C["mlamask"]
                ycol = (512 if not fox else 768) + h * 64
                sbk = bank[s % NSB]
                PT = pt[s % NSB]
                g = gblk[(ui, bi)]
                ob = bank[4 + (g % 2)]
                if fox:
                    B_ = bias[g % 4]
                    self.act(PT[0:kn, 0:wq], sbk[0:kn, 0:wq], AF.Exp, [B_], [PT, sbk], scale=scale, bias=B_[0:kn, jj:jj + 1])
                else:
                    self.act(PT[0:kn, 0:wq], sbk[0:kn, 0:wq], AF.Exp, [], [PT, sbk], scale=scale)
                if jj >= i0:
                    na = tiles[jj][1]
                    self.tt("pool", PT[0:kn, 0:na], PT[0:kn, 0:na], mask[0:kn, 0:na], ALU.mult, [PT, mask], [PT])
                self.mm(ob[0:65, q0 - qb0:q1 - qb0], V[rv][0:kn, jj, :], PT[0:kn, 0:wq], jj == 0, jj == i1, [PT, V[rv]], [ob])
                if jj == i1:
                    ro = g % 2
                    wb = q1 - qb0
                    nsub = len(blk)
                    n = tiles[i0][1]
                    OT = oT[ro]
                    self.cp("dve", OT[0:65, 0:wb], ob[0:65, 0:wb], [], [OT, ob])
                    tb = bank[6 + (g % 2)]
                    for x in range(nsub):
                        self.tr(tb[0:n, x * 65:(x + 1) * 65], OT[0:65, x * 128:x * 128 + n], identf[0:65, 0:65], [OT, identf], [tb])
                    tbv = tb[0:n, 0:nsub * 65].rearrange("p (x c) -> p x c", c=65)
                    P.op("dve", lambda e, tbv=tbv, n=n, ro=ro, nsub=nsub: e.reciprocal(out=rden[ro][0:n, 0:nsub], in_=tbv[:, :, 64]),
                         reads=[], writes=[rden[ro], tb])
                    self.tt("dve", osb[ro][0:n, 0:nsub, :], tbv[:, :, 0:64], rden[ro][0:n, 0:nsub].unsqueeze(2).broadcast_to([n, nsub, 64]),
                            ALU.mult, [rden[ro]], [osb[ro], tb])
                    if nsub == 1:
                        P.dma("sp", S["Y"][qb0:qb0 + n, ycol:ycol + 64], osb[ro][0:n, 0, :], dso[ro], reads=[osb[ro]], writes=[Buf()])
                    else:
                        P.dma("sp", S["Y"][qb0:qb0 + nsub * 128, ycol:ycol + 64].rearrange("(x p) c -> p x c", p=128), osb[ro][:, 0:nsub, :],
                              dso[ro], reads=[osb[ro]], writes=[Buf()])
            for s in range(len(steps) + LA):
                if s < len(steps):
                    emit_qk(s)
                if s >= LA:
                    emit_rest(s - LA)
            self.barrier([(d.key, d.cnt) for d in dso if d.cnt > 0])

    def phase_1c(self, l, hsrc):
        P, I, S, C, nc = self.P, self.I, self.S, self.C, self.nc
        bank = self.bank
        NT, T = self.NT, self.T
        moe = (l % 2 == 1)
        with contextlib.ExitStack() as st:
            sb = lambda name, shape, dt=F32: self.sb(st, "c_" + name, shape, dt)
            dsw = P.dsem()
            wout = sb("wout", [128, 8, D], BF16)
            wv = I["w_out"][l].rearrange("(k p) c -> p k c", p=128)
            for k in range(8):
                P.dma("pool", wout[:, k, :], wv[:, k, :], dsw, writes=[wout], newgroup=False)
            og_bc = sb("og_bc", [128, D])
            P.dma("sp", og_bc[:], I["og"][l].partition_broadcast(128), dsw, writes=[og_bc], newgroup=False)
            g2T = sb("g2T", [128, 8])
            P.dma("sp", g2T[:], I["g2T"][l], dsw, writes=[g2T], newgroup=False)
            if moe:
                wr = sb("wr", [128, 8, NEXP])
                P.dma("sp", wr[:], I["moe_wr"].rearrange("(k p) e -> p k e", p=128), dsw, writes=[wr], newgroup=False)
                P.commit(dsw, [wr])
            P.commit(dsw, [wout, og_bc, g2T])
            R2 = 2
            Yt = self.ring(st, "c_Yt", [128, D], F32, R2)
            Ht = self.ring(st, "c_Ht", [128, D], F32, R2)
            dsl = [P.dsem() for _ in range(R2)]
            junk = sb("junk", [128, D])
            ss = self.ring(st, "c_ss", [128, 16], F32, R2)
            yn = sb("yn", [128, D], BF16)
            ynT = sb("ynT", [128, 8, 128], BF16)
            hm = self.ring(st, "c_hm", [128, D], F32, R2)
            dsh = [P.dsem() for _ in range(R2)]
            u2b = sb("u2b", [128, D], BF16)
            u2f = sb("u2f", [128, D], F32)
            u2Tf = sb("u2Tf", [128, 8, 128], F32)
            u2T = self.ring(st, "c_u2T", [128, 8, 128], BF16, R2)
            dsu = [P.dsem() for _ in range(R2)]
            gl = sb("gl", [128, 8]); gl2 = sb("gl2", [128, 8]); gm1 = sb("gm1", [128, 8]); gm2 = sb("gm2", [128, 8])
            gsc = sb("gsc", [128, 8])
            gT = self.ring(st, "c_gT", [128, 8], F32, R2)
            identb = C["identb"]; identf = C["c_ident"]

            def load(jn):
                pp, nn = self.tiles[jn]
                rr = jn % R2
                P.dma("sp", Yt[rr][0:nn, :], S["Y"][pp:pp + nn, :], dsl[rr], writes=[Yt[rr]])
                P.dma("sp", Ht[rr][0:nn, :], hsrc[pp:pp + nn, :], dsl[rr], writes=[Ht[rr]], newgroup=False)
                P.commit(dsl[rr], [Yt[rr], Ht[rr]])
            for j, (p0, n) in enumerate(self.tiles):
                r = j % R2
                if j == 0:
                    load(0)
                if j + 1 < len(self.tiles):
                    load(j + 1)
                Y_, H_, SS, HM, U2T = Yt[r], Ht[r], ss[r], hm[r], u2T[r]
                self.act(junk[0:n, :], Y_[0:n, :], AF.Square, [Y_], [junk])
                P.op("dve", lambda e, n=n, SS=SS: e.tensor_reduce(out=SS[0:n, 0:4], in_=junk[0:n, :].rearrange("p (g c) -> p g c", g=4), axis=AX.X, op=ALU.add),
                     reads=[junk], writes=[SS])
                self.rsq(SS[0:n, 0:4], SS[0:n, 0:4], 1.0 / 256, [SS])
                for g in range(4):
                    self.stt("dve", yn[0:n, g * 256:(g + 1) * 256], Y_[0:n, g * 256:(g + 1) * 256], SS[0:n, g:g + 1],
                             og_bc[0:n, g * 256:(g + 1) * 256], ALU.mult, ALU.mult, [Y_, SS, og_bc], [yn])
                b0 = bank[0]
                b0v = b0.t[:].bitcast(BF16)
                for k in range(8):
                    self.tr(b0v[:, k * 128:k * 128 + n], yn[0:n, k * 128:(k + 1) * 128], identb[0:n, 0:n], [yn, identb], [b0])
                self.cp("act", ynT[:, :, 0:n], b0v[:, :].rearrange("p (k t) -> p k t", k=8)[:, :, 0:n], [], [ynT, b0])
                for c in range(2):
                    bo = bank[2 + c]
                    for k in range(8):
                        self.mm(bo[0:n, 0:512], ynT[:, k, 0:n], wout[:, k, c * 512:(c + 1) * 512], k == 0, k == 7, [ynT, wout], [bo])
                    self.tt("dve", HM[0:n, c * 512:(c + 1) * 512], bo[0:n, 0:512], H_[0:n, c * 512:(c + 1) * 512], ALU.add, [H_], [HM, bo])
                P.dma("sp", S["hmid"][p0:p0 + n, :], HM[0:n, :], dsh[r], reads=[HM], writes=[Buf()])
                P.op("dve", lambda e, SS=SS: e.memset(SS[:, 8:9], 0.0), writes=[SS])
                self.act(junk[0:n, :], HM[0:n, :], AF.Square, [HM, SS], [junk, SS], accum_out=SS[0:n, 8:9])
                self.rsq(SS[0:n, 9:10], SS[0:n, 8:9], 1.0 / D, [SS])
                b1 = bank[1]
                if not moe:
                    self.act(u2b[0:n, :], HM[0:n, :], AF.Copy, [HM, SS], [u2b], scale=SS[0:n, 9:10])
                    b1v = b1.t[:].bitcast(BF16)
                    for k in range(8):
                        self.tr(b1v[:, k * 128:k * 128 + n], u2b[0:n, k * 128:(k + 1) * 128], identb[0:n, 0:n], [u2b, identb], [b1])
                    self.tt("dve", U2T[:, :, 0:n], b1v[:, :].rearrange("p (k t) -> p k t", k=8)[:, :, 0:n],
                            g2T[:, :].unsqueeze(2).broadcast_to([128, 8, n]), ALU.mult, [g2T], [U2T, b1])
                else:
                    self.act(u2f[0:n, :], HM[0:n, :], AF.Copy, [HM, SS], [u2f], scale=SS[0:n, 9:10])
                    for half in range(2):
                        bb = bank[5 + half]
                        for kk in range(4):
                            k = half * 4 + kk
                            self.tr(bb[:, kk * 128:kk * 128 + n], u2f[0:n, k * 128:(k + 1) * 128], identf[0:n, 0:n], [u2f, identf], [bb])
                        self.tt("dve", u2Tf[:, half * 4:half * 4 + 4, 0:n], bb[:, :].rearrange("p (k t) -> p k t", k=4)[:, :, 0:n],
                                g2T[:, half * 4:half * 4 + 4].unsqueeze(2).broadcast_to([128, 4, n]), ALU.mult, [g2T], [u2Tf, bb])
                    self.cp("pool", U2T[:, :, 0:n], u2Tf[:, :, 0:n], [u2Tf], [U2T])
                    b7 = bank[7]
                    for k in range(8):
                        self.mm(b7[0:n, 0:NEXP], u2Tf[:, k, 0:n], wr[:, k, :], k == 0, k == 7, [u2Tf, wr], [b7])
                    self.cp("act", gl[0:n, :], b7[0:n, 0:NEXP], [], [gl, b7])
                    P.op("dve", lambda e, n=n: e.tensor_reduce(out=gsc[0:n, 0:1], in_=gl[0:n, :], axis=AX.X, op=ALU.max), reads=[gl], writes=[gsc])
                    self.ts("dve", gm1[0:n, :], gl[0:n, :], gsc[0:n, 0:1], None, ALU.is_equal, None, [gl, gsc], [gm1])
                    self.stt("dve", gl2[0:n, :], gm1[0:n, :], -1e30, gl[0:n, :], ALU.mult, ALU.add, [gm1, gl], [gl2])
                    P.op("dve", lambda e, n=n: e.tensor_reduce(out=gsc[0:n, 1:2], in_=gl2[0:n, :], axis=AX.X, op=ALU.max), reads=[gl2], writes=[gsc])
                    self.ts("dve", gm2[0:n, :], gl2[0:n, :], gsc[0:n, 1:2], None, ALU.is_equal, None, [gl2, gsc], [gm2])
                    self.tt("dve", gsc[0:n, 2:3], gsc[0:n, 1:2], gsc[0:n, 0:1], ALU.subtract, [gsc], [gsc])
                    self.act(gsc[0:n, 3:4], gsc[0:n, 2:3], AF.Exp, [gsc], [gsc])
                    self.ts("dve", gsc[0:n, 4:5], gsc[0:n, 3:4], 1.0, None, ALU.add, None, [gsc], [gsc])
                    P.op("dve", lambda e, n=n: e.reciprocal(out=gsc[0:n, 4:5], in_=gsc[0:n, 4:5]), reads=[gsc], writes=[gsc])
                    self.tt("dve", gsc[0:n, 5:6], gsc[0:n, 3:4], gsc[0:n, 4:5], ALU.mult, [gsc], [gsc])
                    gate = gT[r]
                    self.ts("dve", gate[0:n, :], gm1[0:n, :], gsc[0:n, 4:5], None, ALU.mult, None, [gm1, gsc], [gate])
                    self.stt("dve", gate[0:n, :], gm2[0:n, :], gsc[0:n, 5:6], gate[0:n, :], ALU.mult, ALU.add, [gm2, gsc, gate], [gate])
                    P.dma("sp", S["gate"][p0:p0 + n, :], gate[0:n, :], dsu[r], reads=[gate], writes=[Buf()])
                P.dma("sp", S["u2T"].rearrange("(k p) t -> p k t", p=128)[:, :, p0:p0 + n], U2T[:, :, 0:n], dsu[r], reads=[U2T], writes=[Buf()],
                      newgroup=not moe)
                P.commit(dsu[r], [U2T, gT[r]])
            self.barrier([(d.key, d.cnt) for d in dsh + dsu if d.cnt > 0])

    def ffn_core(self, st, tok_sets, wsets, out_fn, blend, gated):
        P, I, S, C, nc = self.P, self.I, self.S, self.C, self.nc
        bank = self.bank
        sb = lambda name, shape, dt=F32: self.sb(st, "f_" + name, shape, dt)
        TSMAX = max(sum(n for (_, n) in ts) for ts in tok_sets)
        NTT = max(len(ts) for ts in tok_sets)
        u2 = sb("u2", [128, 8, TSMAX], BF16)
        acc = sb("acc", [128, NTT, D], F32)
        dsx = P.dsem(); dsa = P.dsem(); dsg = P.dsem()
        SW = 256
        if blend:
            u2B = self.ring(st, "f_u2B", [128, 8, SW], BF16, 2)
            dsxb = [P.dsem() for _ in range(2)]
            accB = self.ring(st, "f_accB", [128, D], F32, 2)
            dsb = [P.dsem() for _ in range(2)]
            selA = C["sel"][:, 0:1]; selB = C["sel"][:, 1:2]
        if gated:
            gt = sb("gt", [128, NTT, NEXP], F32)
            gtB = sb("gtB", [128, NTT, NEXP], F32)
        GW = 512
        wg = self.ring(st, "f_wg", [128, 8, GW], BF16, 2)
        wu = self.ring(st, "f_wu", [128, 8, GW], BF16, 2)
        wd = self.ring(st, "f_wd", [128, 4, D], BF16, 2)
        dsw = [P.dsem() for _ in range(2)]
        sg = self.ring(st, "f_sg", [128, 512], F32, 2)
        hid = self.ring(st, "f_hid", [128, 512], BF16, 8)
        dso = [P.dsem() for _ in range(4)]
        u2Tv = S["u2T"].rearrange("(k p) t -> p k t", p=128)
        groups = []
        for (wg_ap, wu_ap, wd_ap, F, e) in wsets:
            f0 = 0
            while f0 < F:
                fw = min(GW, F - f0)
                groups.append((wg_ap, wu_ap, wd_ap, f0, fw, e))
                f0 += fw
        gcount = 0
        hcount = 0

        def loadw(gi):
            wg_ap, wu_ap, wd_ap, f0, fw, e = groups[gi % len(groups)]
            r = gi % 2
            wgv = wg_ap.rearrange("(k p) f -> p k f", p=128)
            wuv = wu_ap.rearrange("(k p) f -> p k f", p=128)
            for k in range(0, 8, 4):
                P.dma("pool", wg[r][:, k:k + 4, 0:fw], wgv[:, k:k + 4, f0:f0 + fw], dsw[r], writes=[wg[r]], newgroup=(k == 0))
            for k in range(0, 8, 4):
                P.dma("pool", wu[r][:, k:k + 4, 0:fw], wuv[:, k:k + 4, f0:f0 + fw], dsw[r], writes=[wu[r]], newgroup=False)
            nb = fw // 128
            P.dma("pool", wd[r][:, 0:nb, :], wd_ap[f0:f0 + fw, :].rearrange("(b p) d -> p b d", p=128), dsw[r], writes=[wd[r]], newgroup=False)
            P.commit(dsw[r], [wg[r], wu[r], wd[r]])
        total_groups = len(groups) * len(tok_sets)
        loadw(0)
        nstage = 0
        for si, ts in enumerate(tok_sets):
            col = 0
            cols = []
            for ti, (rows, n) in enumerate(ts):
                cols.append(col)
                col += n
            TS = col
            if not blend:
                r00 = ts[0][0]
                for ti, (rows, n) in enumerate(ts):
                    assert rows == r00 + cols[ti]
                    P.dma("sp", acc[0:n, ti, :], S["hmid"][rows:rows + n, :], dsa, writes=[acc], newgroup=(ti == 0))
                for c0 in range(0, TS, 512):
                    cw_ = min(512, TS - c0)
                    P.dma("sp", u2[:, :, c0:c0 + cw_], u2Tv[:, :, r00 + c0:r00 + c0 + cw_], dsx, writes=[u2], newgroup=(c0 == 0))
                P.commit(dsx, [u2])
                P.commit(dsa, [acc])
            else:
                rA0, rB0 = ts[0][0]
                for ti, (rows, n) in enumerate(ts):
                    rA, rB = rows
                    assert rA == rA0 + cols[ti] and rB == rB0 + cols[ti]
                    P.dma("sp", acc[0:n, ti, :], S["hmid"][rA:rA + n, :], dsa, writes=[acc], newgroup=(ti == 0))
                    if gated:
                        P.dma("sp", gt[0:n, ti, :], S["gate"][rA:rA + n, :], dsg, writes=[gt], newgroup=(ti == 0))
                        P.dma("sp", gtB[0:n, ti, :], S["gate"][rB:rB + n, :], dsg, writes=[gtB], newgroup=False)
                P.commit(dsa, [acc])
                if gated:
                    P.commit(dsg, [gt, gtB])
                for c0 in range(0, TS, 512):
                    cw_ = min(512, TS - c0)
                    P.dma("sp", u2[:, :, c0:c0 + cw_], u2Tv[:, :, rA0 + c0:rA0 + c0 + cw_], dsx, writes=[u2], newgroup=(c0 == 0))
                P.commit(dsx, [u2])
                for ti, (rows, n) in enumerate(ts):
                    rA, rB = rows
                    ab = accB[ti % 2]
                    P.dma("sp", ab[0:n, :], S["hmid"][rB:rB + n, :], dsb[ti % 2], writes=[ab])
                    self.act(acc[0:n, ti, :], acc[0:n, ti, :], AF.Copy, [acc, C["sel"]], [acc], scale=selA[0:n, :])
                    self.stt("dve", acc[0:n, ti, :], ab[0:n, :], selB[0:n, :], acc[0:n, ti, :], ALU.mult, ALU.add, [ab, C["sel"], acc], [acc])
                for c0 in range(0, TS, SW):
                    cw_ = min(SW, TS - c0)
                    ub_ = u2B[nstage % 2]
                    P.dma("sp", ub_[:, :, 0:cw_], u2Tv[:, :, rB0 + c0:rB0 + c0 + cw_], dsxb[nstage % 2], writes=[ub_])
                    nstage += 1
                    self.act(u2[:, :, c0:c0 + cw_], u2[:, :, c0:c0 + cw_], AF.Copy, [u2, C["sel"]], [u2], scale=selA)
                    self.stt("dve", u2[:, :, c0:c0 + cw_], ub_[:, :, 0:cw_], selB, u2[:, :, c0:c0 + cw_], ALU.mult, ALU.add, [ub_, C["sel"], u2], [u2])
                if gated:
                    self.act(gt[:], gt[:], AF.Copy, [gt, C["sel"]], [gt], scale=selA)
                    self.stt("dve", gt[:], gtB[:], selB, gt[:], ALU.mult, ALU.add, [gtB, C["sel"], gt], [gt])
            chunks = []
            cur = []
            cw_ = 0
            for ti, (rows, n) in enumerate(ts):
                if cw_ + n > 512:
                    chunks.append(cur); cur = []; cw_ = 0
                cur.append(ti); cw_ += n
            if cur:
                chunks.append(cur)
            for gi_local in range(len(groups)):
                gi = gcount
                gcount += 1
                wg_ap, wu_ap, wd_ap, f0, fw, e = groups[gi_local]
                r = gi % 2
                if gi + 1 < total_groups:
                    loadw(gi + 1)
                nb = fw // 128
                for ch in chunks:
                    c0 = cols[ch[0]]
                    cw = sum(ts[ti][1] for ti in ch)
                    hslots = []
                    for fb in range(nb):
                        bg = bank[(hcount % 2) * 2]
                        bu = bank[(hcount % 2) * 2 + 1]
                        H_ = hid[hcount % 8]
                        SG = sg[hcount % 2]
                        hcount += 1
                        for k in range(8):
                            self.mm(bg[:, 0:cw], wg[r][:, k, fb * 128:(fb + 1) * 128], u2[:, k, c0:c0 + cw], k == 0, k == 7, [wg[r], u2], [bg])
                        for k in range(8):
                            self.mm(bu[:, 0:cw], wu[r][:, k, fb * 128:(fb + 1) * 128], u2[:, k, c0:c0 + cw], k == 0, k == 7, [wu[r], u2], [bu])
                        self.act(SG[:, 0:cw], bg[:, 0:cw], AF.Silu, [], [SG, bg])
                        self.tt("dve", H_[:, 0:cw], bu[:, 0:cw], SG[:, 0:cw], ALU.mult, [SG], [H_, bu])
                        hslots.append(H_)
                    for ti in ch:
                        n = ts[ti][1]
                        lc = cols[ti] - c0
                        for half in range(2):
                            bd = bank[4 + 2 * (ti % 2) + half]
                            for fb in range(nb):
                                self.mm(bd[0:n, 0:512], hslots[fb][:, lc:lc + n], wd[r][:, fb, half * 512:(half + 1) * 512], fb == 0, fb == nb - 1,
                                        [hslots[fb], wd[r]], [bd])
                            a_ = acc[0:n, ti, half * 512:(half + 1) * 512]
                            if gated:
                                self.stt("dve", a_, bd[0:n, 0:512], gt[0:n, ti, e:e + 1], a_, ALU.mult, ALU.add, [acc, gt], [acc, bd])
                            else:
                                self.tt("dve", a_, bd[0:n, 0:512], a_, ALU.add, [acc], [acc, bd])
            for ti, (rows, n) in enumerate(ts):
                dst = out_fn(si, ti)
                if dst is None:
                    continue
                ev = P.dma("sp", dst, acc[0:n, ti, :], dso[ti % 4], reads=[acc], writes=[Buf()])
                self.final_evs.append(ev)
        self.barrier([(d.key, d.cnt) for d in dso if d.cnt > 0])

    def phase_ffn(self, hdst):
        I = self.I
        T = self.T
        rows = [(r0, min(128, T - r0)) for r0 in range(0, T, 128)]
        TSN = 16
        tok_sets = [rows[i:i + TSN] for i in range(0, len(rows), TSN)]
        if len(tok_sets) > 1 and len(tok_sets[-1]) == 1:
            tok_sets[-2] = tok_sets[-2] + tok_sets[-1]
            tok_sets.pop()
        final = hdst is None

        def out_fn(si, ti):
            r0, n = tok_sets[si][ti]
            if not final:
                return hdst[r0:r0 + n, :]
            lo = max(r0, NMETA)
            return None if True else None
        with contextlib.ExitStack() as st:
            self.ffn_core(st, tok_sets, [(I["ffn_wg"], I["ffn_wu"], I["ffn_wd"], D_FF, 0)], out_fn, blend=False, gated=False)

    def phase_moe(self):
        I = self.I
        NTOK = self.moe_tokens
        ntile = NTOK // 128
        TSN = 16
        tiles_ab = [((NMETA + 128 * i, NMETA + NTOK + 128 * i), 128) for i in range(ntile)]
        tok_sets = [tiles_ab[i:i + TSN] for i in range(0, ntile, TSN)]
        out = self.out_ap

        def out_fn(si, ti):
            i = si * TSN + ti
            return out[128 * i:128 * (i + 1), :]
        wsets = [(I["moe_wg"][e], I["moe_wu"][e], I["moe_wd"][e], D_FFE, e) for e in range(NEXP)]
        with contextlib.ExitStack() as st:
            self.ffn_core(st, tok_sets, wsets, out_fn, blend=True, gated=True)


def prep_inputs(inp, b, half, NT, depth=2):
    f = lambda a: np.ascontiguousarray(np.asarray(a, dtype=np.float32))
    T = NMETA + 128 * NT
    m = {}
    m["xin"] = f(np.concatenate([inp["meta"], inp["x"][b]], axis=0))
    sel = np.zeros((128, 2), np.float32)
    sel[:, half] = 1.0
    m["sel"] = sel
    m.update(make_consts(T))
    L = depth
    colmaj = lambda v: f(np.asarray(v).reshape(L, -1, 128).transpose(0, 2, 1))
    m["w_in"] = f(inp["w_in"][:L]); m["w_out"] = f(inp["w_out"][:L])
    m["g1T"] = colmaj(inp["norm1_g"][:L]); m["g2T"] = colmaj(inp["norm2_g"][:L])
    m["og"] = f(inp["out_norm_g"][:L])
    cw = np.asarray(inp["lru_conv_w"][:L])
    m["conv_wT"] = f(cw.transpose(0, 2, 1).reshape(L, 2, 128, 4).transpose(0, 2, 1, 3))
    m["conv_b"] = colmaj(inp["lru_conv_b"][:L])
    m["lru_ba"] = colmaj(np.asarray(inp["lru_ba"][:L]).reshape(L, 256))
    m["lru_bx"] = colmaj(np.asarray(inp["lru_bx"][:L]).reshape(L, 256))
    m["lru_lam"] = colmaj(inp["lru_lambda"][:L])
    m["lru_wa"] = f(inp["lru_wa"][:L]); m["lru_wx"] = f(inp["lru_wx"][:L])
    m["hg_lb"] = f(inp["hg_lb_logits"][:2])
    for k in ["mla_gq", "mla_w_uq", "mla_gkv", "mla_w_ukv", "mla_gqn", "mla_gkn", "fox_gqn", "fox_gkn", "fox_bf"]:
        m[k] = f(inp[k][:L])
    m["ffn_wg"] = f(inp["ffn_w_gate"][0]); m["ffn_wu"] = f(inp["ffn_w_up"][0]); m["ffn_wd"] = f(inp["ffn_w_down"][0])
    if depth > 1:
        m["moe_wr"] = f(inp["moe_w_router"][0]); m["moe_wg"] = f(inp["moe_w_gate"][0])
        m["moe_wu"] = f(inp["moe_w_up"][0]); m["moe_wd"] = f(inp["moe_w_down"][0])
    return m


_CACHE = {}


def _get_program(NT):
    if NT not in _CACHE:
        b = Builder(NT, depth=2, debug=False)
        nc = b.build()
        _CACHE[NT] = (b, nc)
    return _CACHE[NT]


def kernel(**inputs):
    x = np.asarray(inputs["x"])
    Bsz, SEQ, _ = x.shape
    NT = SEQ // 128
    b, nc = _get_program(NT)
    half_tokens = b.moe_tokens
    n_cores = 8
    in_maps = []
    shared = None
    for c in range(n_cores):
        bi, half = c % Bsz, c // Bsz
        if shared is None:
            m = prep_inputs(inputs, bi, half, NT, depth=2)
            shared = m
        else:
            m = dict(shared)
            m["xin"] = np.ascontiguousarray(np.concatenate([np.asarray(inputs["meta"], np.float32), np.asarray(x[bi], np.float32)], axis=0))
            sel = np.zeros((128, 2), np.float32)
            sel[:, half] = 1.0
            m["sel"] = sel
        in_maps.append({k: v for k, v in m.items() if k in b.inputs})
    res = run_bass_kernel_spmd(nc, in_maps, core_ids=list(range(n_cores)))
    out = np.empty((Bsz, SEQ, D), np.float32)
    for c in range(n_cores):
        bi, half = c % Bsz, c // Bsz
        out[bi, half * half_tokens:(half + 1) * half_tokens] = np.asarray(res.results[c]["out"])
    return out
```

```python
import contextlib
import os
import numpy as np
import ml_dtypes
import concourse.bass as bass
import concourse.mybir as mybir
from concourse.bass_utils import run_bass_kernel_spmd

F32 = mybir.dt.float32
BF16 = mybir.dt.bfloat16
AF = mybir.ActivationFunctionType
ALU = mybir.AluOpType
AX = mybir.AxisListType

ENGS = ("pe", "act", "dve", "pool", "sp")
D = 1024
NMETA = 16
EPS = 1e-6
IN_COLS = 2660
D_FF = 2816
D_FFE = 3584
NEXP = 8


class Buf:
    __slots__ = ("name", "w", "r")

    def __init__(self, name=""):
        self.name = name
        self.w = None
        self.r = {}


class DSem:
    __slots__ = ("h", "cnt", "key")

    def __init__(self, h, key):
        self.h = h
        self.cnt = 0
        self.key = key


class TT:
    __slots__ = ("t", "b")

    def __init__(self, t, b=None, name=""):
        self.t = t
        self.b = b if b is not None else Buf(name)

    def __getitem__(self, k):
        return self.t[k]


def _bufs(xs):
    out = []
    for x in xs:
        if x is None:
            continue
        out.append(x.b if isinstance(x, TT) else x)
    return out


class Prog:
    def __init__(self, nc, stack):
        self.nc = nc
        self.stack = stack
        self.q = {e: [] for e in ENGS}
        self.cnt = {e: 0 for e in ENGS}
        self.seen = {e: {} for e in ENGS}
        self.semh = {}
        for e in ENGS:
            h = stack.enter_context(nc.semaphore("es_" + e))
            self.semh[("E", e)] = h
        self.nd = 0
        self.all_dsems = []

    def dsem(self, name=None):
        self.nd += 1
        key = ("D", self.nd)
        h = self.stack.enter_context(self.nc.semaphore(name or f"ds{self.nd}"))
        self.semh[key] = h
        d = DSem(h, key)
        self.all_dsems.append(d)
        return d

    def _waits(self, eng, reads, writes, extra=()):
        need = {}

        def add(ev, raw):
            if ev is None:
                return
            k, v = ev
            if k == ("E", eng) and not raw:
                return
            if need.get(k, 0) < v:
                need[k] = v
        for b in reads:
            add(b.w, True)
        for b in writes:
            add(b.w, False)
            for k, v in b.r.items():
                add((k, v), False)
        for ev in extra:
            add(ev, True)
        out = []
        seen = self.seen[eng]
        for k, v in need.items():
            if seen.get(k, 0) < v:
                seen[k] = v
                out.append((k, v))
        return out

    def _mark(self, ev, reads, writes):
        k, v = ev
        for b in reads:
            if b.r.get(k, 0) < v:
                b.r[k] = v
        for b in writes:
            b.w = ev
            b.r = {}

    def op(self, eng, fn, reads=(), writes=()):
        reads = _bufs(reads)
        writes = _bufs(writes)
        waits = self._waits(eng, reads, writes)
        self.cnt[eng] += 1
        ev = (("E", eng), self.cnt[eng])
        self._mark(ev, reads, writes)
        self.q[eng].append((waits, fn, (("E", eng), 1)))
        return ev

    def dma(self, eng, out, in_, ds, reads=(), writes=(), newgroup=True, **kw):
        reads = _bufs(reads)
        writes = _bufs(writes)
        extra = ()
        if newgroup and ds.cnt > 0:
            extra = ((ds.key, ds.cnt),)
        waits = self._waits(eng, reads, writes, extra)
        ds.cnt += 16
        ev = (ds.key, ds.cnt)
        self._mark(ev, reads, writes)
        self.q[eng].append((waits, lambda e: e.dma_start(out=out, in_=in_, **kw), (ds.key, 16)))
        return ev

    def commit(self, ds, bufs):
        for b in _bufs(bufs):
            if b.w is not None and b.w[0] == ds.key:
                b.w = (ds.key, ds.cnt)
            if ds.key in b.r:
                b.r[ds.key] = ds.cnt

    def wait_all(self, eng, evs):
        waits = self._waits(eng, (), (), evs)
        self.q[eng].append((waits, None, None))

    def emit(self):
        nc = self.nc
        semh = self.semh
        with nc.Block() as block:
            def mk(ename):
                def body(e):
                    for waits, fn, inc in self.q[ename]:
                        for k, v in waits:
                            e.wait_ge(semh[k], v)
                        if fn is None:
                            continue
                        ins = fn(e)
                        if inc is not None:
                            ins.then_inc(semh[inc[0]], inc[1])
                return body
            block.tensor(mk("pe"))
            block.scalar(mk("act"))
            block.vector(mk("dve"))
            block.gpsimd(mk("pool"))
            block.sync(mk("sp"))


def make_consts(T):
    s = np.arange(128)[:, None]
    t = np.arange(128)[None, :]
    c = {}
    c["c_ident"] = np.eye(128, dtype=np.float32)
    c["c_triblk"] = ((s // 32 == t // 32) & (s <= t)).astype(np.float32)
    c["c_blkones"] = (s // 32 == t // 32).astype(np.float32)
    c["c_chunkind"] = (s // 32 == np.arange(4)[None, :]).astype(np.float32)
    c["c_trifull"] = (s <= t).astype(np.float32)
    c["c_sel127"] = np.broadcast_to((s == 127), (128, 128)).astype(np.float32).copy()
    c["c_sel15"] = np.broadcast_to((s == 15), (128, 128)).astype(np.float32).copy()
    c["c_sel64"] = np.broadcast_to((s == 64), (128, 128)).astype(np.float32).copy()
    c["c_sel8"] = np.broadcast_to((s == 8), (128, 128)).astype(np.float32).copy()
    c["c_mask_mla"] = (s // 64 <= t // 64).astype(np.float32)
    pos = np.arange(T, dtype=np.float32)
    inv = (10000.0 ** (-np.arange(16, dtype=np.float32) / 16)).astype(np.float32)
    ang = pos[:, None] * inv[None, :]
    c["c_cos"] = np.cos(ang).astype(np.float32)
    c["c_sin"] = np.sin(ang).astype(np.float32)
    return c


def tile_list(NT):
    return [(0, NMETA)] + [(NMETA + 128 * i, 128) for i in range(NT)]


class Builder:
    def __init__(self, NT, depth=2, debug=False, stop_after=None, moe_tokens=None):
        self.NT = NT
        self.T = NMETA + 128 * NT
        self.depth = depth
        self.debug = debug
        self.stop_after = stop_after
        self.tiles = tile_list(NT)
        self.nc = bass.Bass("TRN2", target_bir_lowering=False)
        self.inputs = {}
        self.outputs = {}
        self.moe_tokens = moe_tokens if moe_tokens is not None else (128 * NT) // 2

    def din(self, name, shape, dt=F32):
        t = self.nc.dram_tensor(name, list(shape), dt, kind="ExternalInput")
        self.inputs[name] = (tuple(shape), dt)
        return t.ap()

    def dscratch(self, name, shape, dt=F32, dbg=False):
        if dbg and self.debug:
            t = self.nc.dram_tensor(name, list(shape), dt, kind="ExternalOutput")
            self.outputs[name] = (tuple(shape), dt)
        else:
            t = self.nc.dram_tensor(name, list(shape), dt, kind="Internal")
        return t.ap()

    def dout(self, name, shape, dt=F32):
        t = self.nc.dram_tensor(name, list(shape), dt, kind="ExternalOutput")
        self.outputs[name] = (tuple(shape), dt)
        return t.ap()

    def sb(self, st, name, shape, dt=F32):
        self._uid = getattr(self, "_uid", 0) + 1
        return TT(st.enter_context(self.nc.sbuf_tensor(f"s{self._uid}_{name}", list(shape), dt)), name=name)

    def ring(self, st, name, shape, dt, n):
        return [self.sb(st, f"{name}{i}", shape, dt) for i in range(n)]

    def build(self):
        nc = self.nc
        T = self.T
        NT = self.NT
        depth = self.depth
        I = {}
        I["xin"] = self.din("xin", [T, D])
        I["sel"] = self.din("sel", [128, 2])
        for nm, shp in [("c_ident", [128, 128]), ("c_triblk", [128, 128]), ("c_blkones", [128, 128]),
                        ("c_chunkind", [128, 4]), ("c_trifull", [128, 128]), ("c_sel127", [128, 128]),
                        ("c_sel15", [128, 128]), ("c_sel64", [128, 128]), ("c_sel8", [128, 128]),
                        ("c_mask_mla", [128, 128]), ("c_cos", [T, 16]), ("c_sin", [T, 16])]:
            I[nm] = self.din(nm, shp)
        L = depth
        I["w_in"] = self.din("w_in", [L, D, IN_COLS])
        I["w_out"] = self.din("w_out", [L, D, D])
        I["g1T"] = self.din("g1T", [L, 128, 8])
        I["g2T"] = self.din("g2T", [L, 128, 8])
        I["og"] = self.din("og", [L, D])
        I["conv_wT"] = self.din("conv_wT", [L, 128, 2, 4])
        I["conv_b"] = self.din("conv_b", [L, 128, 2])
        I["lru_ba"] = self.din("lru_ba", [L, 128, 2])
        I["lru_bx"] = self.din("lru_bx", [L, 128, 2])
        I["lru_lam"] = self.din("lru_lam", [L, 128, 2])
        I["lru_wa"] = self.din("lru_wa", [L, 4, 64, 64])
        I["lru_wx"] = self.din("lru_wx", [L, 4, 64, 64])
        I["hg_lb"] = self.din("hg_lb", [2, 256])
        I["mla_gq"] = self.din("mla_gq", [L, 192])
        I["mla_w_uq"] = self.din("mla_w_uq", [L, 192, 384])
        I["mla_gkv"] = self.din("mla_gkv", [L, 128])
        I["mla_w_ukv"] = self.din("mla_w_ukv", [L, 128, 512])
        I["mla_gqn"] = self.din("mla_gqn", [L, 96])
        I["mla_gkn"] = self.din("mla_gkn", [L, 96])
        I["fox_gqn"] = self.din("fox_gqn", [L, 64])
        I["fox_gkn"] = self.din("fox_gkn", [L, 64])
        I["fox_bf"] = self.din("fox_bf", [L, 4])
        I["ffn_wg"] = self.din("ffn_wg", [D, D_FF])
        I["ffn_wu"] = self.din("ffn_wu", [D, D_FF])
        I["ffn_wd"] = self.din("ffn_wd", [D_FF, D])
        if depth > 1:
            I["moe_wr"] = self.din("moe_wr", [D, NEXP])
            I["moe_wg"] = self.din("moe_wg", [NEXP, D, D_FFE])
            I["moe_wu"] = self.din("moe_wu", [NEXP, D, D_FFE])
            I["moe_wd"] = self.din("moe_wd", [NEXP, D_FFE, D])
        self.I = I
        S = {}
        dbg = True
        S["hA"] = self.dscratch("hA", [T, D], F32, dbg)
        S["hmid"] = self.dscratch("hmid", [T, D], F32, dbg)
        S["Y"] = self.dscratch("Y", [T, D], F32, dbg)
        S["u2T"] = self.dscratch("u2T", [D, T], BF16, dbg)
        S["QTc"] = self.dscratch("QTc", [4, 96, T], BF16, dbg)
        S["KTc"] = self.dscratch("KTc", [4, 96, T], BF16, dbg)
        S["Vc"] = self.dscratch("Vc", [4, 128, NT + 1, 65], BF16, dbg)
        S["QTd"] = self.dscratch("QTd", [2, 128, T], BF16, dbg)
        S["KTd"] = self.dscratch("KTd", [2, 128, T], BF16, dbg)
        S["Vd"] = self.dscratch("Vd", [4, 128, NT + 1, 65], BF16, dbg)
        S["gate"] = self.dscratch("gate", [T, NEXP], F32, dbg)
        self.S = S
        n_out = self.moe_tokens if depth > 1 else 128 * NT
        self.out_ap = self.dout("out", [n_out, D])

        with contextlib.ExitStack() as st:
            P = Prog(nc, st)
            self.P = P
            self.bank = [TT(st.enter_context(nc.psum_tensor(f"bank{i}", [128, 512], F32)), name=f"bank{i}")
                         for i in range(8)]
            with contextlib.ExitStack() as cst:
                self.load_consts(cst)
                for l in range(depth):
                    hsrc = I["xin"] if l == 0 else S["hA"]
                    self.phase_1a(l, hsrc)
                    if self.stop_after == ("1a", l):
                        break
                    self.phase_1b(l)
                    if self.stop_after == ("1b", l):
                        break
                    self.phase_1c(l, hsrc)
                    if self.stop_after == ("1c", l):
                        break
                    if l == 0:
                        self.phase_ffn(S["hA"])
                    else:
                        self.phase_moe()
            P.wait_all("sp", self.final_evs)
            P.emit()
        return nc

    def load_consts(self, st):
        P = self.P
        I = self.I
        self.final_evs = []
        self.ds_const = P.dsem("ds_const")
        C = {}
        for nm in ["c_ident", "c_triblk", "c_blkones", "c_trifull", "c_sel127", "c_sel15", "c_sel64", "c_sel8",
                   "c_mask_mla"]:
            C[nm] = self.sb(st, nm, [128, 128], F32)
            P.dma("sp", C[nm][:], I[nm][:, :], self.ds_const, writes=[C[nm]], newgroup=False)
        C["c_chunkind"] = self.sb(st, "c_chunkind", [128, 4], F32)
        P.dma("sp", C["c_chunkind"][:], I["c_chunkind"][:, :], self.ds_const, writes=[C["c_chunkind"]], newgroup=False)
        C["sel"] = self.sb(st, "sel", [128, 2], F32)
        P.dma("sp", C["sel"][:], I["sel"][:, :], self.ds_const, writes=[C["sel"]], newgroup=False)
        P.commit(self.ds_const, list(C.values()))
        C["identb"] = self.sb(st, "identb", [128, 128], BF16)
        P.op("pool", lambda e: e.tensor_copy(out=C["identb"][:], in_=C["c_ident"][:]), reads=[C["c_ident"]], writes=[C["identb"]])
        C["hgmask"] = self.sb(st, "hgmask", [128, 128], BF16)
        P.op("pool", lambda e: e.tensor_copy(out=C["hgmask"][:], in_=C["c_triblk"][:]), reads=[C["c_triblk"]], writes=[C["hgmask"]])
        C["foxmask"] = self.sb(st, "foxmask", [128, 128], BF16)
        P.op("pool", lambda e: e.tensor_copy(out=C["foxmask"][:], in_=C["c_trifull"][:]), reads=[C["c_trifull"]], writes=[C["foxmask"]])
        C["mlamask"] = self.sb(st, "mlamask", [128, 128], BF16)
        P.op("pool", lambda e: e.tensor_copy(out=C["mlamask"][:], in_=C["c_mask_mla"][:]), reads=[C["c_mask_mla"]], writes=[C["mlamask"]])
        self.C = C
        self.call = self.sb(st, "c_all", [128, self.NT + 1, 4], F32)
        P.op("pool", lambda e: e.memset(self.call[:], 0.0), writes=[self.call])

    def mm(self, out, lhsT, rhs, start, stop, reads, writes, **kw):
        self.P.op("pe", lambda e: e.matmul(out, lhsT=lhsT, rhs=rhs, start=start, stop=stop, **kw), reads=reads, writes=writes)

    def tr(self, out, in_, ident, reads, writes):
        self.P.op("pe", lambda e: e.transpose(out=out, in_=in_, identity=ident), reads=reads, writes=writes)

    def act(self, out, in_, func, reads, writes, **kw):
        self.P.op("act", lambda e: e.activation(out=out, in_=in_, func=func, **kw), reads=reads, writes=writes)

    def tt(self, eng, out, in0, in1, op, reads, writes):
        self.P.op(eng, lambda e: e.tensor_tensor(out=out, in0=in0, in1=in1, op=op), reads=reads, writes=writes)

    def ts(self, eng, out, in0, s1, s2, op0, op1, reads, writes):
        if op1 is None:
            self.P.op(eng, lambda e: e.tensor_scalar(out=out, in0=in0, scalar1=s1, scalar2=None, op0=op0), reads=reads, writes=writes)
        else:
            self.P.op(eng, lambda e: e.tensor_scalar(out=out, in0=in0, scalar1=s1, scalar2=s2, op0=op0, op1=op1), reads=reads, writes=writes)

    def stt(self, eng, out, in0, scalar, in1, op0, op1, reads, writes):
        self.P.op(eng, lambda e: e.scalar_tensor_tensor(out=out, in0=in0, scalar=scalar, in1=in1, op0=op0, op1=op1), reads=reads, writes=writes)

    def cp(self, eng, out, in_, reads, writes):
        if eng == "act":
            self.P.op("act", lambda e: e.copy(out=out, in_=in_), reads=reads, writes=writes)
        else:
            self.P.op(eng, lambda e: e.tensor_copy(out=out, in_=in_), reads=reads, writes=writes)

    def rsq(self, out, in_, scale, rw):
        self.act(out, in_, AF.Ln, rw, rw, scale=scale, bias=EPS)
        self.act(out, out, AF.Exp, rw, rw, scale=-0.5)

    def sigm(self, out, in_, reads, writes, bias=None):
        if bias is None:
            self.act(out, in_, AF.Exp, reads, writes, scale=-1.0)
        else:
            self.act(out, in_, AF.Exp, reads, writes, scale=-1.0, bias=bias)
        w2 = [writes[0]]
        self.act(out, out, AF.Ln, w2, w2, bias=1.0)
        self.act(out, out, AF.Exp, w2, w2, scale=-1.0)

    def rstd(self, ss, out, n, scale, reads_w):
        self.act(out, ss, AF.Sqrt, reads=reads_w, writes=reads_w, scale=scale, bias=EPS)
        self.P.op("dve", lambda e: e.reciprocal(out=out, in_=out), reads=reads_w, writes=reads_w)

    def phase_1a(self, l, hsrc):
        P, I, S, C, nc = self.P, self.I, self.S, self.C, self.nc
        bank = self.bank
        NT, T = self.NT, self.T
        call = self.call
        with contextlib.ExitStack() as st:
            sb = lambda name, shape, dt=F32: self.sb(st, "a_" + name, shape, dt)
            dsw = P.dsem()
            dsl = P.dsem()
            w_in = sb("w_in", [128, 8, IN_COLS], BF16)
            wv = I["w_in"][l].rearrange("(k p) c -> p k c", p=128)
            for k in range(8):
                P.dma("pool", w_in[:, k, :], wv[:, k, :], dsw, writes=[w_in], newgroup=False)
            wuq = sb("wuq", [128, 2, 384], BF16)
            P.dma("pool", wuq[:, 0, :], I["mla_w_uq"][l, 0:128, :], dsw, writes=[wuq], newgroup=False)
            P.dma("pool", wuq[0:64, 1, :], I["mla_w_uq"][l, 128:192, :], dsw, writes=[wuq], newgroup=False)
            wukv = sb("wukv", [128, 512], BF16)
            P.dma("pool", wukv[:], I["mla_w_ukv"][l, :, :], dsw, writes=[wukv], newgroup=False)
            g1T = sb("g1T", [128, 8])
            P.dma("sp", g1T[:], I["g1T"][l], dsl, writes=[g1T], newgroup=False)
            cw = sb("cw", [128, 2, 4]); cb = sb("cb", [128, 2]); ba = sb("ba", [128, 2]); bx = sb("bx", [128, 2])
            lam = sb("lam", [128, 2])
            for tdst, nm in [(cw, "conv_wT"), (cb, "conv_b"), (ba, "lru_ba"), (bx, "lru_bx"), (lam, "lru_lam")]:
                P.dma("sp", tdst[:], I[nm][l], dsl, writes=[tdst], newgroup=False)
            waf = sb("waf", [128, 2, 128]); wxf = sb("wxf", [128, 2, 128])
            wab = sb("wab", [128, 2, 128], BF16); wxb = sb("wxb", [128, 2, 128], BF16)
            P.op("pool", lambda e: e.memset(waf[:], 0.0), writes=[waf])
            P.op("pool", lambda e: e.memset(wxf[:], 0.0), writes=[wxf])
            for nb in range(4):
                r0 = (nb % 2) * 64
                P.dma("sp", waf[r0:r0 + 64, nb // 2, r0:r0 + 64], I["lru_wa"][l, nb], dsl, writes=[waf], newgroup=False)
                P.dma("sp", wxf[r0:r0 + 64, nb // 2, r0:r0 + 64], I["lru_wx"][l, nb], dsl, writes=[wxf], newgroup=False)
            defer = []
            nba = sb("nba", [128, 2]); nbx = sb("nbx", [128, 2])
            defer.append(lambda: self.ts("dve", nba[:], ba[:], -1.0, None, ALU.mult, None, [ba], [nba]))
            defer.append(lambda: self.ts("dve", nbx[:], bx[:], -1.0, None, ALU.mult, None, [bx], [nbx]))
            defer.append(lambda: self.cp("pool", wab[:], waf[:], [waf], [wab]))
            defer.append(lambda: self.cp("pool", wxb[:], wxf[:], [wxf], [wxb]))
            cA = sb("cA", [128, 2]); cA2 = sb("cA2", [128, 2])
            defer.append(lambda: self.act(cA[:], lam[:], AF.Exp, [lam], [cA], scale=-1.0))
            defer.append(lambda: self.act(cA[:], cA[:], AF.Ln, [cA], [cA], bias=1.0))
            defer.append(lambda: self.ts("dve", cA2[:], cA[:], -16.0, None, ALU.mult, None, [cA], [cA2]))
            defer.append(lambda: self.ts("dve", cA[:], cA[:], -8.0, None, ALU.mult, None, [cA], [cA]))
            bcs = []

            def bc(name, src, w):
                t = sb(name, [128, w])
                P.dma("sp", t[:], src.partition_broadcast(128), dsl, writes=[t], newgroup=False)
                bcs.append(t)
                return t
            gq_bc = bc("gq_bc", I["mla_gq"][l], 192)
            gkv_bc = bc("gkv_bc", I["mla_gkv"][l], 128)
            gqn_bc = bc("gqn_bc", I["mla_gqn"][l], 96)
            gkn_bc = bc("gkn_bc", I["mla_gkn"][l], 96)
            fgq_bc = bc("fgq_bc", I["fox_gqn"][l], 64)
            fgk_bc = bc("fgk_bc", I["fox_gkn"][l], 64)
            fbf_bc = bc("fbf_bc", I["fox_bf"][l], 4)
            lb_bc = sb("lb_bc", [128, 256]); oml_bc = sb("oml_bc", [128, 256])
            if l == 0:
                P.op("pool", lambda e: e.memset(lb_bc[:], 0.0), writes=[lb_bc])
                P.op("pool", lambda e: e.memset(oml_bc[:], 1.0), writes=[oml_bc])
            else:
                lg0 = bc("lg0", I["hg_lb"][0], 256)
                lg1 = bc("lg1", I["hg_lb"][1], 256)
                defer.append(lambda: self.tt("dve", lb_bc[:], lg1[:], lg0[:], ALU.subtract, [lg0, lg1], [lb_bc]))
                defer.append(lambda: self.act(lb_bc[:], lb_bc[:], AF.Sigmoid, [lb_bc], [lb_bc]))
                defer.append(lambda: self.ts("dve", oml_bc[:], lb_bc[:], -1.0, 1.0, ALU.mult, ALU.add, [lb_bc], [oml_bc]))
            P.commit(dsw, [w_in, wuq, wukv])
            P.commit(dsl, [g1T, cw, cb, ba, bx, lam, waf, wxf] + bcs)
            for fn in defer:
                fn()
            xe = sb("xe", [128, 2, 3 + 128])
            P.op("pool", lambda e: e.memset(xe[:], 0.0), writes=[xe])
            hprev = sb("hprev", [128, 2])
            P.op("pool", lambda e: e.memset(hprev[:], 0.0), writes=[hprev])
            Sst = [sb(f"Sst{h}", [64, 64]) for h in range(4)]
            for h in range(4):
                P.op("pool", lambda e, h=h: e.memset(Sst[h][:], 0.0), writes=[Sst[h]])
            Sbf = [[sb(f"Sbf{h}_{r}", [64, 64], BF16) for r in range(2)] for h in range(4)]
            hqm = [sb(f"hqm{c}", [64, 4, 128], BF16) for c in range(4)]
            hkem = [sb(f"hkem{c}", [128, 256], BF16) for c in range(4)]
            for c in range(4):
                P.op("pool", lambda e, c=c: e.memset(hqm[c][:], 0.0), writes=[hqm[c]])
            R2 = 2
            junk = sb("junk", [128, D], BF16)
            ss = self.ring(st, "a_ss", [128, 8], F32, R2)
            ub = self.ring(st, "a_ub", [128, D], BF16, R2)
            uT = self.ring(st, "a_uT", [128, 8, 128], BF16, R2)
            zt = self.ring(st, "a_zt", [128, 2148], F32, R2)
            ysb = self.ring(st, "a_ysb", [128, 512], F32, R2)
            dsy = [P.dsem() for _ in range(R2)]
            dsq = [P.dsem() for _ in range(R2)]
            lu = sb("lu", [128, 128]); lub = sb("lub", [128, 128], BF16)
            lr = sb("lr", [128, 128]); li = sb("li", [128, 128]); la = sb("la", [128, 128]); lm = sb("lm", [128, 128])
            lbb = sb("lbb", [128, 128]); lhs = sb("lhs", [128, 128]); lg = sb("lg", [128, 2, 128]); lya = sb("lya", [128, 2, 128])
            hsig = sb("hsig", [128, 256]); hlogf = sb("hlogf", [128, 256]); hkk = sb("hkk", [128, 256])
            heb = sb("heb", [128, 256]); henb = sb("henb", [128, 256]); hebe = sb("hebe", [128, 256])
            hqd = sb("hqd", [128, 256], BF16); hkd = sb("hkd", [128, 256], BF16); hke = sb("hke", [128, 256], BF16)
            hkdf = sb("hkdf", [128, 256]); hvb = sb("hvb", [128, 256], BF16); hsg = sb("hsg", [128, 256])
            hqdT = sb("hqdT", [64, 4, 128], BF16); hkdT = sb("hkdT", [64, 4, 128], BF16)
            hdec = sb("hdec", [64, 4, 4]); hattm = sb("hattm", [128, 4, 128], BF16)
            mjunk = sb("mjunk", [128, 512]); mss = sb("mss", [128, 16])
            cqn = sb("cqn", [128, 192], BF16); ckvn = sb("ckvn", [128, 128], BF16)
            cqT = sb("cqT", [128, 2, 128], BF16); ckvT = sb("ckvT", [128, 128], BF16)
            qf = sb("qf", [128, 4, 96]); kf = sb("kf", [128, 4, 96])
            qb = sb("qb", [128, 4, 96], BF16); kb = sb("kb", [128, 4, 96], BF16)
            rt = sb("rt", [128, 4, 4, 16])
            vaug = self.ring(st, "a_vaug", [128, 4, 65], BF16, R2)
            qkT = self.ring(st, "a_qkT", [96, 8, 128], BF16, R2)
            fqf = sb("fqf", [128, 4, 64]); fqb = sb("fqb", [128, 256], BF16); fkb = sb("fkb", [128, 256], BF16)
            fvaug = self.ring(st, "a_fvaug", [128, 4, 65], BF16, R2)
            fqkT = self.ring(st, "a_fqkT", [128, 4, 128], BF16, R2)
            flog = sb("flog", [128, 4])
            for r in range(R2):
                P.op("pool", lambda e, r=r: e.memset(vaug[r][:], 1.0), writes=[vaug[r]])
                P.op("pool", lambda e, r=r: e.memset(fvaug[r][:], 1.0), writes=[fvaug[r]])
            identb = C["identb"]; identf = C["c_ident"]

            R3 = 3
            ht = self.ring(st, "a_ht3", [128, D], F32, R3)
            cs = self.ring(st, "a_cs3", [128, 32], F32, R3)
            dsh = [P.dsem() for _ in range(R3)]
            ntl = len(self.tiles)
            b0 = bank[0]
            b0v = b0.t[:].bitcast(BF16)
            b1 = bank[1]

            def load(jn):
                pp, nn = self.tiles[jn]
                rr = jn % R3
                P.dma("sp", ht[rr][0:nn, :], hsrc[pp:pp + nn, :], dsh[rr], writes=[ht[rr]])
                P.dma("sp", cs[rr][0:nn, 0:16], I["c_cos"][pp:pp + nn, :], dsh[rr], writes=[cs[rr]], newgroup=False)
                P.dma("sp", cs[rr][0:nn, 16:32], I["c_sin"][pp:pp + nn, :], dsh[rr], writes=[cs[rr]], newgroup=False)
                P.commit(dsh[rr], [ht[rr], cs[rr]])

            def zpro(j):
                p0, n = self.tiles[j]
                r = j % R2
                H, SS, UB, UT = ht[j % R3], ss[r], ub[r], uT[r]
                P.op("dve", lambda e, SS=SS: e.memset(SS[:], 0.0), writes=[SS])
                self.act(junk[0:n, :], H[0:n, :], AF.Square, [H, SS], [junk, SS], accum_out=SS[0:n, 0:1])
                self.rsq(SS[0:n, 1:2], SS[0:n, 0:1], 1.0 / D, [SS])
                self.act(UB[0:n, :], H[0:n, :], AF.Copy, [H, SS], [UB], scale=SS[0:n, 1:2])
                for k in range(8):
                    self.tr(b0v[:, k * 128:k * 128 + n], UB[0:n, k * 128:(k + 1) * 128], identb[0:n, 0:n], [UB, identb], [b0])
                self.tt("dve", UT[:, :, 0:n], b0v[:, :].rearrange("p (k t) -> p k t", k=8)[:, :, 0:n],
                        g1T[:, :].unsqueeze(2).broadcast_to([128, 8, n]), ALU.mult, [g1T], [UT, b0])

            def zgroups(j):
                p0, n = self.tiles[j]
                r = j % R2
                UT, ZT = uT[r], zt[r]
                gl = []

                def lru_mm():
                    for m in range(4):
                        for k in range(8):
                            self.mm(b1[:, m * 128:m * 128 + n], w_in[:, k, m * 128:(m + 1) * 128], UT[:, k, 0:n], k == 0, k == 7, [w_in, UT], [b1])
                gl.append(lru_mm)
                chunks = [(512, 1024), (1024, 1536), (1536, 1888), (1888, 2400), (2400, 2660)]
                for ci, (c0, c1) in enumerate(chunks):
                    def zc(ci=ci, c0=c0, c1=c1):
                        bz = bank[2 + (ci % 2)]
                        wd = c1 - c0
                        for k in range(8):
                            self.mm(bz[0:n, 0:wd], UT[:, k, 0:n], w_in[:, k, c0:c1], k == 0, k == 7, [UT, w_in], [bz])
                        self.cp("act" if ci % 2 == 0 else "dve", ZT[0:n, c0 - 512:c1 - 512], bz[0:n, 0:wd], [], [ZT, bz])
                    gl.append(zc)
                return gl

            def chains(j, fill):
                p0, n = self.tiles[j]
                r = j % R2
                ZT, YS, CS = zt[r], ysb[r], cs[j % R3]

                def fillpop():
                    if fill:
                        fill.pop(0)()
                steps = os.environ.get("K_STEPS", "lru,hgrn,mla,fox").split(",")
                for cc in (range(2) if "lru" in steps else []):
                    self.act(lg[:, cc, 0:n], b1[:, (2 + cc) * 128:(2 + cc) * 128 + n], AF.Gelu_apprx_tanh, [], [lg, b1])
                for cc in (range(2) if "lru" in steps else []):
                    self.cp("act", xe[:, cc, 3:3 + n], b1[:, cc * 128:cc * 128 + n], [], [xe, b1])
                fillpop()
                for cc in (range(2) if "lru" in steps else []):
                    self.ts("dve", lu[:, 0:n], xe[:, cc, 0:n], cw[:, cc, 0:1], cb[:, cc:cc + 1], ALU.mult, ALU.add, [xe, cw, cb], [lu])
                    for jj in range(1, 4):
                        self.stt("dve", lu[:, 0:n], xe[:, cc, jj:jj + n], cw[:, cc, jj:jj + 1], lu[:, 0:n], ALU.mult, ALU.add, [xe, cw, lu], [lu])
                    self.cp("pool", xe[:, cc, 0:3], xe[:, cc, n:n + 3], [xe], [xe])
                    self.cp("pool", lub[:, 0:n], lu[:, 0:n], [lu], [lub])
                    b4 = bank[4]
                    self.mm(b4[:, 0:n], wab[:, cc, :], lub[:, 0:n], True, True, [wab, lub], [b4])
                    self.mm(b4[:, 128:128 + n], wxb[:, cc, :], lub[:, 0:n], True, True, [wxb, lub], [b4])
                    self.sigm(lr[:, 0:n], b4[:, 0:n], [nba], [lr, b4], bias=nba[:, cc:cc + 1])
                    self.sigm(li[:, 0:n], b4[:, 128:128 + n], [nbx], [li, b4], bias=nbx[:, cc:cc + 1])
                    self.act(la[:, 0:n], lr[:, 0:n], AF.Exp, [lr, cA], [la], scale=cA[:, cc:cc + 1])
                    self.act(lm[:, 0:n], lr[:, 0:n], AF.Exp, [lr, cA2], [lm], scale=cA2[:, cc:cc + 1])
                    self.act(lm[:, 0:n], lm[:, 0:n], AF.Ln, [lm], [lm], scale=-1.0, bias=1.0)
                    self.act(lm[:, 0:n], lm[:, 0:n], AF.Exp, [lm], [lm], scale=0.5)
                    self.tt("dve", lbb[:, 0:n], lm[:, 0:n], li[:, 0:n], ALU.mult, [lm, li], [lbb])
                    self.tt("dve", lbb[:, 0:n], lbb[:, 0:n], lu[:, 0:n], ALU.mult, [lbb, lu], [lbb])
                    P.op("dve", lambda e, n=n, cc=cc: e.tensor_tensor_scan(out=lhs[:, 0:n], data0=la[:, 0:n], data1=lbb[:, 0:n],
                                                                        initial=hprev[:, cc:cc + 1], op0=ALU.mult, op1=ALU.add),
                         reads=[la, lbb, hprev], writes=[lhs])
                    self.cp("dve", hprev[:, cc:cc + 1], lhs[:, n - 1:n], [lhs], [hprev])
                    self.tt("dve", lya[:, cc, 0:n], lhs[:, 0:n], lg[:, cc, 0:n], ALU.mult, [lhs, lg], [lya])
                    fillpop()
                b5 = bank[5]
                for cc in (range(2) if "lru" in steps else []):
                    self.tr(b5[0:n, cc * 128:(cc + 1) * 128], lya[:, cc, 0:n], identf[:, :], [lya, identf], [b5])
                if "lru" in steps:
                    self.cp("act", YS[0:n, 0:256], b5[0:n, 0:256], [], [YS, b5])
                fillpop()
                if "hgrn" in steps:
                    nch = (n + 31) // 32
                    hq_ = ZT[0:n, 0:256]; hf_ = ZT[0:n, 256:512]; hv_ = ZT[0:n, 512:768]; hg_ = ZT[0:n, 768:1024]
                    self.sigm(hsig[0:n, :], hf_, [ZT], [hsig])
                    self.tt("dve", hsig[0:n, :], hsig[0:n, :], oml_bc[0:n, :], ALU.mult, [hsig, oml_bc], [hsig])
                    self.tt("dve", hsig[0:n, :], hsig[0:n, :], lb_bc[0:n, :], ALU.add, [hsig, lb_bc], [hsig])
                    self.act(hlogf[0:n, :], hsig[0:n, :], AF.Ln, [hsig], [hlogf])
                    self.ts("pool", hkk[0:n, :], hsig[0:n, :], -1.0, 1.0, ALU.mult, ALU.add, [hsig], [hkk])
                    b4 = bank[4]
                    self.mm(b4[0:n, 0:256], C["c_triblk"][0:n, 0:n], hlogf[0:n, :], True, True, [C["c_triblk"], hlogf], [b4])
                    b6 = bank[6]
                    self.mm(b6[0:n, 0:256], C["c_blkones"][0:n, 0:n], hlogf[0:n, :], True, True, [C["c_blkones"], hlogf], [b6])
                    self.act(heb[0:n, :], b4[0:n, 0:256], AF.Exp, [], [heb, b4])
                    self.act(henb[0:n, :], b4[0:n, 0:256], AF.Exp, [], [henb, b4], scale=-1.0)
                    self.act(hebe[0:n, :], b6[0:n, 0:256], AF.Exp, [], [hebe, b6])
                    self.tt("dve", hqd[0:n, :], hq_, heb[0:n, :], ALU.mult, [ZT, heb], [hqd])
                    self.tt("dve", hkdf[0:n, :], hkk[0:n, :], henb[0:n, :], ALU.mult, [hkk, henb], [hkdf])
                    self.cp("pool", hkd[0:n, :], hkdf[0:n, :], [hkdf], [hkd])
                    self.tt("dve", hke[0:n, :], hkdf[0:n, :], hebe[0:n, :], ALU.mult, [hkdf, hebe], [hke])
                    self.cp("pool", hvb[0:n, :], hv_, [ZT], [hvb])
                    self.sigm(hsg[0:n, :], hg_, [ZT], [hsg])
                    self.tt("pool", hsg[0:n, :], hsg[0:n, :], hg_, ALU.mult, [hsg, ZT], [hsg])
                    b7 = bank[7]
                    for h in range(4):
                        self.mm(b7[0:64, h * 4:h * 4 + nch], hlogf[0:n, h * 64:(h + 1) * 64], C["c_chunkind"][0:n, 0:nch], True, True,
                                [hlogf, C["c_chunkind"]], [b7])
                    self.act(hdec[:, :, 0:nch], b7[0:64, 0:16].rearrange("p (a c) -> p a c", a=4)[:, :, 0:nch], AF.Exp, [], [hdec, b7])
                    b5v = b5.t[:].bitcast(BF16)
                    for h in range(4):
                        self.tr(b5v[0:64, h * 128:h * 128 + n], hqd[0:n, h * 64:(h + 1) * 64], identb[0:n, 0:n], [hqd, identb], [b5])
                        self.tr(b5v[0:64, 512 + h * 128:512 + h * 128 + n], hkd[0:n, h * 64:(h + 1) * 64], identb[0:n, 0:n], [hkd, identb], [b5])
                    b5q = b5v[0:64, 0:512].rearrange("p (a t) -> p a t", a=4)
                    self.cp("act", hqdT[:, :, 0:n], b5q[:, :, 0:n], [], [hqdT, b5])
                    self.cp("act", hkdT[:, :, 0:n], b5v[0:64, 512:1024].rearrange("p (a t) -> p a t", a=4)[:, :, 0:n], [], [hkdT, b5])
                    for c in range(nch):
                        cn = min(32, n - 32 * c)
                        self.cp("pool", hqm[c][:, :, 32 * c:32 * c + cn], hqdT[:, :, 32 * c:32 * c + cn], [hqdT], [hqm[c]])
                        if c % 2 == 0:
                            self.ts("dve", hkem[c][0:n, :], hke[0:n, :], C["c_chunkind"][0:n, c:c + 1], None, ALU.mult, None,
                                    [hke, C["c_chunkind"]], [hkem[c]])
                        else:
                            self.act(hkem[c][0:n, :], hke[0:n, :], AF.Copy, [hke, C["c_chunkind"]], [hkem[c]], scale=C["c_chunkind"][0:n, c:c + 1])
                    for h in range(4):
                        self.mm(b6[0:n, h * 128:h * 128 + n], hkdT[:, h, 0:n], hqdT[:, h, 0:n], True, True, [hkdT, hqdT], [b6])
                    self.tt("dve", hattm[0:n, :, 0:n], b6[0:n, :].rearrange("p (h t) -> p h t", h=4)[:, :, 0:n],
                            C["hgmask"][0:n, 0:n].unsqueeze(1).broadcast_to([n, 4, n]), ALU.mult, [C["hgmask"]], [hattm, b6])
                    for h in range(4):
                        bu = bank[2 + h // 2]
                        for c in range(nch):
                            col = ((h % 2) * 4 + c) * 64
                            self.mm(bu[0:64, col:col + 64], hkem[c][0:n, h * 64:(h + 1) * 64], hvb[0:n, h * 64:(h + 1) * 64], True, True,
                                    [hkem[c], hvb], [bu])
                    for h in range(4):
                        self.mm(b4[0:n, h * 64:(h + 1) * 64], hattm[0:n, h, 0:n], hvb[0:n, h * 64:(h + 1) * 64], h == 0, False, [hattm, hvb], [b4],
                                skip_group_check=True)
                    for c in range(nch):
                        for h in range(4):
                            bu = bank[2 + h // 2]
                            gidx = (j - 1) * 4 + c + 1 if j > 0 else 0
                            slot_prev = Sbf[h][(gidx - 1) % 2]
                            if gidx > 0:
                                self.mm(b4[0:n, h * 64:(h + 1) * 64], hqm[c][:, h, 0:n], slot_prev[:, :], False, True, [hqm[c], slot_prev], [b4],
                                        skip_group_check=True)
                            col = ((h % 2) * 4 + c) * 64
                            self.stt("dve", Sst[h][:], Sst[h][:], hdec[:, h, c:c + 1], bu[0:64, col:col + 64], ALU.mult, ALU.add,
                                     [Sst[h], hdec], [Sst[h], bu])
                            slot = Sbf[h][gidx % 2]
                            self.cp("pool", slot[:], Sst[h][:], [Sst[h]], [slot])
                    self.tt("dve", YS[0:n, 256:512], b4[0:n, 0:256], hsg[0:n, :], ALU.mult, [hsg], [YS, b4])
                    P.dma("sp", S["Y"][p0:p0 + n, 0:512], YS[0:n, :], dsy[r], reads=[YS], writes=[Buf()])
                fillpop()
                if "mla" in steps:
                    cq_ = ZT[0:n, 1024:1216]; ckv_ = ZT[0:n, 1216:1344]; kr_ = ZT[0:n, 1344:1376]
                    P.op("dve", lambda e: e.memset(mss[:], 0.0), writes=[mss])
                    self.act(mjunk[0:n, 0:192], cq_, AF.Square, [ZT, mss], [mjunk, mss], accum_out=mss[0:n, 0:1])
                    self.act(mjunk[0:n, 0:128], ckv_, AF.Square, [ZT, mss], [mjunk, mss], accum_out=mss[0:n, 1:2])
                    self.rsq(mss[0:n, 2:3], mss[0:n, 0:1], 1.0 / 192, [mss])
                    self.rsq(mss[0:n, 3:4], mss[0:n, 1:2], 1.0 / 128, [mss])
                    self.stt("dve", cqn[0:n, :], cq_, mss[0:n, 2:3], gq_bc[0:n, :], ALU.mult, ALU.mult, [ZT, mss, gq_bc], [cqn])
                    self.stt("dve", ckvn[0:n, :], ckv_, mss[0:n, 3:4], gkv_bc[0:n, :], ALU.mult, ALU.mult, [ZT, mss, gkv_bc], [ckvn])
                    self.tr(b5v[:, 0:n], cqn[0:n, 0:128], identb[0:n, 0:n], [cqn, identb], [b5])
                    self.tr(b5v[0:64, 128:128 + n], cqn[0:n, 128:192], identb[0:n, 0:n], [cqn, identb], [b5])
                    self.tr(b5v[:, 256:256 + n], ckvn[0:n, 0:128], identb[0:n, 0:n], [ckvn, identb], [b5])
                    self.cp("act", cqT[:, 0, 0:n], b5v[:, 0:n], [], [cqT, b5])
                    self.cp("act", cqT[0:64, 1, 0:n], b5v[0:64, 128:128 + n], [], [cqT, b5])
                    self.cp("act", ckvT[:, 0:n], b5v[:, 256:256 + n], [], [ckvT, b5])
                    self.mm(b6[0:n, 0:384], cqT[:, 0, 0:n], wuq[:, 0, :], True, False, [cqT, wuq], [b6])
                    self.mm(b6[0:n, 0:384], cqT[0:64, 1, 0:n], wuq[0:64, 1, :], False, True, [cqT, wuq], [b6])
                    self.mm(b7[0:n, 0:512], ckvT[:, 0:n], wukv[:, :], True, True, [ckvT, wukv], [b7])
                    self.cp("act", qf[0:n, :, :], b6[0:n, 0:384].rearrange("p (h c) -> p h c", h=4), [], [qf, b6])
                    b7h = b7[0:n, 0:512].rearrange("p (h c) -> p h c", h=4)
                    self.cp("dve", kf[0:n, :, 0:64], b7h[:, :, 0:64], [], [kf, b7])
                    VA = vaug[r]
                    self.cp("act", VA[0:n, :, 0:64], b7h[:, :, 64:128], [], [VA, b7])
                    self.cp("pool", kf[0:n, :, 64:96], kr_.unsqueeze(1).broadcast_to([n, 4, 32]), [ZT], [kf])
                    for (src, gbc, dst, col) in [(qf, gqn_bc, qb, 4), (kf, gkn_bc, kb, 8)]:
                        self.act(mjunk[0:n, 0:384], src[0:n, :, :].rearrange("p h c -> p (h c)"), AF.Square, [src], [mjunk])
                        P.op("dve", lambda e, n=n, col=col: e.tensor_reduce(out=mss[0:n, col:col + 4], in_=mjunk[0:n, 0:384].rearrange("p (h c) -> p h c", h=4),
                                                                        axis=AX.X, op=ALU.add), reads=[mjunk], writes=[mss])
                        self.rsq(mss[0:n, col:col + 4], mss[0:n, col:col + 4], 1.0 / 96, [mss])
                        self.tt("dve", src[0:n, :, :], src[0:n, :, :], mss[0:n, col:col + 4].unsqueeze(2).broadcast_to([n, 4, 96]), ALU.mult, [src, mss], [src])
                        self.tt("pool", src[0:n, :, :], src[0:n, :, :], gbc[0:n, :].unsqueeze(1).broadcast_to([n, 4, 96]), ALU.mult, [src, gbc], [src])
                        cosb = CS[0:n, 0:16].unsqueeze(1).broadcast_to([n, 4, 16])
                        sinb = CS[0:n, 16:32].unsqueeze(1).broadcast_to([n, 4, 16])
                        t1 = src[0:n, :, 64:80]; t2 = src[0:n, :, 80:96]
                        self.tt("dve", rt[0:n, :, 0, :], t1, cosb, ALU.mult, [src, CS], [rt])
                        self.tt("pool", rt[0:n, :, 1, :], t2, sinb, ALU.mult, [src, CS], [rt])
                        self.tt("dve", rt[0:n, :, 2, :], t1, sinb, ALU.mult, [src, CS], [rt])
                        self.tt("pool", rt[0:n, :, 3, :], t2, cosb, ALU.mult, [src, CS], [rt])
                        self.cp("act", dst[0:n, :, 0:64], src[0:n, :, 0:64], [src], [dst])
                        self.tt("dve", dst[0:n, :, 64:80], rt[0:n, :, 0, :], rt[0:n, :, 1, :], ALU.subtract, [rt], [dst])
                        self.tt("dve", dst[0:n, :, 80:96], rt[0:n, :, 2, :], rt[0:n, :, 3, :], ALU.add, [rt], [dst])
                    QK = qkT[r]
                    for h in range(4):
                        self.tr(b5v[0:96, h * 128:h * 128 + n], qb[0:n, h, :], identb[0:n, 0:n], [qb, identb], [b5])
                        self.tr(b5v[0:96, (4 + h) * 128:(4 + h) * 128 + n], kb[0:n, h, :], identb[0:n, 0:n], [kb, identb], [b5])
                    self.cp("act", QK[:, :, 0:n], b5v[0:96, :].rearrange("p (a t) -> p a t", a=8)[:, :, 0:n], [], [QK, b5])
                    P.dma("sp", S["QTc"][:, :, p0:p0 + n].rearrange("h d t -> d h t"), QK[:, 0:4, 0:n], dsq[r], reads=[QK], writes=[Buf()])
                    P.dma("sp", S["KTc"][:, :, p0:p0 + n].rearrange("h d t -> d h t"), QK[:, 4:8, 0:n], dsq[r], reads=[QK], writes=[Buf()], newgroup=False)
                    P.dma("sp", S["Vc"][:, 0:n, j, :].rearrange("h p c -> p h c"), VA[0:n, :, :], dsq[r], reads=[VA], writes=[Buf()], newgroup=False)
                fillpop()
                if "fox" in steps:
                    fq_ = ZT[0:n, 1376:1632]; fk_ = ZT[0:n, 1632:1888]; fv_ = ZT[0:n, 1888:2144]; ff_ = ZT[0:n, 2144:2148]
                    for (src, gbc, dst, col) in [(fq_, fgq_bc, fqb, 12), (fk_, fgk_bc, fkb, 12)]:
                        self.act(mjunk[0:n, 0:256], src, AF.Square, [ZT], [mjunk])
                        P.op("dve", lambda e, n=n, col=col: e.tensor_reduce(out=mss[0:n, col:col + 4], in_=mjunk[0:n, 0:256].rearrange("p (h c) -> p h c", h=4),
                                                                        axis=AX.X, op=ALU.add), reads=[mjunk], writes=[mss])
                        self.rsq(mss[0:n, col:col + 4], mss[0:n, col:col + 4], 1.0 / 64, [mss])
                        self.tt("dve", fqf[0:n, :, :], src.rearrange("p (h c) -> p h c", h=4), mss[0:n, col:col + 4].unsqueeze(2).broadcast_to([n, 4, 64]), ALU.mult, [ZT, mss], [fqf])
                        self.tt("pool", dst[0:n, :].rearrange("p (h c) -> p h c", h=4), fqf[0:n, :, :], gbc[0:n, :].unsqueeze(1).broadcast_to([n, 4, 64]), ALU.mult, [fqf, gbc], [dst])
                    FV = fvaug[r]
                    self.cp("act", FV[0:n, :, 0:64], fv_.rearrange("p (h c) -> p h c", h=4), [ZT], [FV])
                    FQK = fqkT[r]
                    for hp in range(2):
                        self.tr(b5v[:, hp * 128:hp * 128 + n], fqb[0:n, hp * 128:(hp + 1) * 128], identb[0:n, 0:n], [fqb, identb], [b5])
                        self.tr(b5v[:, (2 + hp) * 128:(2 + hp) * 128 + n], fkb[0:n, hp * 128:(hp + 1) * 128], identb[0:n, 0:n], [fkb, identb], [b5])
                    self.cp("act", FQK[:, :, 0:n], b5v[:, 0:512].rearrange("p (a t) -> p a t", a=4)[:, :, 0:n], [], [FQK, b5])
                    P.dma("sp", S["QTd"][:, :, p0:p0 + n].rearrange("a d t -> d a t"), FQK[:, 0:2, 0:n], dsq[r], reads=[FQK], writes=[Buf()], newgroup=False)
                    P.dma("sp", S["KTd"][:, :, p0:p0 + n].rearrange("a d t -> d a t"), FQK[:, 2:4, 0:n], dsq[r], reads=[FQK], writes=[Buf()], newgroup=False)
                    P.dma("sp", S["Vd"][:, 0:n, j, :].rearrange("h p c -> p h c"), FV[0:n, :, :], dsq[r], reads=[FV], writes=[Buf()], newgroup=False)
                    P.commit(dsq[r], [qkT[r], vaug[r], FQK, FV])
                    self.tt("dve", flog[0:n, :], ff_, fbf_bc[0:n, :], ALU.add, [ZT, fbf_bc], [flog])
                    self.act(flog[0:n, :], flog[0:n, :], AF.Exp, [flog], [flog], scale=-1.0)
                    self.act(flog[0:n, :], flog[0:n, :], AF.Ln, [flog], [flog], bias=1.0)
                    self.ts("dve", flog[0:n, :], flog[0:n, :], -1.0, None, ALU.mult, None, [flog], [flog])
                    if j == 0:
                        self.mm(b6[0:n, 0:4], C["c_trifull"][0:n, 0:n], flog[0:n, :], True, True, [C["c_trifull"], flog], [b6])
                    else:
                        pn = self.tiles[j - 1][1]
                        selp = C["c_sel15"] if pn == 16 else C["c_sel127"]
                        self.mm(b6[0:n, 0:4], C["c_trifull"][0:n, 0:n], flog[0:n, :], True, False, [C["c_trifull"], flog], [b6])
                        self.mm(b6[0:n, 0:4], selp[0:pn, 0:n], call[0:pn, j - 1, :], False, True, [selp, call], [b6])
                    self.cp("dve", call[0:n, j, :], b6[0:n, 0:4], [], [call, b6])
                while fill:
                    fillpop()
            load(0)
            if ntl > 1:
                load(1)
            zpro(0)
            for g_ in zgroups(0):
                g_()
            for j in range(ntl):
                if j + 2 < ntl:
                    load(j + 2)
                fill = []
                if j + 1 < ntl:
                    zpro(j + 1)
                    fill = zgroups(j + 1)
                chains(j, fill)
            evs = [(d.key, d.cnt) for d in dsy + dsq if d.cnt > 0]
            self.barrier(evs)

    def barrier(self, evs=()):
        P = self.P
        evs = list(evs)
        for e in ("pe", "act", "dve", "pool"):
            if P.cnt[e] > 0:
                evs.append((("E", e), P.cnt[e]))
        for d in P.all_dsems:
            if d.cnt > 0:
                evs.append((d.key, d.cnt))
        for e in ("sp", "pool", "act", "pe", "dve"):
            P.wait_all(e, evs)

    def phase_1b(self, l):
        P, I, S, C, nc = self.P, self.I, self.S, self.C, self.nc
        bank = self.bank
        NT, T = self.NT, self.T
        call = self.call
        tiles = self.tiles
        WMAX = 4

        def make_blocks(W):
            blocks = [[0]]
            i = 1
            while i < len(tiles):
                blocks.append(list(range(i, min(i + W, len(tiles)))))
                i += W
            return blocks
        blocks_of = {"mla": make_blocks(4), "fox": make_blocks(4)}
        fsub = []
        fsub_of = {}
        for bi_, blk_ in enumerate(blocks_of["fox"]):
            for hi_ in range(0, len(blk_), 2):
                fsub_of[(bi_, hi_ // 2)] = len(fsub)
                fsub.append((bi_, hi_ // 2, blk_[hi_:hi_ + 2]))
        with contextlib.ExitStack() as st:
            sb = lambda name, shape, dt=F32: self.sb(st, "b_" + name, shape, dt)
            fblocks = fsub
            b7 = bank[7]
            for bi, (_b, _h, blk) in enumerate(fblocks):
                i = blk[0]
                n = tiles[i][1]
                if len(blk) > 1:
                    sel = C["c_sel127"]
                else:
                    sel = C["c_sel8"] if n == 16 else C["c_sel64"]
                self.mm(b7[:, bi * 4:(bi + 1) * 4], sel[0:n, :], call[0:n, i, :], True, True, [sel, call], [b7])
            cref = sb("cref", [128, len(fblocks), 4])
            self.cp("dve", cref[:].rearrange("p a b -> p (a b)"), b7[:, 0:len(fblocks) * 4], [], [cref, b7])
            negc = sb("negc", [128, 4, NT + 1])
            self.ts("dve", negc[:], call[:].rearrange("p j h -> p h j"), -1.0, None, ALU.mult, None, [call], [negc])
            bias = self.ring(st, "b_bias", [128, NT + 1], F32, 8)
            KT = self.ring(st, "b_KT", [128, T], BF16, 2)
            QT = self.ring(st, "b_QT", [128, T], BF16, 2)
            V = self.ring(st, "b_V", [128, NT + 1, 65], BF16, 2)
            dsk = [P.dsem() for _ in range(2)]
            dsv = [P.dsem() for _ in range(2)]
            LA = 3
            NSB = 4
            pt = self.ring(st, "b_pt", [128, WMAX * 128], BF16, NSB)
            oT = self.ring(st, "b_oT", [65, WMAX * 128], F32, 2)
            osb = self.ring(st, "b_osb", [128, WMAX, 64], F32, 2)
            rden = self.ring(st, "b_rden", [128, WMAX], F32, 2)
            dso = [P.dsem() for _ in range(2)]
            identf = C["c_ident"]
            units = []
            for h in range(4):
                units.append(("mla", h))
            for h in range(4):
                units.append(("fox", h))
            loaded = {}
            nload = [0]

            def load(u):
                kind, h = u
                if kind == "mla":
                    key = ("mla", h)
                else:
                    key = ("fox", h // 2)
                if key in loaded:
                    return loaded[key]
                r = nload[0] % 2
                nload[0] += 1
                if kind == "mla":
                    P.dma("sp", KT[r][0:96, :], S["KTc"][h], dsk[r], writes=[KT[r]])
                    P.dma("sp", QT[r][0:96, :], S["QTc"][h], dsk[r], writes=[QT[r]], newgroup=False)
                else:
                    P.dma("sp", KT[r][:, :], S["KTd"][h // 2], dsk[r], writes=[KT[r]])
                    P.dma("sp", QT[r][:, :], S["QTd"][h // 2], dsk[r], writes=[QT[r]], newgroup=False)
                P.commit(dsk[r], [KT[r], QT[r]])
                loaded[key] = r
                return r
            nv = [0]

            def loadv(u):
                kind, h = u
                r = nv[0] % 2
                nv[0] += 1
                src = S["Vc"] if kind == "mla" else S["Vd"]
                P.dma("sp", V[r][:], src[h], dsv[r], writes=[V[r]])
                return r
            ring_of = {}
            steps = []
            ustart = {}
            uend = {}
            gblk = {}
            ng = 0
            for ui, (kind, h) in enumerate(units):
                ustart[ui] = len(steps)
                for bi, blk in enumerate(blocks_of[kind]):
                    gblk[(ui, bi)] = ng
                    ng += 1
                    for jj in range(blk[-1] + 1):
                        steps.append((ui, bi, jj))
                uend[ui] = len(steps)

            def geom(s):
                ui, bi, jj = steps[s]
                kind, h = units[ui]
                blk = blocks_of[kind][bi]
                i0, i1 = blk[0], blk[-1]
                a = max(i0, jj)
                qb0 = tiles[i0][0]
                q0 = tiles[a][0]
                q1 = tiles[i1][0] + tiles[i1][1]
                return ui, bi, jj, kind, h, blk, i0, i1, a, qb0, q0, q1

            def emit_qk(s):
                ui, bi, jj, kind, h, blk, i0, i1, a, qb0, q0, q1 = geom(s)
                fox = kind == "fox"
                if ui not in ring_of:
                    ring_of[ui] = (load(units[ui]), loadv(units[ui]))
                r, rv = ring_of[ui]
                if s - ustart[ui] == min(LA + 1, uend[ui] - ustart[ui] - 1) and ui + 1 < len(units) and (ui + 1) not in ring_of:
                    ring_of[ui + 1] = (load(units[ui + 1]), loadv(units[ui + 1]))
                if fox and jj == 0:
                    for hi in range((len(blk) + 1) // 2):
                        si = fsub_of[(bi, hi)]
                        tl = fsub[si][2][-1]
                        B_ = bias[(gblk[(ui, bi)] % 4) * 2 + hi]
                        self.ts("dve", B_[:, 0:tl + 1], negc[:, h, 0:tl + 1], cref[:, si, h:h + 1], None, ALU.add, None, [negc, cref], [B_])
                rows = slice(0, 96) if not fox else slice((h % 2) * 64, (h % 2) * 64 + 64)
                k0, kn = tiles[jj]
                sbk = bank[s % NSB]
                self.mm(sbk[0:kn, 0:q1 - q0], KT[r][rows, k0:k0 + kn], QT[r][rows, q0:q1], True, True, [KT[r], QT[r]], [sbk])

            def emit_rest(s):
                ui, bi, jj, kind, h, blk, i0, i1, a, qb0, q0, q1 = geom(s)
                fox = kind == "fox"
                r, rv = ring_of[ui]
                k0, kn = tiles[jj]
                wq = q1 - q0
                scale = (96.0 if not fox else 64.0) ** -0.5
                mask = C["foxmask"] if fox else C["mlamask"]
                ycol = (512 if not fox else 768) + h * 64
                sbk = bank[s % NSB]
                PT = pt[s % NSB]
                g = gblk[(ui, bi)]
                ob = bank[4 + (g % 2)]
                if fox:
                    for hi in range((len(blk) + 1) // 2):
                        sub = fsub[fsub_of[(bi, hi)]][2]
                        if sub[-1] < a:
                            continue
                        lo = max(sub[0], a)
                        c_lo = tiles[lo][0] - q0
                        c_hi = tiles[sub[-1]][0] + tiles[sub[-1]][1] - q0
                        B_ = bias[(g % 4) * 2 + hi]
                        self.act(PT[0:kn, c_lo:c_hi], sbk[0:kn, c_lo:c_hi], AF.Exp, [B_], [PT, sbk], scale=scale, bias=B_[0:kn, jj:jj + 1])
                else:
                    self.act(PT[0:kn, 0:wq], sbk[0:kn, 0:wq], AF.Exp, [], [PT, sbk], scale=scale)
                if jj >= i0:
                    na = tiles[jj][1]
                    self.tt("pool", PT[0:kn, 0:na], PT[0:kn, 0:na], mask[0:kn, 0:na], ALU.mult, [PT, mask], [PT])
                self.mm(ob[0:65, q0 - qb0:q1 - qb0], V[rv][0:kn, jj, :], PT[0:kn, 0:wq], jj == 0, jj == i1, [PT, V[rv]], [ob])
                if jj == i1:
                    ro = g % 2
                    wb = q1 - qb0
                    nsub = len(blk)
                    n = tiles[i0][1]
                    OT = oT[ro]
                    self.cp("dve", OT[0:65, 0:wb], ob[0:65, 0:wb], [], [OT, ob])
                    tb = bank[6 + (g % 2)]
                    for x in range(nsub):
                        self.tr(tb[0:n, x * 65:(x + 1) * 65], OT[0:65, x * 128:x * 128 + n], identf[0:65, 0:65], [OT, identf], [tb])
                    tbv = tb[0:n, 0:nsub * 65].rearrange("p (x c) -> p x c", c=65)
                    P.op("dve", lambda e, tbv=tbv, n=n, ro=ro, nsub=nsub: e.reciprocal(out=rden[ro][0:n, 0:nsub], in_=tbv[:, :, 64]),
                         reads=[], writes=[rden[ro], tb])
                    self.tt("dve", osb[ro][0:n, 0:nsub, :], tbv[:, :, 0:64], rden[ro][0:n, 0:nsub].unsqueeze(2).broadcast_to([n, nsub, 64]),
                            ALU.mult, [rden[ro]], [osb[ro], tb])
                    if nsub == 1:
                        P.dma("sp", S["Y"][qb0:qb0 + n, ycol:ycol + 64], osb[ro][0:n, 0, :], dso[ro], reads=[osb[ro]], writes=[Buf()])
                    else:
                        P.dma("sp", S["Y"][qb0:qb0 + nsub * 128, ycol:ycol + 64].rearrange("(x p) c -> p x c", p=128), osb[ro][:, 0:nsub, :],
                              dso[ro], reads=[osb[ro]], writes=[Buf()])
            for s in range(len(steps) + LA):
                if s < len(steps):
                    emit_qk(s)
                if s >= LA:
                    emit_rest(s - LA)
            self.barrier([(d.key, d.cnt) for d in dso if d.cnt > 0])

    def phase_1c(self, l, hsrc):
        P, I, S, C, nc = self.P, self.I, self.S, self.C, self.nc
        bank = self.bank
        NT, T = self.NT, self.T
        moe = (l % 2 == 1)
        with contextlib.ExitStack() as st:
            sb = lambda name, shape, dt=F32: self.sb(st, "c_" + name, shape, dt)
            dsw = P.dsem()
            wout = sb("wout", [128, 8, D], BF16)
            wv = I["w_out"][l].rearrange("(k p) c -> p k c", p=128)
            for k in range(8):
                P.dma("pool", wout[:, k, :], wv[:, k, :], dsw, writes=[wout], newgroup=False)
            og_bc = sb("og_bc", [128, D])
            P.dma("sp", og_bc[:], I["og"][l].partition_broadcast(128), dsw, writes=[og_bc], newgroup=False)
            g2T = sb("g2T", [128, 8])
            P.dma("sp", g2T[:], I["g2T"][l], dsw, writes=[g2T], newgroup=False)
            if moe:
                wr = sb("wr", [128, 8, NEXP])
                P.dma("sp", wr[:], I["moe_wr"].rearrange("(k p) e -> p k e", p=128), dsw, writes=[wr], newgroup=False)
                P.commit(dsw, [wr])
            P.commit(dsw, [wout, og_bc, g2T])
            R2 = 2
            R3 = 3
            Yt = self.ring(st, "c_Yt", [128, D], F32, R3)
            Ht = self.ring(st, "c_Ht", [128, D], F32, R3)
            dsl = [P.dsem() for _ in range(R3)]
            junk = sb("junk", [128, D])
            ss = self.ring(st, "c_ss", [128, 16], F32, R2)
            yn = sb("yn", [128, D], BF16)
            ynT_r = self.ring(st, "c_ynT", [128, 8, 128], BF16, R2)
            junkA = sb("junkA", [128, D])
            ssA = self.ring(st, "c_ssA", [128, 4], F32, R2)
            hm = self.ring(st, "c_hm", [128, D], F32, R2)
            dsh = [P.dsem() for _ in range(R2)]
            u2b = sb("u2b", [128, D], BF16)
            u2f = sb("u2f", [128, D], F32)
            u2Tf = sb("u2Tf", [128, 8, 128], F32)
            u2T = self.ring(st, "c_u2T", [128, 8, 128], BF16, R2)
            dsu = [P.dsem() for _ in range(R2)]
            gl = sb("gl", [128, 8]); gl2 = sb("gl2", [128, 8]); gm1 = sb("gm1", [128, 8]); gm2 = sb("gm2", [128, 8])
            gsc = sb("gsc", [128, 8])
            gT = self.ring(st, "c_gT", [128, 8], F32, R2)
            identb = C["identb"]; identf = C["c_ident"]

            def load(jn):
                pp, nn = self.tiles[jn]
                rr = jn % R3
                P.dma("sp", Yt[rr][0:nn, :], S["Y"][pp:pp + nn, :], dsl[rr], writes=[Yt[rr]])
                P.dma("sp", Ht[rr][0:nn, :], hsrc[pp:pp + nn, :], dsl[rr], writes=[Ht[rr]], newgroup=False)
                P.commit(dsl[rr], [Yt[rr], Ht[rr]])
            def stage_a(j):
                p0, n = self.tiles[j]
                Y_ = Yt[j % R3]
                SA = ssA[j % R2]
                ynT = ynT_r[j % R2]
                self.act(junkA[0:n, :], Y_[0:n, :], AF.Square, [Y_], [junkA])
                P.op("dve", lambda e, n=n, SA=SA: e.tensor_reduce(out=SA[0:n, 0:4], in_=junkA[0:n, :].rearrange("p (g c) -> p g c", g=4), axis=AX.X, op=ALU.add),
                     reads=[junkA], writes=[SA])
                self.rsq(SA[0:n, 0:4], SA[0:n, 0:4], 1.0 / 256, [SA])
                for g in range(4):
                    self.stt("dve", yn[0:n, g * 256:(g + 1) * 256], Y_[0:n, g * 256:(g + 1) * 256], SA[0:n, g:g + 1],
                             og_bc[0:n, g * 256:(g + 1) * 256], ALU.mult, ALU.mult, [Y_, SA, og_bc], [yn])
                b0 = bank[0]
                b0v = b0.t[:].bitcast(BF16)
                for k in range(8):
                    self.tr(b0v[:, k * 128:k * 128 + n], yn[0:n, k * 128:(k + 1) * 128], identb[0:n, 0:n], [yn, identb], [b0])
                self.cp("act", ynT[:, :, 0:n], b0v[:, :].rearrange("p (k t) -> p k t", k=8)[:, :, 0:n], [], [ynT, b0])
            ntl = len(self.tiles)
            for jn in range(min(3, ntl)):
                load(jn)
            stage_a(0)
            for j, (p0, n) in enumerate(self.tiles):
                r = j % R2
                if j + 1 < ntl:
                    stage_a(j + 1)
                Y_, H_, SS, HM, U2T = Yt[j % R3], Ht[j % R3], ss[r], hm[r], u2T[r]
                ynT = ynT_r[r]
                for c in range(2):
                    bo = bank[2 + c]
                    for k in range(8):
                        self.mm(bo[0:n, 0:512], ynT[:, k, 0:n], wout[:, k, c * 512:(c + 1) * 512], k == 0, k == 7, [ynT, wout], [bo])
                    self.tt("dve", HM[0:n, c * 512:(c + 1) * 512], bo[0:n, 0:512], H_[0:n, c * 512:(c + 1) * 512], ALU.add, [H_], [HM, bo])
                P.dma("sp", S["hmid"][p0:p0 + n, :], HM[0:n, :], dsh[r], reads=[HM], writes=[Buf()])
                P.op("dve", lambda e, SS=SS: e.memset(SS[:, 8:9], 0.0), writes=[SS])
                self.act(junk[0:n, :], HM[0:n, :], AF.Square, [HM, SS], [junk, SS], accum_out=SS[0:n, 8:9])
                self.rsq(SS[0:n, 9:10], SS[0:n, 8:9], 1.0 / D, [SS])
                b1 = bank[1]
                if not moe:
                    self.act(u2b[0:n, :], HM[0:n, :], AF.Copy, [HM, SS], [u2b], scale=SS[0:n, 9:10])
                    b1v = b1.t[:].bitcast(BF16)
                    for k in range(8):
                        self.tr(b1v[:, k * 128:k * 128 + n], u2b[0:n, k * 128:(k + 1) * 128], identb[0:n, 0:n], [u2b, identb], [b1])
                    self.tt("dve", U2T[:, :, 0:n], b1v[:, :].rearrange("p (k t) -> p k t", k=8)[:, :, 0:n],
                            g2T[:, :].unsqueeze(2).broadcast_to([128, 8, n]), ALU.mult, [g2T], [U2T, b1])
                else:
                    self.act(u2f[0:n, :], HM[0:n, :], AF.Copy, [HM, SS], [u2f], scale=SS[0:n, 9:10])
                    for half in range(2):
                        bb = bank[5 + half]
                        for kk in range(4):
                            k = half * 4 + kk
                            self.tr(bb[:, kk * 128:kk * 128 + n], u2f[0:n, k * 128:(k + 1) * 128], identf[0:n, 0:n], [u2f, identf], [bb])
                        self.tt("dve", u2Tf[:, half * 4:half * 4 + 4, 0:n], bb[:, :].rearrange("p (k t) -> p k t", k=4)[:, :, 0:n],
                                g2T[:, half * 4:half * 4 + 4].unsqueeze(2).broadcast_to([128, 4, n]), ALU.mult, [g2T], [u2Tf, bb])
                    self.cp("pool", U2T[:, :, 0:n], u2Tf[:, :, 0:n], [u2Tf], [U2T])
                    b7 = bank[7]
                    for k in range(8):
                        self.mm(b7[0:n, 0:NEXP], u2Tf[:, k, 0:n], wr[:, k, :], k == 0, k == 7, [u2Tf, wr], [b7])
                    self.cp("act", gl[0:n, :], b7[0:n, 0:NEXP], [], [gl, b7])
                    P.op("dve", lambda e, n=n: e.tensor_reduce(out=gsc[0:n, 0:1], in_=gl[0:n, :], axis=AX.X, op=ALU.max), reads=[gl], writes=[gsc])
                    self.ts("dve", gm1[0:n, :], gl[0:n, :], gsc[0:n, 0:1], None, ALU.is_equal, None, [gl, gsc], [gm1])
                    self.stt("dve", gl2[0:n, :], gm1[0:n, :], -1e30, gl[0:n, :], ALU.mult, ALU.add, [gm1, gl], [gl2])
                    P.op("dve", lambda e, n=n: e.tensor_reduce(out=gsc[0:n, 1:2], in_=gl2[0:n, :], axis=AX.X, op=ALU.max), reads=[gl2], writes=[gsc])
                    self.ts("dve", gm2[0:n, :], gl2[0:n, :], gsc[0:n, 1:2], None, ALU.is_equal, None, [gl2, gsc], [gm2])
                    self.tt("dve", gsc[0:n, 2:3], gsc[0:n, 1:2], gsc[0:n, 0:1], ALU.subtract, [gsc], [gsc])
                    self.act(gsc[0:n, 3:4], gsc[0:n, 2:3], AF.Exp, [gsc], [gsc])
                    self.ts("dve", gsc[0:n, 4:5], gsc[0:n, 3:4], 1.0, None, ALU.add, None, [gsc], [gsc])
                    P.op("dve", lambda e, n=n: e.reciprocal(out=gsc[0:n, 4:5], in_=gsc[0:n, 4:5]), reads=[gsc], writes=[gsc])
                    self.tt("dve", gsc[0:n, 5:6], gsc[0:n, 3:4], gsc[0:n, 4:5], ALU.mult, [gsc], [gsc])
                    gate = gT[r]
                    self.ts("dve", gate[0:n, :], gm1[0:n, :], gsc[0:n, 4:5], None, ALU.mult, None, [gm1, gsc], [gate])
                    self.stt("dve", gate[0:n, :], gm2[0:n, :], gsc[0:n, 5:6], gate[0:n, :], ALU.mult, ALU.add, [gm2, gsc, gate], [gate])
                    P.dma("sp", S["gate"][p0:p0 + n, :], gate[0:n, :], dsu[r], reads=[gate], writes=[Buf()])
                P.dma("sp", S["u2T"].rearrange("(k p) t -> p k t", p=128)[:, :, p0:p0 + n], U2T[:, :, 0:n], dsu[r], reads=[U2T], writes=[Buf()],
                      newgroup=not moe)
                P.commit(dsu[r], [U2T, gT[r]])
                if j + 3 < ntl:
                    load(j + 3)
            self.barrier([(d.key, d.cnt) for d in dsh + dsu if d.cnt > 0])

    def ffn_core(self, st, tok_sets, wsets, out_fn, blend, gated):
        P, I, S, C, nc = self.P, self.I, self.S, self.C, self.nc
        bank = self.bank
        sb = lambda name, shape, dt=F32: self.sb(st, "f_" + name, shape, dt)
        TSMAX = max(sum(n for (_, n) in ts) for ts in tok_sets)
        NTT = max(len(ts) for ts in tok_sets)
        u2 = sb("u2", [128, 8, TSMAX], BF16)
        acc = sb("acc", [128, NTT, D], F32)
        dsx = P.dsem(); dsa = P.dsem(); dsg = P.dsem()
        SW = 256
        if blend:
            u2B = self.ring(st, "f_u2B", [128, 8, SW], BF16, 2)
            dsxb = [P.dsem() for _ in range(2)]
            accB = self.ring(st, "f_accB", [128, D], F32, 2)
            dsb = [P.dsem() for _ in range(2)]
            selA = C["sel"][:, 0:1]; selB = C["sel"][:, 1:2]
        if gated:
            gt = sb("gt", [128, NTT, NEXP], F32)
            gtB = sb("gtB", [128, NTT, NEXP], F32)
        GW = 512
        wg = self.ring(st, "f_wg", [128, 8, GW], BF16, 2)
        wu = self.ring(st, "f_wu", [128, 8, GW], BF16, 2)
        wd = self.ring(st, "f_wd", [128, 4, D], BF16, 2)
        dsw = [P.dsem() for _ in range(2)]
        sg = self.ring(st, "f_sg", [128, 512], F32, 2)
        hid = self.ring(st, "f_hid", [128, 512], BF16, 8)
        dso = [P.dsem() for _ in range(4)]
        u2Tv = S["u2T"].rearrange("(k p) t -> p k t", p=128)
        groups = []
        for (wg_ap, wu_ap, wd_ap, F, e) in wsets:
            f0 = 0
            while f0 < F:
                fw = min(GW, F - f0)
                groups.append((wg_ap, wu_ap, wd_ap, f0, fw, e))
                f0 += fw
        gcount = 0
        hcount = 0

        def loadw(gi):
            wg_ap, wu_ap, wd_ap, f0, fw, e = groups[gi % len(groups)]
            r = gi % 2
            wgv = wg_ap.rearrange("(k p) f -> p k f", p=128)
            wuv = wu_ap.rearrange("(k p) f -> p k f", p=128)
            for k in range(0, 8, 4):
                P.dma("pool", wg[r][:, k:k + 4, 0:fw], wgv[:, k:k + 4, f0:f0 + fw], dsw[r], writes=[wg[r]], newgroup=(k == 0))
            for k in range(0, 8, 4):
                P.dma("pool", wu[r][:, k:k + 4, 0:fw], wuv[:, k:k + 4, f0:f0 + fw], dsw[r], writes=[wu[r]], newgroup=False)
            nb = fw // 128
            P.dma("pool", wd[r][:, 0:nb, :], wd_ap[f0:f0 + fw, :].rearrange("(b p) d -> p b d", p=128), dsw[r], writes=[wd[r]], newgroup=False)
            P.commit(dsw[r], [wg[r], wu[r], wd[r]])
        total_groups = len(groups) * len(tok_sets)
        loadw(0)
        nstage = 0
        for si, ts in enumerate(tok_sets):
            col = 0
            cols = []
            for ti, (rows, n) in enumerate(ts):
                cols.append(col)
                col += n
            TS = col
            if not blend:
                r00 = ts[0][0]
                for ti, (rows, n) in enumerate(ts):
                    assert rows == r00 + cols[ti]
                    P.dma("sp", acc[0:n, ti, :], S["hmid"][rows:rows + n, :], dsa, writes=[acc], newgroup=(ti == 0))
                for c0 in range(0, TS, 512):
                    cw_ = min(512, TS - c0)
                    P.dma("sp", u2[:, :, c0:c0 + cw_], u2Tv[:, :, r00 + c0:r00 + c0 + cw_], dsx, writes=[u2], newgroup=(c0 == 0))
                P.commit(dsx, [u2])
                P.commit(dsa, [acc])
            else:
                rA0, rB0 = ts[0][0]
                for ti, (rows, n) in enumerate(ts):
                    rA, rB = rows
                    assert rA == rA0 + cols[ti] and rB == rB0 + cols[ti]
                    P.dma("sp", acc[0:n, ti, :], S["hmid"][rA:rA + n, :], dsa, writes=[acc], newgroup=(ti == 0))
                    if gated:
                        P.dma("sp", gt[0:n, ti, :], S["gate"][rA:rA + n, :], dsg, writes=[gt], newgroup=(ti == 0))
                        P.dma("sp", gtB[0:n, ti, :], S["gate"][rB:rB + n, :], dsg, writes=[gtB], newgroup=False)
                P.commit(dsa, [acc])
                if gated:
                    P.commit(dsg, [gt, gtB])
                for c0 in range(0, TS, 512):
                    cw_ = min(512, TS - c0)
                    P.dma("sp", u2[:, :, c0:c0 + cw_], u2Tv[:, :, rA0 + c0:rA0 + c0 + cw_], dsx, writes=[u2], newgroup=(c0 == 0))
                P.commit(dsx, [u2])
                for ti, (rows, n) in enumerate(ts):
                    rA, rB = rows
                    ab = accB[ti % 2]
                    P.dma("sp", ab[0:n, :], S["hmid"][rB:rB + n, :], dsb[ti % 2], writes=[ab])
                    self.act(acc[0:n, ti, :], acc[0:n, ti, :], AF.Copy, [acc, C["sel"]], [acc], scale=selA[0:n, :])
                    self.stt("dve", acc[0:n, ti, :], ab[0:n, :], selB[0:n, :], acc[0:n, ti, :], ALU.mult, ALU.add, [ab, C["sel"], acc], [acc])
                for c0 in range(0, TS, SW):
                    cw_ = min(SW, TS - c0)
                    ub_ = u2B[nstage % 2]
                    P.dma("sp", ub_[:, :, 0:cw_], u2Tv[:, :, rB0 + c0:rB0 + c0 + cw_], dsxb[nstage % 2], writes=[ub_])
                    nstage += 1
                    self.act(u2[:, :, c0:c0 + cw_], u2[:, :, c0:c0 + cw_], AF.Copy, [u2, C["sel"]], [u2], scale=selA)
                    self.stt("dve", u2[:, :, c0:c0 + cw_], ub_[:, :, 0:cw_], selB, u2[:, :, c0:c0 + cw_], ALU.mult, ALU.add, [ub_, C["sel"], u2], [u2])
                if gated:
                    self.act(gt[:], gt[:], AF.Copy, [gt, C["sel"]], [gt], scale=selA)
                    self.stt("dve", gt[:], gtB[:], selB, gt[:], ALU.mult, ALU.add, [gtB, C["sel"], gt], [gt])
            chunks = []
            cur = []
            cw_ = 0
            for ti, (rows, n) in enumerate(ts):
                if cw_ + n > 512:
                    chunks.append(cur); cur = []; cw_ = 0
                cur.append(ti); cw_ += n
            if cur:
                chunks.append(cur)
            for gi_local in range(len(groups)):
                gi = gcount
                gcount += 1
                wg_ap, wu_ap, wd_ap, f0, fw, e = groups[gi_local]
                r = gi % 2
                if gi + 1 < total_groups:
                    loadw(gi + 1)
                nb = fw // 128
                for ch in chunks:
                    c0 = cols[ch[0]]
                    cw = sum(ts[ti][1] for ti in ch)
                    hslots = []
                    for fb in range(nb):
                        bg = bank[(hcount % 2) * 2]
                        bu = bank[(hcount % 2) * 2 + 1]
                        H_ = hid[hcount % 8]
                        SG = sg[hcount % 2]
                        hcount += 1
                        for k in range(8):
                            self.mm(bg[:, 0:cw], wg[r][:, k, fb * 128:(fb + 1) * 128], u2[:, k, c0:c0 + cw], k == 0, k == 7, [wg[r], u2], [bg])
                        for k in range(8):
                            self.mm(bu[:, 0:cw], wu[r][:, k, fb * 128:(fb + 1) * 128], u2[:, k, c0:c0 + cw], k == 0, k == 7, [wu[r], u2], [bu])
                        self.act(SG[:, 0:cw], bg[:, 0:cw], AF.Silu, [], [SG, bg])
                        self.tt("dve", H_[:, 0:cw], bu[:, 0:cw], SG[:, 0:cw], ALU.mult, [SG], [H_, bu])
                        hslots.append(H_)
                    for ti in ch:
                        n = ts[ti][1]
                        lc = cols[ti] - c0
                        for half in range(2):
                            bd = bank[4 + 2 * (ti % 2) + half]
                            for fb in range(nb):
                                self.mm(bd[0:n, 0:512], hslots[fb][:, lc:lc + n], wd[r][:, fb, half * 512:(half + 1) * 512], fb == 0, fb == nb - 1,
                                        [hslots[fb], wd[r]], [bd])
                            a_ = acc[0:n, ti, half * 512:(half + 1) * 512]
                            if gated:
                                self.stt("dve", a_, bd[0:n, 0:512], gt[0:n, ti, e:e + 1], a_, ALU.mult, ALU.add, [acc, gt], [acc, bd])
                            else:
                                self.tt("dve", a_, bd[0:n, 0:512], a_, ALU.add, [acc], [acc, bd])
            for ti, (rows, n) in enumerate(ts):
                dst = out_fn(si, ti)
                if dst is None:
                    continue
                ev = P.dma("sp", dst, acc[0:n, ti, :], dso[ti % 4], reads=[acc], writes=[Buf()])
                self.final_evs.append(ev)
        self.barrier([(d.key, d.cnt) for d in dso if d.cnt > 0])

    def phase_ffn(self, hdst):
        I = self.I
        T = self.T
        rows = [(r0, min(128, T - r0)) for r0 in range(0, T, 128)]
        TSN = 16
        tok_sets = [rows[i:i + TSN] for i in range(0, len(rows), TSN)]
        if len(tok_sets) > 1 and len(tok_sets[-1]) == 1:
            tok_sets[-2] = tok_sets[-2] + tok_sets[-1]
            tok_sets.pop()
        final = hdst is None

        def out_fn(si, ti):
            r0, n = tok_sets[si][ti]
            if not final:
                return hdst[r0:r0 + n, :]
            lo = max(r0, NMETA)
            return None if True else None
        with contextlib.ExitStack() as st:
            self.ffn_core(st, tok_sets, [(I["ffn_wg"], I["ffn_wu"], I["ffn_wd"], D_FF, 0)], out_fn, blend=False, gated=False)

    def phase_moe(self):
        I = self.I
        NTOK = self.moe_tokens
        ntile = NTOK // 128
        TSN = 16
        tiles_ab = [((NMETA + 128 * i, NMETA + NTOK + 128 * i), 128) for i in range(ntile)]
        tok_sets = [tiles_ab[i:i + TSN] for i in range(0, ntile, TSN)]
        out = self.out_ap

        def out_fn(si, ti):
            i = si * TSN + ti
            return out[128 * i:128 * (i + 1), :]
        wsets = [(I["moe_wg"][e], I["moe_wu"][e], I["moe_wd"][e], D_FFE, e) for e in range(NEXP)]
        with contextlib.ExitStack() as st:
            self.ffn_core(st, tok_sets, wsets, out_fn, blend=True, gated=True)


def prep_inputs(inp, b, half, NT, depth=2):
    f = lambda a: np.ascontiguousarray(np.asarray(a, dtype=np.float32))
    T = NMETA + 128 * NT
    m = {}
    m["xin"] = f(np.concatenate([inp["meta"], inp["x"][b]], axis=0))
    sel = np.zeros((128, 2), np.float32)
    sel[:, half] = 1.0
    m["sel"] = sel
    m.update(make_consts(T))
    L = depth
    colmaj = lambda v: f(np.asarray(v).reshape(L, -1, 128).transpose(0, 2, 1))
    m["w_in"] = f(inp["w_in"][:L]); m["w_out"] = f(inp["w_out"][:L])
    m["g1T"] = colmaj(inp["norm1_g"][:L]); m["g2T"] = colmaj(inp["norm2_g"][:L])
    m["og"] = f(inp["out_norm_g"][:L])
    cw = np.asarray(inp["lru_conv_w"][:L])
    m["conv_wT"] = f(cw.transpose(0, 2, 1).reshape(L, 2, 128, 4).transpose(0, 2, 1, 3))
    m["conv_b"] = colmaj(inp["lru_conv_b"][:L])
    m["lru_ba"] = colmaj(np.asarray(inp["lru_ba"][:L]).reshape(L, 256))
    m["lru_bx"] = colmaj(np.asarray(inp["lru_bx"][:L]).reshape(L, 256))
    m["lru_lam"] = colmaj(inp["lru_lambda"][:L])
    m["lru_wa"] = f(inp["lru_wa"][:L]); m["lru_wx"] = f(inp["lru_wx"][:L])
    m["hg_lb"] = f(inp["hg_lb_logits"][:2])
    for k in ["mla_gq", "mla_w_uq", "mla_gkv", "mla_w_ukv", "mla_gqn", "mla_gkn", "fox_gqn", "fox_gkn", "fox_bf"]:
        m[k] = f(inp[k][:L])
    m["ffn_wg"] = f(inp["ffn_w_gate"][0]); m["ffn_wu"] = f(inp["ffn_w_up"][0]); m["ffn_wd"] = f(inp["ffn_w_down"][0])
    if depth > 1:
        m["moe_wr"] = f(inp["moe_w_router"][0]); m["moe_wg"] = f(inp["moe_w_gate"][0])
        m["moe_wu"] = f(inp["moe_w_up"][0]); m["moe_wd"] = f(inp["moe_w_down"][0])
    return m


_CACHE = {}


def _get_program(NT):
    if NT not in _CACHE:
        b = Builder(NT, depth=2, debug=False)
        nc = b.build()
        _CACHE[NT] = (b, nc)
    return _CACHE[NT]


def kernel(**inputs):
    x = np.asarray(inputs["x"])
    Bsz, SEQ, _ = x.shape
    NT = SEQ // 128
    b, nc = _get_program(NT)
    half_tokens = b.moe_tokens
    n_cores = 8
    in_maps = []
    shared = None
    for c in range(n_cores):
        bi, half = c % Bsz, c // Bsz
        if shared is None:
            m = prep_inputs(inputs, bi, half, NT, depth=2)
            shared = m
        else:
            m = dict(shared)
            m["xin"] = np.ascontiguousarray(np.concatenate([np.asarray(inputs["meta"], np.float32), np.asarray(x[bi], np.float32)], axis=0))
            sel = np.zeros((128, 2), np.float32)
            sel[:, half] = 1.0
            m["sel"] = sel
        in_maps.append({k: v for k, v in m.items() if k in b.inputs})
    res = run_bass_kernel_spmd(nc, in_maps, core_ids=list(range(n_cores)))
    out = np.empty((Bsz, SEQ, D), np.float32)
    for c in range(n_cores):
        bi, half = c % Bsz, c // Bsz
        out[bi, half * half_tokens:(half + 1) * half_tokens] = np.asarray(res.results[c]["out"])
    return out
```

```python
import contextlib
import os
import numpy as np
import ml_dtypes
import concourse.bass as bass
import concourse.mybir as mybir
from concourse.bass_utils import run_bass_kernel_spmd

F32 = mybir.dt.float32
BF16 = mybir.dt.bfloat16
AF = mybir.ActivationFunctionType
ALU = mybir.AluOpType
AX = mybir.AxisListType

ENGS = ("pe", "act", "dve", "pool", "sp")
D = 1024
NMETA = 16
EPS = 1e-6
IN_COLS = 2660
D_FF = 2816
D_FFE = 3584
NEXP = 8


class Buf:
    __slots__ = ("name", "w", "r")

    def __init__(self, name=""):
        self.name = name
        self.w = None
        self.r = {}


class DSem:
    __slots__ = ("h", "cnt", "key")

    def __init__(self, h, key):
        self.h = h
        self.cnt = 0
        self.key = key


class TT:
    __slots__ = ("t", "b")

    def __init__(self, t, b=None, name=""):
        self.t = t
        self.b = b if b is not None else Buf(name)

    def __getitem__(self, k):
        return self.t[k]


def _bufs(xs):
    out = []
    for x in xs:
        if x is None:
            continue
        out.append(x.b if isinstance(x, TT) else x)
    return out


class Prog:
    def __init__(self, nc, stack):
        self.nc = nc
        self.stack = stack
        self.q = {e: [] for e in ENGS}
        self.cnt = {e: 0 for e in ENGS}
        self.seen = {e: {} for e in ENGS}
        self.semh = {}
        for e in ENGS:
            h = stack.enter_context(nc.semaphore("es_" + e))
            self.semh[("E", e)] = h
        self.nd = 0
        self.all_dsems = []

    def dsem(self, name=None):
        self.nd += 1
        key = ("D", self.nd)
        h = self.stack.enter_context(self.nc.semaphore(name or f"ds{self.nd}"))
        self.semh[key] = h
        d = DSem(h, key)
        self.all_dsems.append(d)
        return d

    def _waits(self, eng, reads, writes, extra=(), skipkey=None):
        need = {}

        def add(ev, raw):
            if ev is None:
                return
            k, v = ev
            if k == skipkey:
                return
            if k == ("E", eng) and not raw:
                return
            if need.get(k, 0) < v:
                need[k] = v
        for b in reads:
            add(b.w, True)
        for b in writes:
            add(b.w, False)
            for k, v in b.r.items():
                add((k, v), False)
        for ev in extra:
            add(ev, True)
        out = []
        seen = self.seen[eng]
        for k, v in need.items():
            if seen.get(k, 0) < v:
                seen[k] = v
                out.append((k, v))
        return out

    def _mark(self, ev, reads, writes):
        k, v = ev
        for b in reads:
            if b.r.get(k, 0) < v:
                b.r[k] = v
        for b in writes:
            b.w = ev
            b.r = {}

    def op(self, eng, fn, reads=(), writes=()):
        reads = _bufs(reads)
        writes = _bufs(writes)
        waits = self._waits(eng, reads, writes)
        self.cnt[eng] += 1
        ev = (("E", eng), self.cnt[eng])
        self._mark(ev, reads, writes)
        self.q[eng].append((waits, fn, (("E", eng), 1)))
        return ev

    def dma(self, eng, out, in_, ds, reads=(), writes=(), newgroup=True, **kw):
        reads = _bufs(reads)
        writes = _bufs(writes)
        extra = ()
        if newgroup and ds.cnt > 0:
            extra = ((ds.key, ds.cnt),)
        waits = self._waits(eng, reads, writes, extra, skipkey=(None if newgroup else ds.key))
        ds.cnt += 16
        ev = (ds.key, ds.cnt)
        self._mark(ev, reads, writes)
        self.q[eng].append((waits, lambda e: e.dma_start(out=out, in_=in_, **kw), (ds.key, 16)))
        return ev

    def commit(self, ds, bufs):
        for b in _bufs(bufs):
            if b.w is not None and b.w[0] == ds.key:
                b.w = (ds.key, ds.cnt)
            if ds.key in b.r:
                b.r[ds.key] = ds.cnt

    def wait_all(self, eng, evs):
        waits = self._waits(eng, (), (), evs)
        self.q[eng].append((waits, None, None))

    def emit(self):
        nc = self.nc
        semh = self.semh
        with nc.Block() as block:
            def mk(ename):
                def body(e):
                    for waits, fn, inc in self.q[ename]:
                        for k, v in waits:
                            e.wait_ge(semh[k], v)
                        if fn is None:
                            continue
                        ins = fn(e)
                        if inc is not None:
                            ins.then_inc(semh[inc[0]], inc[1])
                return body
            block.tensor(mk("pe"))
            block.scalar(mk("act"))
            block.vector(mk("dve"))
            block.gpsimd(mk("pool"))
            block.sync(mk("sp"))


def make_consts(T):
    s = np.arange(128)[:, None]
    t = np.arange(128)[None, :]
    c = {}
    c["c_ident"] = np.eye(128, dtype=np.float32)
    c["c_triblk"] = ((s // 32 == t // 32) & (s <= t)).astype(np.float32)
    c["c_blkones"] = (s // 32 == t // 32).astype(np.float32)
    c["c_chunkind"] = (s // 32 == np.arange(4)[None, :]).astype(np.float32)
    c["c_trifull"] = (s <= t).astype(np.float32)
    c["c_sel127"] = np.broadcast_to((s == 127), (128, 128)).astype(np.float32).copy()
    c["c_sel15"] = np.broadcast_to((s == 15), (128, 128)).astype(np.float32).copy()
    c["c_sel64"] = np.broadcast_to((s == 64), (128, 128)).astype(np.float32).copy()
    c["c_sel8"] = np.broadcast_to((s == 8), (128, 128)).astype(np.float32).copy()
    c["c_mask_mla"] = (s // 64 <= t // 64).astype(np.float32)
    pos = np.arange(T, dtype=np.float32)
    inv = (10000.0 ** (-np.arange(16, dtype=np.float32) / 16)).astype(np.float32)
    ang = pos[:, None] * inv[None, :]
    c["c_cos"] = np.cos(ang).astype(np.float32)
    c["c_sin"] = np.sin(ang).astype(np.float32)
    return c


def tile_list(NT):
    return [(0, NMETA)] + [(NMETA + 128 * i, 128) for i in range(NT)]


class Builder:
    def __init__(self, NT, depth=2, debug=False, stop_after=None, moe_tokens=None):
        self.NT = NT
        self.T = NMETA + 128 * NT
        self.depth = depth
        self.debug = debug
        self.stop_after = stop_after
        self.tiles = tile_list(NT)
        self.nc = bass.Bass("TRN2", target_bir_lowering=False)
        self.inputs = {}
        self.outputs = {}
        self.moe_tokens = moe_tokens if moe_tokens is not None else (128 * NT) // 2

    def din(self, name, shape, dt=F32):
        t = self.nc.dram_tensor(name, list(shape), dt, kind="ExternalInput")
        self.inputs[name] = (tuple(shape), dt)
        return t.ap()

    def dscratch(self, name, shape, dt=F32, dbg=False):
        if dbg and self.debug:
            t = self.nc.dram_tensor(name, list(shape), dt, kind="ExternalOutput")
            self.outputs[name] = (tuple(shape), dt)
        else:
            t = self.nc.dram_tensor(name, list(shape), dt, kind="Internal")
        return t.ap()

    def dout(self, name, shape, dt=F32):
        t = self.nc.dram_tensor(name, list(shape), dt, kind="ExternalOutput")
        self.outputs[name] = (tuple(shape), dt)
        return t.ap()

    def sb(self, st, name, shape, dt=F32):
        self._uid = getattr(self, "_uid", 0) + 1
        return TT(st.enter_context(self.nc.sbuf_tensor(f"s{self._uid}_{name}", list(shape), dt)), name=name)

    def ring(self, st, name, shape, dt, n):
        return [self.sb(st, f"{name}{i}", shape, dt) for i in range(n)]

    def build(self):
        nc = self.nc
        T = self.T
        NT = self.NT
        depth = self.depth
        I = {}
        I["xin"] = self.din("xin", [T, D])
        I["sel"] = self.din("sel", [128, 2])
        for nm, shp in [("c_ident", [128, 128]), ("c_triblk", [128, 128]), ("c_blkones", [128, 128]),
                        ("c_chunkind", [128, 4]), ("c_trifull", [128, 128]), ("c_sel127", [128, 128]),
                        ("c_sel15", [128, 128]), ("c_sel64", [128, 128]), ("c_sel8", [128, 128]),
                        ("c_mask_mla", [128, 128]), ("c_cos", [T, 16]), ("c_sin", [T, 16])]:
            I[nm] = self.din(nm, shp)
        L = depth
        I["w_in"] = self.din("w_in", [L, D, IN_COLS])
        I["w_out"] = self.din("w_out", [L, D, D])
        I["g1T"] = self.din("g1T", [L, 128, 8])
        I["g2T"] = self.din("g2T", [L, 128, 8])
        I["og"] = self.din("og", [L, D])
        I["conv_wT"] = self.din("conv_wT", [L, 128, 2, 4])
        I["conv_b"] = self.din("conv_b", [L, 128, 2])
        I["lru_ba"] = self.din("lru_ba", [L, 128, 2])
        I["lru_bx"] = self.din("lru_bx", [L, 128, 2])
        I["lru_lam"] = self.din("lru_lam", [L, 128, 2])
        I["lru_wa"] = self.din("lru_wa", [L, 4, 64, 64])
        I["lru_wx"] = self.din("lru_wx", [L, 4, 64, 64])
        I["hg_lb"] = self.din("hg_lb", [2, 256])
        I["mla_gq"] = self.din("mla_gq", [L, 192])
        I["mla_w_uq"] = self.din("mla_w_uq", [L, 192, 384])
        I["mla_gkv"] = self.din("mla_gkv", [L, 128])
        I["mla_w_ukv"] = self.din("mla_w_ukv", [L, 128, 512])
        I["mla_gqn"] = self.din("mla_gqn", [L, 96])
        I["mla_gkn"] = self.din("mla_gkn", [L, 96])
        I["fox_gqn"] = self.din("fox_gqn", [L, 64])
        I["fox_gkn"] = self.din("fox_gkn", [L, 64])
        I["fox_bf"] = self.din("fox_bf", [L, 4])
        I["ffn_wg"] = self.din("ffn_wg", [D, D_FF])
        I["ffn_wu"] = self.din("ffn_wu", [D, D_FF])
        I["ffn_wd"] = self.din("ffn_wd", [D_FF, D])
        if depth > 1:
            I["moe_wr"] = self.din("moe_wr", [D, NEXP])
            I["moe_wg"] = self.din("moe_wg", [NEXP, D, D_FFE])
            I["moe_wu"] = self.din("moe_wu", [NEXP, D, D_FFE])
            I["moe_wd"] = self.din("moe_wd", [NEXP, D_FFE, D])
        self.I = I
        S = {}
        dbg = True
        S["hA"] = self.dscratch("hA", [T, D], F32, dbg)
        S["hmid"] = self.dscratch("hmid", [T, D], F32, dbg)
        S["Y"] = self.dscratch("Y", [T, D], F32, dbg)
        S["u2T"] = self.dscratch("u2T", [D, T], BF16, dbg)
        S["QTc"] = self.dscratch("QTc", [4, 96, T], BF16, dbg)
        S["KTc"] = self.dscratch("KTc", [4, 96, T], BF16, dbg)
        S["Vc"] = self.dscratch("Vc", [4, 128, NT + 1, 65], BF16, dbg)
        S["QTd"] = self.dscratch("QTd", [2, 128, T], BF16, dbg)
        S["KTd"] = self.dscratch("KTd", [2, 128, T], BF16, dbg)
        S["Vd"] = self.dscratch("Vd", [4, 128, NT + 1, 65], BF16, dbg)
        S["gate"] = self.dscratch("gate", [T, NEXP], F32, dbg)
        self.S = S
        n_out = self.moe_tokens if depth > 1 else 128 * NT
        self.out_ap = self.dout("out", [n_out, D])

        with contextlib.ExitStack() as st:
            P = Prog(nc, st)
            self.P = P
            self.bank = [TT(st.enter_context(nc.psum_tensor(f"bank{i}", [128, 512], F32)), name=f"bank{i}")
                         for i in range(8)]
            with contextlib.ExitStack() as cst:
                self.load_consts(cst)
                for l in range(depth):
                    hsrc = I["xin"] if l == 0 else S["hA"]
                    self.phase_1a(l, hsrc)
                    if self.stop_after == ("1a", l):
                        break
                    self.phase_1b(l)
                    if self.stop_after == ("1b", l):
                        break
                    self.phase_1c(l, hsrc)
                    if self.stop_after == ("1c", l):
                        break
                    if l == 0:
                        self.phase_ffn(S["hA"])
                    else:
                        self.phase_moe()
            P.wait_all("sp", self.final_evs)
            P.emit()
        return nc

    def load_consts(self, st):
        P = self.P
        I = self.I
        self.final_evs = []
        self.ds_const = P.dsem("ds_const")
        C = {}
        for nm in ["c_ident", "c_triblk", "c_blkones", "c_trifull", "c_sel127", "c_sel15", "c_sel64", "c_sel8",
                   "c_mask_mla"]:
            C[nm] = self.sb(st, nm, [128, 128], F32)
            P.dma("sp", C[nm][:], I[nm][:, :], self.ds_const, writes=[C[nm]], newgroup=False)
        C["c_chunkind"] = self.sb(st, "c_chunkind", [128, 4], F32)
        P.dma("sp", C["c_chunkind"][:], I["c_chunkind"][:, :], self.ds_const, writes=[C["c_chunkind"]], newgroup=False)
        C["sel"] = self.sb(st, "sel", [128, 2], F32)
        P.dma("sp", C["sel"][:], I["sel"][:, :], self.ds_const, writes=[C["sel"]], newgroup=False)
        P.commit(self.ds_const, list(C.values()))
        C["identb"] = self.sb(st, "identb", [128, 128], BF16)
        P.op("pool", lambda e: e.tensor_copy(out=C["identb"][:], in_=C["c_ident"][:]), reads=[C["c_ident"]], writes=[C["identb"]])
        C["hgmask"] = self.sb(st, "hgmask", [128, 128], BF16)
        P.op("pool", lambda e: e.tensor_copy(out=C["hgmask"][:], in_=C["c_triblk"][:]), reads=[C["c_triblk"]], writes=[C["hgmask"]])
        C["foxmask"] = self.sb(st, "foxmask", [128, 128], BF16)
        P.op("pool", lambda e: e.tensor_copy(out=C["foxmask"][:], in_=C["c_trifull"][:]), reads=[C["c_trifull"]], writes=[C["foxmask"]])
        C["mlamask"] = self.sb(st, "mlamask", [128, 128], BF16)
        P.op("pool", lambda e: e.tensor_copy(out=C["mlamask"][:], in_=C["c_mask_mla"][:]), reads=[C["c_mask_mla"]], writes=[C["mlamask"]])
        self.C = C
        self.call = self.sb(st, "c_all", [128, self.NT + 1, 4], F32)
        P.op("pool", lambda e: e.memset(self.call[:], 0.0), writes=[self.call])

    def mm(self, out, lhsT, rhs, start, stop, reads, writes, **kw):
        self.P.op("pe", lambda e: e.matmul(out, lhsT=lhsT, rhs=rhs, start=start, stop=stop, **kw), reads=reads, writes=writes)

    def tr(self, out, in_, ident, reads, writes):
        self.P.op("pe", lambda e: e.transpose(out=out, in_=in_, identity=ident), reads=reads, writes=writes)

    def act(self, out, in_, func, reads, writes, **kw):
        self.P.op("act", lambda e: e.activation(out=out, in_=in_, func=func, **kw), reads=reads, writes=writes)

    def tt(self, eng, out, in0, in1, op, reads, writes):
        self.P.op(eng, lambda e: e.tensor_tensor(out=out, in0=in0, in1=in1, op=op), reads=reads, writes=writes)

    def ts(self, eng, out, in0, s1, s2, op0, op1, reads, writes):
        if op1 is None:
            self.P.op(eng, lambda e: e.tensor_scalar(out=out, in0=in0, scalar1=s1, scalar2=None, op0=op0), reads=reads, writes=writes)
        else:
            self.P.op(eng, lambda e: e.tensor_scalar(out=out, in0=in0, scalar1=s1, scalar2=s2, op0=op0, op1=op1), reads=reads, writes=writes)

    def stt(self, eng, out, in0, scalar, in1, op0, op1, reads, writes):
        self.P.op(eng, lambda e: e.scalar_tensor_tensor(out=out, in0=in0, scalar=scalar, in1=in1, op0=op0, op1=op1), reads=reads, writes=writes)

    def cp(self, eng, out, in_, reads, writes):
        if eng == "act":
            self.P.op("act", lambda e: e.copy(out=out, in_=in_), reads=reads, writes=writes)
        else:
            self.P.op(eng, lambda e: e.tensor_copy(out=out, in_=in_), reads=reads, writes=writes)

    def rsq(self, out, in_, scale, rw):
        self.act(out, in_, AF.Ln, rw, rw, scale=scale, bias=EPS)
        self.act(out, out, AF.Exp, rw, rw, scale=-0.5)

    def sigm(self, out, in_, reads, writes, bias=None):
        if bias is None:
            self.act(out, in_, AF.Exp, reads, writes, scale=-1.0)
        else:
            self.act(out, in_, AF.Exp, reads, writes, scale=-1.0, bias=bias)
        w2 = [writes[0]]
        self.act(out, out, AF.Ln, w2, w2, bias=1.0)
        self.act(out, out, AF.Exp, w2, w2, scale=-1.0)

    def rstd(self, ss, out, n, scale, reads_w):
        self.act(out, ss, AF.Sqrt, reads=reads_w, writes=reads_w, scale=scale, bias=EPS)
        self.P.op("dve", lambda e: e.reciprocal(out=out, in_=out), reads=reads_w, writes=reads_w)

    def phase_1a(self, l, hsrc):
        P, I, S, C, nc = self.P, self.I, self.S, self.C, self.nc
        bank = self.bank
        NT, T = self.NT, self.T
        call = self.call
        with contextlib.ExitStack() as st:
            sb = lambda name, shape, dt=F32: self.sb(st, "a_" + name, shape, dt)
            dsw = P.dsem()
            dsl = P.dsem()
            w_in = sb("w_in", [128, 8, IN_COLS], BF16)
            wv = I["w_in"][l].rearrange("(k p) c -> p k c", p=128)
            for k in range(8):
                P.dma("pool", w_in[:, k, :], wv[:, k, :], dsw, writes=[w_in], newgroup=False)
            wuq = sb("wuq", [128, 2, 384], BF16)
            P.dma("pool", wuq[:, 0, :], I["mla_w_uq"][l, 0:128, :], dsw, writes=[wuq], newgroup=False)
            P.dma("pool", wuq[0:64, 1, :], I["mla_w_uq"][l, 128:192, :], dsw, writes=[wuq], newgroup=False)
            wukv = sb("wukv", [128, 512], BF16)
            P.dma("pool", wukv[:], I["mla_w_ukv"][l, :, :], dsw, writes=[wukv], newgroup=False)
            g1T = sb("g1T", [128, 8])
            P.dma("sp", g1T[:], I["g1T"][l], dsl, writes=[g1T], newgroup=False)
            cw = sb("cw", [128, 2, 4]); cb = sb("cb", [128, 2]); ba = sb("ba", [128, 2]); bx = sb("bx", [128, 2])
            lam = sb("lam", [128, 2])
            for tdst, nm in [(cw, "conv_wT"), (cb, "conv_b"), (ba, "lru_ba"), (bx, "lru_bx"), (lam, "lru_lam")]:
                P.dma("sp", tdst[:], I[nm][l], dsl, writes=[tdst], newgroup=False)
            waf = sb("waf", [128, 2, 128]); wxf = sb("wxf", [128, 2, 128])
            wab = sb("wab", [128, 2, 128], BF16); wxb = sb("wxb", [128, 2, 128], BF16)
            P.op("pool", lambda e: e.memset(waf[:], 0.0), writes=[waf])
            P.op("pool", lambda e: e.memset(wxf[:], 0.0), writes=[wxf])
            for nb in range(4):
                r0 = (nb % 2) * 64
                P.dma("sp", waf[r0:r0 + 64, nb // 2, r0:r0 + 64], I["lru_wa"][l, nb], dsl, writes=[waf], newgroup=False)
                P.dma("sp", wxf[r0:r0 + 64, nb // 2, r0:r0 + 64], I["lru_wx"][l, nb], dsl, writes=[wxf], newgroup=False)
            defer = []
            nba = sb("nba", [128, 2]); nbx = sb("nbx", [128, 2])
            defer.append(lambda: self.ts("dve", nba[:], ba[:], -1.0, None, ALU.mult, None, [ba], [nba]))
            defer.append(lambda: self.ts("dve", nbx[:], bx[:], -1.0, None, ALU.mult, None, [bx], [nbx]))
            defer.append(lambda: self.cp("pool", wab[:], waf[:], [waf], [wab]))
            defer.append(lambda: self.cp("pool", wxb[:], wxf[:], [wxf], [wxb]))
            cA = sb("cA", [128, 2]); cA2 = sb("cA2", [128, 2])
            defer.append(lambda: self.act(cA[:], lam[:], AF.Exp, [lam], [cA], scale=-1.0))
            defer.append(lambda: self.act(cA[:], cA[:], AF.Ln, [cA], [cA], bias=1.0))
            defer.append(lambda: self.ts("dve", cA2[:], cA[:], -16.0, None, ALU.mult, None, [cA], [cA2]))
            defer.append(lambda: self.ts("dve", cA[:], cA[:], -8.0, None, ALU.mult, None, [cA], [cA]))
            bcs = []

            def bc(name, src, w):
                t = sb(name, [128, w])
                P.dma("sp", t[:], src.partition_broadcast(128), dsl, writes=[t], newgroup=False)
                bcs.append(t)
                return t
            gq_bc = bc("gq_bc", I["mla_gq"][l], 192)
            gkv_bc = bc("gkv_bc", I["mla_gkv"][l], 128)
            gqn_bc = bc("gqn_bc", I["mla_gqn"][l], 96)
            gkn_bc = bc("gkn_bc", I["mla_gkn"][l], 96)
            fgq_bc = bc("fgq_bc", I["fox_gqn"][l], 64)
            fgk_bc = bc("fgk_bc", I["fox_gkn"][l], 64)
            fbf_bc = bc("fbf_bc", I["fox_bf"][l], 4)
            lb_bc = sb("lb_bc", [128, 256]); oml_bc = sb("oml_bc", [128, 256])
            if l == 0:
                P.op("pool", lambda e: e.memset(lb_bc[:], 0.0), writes=[lb_bc])
                P.op("pool", lambda e: e.memset(oml_bc[:], 1.0), writes=[oml_bc])
            else:
                lg0 = bc("lg0", I["hg_lb"][0], 256)
                lg1 = bc("lg1", I["hg_lb"][1], 256)
                defer.append(lambda: self.tt("dve", lb_bc[:], lg1[:], lg0[:], ALU.subtract, [lg0, lg1], [lb_bc]))
                defer.append(lambda: self.act(lb_bc[:], lb_bc[:], AF.Sigmoid, [lb_bc], [lb_bc]))
                defer.append(lambda: self.ts("dve", oml_bc[:], lb_bc[:], -1.0, 1.0, ALU.mult, ALU.add, [lb_bc], [oml_bc]))
            P.commit(dsw, [w_in, wuq, wukv])
            P.commit(dsl, [g1T, cw, cb, ba, bx, lam, waf, wxf] + bcs)
            for fn in defer:
                fn()
            xe = sb("xe", [128, 2, 3 + 128])
            P.op("pool", lambda e: e.memset(xe[:], 0.0), writes=[xe])
            hprev = sb("hprev", [128, 2])
            P.op("pool", lambda e: e.memset(hprev[:], 0.0), writes=[hprev])
            Sst = [sb(f"Sst{h}", [64, 64]) for h in range(4)]
            for h in range(4):
                P.op("pool", lambda e, h=h: e.memset(Sst[h][:], 0.0), writes=[Sst[h]])
            Sbf = [[sb(f"Sbf{h}_{r}", [64, 64], BF16) for r in range(2)] for h in range(4)]
            hqm = [sb(f"hqm{c}", [64, 4, 128], BF16) for c in range(4)]
            hkem = [sb(f"hkem{c}", [128, 256], BF16) for c in range(4)]
            for c in range(4):
                P.op("pool", lambda e, c=c: e.memset(hqm[c][:], 0.0), writes=[hqm[c]])
            R2 = 2
            junk = sb("junk", [128, D], BF16)
            ss = self.ring(st, "a_ss", [128, 8], F32, R2)
            ub = self.ring(st, "a_ub", [128, D], BF16, R2)
            uT = self.ring(st, "a_uT", [128, 8, 128], BF16, R2)
            zt = self.ring(st, "a_zt", [128, 2148], F32, R2)
            ysb = self.ring(st, "a_ysb", [128, 512], F32, R2)
            dsy = [P.dsem() for _ in range(R2)]
            dsq = [P.dsem() for _ in range(R2)]
            lu = sb("lu", [128, 128]); lub = sb("lub", [128, 128], BF16)
            lr = sb("lr", [128, 128]); li = sb("li", [128, 128]); la = sb("la", [128, 128]); lm = sb("lm", [128, 128])
            lbb = sb("lbb", [128, 128]); lhs = sb("lhs", [128, 128]); lg = sb("lg", [128, 2, 128]); lya = sb("lya", [128, 2, 128])
            hsig = sb("hsig", [128, 256]); hlogf = sb("hlogf", [128, 256]); hkk = sb("hkk", [128, 256])
            heb = sb("heb", [128, 256]); henb = sb("henb", [128, 256]); hebe = sb("hebe", [128, 256])
            hqd = sb("hqd", [128, 256], BF16); hkd = sb("hkd", [128, 256], BF16); hke = sb("hke", [128, 256], BF16)
            hkdf = sb("hkdf", [128, 256]); hvb = sb("hvb", [128, 256], BF16); hsg = sb("hsg", [128, 256])
            hqdT = sb("hqdT", [64, 4, 128], BF16); hkdT = sb("hkdT", [64, 4, 128], BF16)
            hdec = sb("hdec", [64, 4, 4]); hattm = sb("hattm", [128, 4, 128], BF16)
            mjunk = sb("mjunk", [128, 512]); mss = sb("mss", [128, 16])
            cqn = sb("cqn", [128, 192], BF16); ckvn = sb("ckvn", [128, 128], BF16)
            cqT = sb("cqT", [128, 2, 128], BF16); ckvT = sb("ckvT", [128, 128], BF16)
            qf = sb("qf", [128, 4, 96]); kf = sb("kf", [128, 4, 96])
            qb = sb("qb", [128, 4, 96], BF16); kb = sb("kb", [128, 4, 96], BF16)
            rt = sb("rt", [128, 4, 4, 16])
            vaug = self.ring(st, "a_vaug", [128, 4, 65], BF16, R2)
            qkT = self.ring(st, "a_qkT", [96, 8, 128], BF16, R2)
            fqf = sb("fqf", [128, 4, 64]); fqb = sb("fqb", [128, 256], BF16); fkb = sb("fkb", [128, 256], BF16)
            fvaug = self.ring(st, "a_fvaug", [128, 4, 65], BF16, R2)
            fqkT = self.ring(st, "a_fqkT", [128, 4, 128], BF16, R2)
            flog = sb("flog", [128, 4])
            for r in range(R2):
                P.op("pool", lambda e, r=r: e.memset(vaug[r][:], 1.0), writes=[vaug[r]])
                P.op("pool", lambda e, r=r: e.memset(fvaug[r][:], 1.0), writes=[fvaug[r]])
            identb = C["identb"]; identf = C["c_ident"]

            R3 = 3
            ht = self.ring(st, "a_ht3", [128, D], F32, R3)
            cs = self.ring(st, "a_cs3", [128, 32], F32, R3)
            dsh = [P.dsem() for _ in range(R3)]
            ntl = len(self.tiles)
            b0 = bank[0]
            b0v = b0.t[:].bitcast(BF16)
            b1 = bank[1]

            def load(jn):
                pp, nn = self.tiles[jn]
                rr = jn % R3
                P.dma("sp", ht[rr][0:nn, :], hsrc[pp:pp + nn, :], dsh[rr], writes=[ht[rr]])
                P.dma("sp", cs[rr][0:nn, 0:16], I["c_cos"][pp:pp + nn, :], dsh[rr], writes=[cs[rr]], newgroup=False)
                P.dma("sp", cs[rr][0:nn, 16:32], I["c_sin"][pp:pp + nn, :], dsh[rr], writes=[cs[rr]], newgroup=False)
                P.commit(dsh[rr], [ht[rr], cs[rr]])

            def zpro(j):
                p0, n = self.tiles[j]
                r = j % R2
                H, SS, UB, UT = ht[j % R3], ss[r], ub[r], uT[r]
                P.op("dve", lambda e, SS=SS: e.memset(SS[:], 0.0), writes=[SS])
                self.act(junk[0:n, :], H[0:n, :], AF.Square, [H, SS], [junk, SS], accum_out=SS[0:n, 0:1])
                self.rsq(SS[0:n, 1:2], SS[0:n, 0:1], 1.0 / D, [SS])
                self.act(UB[0:n, :], H[0:n, :], AF.Copy, [H, SS], [UB], scale=SS[0:n, 1:2])
                for k in range(8):
                    self.tr(b0v[:, k * 128:k * 128 + n], UB[0:n, k * 128:(k + 1) * 128], identb[0:n, 0:n], [UB, identb], [b0])
                self.tt("dve", UT[:, :, 0:n], b0v[:, :].rearrange("p (k t) -> p k t", k=8)[:, :, 0:n],
                        g1T[:, :].unsqueeze(2).broadcast_to([128, 8, n]), ALU.mult, [g1T], [UT, b0])

            def zgroups(j):
                p0, n = self.tiles[j]
                r = j % R2
                UT, ZT = uT[r], zt[r]
                gl = []

                def lru_mm():
                    for m in range(4):
                        for k in range(8):
                            self.mm(b1[:, m * 128:m * 128 + n], w_in[:, k, m * 128:(m + 1) * 128], UT[:, k, 0:n], k == 0, k == 7, [w_in, UT], [b1])
                gl.append(lru_mm)
                chunks = [(512, 1024), (1024, 1536), (1536, 1888), (1888, 2400), (2400, 2660)]
                for ci, (c0, c1) in enumerate(chunks):
                    def zc(ci=ci, c0=c0, c1=c1):
                        bz = bank[2 + (ci % 2)]
                        wd = c1 - c0
                        for k in range(8):
                            self.mm(bz[0:n, 0:wd], UT[:, k, 0:n], w_in[:, k, c0:c1], k == 0, k == 7, [UT, w_in], [bz])
                        self.cp("act" if ci % 2 == 0 else "dve", ZT[0:n, c0 - 512:c1 - 512], bz[0:n, 0:wd], [], [ZT, bz])
                    gl.append(zc)
                return gl

            def chains(j, fill):
                p0, n = self.tiles[j]
                r = j % R2
                ZT, YS, CS = zt[r], ysb[r], cs[j % R3]

                def fillpop():
                    if fill:
                        fill.pop(0)()
                steps = os.environ.get("K_STEPS", "lru,hgrn,mla,fox").split(",")
                for cc in (range(2) if "lru" in steps else []):
                    self.act(lg[:, cc, 0:n], b1[:, (2 + cc) * 128:(2 + cc) * 128 + n], AF.Gelu_apprx_tanh, [], [lg, b1])
                for cc in (range(2) if "lru" in steps else []):
                    self.cp("act", xe[:, cc, 3:3 + n], b1[:, cc * 128:cc * 128 + n], [], [xe, b1])
                fillpop()
                for cc in (range(2) if "lru" in steps else []):
                    self.ts("dve", lu[:, 0:n], xe[:, cc, 0:n], cw[:, cc, 0:1], cb[:, cc:cc + 1], ALU.mult, ALU.add, [xe, cw, cb], [lu])
                    for jj in range(1, 4):
                        self.stt("dve", lu[:, 0:n], xe[:, cc, jj:jj + n], cw[:, cc, jj:jj + 1], lu[:, 0:n], ALU.mult, ALU.add, [xe, cw, lu], [lu])
                    self.cp("pool", xe[:, cc, 0:3], xe[:, cc, n:n + 3], [xe], [xe])
                    self.cp("pool", lub[:, 0:n], lu[:, 0:n], [lu], [lub])
                    b4 = bank[4]
                    self.mm(b4[:, 0:n], wab[:, cc, :], lub[:, 0:n], True, True, [wab, lub], [b4])
                    self.mm(b4[:, 128:128 + n], wxb[:, cc, :], lub[:, 0:n], True, True, [wxb, lub], [b4])
                    self.sigm(lr[:, 0:n], b4[:, 0:n], [nba], [lr, b4], bias=nba[:, cc:cc + 1])
                    self.sigm(li[:, 0:n], b4[:, 128:128 + n], [nbx], [li, b4], bias=nbx[:, cc:cc + 1])
                    self.act(la[:, 0:n], lr[:, 0:n], AF.Exp, [lr, cA], [la], scale=cA[:, cc:cc + 1])
                    self.act(lm[:, 0:n], lr[:, 0:n], AF.Exp, [lr, cA2], [lm], scale=cA2[:, cc:cc + 1])
                    self.act(lm[:, 0:n], lm[:, 0:n], AF.Ln, [lm], [lm], scale=-1.0, bias=1.0)
                    self.act(lm[:, 0:n], lm[:, 0:n], AF.Exp, [lm], [lm], scale=0.5)
                    self.tt("dve", lbb[:, 0:n], lm[:, 0:n], li[:, 0:n], ALU.mult, [lm, li], [lbb])
                    self.tt("dve", lbb[:, 0:n], lbb[:, 0:n], lu[:, 0:n], ALU.mult, [lbb, lu], [lbb])
                    P.op("dve", lambda e, n=n, cc=cc: e.tensor_tensor_scan(out=lhs[:, 0:n], data0=la[:, 0:n], data1=lbb[:, 0:n],
                                                                        initial=hprev[:, cc:cc + 1], op0=ALU.mult, op1=ALU.add),
                         reads=[la, lbb, hprev], writes=[lhs])
                    self.cp("dve", hprev[:, cc:cc + 1], lhs[:, n - 1:n], [lhs], [hprev])
                    self.tt("dve", lya[:, cc, 0:n], lhs[:, 0:n], lg[:, cc, 0:n], ALU.mult, [lhs, lg], [lya])
                    fillpop()
                b5 = bank[5]
                for cc in (range(2) if "lru" in steps else []):
                    self.tr(b5[0:n, cc * 128:(cc + 1) * 128], lya[:, cc, 0:n], identf[:, :], [lya, identf], [b5])
                if "lru" in steps:
                    self.cp("act", YS[0:n, 0:256], b5[0:n, 0:256], [], [YS, b5])
                fillpop()
                if "hgrn" in steps:
                    nch = (n + 31) // 32
                    hq_ = ZT[0:n, 0:256]; hf_ = ZT[0:n, 256:512]; hv_ = ZT[0:n, 512:768]; hg_ = ZT[0:n, 768:1024]
                    self.sigm(hsig[0:n, :], hf_, [ZT], [hsig])
                    self.tt("dve", hsig[0:n, :], hsig[0:n, :], oml_bc[0:n, :], ALU.mult, [hsig, oml_bc], [hsig])
                    self.tt("dve", hsig[0:n, :], hsig[0:n, :], lb_bc[0:n, :], ALU.add, [hsig, lb_bc], [hsig])
                    self.act(hlogf[0:n, :], hsig[0:n, :], AF.Ln, [hsig], [hlogf])
                    self.ts("pool", hkk[0:n, :], hsig[0:n, :], -1.0, 1.0, ALU.mult, ALU.add, [hsig], [hkk])
                    b4 = bank[4]
                    self.mm(b4[0:n, 0:256], C["c_triblk"][0:n, 0:n], hlogf[0:n, :], True, True, [C["c_triblk"], hlogf], [b4])
                    b6 = bank[6]
                    self.mm(b6[0:n, 0:256], C["c_blkones"][0:n, 0:n], hlogf[0:n, :], True, True, [C["c_blkones"], hlogf], [b6])
                    self.act(heb[0:n, :], b4[0:n, 0:256], AF.Exp, [], [heb, b4])
                    self.act(henb[0:n, :], b4[0:n, 0:256], AF.Exp, [], [henb, b4], scale=-1.0)
                    self.act(hebe[0:n, :], b6[0:n, 0:256], AF.Exp, [], [hebe, b6])
                    self.tt("dve", hqd[0:n, :], hq_, heb[0:n, :], ALU.mult, [ZT, heb], [hqd])
                    self.tt("dve", hkdf[0:n, :], hkk[0:n, :], henb[0:n, :], ALU.mult, [hkk, henb], [hkdf])
                    self.cp("pool", hkd[0:n, :], hkdf[0:n, :], [hkdf], [hkd])
                    self.tt("dve", hke[0:n, :], hkdf[0:n, :], hebe[0:n, :], ALU.mult, [hkdf, hebe], [hke])
                    self.cp("pool", hvb[0:n, :], hv_, [ZT], [hvb])
                    self.sigm(hsg[0:n, :], hg_, [ZT], [hsg])
                    self.tt("pool", hsg[0:n, :], hsg[0:n, :], hg_, ALU.mult, [hsg, ZT], [hsg])
                    b7 = bank[7]
                    for h in range(4):
                        self.mm(b7[0:64, h * 4:h * 4 + nch], hlogf[0:n, h * 64:(h + 1) * 64], C["c_chunkind"][0:n, 0:nch], True, True,
                                [hlogf, C["c_chunkind"]], [b7])
                    self.act(hdec[:, :, 0:nch], b7[0:64, 0:16].rearrange("p (a c) -> p a c", a=4)[:, :, 0:nch], AF.Exp, [], [hdec, b7])
                    b5v = b5.t[:].bitcast(BF16)
                    for h in range(4):
                        self.tr(b5v[0:64, h * 128:h * 128 + n], hqd[0:n, h * 64:(h + 1) * 64], identb[0:n, 0:n], [hqd, identb], [b5])
                        self.tr(b5v[0:64, 512 + h * 128:512 + h * 128 + n], hkd[0:n, h * 64:(h + 1) * 64], identb[0:n, 0:n], [hkd, identb], [b5])
                    b5q = b5v[0:64, 0:512].rearrange("p (a t) -> p a t", a=4)
                    self.cp("act", hqdT[:, :, 0:n], b5q[:, :, 0:n], [], [hqdT, b5])
                    self.cp("act", hkdT[:, :, 0:n], b5v[0:64, 512:1024].rearrange("p (a t) -> p a t", a=4)[:, :, 0:n], [], [hkdT, b5])
                    for c in range(nch):
                        cn = min(32, n - 32 * c)
                        self.cp("pool", hqm[c][:, :, 32 * c:32 * c + cn], hqdT[:, :, 32 * c:32 * c + cn], [hqdT], [hqm[c]])
                        if c % 2 == 0:
                            self.ts("dve", hkem[c][0:n, :], hke[0:n, :], C["c_chunkind"][0:n, c:c + 1], None, ALU.mult, None,
                                    [hke, C["c_chunkind"]], [hkem[c]])
                        else:
                            self.act(hkem[c][0:n, :], hke[0:n, :], AF.Copy, [hke, C["c_chunkind"]], [hkem[c]], scale=C["c_chunkind"][0:n, c:c + 1])
                    for h in range(4):
                        self.mm(b6[0:n, h * 128:h * 128 + n], hkdT[:, h, 0:n], hqdT[:, h, 0:n], True, True, [hkdT, hqdT], [b6])
                    self.tt("dve", hattm[0:n, :, 0:n], b6[0:n, :].rearrange("p (h t) -> p h t", h=4)[:, :, 0:n],
                            C["hgmask"][0:n, 0:n].unsqueeze(1).broadcast_to([n, 4, n]), ALU.mult, [C["hgmask"]], [hattm, b6])
                    for h in range(4):
                        bu = bank[2 + h // 2]
                        for c in range(nch):
                            col = ((h % 2) * 4 + c) * 64
                            self.mm(bu[0:64, col:col + 64], hkem[c][0:n, h * 64:(h + 1) * 64], hvb[0:n, h * 64:(h + 1) * 64], True, True,
                                    [hkem[c], hvb], [bu])
                    for h in range(4):
                        self.mm(b4[0:n, h * 64:(h + 1) * 64], hattm[0:n, h, 0:n], hvb[0:n, h * 64:(h + 1) * 64], h == 0, False, [hattm, hvb], [b4],
                                skip_group_check=True)
                    for c in range(nch):
                        for h in range(4):
                            bu = bank[2 + h // 2]
                            gidx = (j - 1) * 4 + c + 1 if j > 0 else 0
                            slot_prev = Sbf[h][(gidx - 1) % 2]
                            if gidx > 0:
                                self.mm(b4[0:n, h * 64:(h + 1) * 64], hqm[c][:, h, 0:n], slot_prev[:, :], False, True, [hqm[c], slot_prev], [b4],
                                        skip_group_check=True)
                            col = ((h % 2) * 4 + c) * 64
                            self.stt("dve", Sst[h][:], Sst[h][:], hdec[:, h, c:c + 1], bu[0:64, col:col + 64], ALU.mult, ALU.add,
                                     [Sst[h], hdec], [Sst[h], bu])
                            slot = Sbf[h][gidx % 2]
                            self.cp("pool", slot[:], Sst[h][:], [Sst[h]], [slot])
                    self.tt("dve", YS[0:n, 256:512], b4[0:n, 0:256], hsg[0:n, :], ALU.mult, [hsg], [YS, b4])
                    P.dma("sp", S["Y"][p0:p0 + n, 0:512], YS[0:n, :], dsy[r], reads=[YS], writes=[Buf()])
                fillpop()
                if "mla" in steps:
                    cq_ = ZT[0:n, 1024:1216]; ckv_ = ZT[0:n, 1216:1344]; kr_ = ZT[0:n, 1344:1376]
                    P.op("dve", lambda e: e.memset(mss[:], 0.0), writes=[mss])
                    self.act(mjunk[0:n, 0:192], cq_, AF.Square, [ZT, mss], [mjunk, mss], accum_out=mss[0:n, 0:1])
                    self.act(mjunk[0:n, 0:128], ckv_, AF.Square, [ZT, mss], [mjunk, mss], accum_out=mss[0:n, 1:2])
                    self.rsq(mss[0:n, 2:3], mss[0:n, 0:1], 1.0 / 192, [mss])
                    self.rsq(mss[0:n, 3:4], mss[0:n, 1:2], 1.0 / 128, [mss])
                    self.stt("dve", cqn[0:n, :], cq_, mss[0:n, 2:3], gq_bc[0:n, :], ALU.mult, ALU.mult, [ZT, mss, gq_bc], [cqn])
                    self.stt("dve", ckvn[0:n, :], ckv_, mss[0:n, 3:4], gkv_bc[0:n, :], ALU.mult, ALU.mult, [ZT, mss, gkv_bc], [ckvn])
                    self.tr(b5v[:, 0:n], cqn[0:n, 0:128], identb[0:n, 0:n], [cqn, identb], [b5])
                    self.tr(b5v[0:64, 128:128 + n], cqn[0:n, 128:192], identb[0:n, 0:n], [cqn, identb], [b5])
                    self.tr(b5v[:, 256:256 + n], ckvn[0:n, 0:128], identb[0:n, 0:n], [ckvn, identb], [b5])
                    self.cp("act", cqT[:, 0, 0:n], b5v[:, 0:n], [], [cqT, b5])
                    self.cp("act", cqT[0:64, 1, 0:n], b5v[0:64, 128:128 + n], [], [cqT, b5])
                    self.cp("act", ckvT[:, 0:n], b5v[:, 256:256 + n], [], [ckvT, b5])
                    self.mm(b6[0:n, 0:384], cqT[:, 0, 0:n], wuq[:, 0, :], True, False, [cqT, wuq], [b6])
                    self.mm(b6[0:n, 0:384], cqT[0:64, 1, 0:n], wuq[0:64, 1, :], False, True, [cqT, wuq], [b6])
                    self.mm(b7[0:n, 0:512], ckvT[:, 0:n], wukv[:, :], True, True, [ckvT, wukv], [b7])
                    self.cp("act", qf[0:n, :, :], b6[0:n, 0:384].rearrange("p (h c) -> p h c", h=4), [], [qf, b6])
                    b7h = b7[0:n, 0:512].rearrange("p (h c) -> p h c", h=4)
                    self.cp("dve", kf[0:n, :, 0:64], b7h[:, :, 0:64], [], [kf, b7])
                    VA = vaug[r]
                    self.cp("act", VA[0:n, :, 0:64], b7h[:, :, 64:128], [], [VA, b7])
                    self.cp("pool", kf[0:n, :, 64:96], kr_.unsqueeze(1).broadcast_to([n, 4, 32]), [ZT], [kf])
                    for (src, gbc, dst, col) in [(qf, gqn_bc, qb, 4), (kf, gkn_bc, kb, 8)]:
                        self.act(mjunk[0:n, 0:384], src[0:n, :, :].rearrange("p h c -> p (h c)"), AF.Square, [src], [mjunk])
                        P.op("dve", lambda e, n=n, col=col: e.tensor_reduce(out=mss[0:n, col:col + 4], in_=mjunk[0:n, 0:384].rearrange("p (h c) -> p h c", h=4),
                                                                        axis=AX.X, op=ALU.add), reads=[mjunk], writes=[mss])
                        self.rsq(mss[0:n, col:col + 4], mss[0:n, col:col + 4], 1.0 / 96, [mss])
                        self.tt("dve", src[0:n, :, :], src[0:n, :, :], mss[0:n, col:col + 4].unsqueeze(2).broadcast_to([n, 4, 96]), ALU.mult, [src, mss], [src])
                        self.tt("pool", src[0:n, :, :], src[0:n, :, :], gbc[0:n, :].unsqueeze(1).broadcast_to([n, 4, 96]), ALU.mult, [src, gbc], [src])
                        cosb = CS[0:n, 0:16].unsqueeze(1).broadcast_to([n, 4, 16])
                        sinb = CS[0:n, 16:32].unsqueeze(1).broadcast_to([n, 4, 16])
                        t1 = src[0:n, :, 64:80]; t2 = src[0:n, :, 80:96]
                        self.tt("dve", rt[0:n, :, 0, :], t1, cosb, ALU.mult, [src, CS], [rt])
                        self.tt("pool", rt[0:n, :, 1, :], t2, sinb, ALU.mult, [src, CS], [rt])
                        self.tt("dve", rt[0:n, :, 2, :], t1, sinb, ALU.mult, [src, CS], [rt])
                        self.tt("pool", rt[0:n, :, 3, :], t2, cosb, ALU.mult, [src, CS], [rt])
                        self.cp("act", dst[0:n, :, 0:64], src[0:n, :, 0:64], [src], [dst])
                        self.tt("dve", dst[0:n, :, 64:80], rt[0:n, :, 0, :], rt[0:n, :, 1, :], ALU.subtract, [rt], [dst])
                        self.tt("dve", dst[0:n, :, 80:96], rt[0:n, :, 2, :], rt[0:n, :, 3, :], ALU.add, [rt], [dst])
                    QK = qkT[r]
                    for h in range(4):
                        self.tr(b5v[0:96, h * 128:h * 128 + n], qb[0:n, h, :], identb[0:n, 0:n], [qb, identb], [b5])
                        self.tr(b5v[0:96, (4 + h) * 128:(4 + h) * 128 + n], kb[0:n, h, :], identb[0:n, 0:n], [kb, identb], [b5])
                    self.cp("act", QK[:, :, 0:n], b5v[0:96, :].rearrange("p (a t) -> p a t", a=8)[:, :, 0:n], [], [QK, b5])
                    P.dma("sp", S["QTc"][:, :, p0:p0 + n].rearrange("h d t -> d h t"), QK[:, 0:4, 0:n], dsq[r], reads=[QK], writes=[Buf()])
                    P.dma("sp", S["KTc"][:, :, p0:p0 + n].rearrange("h d t -> d h t"), QK[:, 4:8, 0:n], dsq[r], reads=[QK], writes=[Buf()], newgroup=False)
                    P.dma("sp", S["Vc"][:, 0:n, j, :].rearrange("h p c -> p h c"), VA[0:n, :, :], dsq[r], reads=[VA], writes=[Buf()], newgroup=False)
                fillpop()
                if "fox" in steps:
                    fq_ = ZT[0:n, 1376:1632]; fk_ = ZT[0:n, 1632:1888]; fv_ = ZT[0:n, 1888:2144]; ff_ = ZT[0:n, 2144:2148]
                    for (src, gbc, dst, col) in [(fq_, fgq_bc, fqb, 12), (fk_, fgk_bc, fkb, 12)]:
                        self.act(mjunk[0:n, 0:256], src, AF.Square, [ZT], [mjunk])
                        P.op("dve", lambda e, n=n, col=col: e.tensor_reduce(out=mss[0:n, col:col + 4], in_=mjunk[0:n, 0:256].rearrange("p (h c) -> p h c", h=4),
                                                                        axis=AX.X, op=ALU.add), reads=[mjunk], writes=[mss])
                        self.rsq(mss[0:n, col:col + 4], mss[0:n, col:col + 4], 1.0 / 64, [mss])
                        self.tt("dve", fqf[0:n, :, :], src.rearrange("p (h c) -> p h c", h=4), mss[0:n, col:col + 4].unsqueeze(2).broadcast_to([n, 4, 64]), ALU.mult, [ZT, mss], [fqf])
                        self.tt("pool", dst[0:n, :].rearrange("p (h c) -> p h c", h=4), fqf[0:n, :, :], gbc[0:n, :].unsqueeze(1).broadcast_to([n, 4, 64]), ALU.mult, [fqf, gbc], [dst])
                    FV = fvaug[r]
                    self.cp("act", FV[0:n, :, 0:64], fv_.rearrange("p (h c) -> p h c", h=4), [ZT], [FV])
                    FQK = fqkT[r]
                    for hp in range(2):
                        self.tr(b5v[:, hp * 128:hp * 128 + n], fqb[0:n, hp * 128:(hp + 1) * 128], identb[0:n, 0:n], [fqb, identb], [b5])
                        self.tr(b5v[:, (2 + hp) * 128:(2 + hp) * 128 + n], fkb[0:n, hp * 128:(hp + 1) * 128], identb[0:n, 0:n], [fkb, identb], [b5])
                    self.cp("act", FQK[:, :, 0:n], b5v[:, 0:512].rearrange("p (a t) -> p a t", a=4)[:, :, 0:n], [], [FQK, b5])
                    P.dma("sp", S["QTd"][:, :, p0:p0 + n].rearrange("a d t -> d a t"), FQK[:, 0:2, 0:n], dsq[r], reads=[FQK], writes=[Buf()], newgroup=False)
                    P.dma("sp", S["KTd"][:, :, p0:p0 + n].rearrange("a d t -> d a t"), FQK[:, 2:4, 0:n], dsq[r], reads=[FQK], writes=[Buf()], newgroup=False)
                    P.dma("sp", S["Vd"][:, 0:n, j, :].rearrange("h p c -> p h c"), FV[0:n, :, :], dsq[r], reads=[FV], writes=[Buf()], newgroup=False)
                    P.commit(dsq[r], [qkT[r], vaug[r], FQK, FV])
                    self.tt("dve", flog[0:n, :], ff_, fbf_bc[0:n, :], ALU.add, [ZT, fbf_bc], [flog])
                    self.act(flog[0:n, :], flog[0:n, :], AF.Exp, [flog], [flog], scale=-1.0)
                    self.act(flog[0:n, :], flog[0:n, :], AF.Ln, [flog], [flog], bias=1.0)
                    self.ts("dve", flog[0:n, :], flog[0:n, :], -1.0, None, ALU.mult, None, [flog], [flog])
                    if j == 0:
                        self.mm(b6[0:n, 0:4], C["c_trifull"][0:n, 0:n], flog[0:n, :], True, True, [C["c_trifull"], flog], [b6])
                    else:
                        pn = self.tiles[j - 1][1]
                        selp = C["c_sel15"] if pn == 16 else C["c_sel127"]
                        self.mm(b6[0:n, 0:4], C["c_trifull"][0:n, 0:n], flog[0:n, :], True, False, [C["c_trifull"], flog], [b6])
                        self.mm(b6[0:n, 0:4], selp[0:pn, 0:n], call[0:pn, j - 1, :], False, True, [selp, call], [b6])
                    self.cp("dve", call[0:n, j, :], b6[0:n, 0:4], [], [call, b6])
                while fill:
                    fillpop()
            load(0)
            if ntl > 1:
                load(1)
            zpro(0)
            for g_ in zgroups(0):
                g_()
            for j in range(ntl):
                if j + 2 < ntl:
                    load(j + 2)
                fill = []
                if j + 1 < ntl:
                    zpro(j + 1)
                    fill = zgroups(j + 1)
                chains(j, fill)
            evs = [(d.key, d.cnt) for d in dsy + dsq if d.cnt > 0]
            self.barrier(evs)

    def barrier(self, evs=()):
        P = self.P
        evs = list(evs)
        for e in ("pe", "act", "dve", "pool"):
            if P.cnt[e] > 0:
                evs.append((("E", e), P.cnt[e]))
        for d in P.all_dsems:
            if d.cnt > 0:
                evs.append((d.key, d.cnt))
        for e in ("sp", "pool", "act", "pe", "dve"):
            P.wait_all(e, evs)

    def phase_1b(self, l):
        P, I, S, C, nc = self.P, self.I, self.S, self.C, self.nc
        bank = self.bank
        NT, T = self.NT, self.T
        call = self.call
        tiles = self.tiles
        WMAX = 4

        def make_blocks(W):
            blocks = [[0]]
            i = 1
            while i < len(tiles):
                blocks.append(list(range(i, min(i + W, len(tiles)))))
                i += W
            return blocks
        blocks_of = {"mla": make_blocks(4), "fox": make_blocks(4)}
        fsub = []
        fsub_of = {}
        for bi_, blk_ in enumerate(blocks_of["fox"]):
            for hi_ in range(0, len(blk_), 2):
                fsub_of[(bi_, hi_ // 2)] = len(fsub)
                fsub.append((bi_, hi_ // 2, blk_[hi_:hi_ + 2]))
        with contextlib.ExitStack() as st:
            sb = lambda name, shape, dt=F32: self.sb(st, "b_" + name, shape, dt)
            fblocks = fsub
            b7 = bank[7]
            for bi, (_b, _h, blk) in enumerate(fblocks):
                i = blk[0]
                n = tiles[i][1]
                if len(blk) > 1:
                    sel = C["c_sel127"]
                else:
                    sel = C["c_sel8"] if n == 16 else C["c_sel64"]
                self.mm(b7[:, bi * 4:(bi + 1) * 4], sel[0:n, :], call[0:n, i, :], True, True, [sel, call], [b7])
            cref = sb("cref", [128, len(fblocks), 4])
            self.cp("dve", cref[:].rearrange("p a b -> p (a b)"), b7[:, 0:len(fblocks) * 4], [], [cref, b7])
            negc = sb("negc", [128, 4, NT + 1])
            self.ts("dve", negc[:], call[:].rearrange("p j h -> p h j"), -1.0, None, ALU.mult, None, [call], [negc])
            bias = self.ring(st, "b_bias", [128, NT + 1], F32, 8)
            KT = self.ring(st, "b_KT", [128, T], BF16, 2)
            QT = self.ring(st, "b_QT", [128, T], BF16, 2)
            V = self.ring(st, "b_V", [128, NT + 1, 65], BF16, 2)
            dsk = [P.dsem() for _ in range(2)]
            dsv = [P.dsem() for _ in range(2)]
            LA = 3
            NSB = 4
            pt = self.ring(st, "b_pt", [128, WMAX * 128], BF16, NSB)
            oT = self.ring(st, "b_oT", [65, WMAX * 128], F32, 2)
            osb = self.ring(st, "b_osb", [128, WMAX, 64], F32, 2)
            rden = self.ring(st, "b_rden", [128, WMAX], F32, 2)
            dso = [P.dsem() for _ in range(2)]
            identf = C["c_ident"]
            units = []
            for h in range(4):
                units.append(("mla", h))
            for h in range(4):
                units.append(("fox", h))
            loaded = {}
            nload = [0]

            def load(u):
                kind, h = u
                if kind == "mla":
                    key = ("mla", h)
                else:
                    key = ("fox", h // 2)
                if key in loaded:
                    return loaded[key]
                r = nload[0] % 2
                nload[0] += 1
                if kind == "mla":
                    P.dma("sp", KT[r][0:96, :], S["KTc"][h], dsk[r], writes=[KT[r]])
                    P.dma("sp", QT[r][0:96, :], S["QTc"][h], dsk[r], writes=[QT[r]], newgroup=False)
                else:
                    P.dma("sp", KT[r][:, :], S["KTd"][h // 2], dsk[r], writes=[KT[r]])
                    P.dma("sp", QT[r][:, :], S["QTd"][h // 2], dsk[r], writes=[QT[r]], newgroup=False)
                P.commit(dsk[r], [KT[r], QT[r]])
                loaded[key] = r
                return r
            nv = [0]

            def loadv(u):
                kind, h = u
                r = nv[0] % 2
                nv[0] += 1
                src = S["Vc"] if kind == "mla" else S["Vd"]
                P.dma("sp", V[r][:], src[h], dsv[r], writes=[V[r]])
                return r
            ring_of = {}
            steps = []
            ustart = {}
            uend = {}
            gblk = {}
            ng = 0
            for ui, (kind, h) in enumerate(units):
                ustart[ui] = len(steps)
                for bi, blk in enumerate(blocks_of[kind]):
                    gblk[(ui, bi)] = ng
                    ng += 1
                    for jj in range(blk[-1] + 1):
                        steps.append((ui, bi, jj))
                uend[ui] = len(steps)

            def geom(s):
                ui, bi, jj = steps[s]
                kind, h = units[ui]
                blk = blocks_of[kind][bi]
                i0, i1 = blk[0], blk[-1]
                a = max(i0, jj)
                qb0 = tiles[i0][0]
                q0 = tiles[a][0]
                q1 = tiles[i1][0] + tiles[i1][1]
                return ui, bi, jj, kind, h, blk, i0, i1, a, qb0, q0, q1

            def emit_qk(s):
                ui, bi, jj, kind, h, blk, i0, i1, a, qb0, q0, q1 = geom(s)
                fox = kind == "fox"
                if ui not in ring_of:
                    ring_of[ui] = (load(units[ui]), loadv(units[ui]))
                r, rv = ring_of[ui]
                if s - ustart[ui] == min(LA + 1, uend[ui] - ustart[ui] - 1) and ui + 1 < len(units) and (ui + 1) not in ring_of:
                    ring_of[ui + 1] = (load(units[ui + 1]), loadv(units[ui + 1]))
                if fox and jj == 0:
                    for hi in range((len(blk) + 1) // 2):
                        si = fsub_of[(bi, hi)]
                        tl = fsub[si][2][-1]
                        B_ = bias[(gblk[(ui, bi)] % 4) * 2 + hi]
                        self.ts("dve", B_[:, 0:tl + 1], negc[:, h, 0:tl + 1], cref[:, si, h:h + 1], None, ALU.add, None, [negc, cref], [B_])
                rows = slice(0, 96) if not fox else slice((h % 2) * 64, (h % 2) * 64 + 64)
                k0, kn = tiles[jj]
                sbk = bank[s % NSB]
                self.mm(sbk[0:kn, 0:q1 - q0], KT[r][rows, k0:k0 + kn], QT[r][rows, q0:q1], True, True, [KT[r], QT[r]], [sbk])

            def emit_rest(s):
                ui, bi, jj, kind, h, blk, i0, i1, a, qb0, q0, q1 = geom(s)
                fox = kind == "fox"
                r, rv = ring_of[ui]
                k0, kn = tiles[jj]
                wq = q1 - q0
                scale = (96.0 if not fox else 64.0) ** -0.5
                mask = C["foxmask"] if fox else C["mlamask"]
                ycol = (512 if not fox else 768) + h * 64
                sbk = bank[s % NSB]
                PT = pt[s % NSB]
                g = gblk[(ui, bi)]
                ob = bank[4 + (g % 2)]
                if fox:
                    for hi in range((len(blk) + 1) // 2):
                        sub = fsub[fsub_of[(bi, hi)]][2]
                        if sub[-1] < a:
                            continue
                        lo = max(sub[0], a)
                        c_lo = tiles[lo][0] - q0
                        c_hi = tiles[sub[-1]][0] + tiles[sub[-1]][1] - q0
                        B_ = bias[(g % 4) * 2 + hi]
                        self.act(PT[0:kn, c_lo:c_hi], sbk[0:kn, c_lo:c_hi], AF.Exp, [B_], [PT, sbk], scale=scale, bias=B_[0:kn, jj:jj + 1])
                else:
                    self.act(PT[0:kn, 0:wq], sbk[0:kn, 0:wq], AF.Exp, [], [PT, sbk], scale=scale)
                if jj >= i0:
                    na = tiles[jj][1]
                    self.tt("pool", PT[0:kn, 0:na], PT[0:kn, 0:na], mask[0:kn, 0:na], ALU.mult, [PT, mask], [PT])
                self.mm(ob[0:65, q0 - qb0:q1 - qb0], V[rv][0:kn, jj, :], PT[0:kn, 0:wq], jj == 0, jj == i1, [PT, V[rv]], [ob])
                if jj == i1:
                    ro = g % 2
                    wb = q1 - qb0
                    nsub = len(blk)
                    n = tiles[i0][1]
                    OT = oT[ro]
                    self.cp("dve", OT[0:65, 0:wb], ob[0:65, 0:wb], [], [OT, ob])
                    tb = bank[6 + (g % 2)]
                    for x in range(nsub):
                        self.tr(tb[0:n, x * 65:(x + 1) * 65], OT[0:65, x * 128:x * 128 + n], identf[0:65, 0:65], [OT, identf], [tb])
                    tbv = tb[0:n, 0:nsub * 65].rearrange("p (x c) -> p x c", c=65)
                    P.op("dve", lambda e, tbv=tbv, n=n, ro=ro, nsub=nsub: e.reciprocal(out=rden[ro][0:n, 0:nsub], in_=tbv[:, :, 64]),
                         reads=[], writes=[rden[ro], tb])
                    self.tt("dve", osb[ro][0:n, 0:nsub, :], tbv[:, :, 0:64], rden[ro][0:n, 0:nsub].unsqueeze(2).broadcast_to([n, nsub, 64]),
                            ALU.mult, [rden[ro]], [osb[ro], tb])
                    if nsub == 1:
                        P.dma("sp", S["Y"][qb0:qb0 + n, ycol:ycol + 64], osb[ro][0:n, 0, :], dso[ro], reads=[osb[ro]], writes=[Buf()])
                    else:
                        P.dma("sp", S["Y"][qb0:qb0 + nsub * 128, ycol:ycol + 64].rearrange("(x p) c -> p x c", p=128), osb[ro][:, 0:nsub, :],
                              dso[ro], reads=[osb[ro]], writes=[Buf()])
            for s in range(len(steps) + LA):
                if s < len(steps):
                    emit_qk(s)
                if s >= LA:
                    emit_rest(s - LA)
            self.barrier([(d.key, d.cnt) for d in dso if d.cnt > 0])

    def phase_1c(self, l, hsrc):
        P, I, S, C, nc = self.P, self.I, self.S, self.C, self.nc
        bank = self.bank
        NT, T = self.NT, self.T
        moe = (l % 2 == 1)
        with contextlib.ExitStack() as st:
            sb = lambda name, shape, dt=F32: self.sb(st, "c_" + name, shape, dt)
            dsw = P.dsem()
            wout = sb("wout", [128, 8, D], BF16)
            wv = I["w_out"][l].rearrange("(k p) c -> p k c", p=128)
            for k in range(8):
                P.dma("pool", wout[:, k, :], wv[:, k, :], dsw, writes=[wout], newgroup=False)
            og_bc = sb("og_bc", [128, D])
            P.dma("sp", og_bc[:], I["og"][l].partition_broadcast(128), dsw, writes=[og_bc], newgroup=False)
            g2T = sb("g2T", [128, 8])
            P.dma("sp", g2T[:], I["g2T"][l], dsw, writes=[g2T], newgroup=False)
            if moe:
                wr = sb("wr", [128, 8, NEXP])
                P.dma("sp", wr[:], I["moe_wr"].rearrange("(k p) e -> p k e", p=128), dsw, writes=[wr], newgroup=False)
                P.commit(dsw, [wr])
            P.commit(dsw, [wout, og_bc, g2T])
            R2 = 2
            R3 = 3
            Yt = self.ring(st, "c_Yt", [128, D], F32, R3)
            Ht = self.ring(st, "c_Ht", [128, D], F32, R3)
            dsl = [P.dsem() for _ in range(R3)]
            junk = sb("junk", [128, D])
            ss = self.ring(st, "c_ss", [128, 16], F32, R2)
            yn = sb("yn", [128, D], BF16)
            ynT_r = self.ring(st, "c_ynT", [128, 8, 128], BF16, R2)
            junkA = sb("junkA", [128, D])
            ssA = self.ring(st, "c_ssA", [128, 4], F32, R2)
            hm = self.ring(st, "c_hm", [128, D], F32, R2)
            dsh = [P.dsem() for _ in range(R2)]
            u2b = sb("u2b", [128, D], BF16)
            u2f = sb("u2f", [128, D], F32)
            u2Tf = sb("u2Tf", [128, 8, 128], F32)
            u2T = self.ring(st, "c_u2T", [128, 8, 128], BF16, R2)
            dsu = [P.dsem() for _ in range(R2)]
            gl = sb("gl", [128, 8]); gl2 = sb("gl2", [128, 8]); gm1 = sb("gm1", [128, 8]); gm2 = sb("gm2", [128, 8])
            gsc = sb("gsc", [128, 8])
            gT = self.ring(st, "c_gT", [128, 8], F32, R2)
            identb = C["identb"]; identf = C["c_ident"]

            def load(jn):
                pp, nn = self.tiles[jn]
                rr = jn % R3
                P.dma("sp", Yt[rr][0:nn, :], S["Y"][pp:pp + nn, :], dsl[rr], writes=[Yt[rr]])
                P.dma("sp", Ht[rr][0:nn, :], hsrc[pp:pp + nn, :], dsl[rr], writes=[Ht[rr]], newgroup=False)
                P.commit(dsl[rr], [Yt[rr], Ht[rr]])
            def stage_a(j):
                p0, n = self.tiles[j]
                Y_ = Yt[j % R3]
                SA = ssA[j % R2]
                ynT = ynT_r[j % R2]
                self.act(junkA[0:n, :], Y_[0:n, :], AF.Square, [Y_], [junkA])
                P.op("dve", lambda e, n=n, SA=SA: e.tensor_reduce(out=SA[0:n, 0:4], in_=junkA[0:n, :].rearrange("p (g c) -> p g c", g=4), axis=AX.X, op=ALU.add),
                     reads=[junkA], writes=[SA])
                self.rsq(SA[0:n, 0:4], SA[0:n, 0:4], 1.0 / 256, [SA])
                for g in range(4):
                    self.stt("dve", yn[0:n, g * 256:(g + 1) * 256], Y_[0:n, g * 256:(g + 1) * 256], SA[0:n, g:g + 1],
                             og_bc[0:n, g * 256:(g + 1) * 256], ALU.mult, ALU.mult, [Y_, SA, og_bc], [yn])
                b0 = bank[0]
                b0v = b0.t[:].bitcast(BF16)
                for k in range(8):
                    self.tr(b0v[:, k * 128:k * 128 + n], yn[0:n, k * 128:(k + 1) * 128], identb[0:n, 0:n], [yn, identb], [b0])
                self.cp("act", ynT[:, :, 0:n], b0v[:, :].rearrange("p (k t) -> p k t", k=8)[:, :, 0:n], [], [ynT, b0])
            ntl = len(self.tiles)
            for jn in range(min(3, ntl)):
                load(jn)
            stage_a(0)
            for j, (p0, n) in enumerate(self.tiles):
                r = j % R2
                if j + 1 < ntl:
                    stage_a(j + 1)
                Y_, H_, SS, HM, U2T = Yt[j % R3], Ht[j % R3], ss[r], hm[r], u2T[r]
                ynT = ynT_r[r]
                for c in range(2):
                    bo = bank[2 + c]
                    for k in range(8):
                        self.mm(bo[0:n, 0:512], ynT[:, k, 0:n], wout[:, k, c * 512:(c + 1) * 512], k == 0, k == 7, [ynT, wout], [bo])
                    self.tt("dve", HM[0:n, c * 512:(c + 1) * 512], bo[0:n, 0:512], H_[0:n, c * 512:(c + 1) * 512], ALU.add, [H_], [HM, bo])
                P.dma("sp", S["hmid"][p0:p0 + n, :], HM[0:n, :], dsh[r], reads=[HM], writes=[Buf()])
                P.op("dve", lambda e, SS=SS: e.memset(SS[:, 8:9], 0.0), writes=[SS])
                self.act(junk[0:n, :], HM[0:n, :], AF.Square, [HM, SS], [junk, SS], accum_out=SS[0:n, 8:9])
                self.rsq(SS[0:n, 9:10], SS[0:n, 8:9], 1.0 / D, [SS])
                b1 = bank[1]
                if not moe:
                    self.act(u2b[0:n, :], HM[0:n, :], AF.Copy, [HM, SS], [u2b], scale=SS[0:n, 9:10])
                    b1v = b1.t[:].bitcast(BF16)
                    for k in range(8):
                        self.tr(b1v[:, k * 128:k * 128 + n], u2b[0:n, k * 128:(k + 1) * 128], identb[0:n, 0:n], [u2b, identb], [b1])
                    self.tt("dve", U2T[:, :, 0:n], b1v[:, :].rearrange("p (k t) -> p k t", k=8)[:, :, 0:n],
                            g2T[:, :].unsqueeze(2).broadcast_to([128, 8, n]), ALU.mult, [g2T], [U2T, b1])
                else:
                    self.act(u2f[0:n, :], HM[0:n, :], AF.Copy, [HM, SS], [u2f], scale=SS[0:n, 9:10])
                    for half in range(2):
                        bb = bank[5 + half]
                        for kk in range(4):
                            k = half * 4 + kk
                            self.tr(bb[:, kk * 128:kk * 128 + n], u2f[0:n, k * 128:(k + 1) * 128], identf[0:n, 0:n], [u2f, identf], [bb])
                        self.tt("dve", u2Tf[:, half * 4:half * 4 + 4, 0:n], bb[:, :].rearrange("p (k t) -> p k t", k=4)[:, :, 0:n],
                                g2T[:, half * 4:half * 4 + 4].unsqueeze(2).broadcast_to([128, 4, n]), ALU.mult, [g2T], [u2Tf, bb])
                    self.cp("pool", U2T[:, :, 0:n], u2Tf[:, :, 0:n], [u2Tf], [U2T])
                    b7 = bank[7]
                    for k in range(8):
                        self.mm(b7[0:n, 0:NEXP], u2Tf[:, k, 0:n], wr[:, k, :], k == 0, k == 7, [u2Tf, wr], [b7])
                    self.cp("act", gl[0:n, :], b7[0:n, 0:NEXP], [], [gl, b7])
                    P.op("dve", lambda e, n=n: e.tensor_reduce(out=gsc[0:n, 0:1], in_=gl[0:n, :], axis=AX.X, op=ALU.max), reads=[gl], writes=[gsc])
                    self.ts("dve", gm1[0:n, :], gl[0:n, :], gsc[0:n, 0:1], None, ALU.is_equal, None, [gl, gsc], [gm1])
                    self.stt("dve", gl2[0:n, :], gm1[0:n, :], -1e30, gl[0:n, :], ALU.mult, ALU.add, [gm1, gl], [gl2])
                    P.op("dve", lambda e, n=n: e.tensor_reduce(out=gsc[0:n, 1:2], in_=gl2[0:n, :], axis=AX.X, op=ALU.max), reads=[gl2], writes=[gsc])
                    self.ts("dve", gm2[0:n, :], gl2[0:n, :], gsc[0:n, 1:2], None, ALU.is_equal, None, [gl2, gsc], [gm2])
                    self.tt("dve", gsc[0:n, 2:3], gsc[0:n, 1:2], gsc[0:n, 0:1], ALU.subtract, [gsc], [gsc])
                    self.act(gsc[0:n, 3:4], gsc[0:n, 2:3], AF.Exp, [gsc], [gsc])
                    self.ts("dve", gsc[0:n, 4:5], gsc[0:n, 3:4], 1.0, None, ALU.add, None, [gsc], [gsc])
                    P.op("dve", lambda e, n=n: e.reciprocal(out=gsc[0:n, 4:5], in_=gsc[0:n, 4:5]), reads=[gsc], writes=[gsc])
                    self.tt("dve", gsc[0:n, 5:6], gsc[0:n, 3:4], gsc[0:n, 4:5], ALU.mult, [gsc], [gsc])
                    gate = gT[r]
                    self.ts("dve", gate[0:n, :], gm1[0:n, :], gsc[0:n, 4:5], None, ALU.mult, None, [gm1, gsc], [gate])
                    self.stt("dve", gate[0:n, :], gm2[0:n, :], gsc[0:n, 5:6], gate[0:n, :], ALU.mult, ALU.add, [gm2, gsc, gate], [gate])
                    P.dma("sp", S["gate"][p0:p0 + n, :], gate[0:n, :], dsu[r], reads=[gate], writes=[Buf()])
                P.dma("sp", S["u2T"].rearrange("(k p) t -> p k t", p=128)[:, :, p0:p0 + n], U2T[:, :, 0:n], dsu[r], reads=[U2T], writes=[Buf()],
                      newgroup=not moe)
                P.commit(dsu[r], [U2T, gT[r]])
                if j + 3 < ntl:
                    load(j + 3)
            self.barrier([(d.key, d.cnt) for d in dsh + dsu if d.cnt > 0])

    def ffn_core(self, st, tok_sets, wsets, out_fn, blend, gated):
        P, I, S, C, nc = self.P, self.I, self.S, self.C, self.nc
        bank = self.bank
        sb = lambda name, shape, dt=F32: self.sb(st, "f_" + name, shape, dt)
        TSMAX = max(sum(n for (_, n) in ts) for ts in tok_sets)
        NTT = max(len(ts) for ts in tok_sets)
        u2 = sb("u2", [128, 8, TSMAX], BF16)
        acc = sb("acc", [128, NTT, D], F32)
        dsx = P.dsem(); dsa = P.dsem(); dsg = P.dsem()
        SW = 256
        if blend:
            u2B = self.ring(st, "f_u2B", [128, 8, SW], BF16, 2)
            dsxb = [P.dsem() for _ in range(2)]
            accB = self.ring(st, "f_accB", [128, D], F32, 2)
            dsb = [P.dsem() for _ in range(2)]
            selA = C["sel"][:, 0:1]; selB = C["sel"][:, 1:2]
        if gated:
            gt = sb("gt", [128, NTT, NEXP], F32)
            gtB = sb("gtB", [128, NTT, NEXP], F32)
        GW = 512
        wg = self.ring(st, "f_wg", [128, 8, GW], BF16, 2)
        wu = self.ring(st, "f_wu", [128, 8, GW], BF16, 2)
        wd = self.ring(st, "f_wd", [128, 4, D], BF16, 2)
        dsw = [P.dsem() for _ in range(2)]
        sg = self.ring(st, "f_sg", [128, 512], F32, 2)
        hid = self.ring(st, "f_hid", [128, 512], BF16, 8)
        dso = [P.dsem() for _ in range(4)]
        u2Tv = S["u2T"].rearrange("(k p) t -> p k t", p=128)
        groups = []
        for (wg_ap, wu_ap, wd_ap, F, e) in wsets:
            f0 = 0
            while f0 < F:
                fw = min(GW, F - f0)
                groups.append((wg_ap, wu_ap, wd_ap, f0, fw, e))
                f0 += fw
        gcount = 0
        hcount = 0

        def loadw(gi):
            wg_ap, wu_ap, wd_ap, f0, fw, e = groups[gi % len(groups)]
            r = gi % 2
            wgv = wg_ap.rearrange("(k p) f -> p k f", p=128)
            wuv = wu_ap.rearrange("(k p) f -> p k f", p=128)
            for k in range(0, 8, 4):
                P.dma("pool", wg[r][:, k:k + 4, 0:fw], wgv[:, k:k + 4, f0:f0 + fw], dsw[r], writes=[wg[r]], newgroup=(k == 0))
            for k in range(0, 8, 4):
                P.dma("pool", wu[r][:, k:k + 4, 0:fw], wuv[:, k:k + 4, f0:f0 + fw], dsw[r], writes=[wu[r]], newgroup=False)
            nb = fw // 128
            P.dma("pool", wd[r][:, 0:nb, :], wd_ap[f0:f0 + fw, :].rearrange("(b p) d -> p b d", p=128), dsw[r], writes=[wd[r]], newgroup=False)
            P.commit(dsw[r], [wg[r], wu[r], wd[r]])
        total_groups = len(groups) * len(tok_sets)
        loadw(0)
        nstage = 0
        for si, ts in enumerate(tok_sets):
            col = 0
            cols = []
            for ti, (rows, n) in enumerate(ts):
                cols.append(col)
                col += n
            TS = col
            if not blend:
                r00 = ts[0][0]
                for ti, (rows, n) in enumerate(ts):
                    assert rows == r00 + cols[ti]
                    P.dma("sp", acc[0:n, ti, :], S["hmid"][rows:rows + n, :], dsa, writes=[acc], newgroup=(ti == 0))
                for c0 in range(0, TS, 512):
                    cw_ = min(512, TS - c0)
                    P.dma("sp", u2[:, :, c0:c0 + cw_], u2Tv[:, :, r00 + c0:r00 + c0 + cw_], dsx, writes=[u2], newgroup=(c0 == 0))
                P.commit(dsx, [u2])
                P.commit(dsa, [acc])
            else:
                rA0, rB0 = ts[0][0]
                for ti, (rows, n) in enumerate(ts):
                    rA, rB = rows
                    assert rA == rA0 + cols[ti] and rB == rB0 + cols[ti]
                    P.dma("sp", acc[0:n, ti, :], S["hmid"][rA:rA + n, :], dsa, writes=[acc], newgroup=(ti == 0))
                    if gated:
                        P.dma("sp", gt[0:n, ti, :], S["gate"][rA:rA + n, :], dsg, writes=[gt], newgroup=(ti == 0))
                        P.dma("sp", gtB[0:n, ti, :], S["gate"][rB:rB + n, :], dsg, writes=[gtB], newgroup=False)
                P.commit(dsa, [acc])
                if gated:
                    P.commit(dsg, [gt, gtB])
                for c0 in range(0, TS, 512):
                    cw_ = min(512, TS - c0)
                    P.dma("sp", u2[:, :, c0:c0 + cw_], u2Tv[:, :, rA0 + c0:rA0 + c0 + cw_], dsx, writes=[u2], newgroup=(c0 == 0))
                P.commit(dsx, [u2])
                for ti, (rows, n) in enumerate(ts):
                    rA, rB = rows
                    ab = accB[ti % 2]
                    P.dma("sp", ab[0:n, :], S["hmid"][rB:rB + n, :], dsb[ti % 2], writes=[ab])
                    self.act(acc[0:n, ti, :], acc[0:n, ti, :], AF.Copy, [acc, C["sel"]], [acc], scale=selA[0:n, :])
                    self.stt("dve", acc[0:n, ti, :], ab[0:n, :], selB[0:n, :], acc[0:n, ti, :], ALU.mult, ALU.add, [ab, C["sel"], acc], [acc])
                for c0 in range(0, TS, SW):
                    cw_ = min(SW, TS - c0)
                    ub_ = u2B[nstage % 2]
                    P.dma("sp", ub_[:, :, 0:cw_], u2Tv[:, :, rB0 + c0:rB0 + c0 + cw_], dsxb[nstage % 2], writes=[ub_])
                    nstage += 1
                    self.act(u2[:, :, c0:c0 + cw_], u2[:, :, c0:c0 + cw_], AF.Copy, [u2, C["sel"]], [u2], scale=selA)
                    self.stt("dve", u2[:, :, c0:c0 + cw_], ub_[:, :, 0:cw_], selB, u2[:, :, c0:c0 + cw_], ALU.mult, ALU.add, [ub_, C["sel"], u2], [u2])
                if gated:
                    self.act(gt[:], gt[:], AF.Copy, [gt, C["sel"]], [gt], scale=selA)
                    self.stt("dve", gt[:], gtB[:], selB, gt[:], ALU.mult, ALU.add, [gtB, C["sel"], gt], [gt])
            chunks = []
            cur = []
            cw_ = 0
            for ti, (rows, n) in enumerate(ts):
                if cw_ + n > 512:
                    chunks.append(cur); cur = []; cw_ = 0
                cur.append(ti); cw_ += n
            if cur:
                chunks.append(cur)
            for gi_local in range(len(groups)):
                gi = gcount
                gcount += 1
                wg_ap, wu_ap, wd_ap, f0, fw, e = groups[gi_local]
                r = gi % 2
                if gi + 1 < total_groups:
                    loadw(gi + 1)
                nb = fw // 128
                for ch in chunks:
                    c0 = cols[ch[0]]
                    cw = sum(ts[ti][1] for ti in ch)
                    hslots = []
                    for fb in range(nb):
                        bg = bank[(hcount % 2) * 2]
                        bu = bank[(hcount % 2) * 2 + 1]
                        H_ = hid[hcount % 8]
                        SG = sg[hcount % 2]
                        hcount += 1
                        for k in range(8):
                            self.mm(bg[:, 0:cw], wg[r][:, k, fb * 128:(fb + 1) * 128], u2[:, k, c0:c0 + cw], k == 0, k == 7, [wg[r], u2], [bg])
                        for k in range(8):
                            self.mm(bu[:, 0:cw], wu[r][:, k, fb * 128:(fb + 1) * 128], u2[:, k, c0:c0 + cw], k == 0, k == 7, [wu[r], u2], [bu])
                        self.act(SG[:, 0:cw], bg[:, 0:cw], AF.Silu, [], [SG, bg])
                        self.tt("dve", H_[:, 0:cw], bu[:, 0:cw], SG[:, 0:cw], ALU.mult, [SG], [H_, bu])
                        hslots.append(H_)
                    for ti in ch:
                        n = ts[ti][1]
                        lc = cols[ti] - c0
                        for half in range(2):
                            bd = bank[4 + 2 * (ti % 2) + half]
                            for fb in range(nb):
                                self.mm(bd[0:n, 0:512], hslots[fb][:, lc:lc + n], wd[r][:, fb, half * 512:(half + 1) * 512], fb == 0, fb == nb - 1,
                                        [hslots[fb], wd[r]], [bd])
                            a_ = acc[0:n, ti, half * 512:(half + 1) * 512]
                            if gated:
                                self.stt("dve", a_, bd[0:n, 0:512], gt[0:n, ti, e:e + 1], a_, ALU.mult, ALU.add, [acc, gt], [acc, bd])
                            else:
                                self.tt("dve", a_, bd[0:n, 0:512], a_, ALU.add, [acc], [acc, bd])
            for ti, (rows, n) in enumerate(ts):
                dst = out_fn(si, ti)
                if dst is None:
                    continue
                ev = P.dma("sp", dst, acc[0:n, ti, :], dso[ti % 4], reads=[acc], writes=[Buf()])
                self.final_evs.append(ev)
        self.barrier([(d.key, d.cnt) for d in dso if d.cnt > 0])

    def phase_ffn(self, hdst):
        I = self.I
        T = self.T
        rows = [(r0, min(128, T - r0)) for r0 in range(0, T, 128)]
        TSN = 16
        tok_sets = [rows[i:i + TSN] for i in range(0, len(rows), TSN)]
        if len(tok_sets) > 1 and len(tok_sets[-1]) == 1:
            tok_sets[-2] = tok_sets[-2] + tok_sets[-1]
            tok_sets.pop()
        final = hdst is None

        def out_fn(si, ti):
            r0, n = tok_sets[si][ti]
            if not final:
                return hdst[r0:r0 + n, :]
            lo = max(r0, NMETA)
            return None if True else None
        with contextlib.ExitStack() as st:
            self.ffn_core(st, tok_sets, [(I["ffn_wg"], I["ffn_wu"], I["ffn_wd"], D_FF, 0)], out_fn, blend=False, gated=False)

    def phase_moe(self):
        I = self.I
        NTOK = self.moe_tokens
        ntile = NTOK // 128
        TSN = 16
        tiles_ab = [((NMETA + 128 * i, NMETA + NTOK + 128 * i), 128) for i in range(ntile)]
        tok_sets = [tiles_ab[i:i + TSN] for i in range(0, ntile, TSN)]
        out = self.out_ap

        def out_fn(si, ti):
            i = si * TSN + ti
            return out[128 * i:128 * (i + 1), :]
        wsets = [(I["moe_wg"][e], I["moe_wu"][e], I["moe_wd"][e], D_FFE, e) for e in range(NEXP)]
        with contextlib.ExitStack() as st:
            self.ffn_core(st, tok_sets, wsets, out_fn, blend=True, gated=True)


def prep_inputs(inp, b, half, NT, depth=2):
    f = lambda a: np.ascontiguousarray(np.asarray(a, dtype=np.float32))
    T = NMETA + 128 * NT
    m = {}
    m["xin"] = f(np.concatenate([inp["meta"], inp["x"][b]], axis=0))
    sel = np.zeros((128, 2), np.float32)
    sel[:, half] = 1.0
    m["sel"] = sel
    m.update(make_consts(T))
    L = depth
    colmaj = lambda v: f(np.asarray(v).reshape(L, -1, 128).transpose(0, 2, 1))
    m["w_in"] = f(inp["w_in"][:L]); m["w_out"] = f(inp["w_out"][:L])
    m["g1T"] = colmaj(inp["norm1_g"][:L]); m["g2T"] = colmaj(inp["norm2_g"][:L])
    m["og"] = f(inp["out_norm_g"][:L])
    cw = np.asarray(inp["lru_conv_w"][:L])
    m["conv_wT"] = f(cw.transpose(0, 2, 1).reshape(L, 2, 128, 4).transpose(0, 2, 1, 3))
    m["conv_b"] = colmaj(inp["lru_conv_b"][:L])
    m["lru_ba"] = colmaj(np.asarray(inp["lru_ba"][:L]).reshape(L, 256))
    m["lru_bx"] = colmaj(np.asarray(inp["lru_bx"][:L]).reshape(L, 256))
    m["lru_lam"] = colmaj(inp["lru_lambda"][:L])
    m["lru_wa"] = f(inp["lru_wa"][:L]); m["lru_wx"] = f(inp["lru_wx"][:L])
    m["hg_lb"] = f(inp["hg_lb_logits"][:2])
    for k in ["mla_gq", "mla_w_uq", "mla_gkv", "mla_w_ukv", "mla_gqn", "mla_gkn", "fox_gqn", "fox_gkn", "fox_bf"]:
        m[k] = f(inp[k][:L])
    m["ffn_wg"] = f(inp["ffn_w_gate"][0]); m["ffn_wu"] = f(inp["ffn_w_up"][0]); m["ffn_wd"] = f(inp["ffn_w_down"][0])
    if depth > 1:
        m["moe_wr"] = f(inp["moe_w_router"][0]); m["moe_wg"] = f(inp["moe_w_gate"][0])
        m["moe_wu"] = f(inp["moe_w_up"][0]); m["moe_wd"] = f(inp["moe_w_down"][0])
    return m


_CACHE = {}


def _get_program(NT):
    if NT not in _CACHE:
        b = Builder(NT, depth=2, debug=False)
        nc = b.build()
        _CACHE[NT] = (b, nc)
    return _CACHE[NT]


def kernel(**inputs):
    x = np.asarray(inputs["x"])
    Bsz, SEQ, _ = x.shape
    NT = SEQ // 128
    b, nc = _get_program(NT)
    half_tokens = b.moe_tokens
    n_cores = 8
    in_maps = []
    shared = None
    for c in range(n_cores):
        bi, half = c % Bsz, c // Bsz
        if shared is None:
            m = prep_inputs(inputs, bi, half, NT, depth=2)
            shared = m
        else:
            m = dict(shared)
            m["xin"] = np.ascontiguousarray(np.concatenate([np.asarray(inputs["meta"], np.float32), np.asarray(x[bi], np.float32)], axis=0))
            sel = np.zeros((128, 2), np.float32)
            sel[:, half] = 1.0
            m["sel"] = sel
        in_maps.append({k: v for k, v in m.items() if k in b.inputs})
    res = run_bass_kernel_spmd(nc, in_maps, core_ids=list(range(n_cores)))
    out = np.empty((Bsz, SEQ, D), np.float32)
    for c in range(n_cores):
        bi, half = c % Bsz, c // Bsz
        out[bi, half * half_tokens:(half + 1) * half_tokens] = np.asarray(res.results[c]["out"])
    return out
```
